# Optimizing a Trainium2 kernel written in Bass

```python
import jax
import jax.numpy as jnp
from jax import lax
import numpy as np

D_MODEL = 1024
BATCH = 16
SEQ = 2048
DEPTH = 2

HEAD_DIM = 64
ROPE_THETA = 10000.0
RMS_EPS = 1e-6
N_BRANCH = 3

A_HEADS = 6
A_WIDTH = A_HEADS * HEAD_DIM
A_BLOCK = 256
A_TOPK = 3
A_QCHUNK = 32

B_HEADS = 6
B_WIDTH = B_HEADS * HEAD_DIM
B_KV_DIM = HEAD_DIM
B_TOPK = 256
B_IDX_HEADS = 4
B_IDX_DIM = HEAD_DIM
B_QCHUNK = 128

C_GROUPS = ((128, 1), (512, 4), (2048, 16))
C_SLOTS = 4
C_HEADS = C_SLOTS * len(C_GROUPS)
C_WIDTH = C_SLOTS * HEAD_DIM
C_BLOCK = 128

IN_SPLITS = (A_WIDTH, A_WIDTH, A_WIDTH, A_WIDTH,
             B_WIDTH, B_KV_DIM, B_KV_DIM, B_WIDTH, B_IDX_HEADS * B_IDX_DIM, B_IDX_DIM, B_IDX_HEADS,
             C_HEADS * HEAD_DIM, C_HEADS * HEAD_DIM, C_HEADS * HEAD_DIM, C_WIDTH,
             N_BRANCH * D_MODEL)
IN_WIDTH = sum(IN_SPLITS)
IN_OFFSETS = tuple(int(o) for o in np.cumsum(IN_SPLITS)[:-1])

kernel_name = 'hybrid_moba_dsa_dilated_gated'


def rmsnorm(x, g):
    xf = x.astype(jnp.float32)
    y = xf * lax.rsqrt(jnp.mean(xf * xf, axis=-1, keepdims=True) + RMS_EPS)
    return (y * g.astype(jnp.float32)).astype(x.dtype)


def rope_tables(seq_len):
    inv_freq = 1.0 / (ROPE_THETA ** (jnp.arange(0, HEAD_DIM, 2, dtype=jnp.float32) / HEAD_DIM))
    ang = jnp.arange(seq_len, dtype=jnp.float32)[:, None] * inv_freq[None, :]
    return jnp.cos(ang), jnp.sin(ang)


def apply_rope(x, cos, sin):
    half = HEAD_DIM // 2
    c = cos[None, :, None, :].astype(x.dtype)
    s = sin[None, :, None, :].astype(x.dtype)
    x1, x2 = x[..., :half], x[..., half:]
    return jnp.concatenate([x1 * c - x2 * s, x1 * s + x2 * c], axis=-1)


def moba_attention(q, k, v):
    bsz, seq, nh, hd = q.shape
    scale = hd ** -0.5
    n_blk = -(-seq // A_BLOCK)
    pad = n_blk * A_BLOCK - seq
    qh = q.transpose(0, 2, 1, 3)
    kb = jnp.pad(k.transpose(0, 2, 1, 3), ((0, 0), (0, 0), (0, pad), (0, 0))).reshape(bsz, nh, n_blk, A_BLOCK, hd)
    vb = jnp.pad(v.transpose(0, 2, 1, 3), ((0, 0), (0, 0), (0, pad), (0, 0))).reshape(bsz, nh, n_blk, A_BLOCK, hd)
    k_mean = jnp.mean(kb.astype(jnp.float32), axis=3)
    topk = max(1, min(A_TOPK, n_blk - 1))
    b_ix = jnp.arange(bsz)[:, None, None, None]
    h_ix = jnp.arange(nh)[None, :, None, None]
    blk_ids = jnp.arange(n_blk)
    n_sel = topk * A_BLOCK

    def chunk(ci):
        t0 = ci * A_QCHUNK
        own = t0 // A_BLOCK
        t_pos = t0 + jnp.arange(A_QCHUNK)
        qc = lax.dynamic_slice_in_dim(qh, t0, A_QCHUNK, axis=2)
        k_own = lax.dynamic_index_in_dim(kb, own, axis=2, keepdims=False)
        v_own = lax.dynamic_index_in_dim(vb, own, axis=2, keepdims=False)
        kpos_own = own * A_BLOCK + jnp.arange(A_BLOCK)
        s_own = jnp.einsum('bhqd,bhkd->bhqk', qc, k_own, preferred_element_type=jnp.float32) * scale
        s_own = jnp.where(kpos_own[None, :] <= t_pos[:, None], s_own, -jnp.inf)
        gate = jnp.einsum('bhqd,bhnd->bhqn', qc.astype(jnp.float32), k_mean)
        gate = jnp.where(blk_ids < own, gate, -jnp.inf)
        g_val, g_idx = lax.top_k(gate, topk)
        sel_ok = jnp.isfinite(g_val)
        k_sel = kb[b_ix, h_ix, g_idx]
        v_sel = vb[b_ix, h_ix, g_idx]
        s_sel = jnp.einsum('bhqd,bhqnkd->bhqnk', qc, k_sel, preferred_element_type=jnp.float32) * scale
        s_sel = jnp.where(sel_ok[..., None], s_sel, -jnp.inf)
        scores = jnp.concatenate([s_sel.reshape(bsz, nh, A_QCHUNK, n_sel), s_own], axis=-1)
        p = jax.nn.softmax(scores, axis=-1).astype(v.dtype)
        p_sel = p[..., :n_sel].reshape(bsz, nh, A_QCHUNK, topk, A_BLOCK)
        out = jnp.einsum('bhqnk,bhqnkd->bhqd', p_sel, v_sel)
        return out + jnp.einsum('bhqk,bhkd->bhqd', p[..., n_sel:], v_own)

    outs = lax.map(chunk, jnp.arange(seq // A_QCHUNK))
    return outs.transpose(1, 0, 3, 2, 4).reshape(bsz, seq, nh, hd)


def dsa_attention(q, k, v, iq, ik, iw):
    bsz, seq, nh, hd = q.shape
    scale = hd ** -0.5
    n_keep = min(B_TOPK, seq // 4)
    b_ix = jnp.arange(bsz)[:, None, None]
    key_pos = jnp.arange(seq)
    iw = iw.astype(jnp.float32) * (B_IDX_HEADS * B_IDX_DIM) ** -0.5

    def chunk(ci):
        t0 = ci * B_QCHUNK
        t_pos = t0 + jnp.arange(B_QCHUNK)
        iq_c = lax.dynamic_slice_in_dim(iq, t0, B_QCHUNK, axis=1)
        iw_c = lax.dynamic_slice_in_dim(iw, t0, B_QCHUNK, axis=1)
        logit = jnp.einsum('bqhd,bsd->bqhs', iq_c, ik, preferred_element_type=jnp.float32)
        idx_score = jnp.einsum('bqhs,bqh->bqs', jax.nn.relu(logit), iw_c)
        idx_score = jnp.where(key_pos[None, :] <= t_pos[:, None], idx_score, -jnp.inf)
        _, sel = lax.top_k(idx_score, n_keep)
        sel_ok = sel <= t_pos[None, :, None]
        k_g = k[b_ix, sel]
        v_g = v[b_ix, sel]
        qc = lax.dynamic_slice_in_dim(q, t0, B_QCHUNK, axis=1)
        s = jnp.einsum('bqhd,bqkd->bqhk', qc, k_g, preferred_element_type=jnp.float32) * scale
        s = jnp.where(sel_ok[:, :, None, :], s, -jnp.inf)
        p = jax.nn.softmax(s, axis=-1).astype(v.dtype)
        return jnp.einsum('bqhk,bqkd->bqhd', p, v_g)

    outs = lax.map(chunk, jnp.arange(seq // B_QCHUNK))
    return outs.transpose(1, 0, 2, 3, 4).reshape(bsz, seq, nh, hd)


def dilated_group(q, k, v, steps, dil):
    bsz, seq, nh, hd = q.shape
    n_sub = seq // dil
    n_blk = -(-n_sub // C_BLOCK)
    pad = n_blk * C_BLOCK - n_sub

    def to_sub(t):
        t = t.reshape(bsz, n_sub, dil, nh, hd).transpose(0, 2, 3, 1, 4)
        return jnp.pad(t, ((0, 0), (0, 0), (0, 0), (0, pad), (0, 0)))

    def band(t):
        tb = jnp.pad(to_sub(t), ((0, 0), (0, 0), (0, 0), (C_BLOCK, 0), (0, 0)))
        tb = tb.reshape(bsz, dil, nh, n_blk + 1, C_BLOCK, hd)
        return jnp.concatenate([tb[:, :, :, :-1], tb[:, :, :, 1:]], axis=4)

    qs = to_sub(q).reshape(bsz, dil, nh, n_blk, C_BLOCK, hd)
    kw = band(k)
    vw = band(v)
    s = jnp.einsum('brhnqd,brhnkd->brhnqk', qs, kw, preferred_element_type=jnp.float32) * hd ** -0.5
    blk = jnp.arange(n_blk)[:, None, None]
    qi = jnp.arange(C_BLOCK)[None, :, None]
    ki = jnp.arange(2 * C_BLOCK)[None, None, :]
    dist = C_BLOCK + qi - ki
    ok = (dist >= 0) & (dist <= steps) & (blk * C_BLOCK + ki >= C_BLOCK)
    s = jnp.where(ok, s, -jnp.inf)
    lse = jax.nn.logsumexp(s, axis=-1)
    p = jnp.exp(s - lse[..., None]).astype(v.dtype)
    o = jnp.einsum('brhnqk,brhnkd->brhnqd', p, vw)
    o = o.reshape(bsz, dil, nh, n_blk * C_BLOCK, hd)[:, :, :, :n_sub]
    o = o.transpose(0, 3, 1, 2, 4).reshape(bsz, seq, nh, hd)
    lse = lse.reshape(bsz, dil, nh, n_blk * C_BLOCK)[..., :n_sub]
    lse = lse.transpose(0, 3, 1, 2).reshape(bsz, seq, nh)
    return o, lse


def dilated_mixture(q, k, v):
    outs = []
    lses = []
    for g, (window, dil) in enumerate(C_GROUPS):
        sl = slice(g * C_SLOTS, (g + 1) * C_SLOTS)
        o, l = dilated_group(q[:, :, sl], k[:, :, sl], v[:, :, sl], window // dil, dil)
        outs.append(o)
        lses.append(l)
    w = jax.nn.softmax(jnp.stack(lses, axis=0), axis=0)
    return jnp.sum(w[..., None] * jnp.stack(outs, axis=0), axis=0).astype(q.dtype)


def hybrid_layer(x, norm_g, w_in, w_br_a, w_br_b, w_br_c, w_out, cos, sin):
    bsz, seq, _ = x.shape
    h = rmsnorm(x, norm_g)
    proj = h @ w_in
    (a_q, a_k, a_v, a_g, b_q, b_k, b_v, b_g, i_q, i_k, i_w,
     c_q, c_k, c_v, c_g, m_g) = jnp.split(proj, IN_OFFSETS, axis=-1)

    def heads(t, n):
        return t.reshape(bsz, seq, n, -1)

    def rope1(t):
        return apply_rope(t[:, :, None, :], cos, sin)[:, :, 0, :]

    ya = moba_attention(apply_rope(heads(a_q, A_HEADS), cos, sin),
                        apply_rope(heads(a_k, A_HEADS), cos, sin),
                        heads(a_v, A_HEADS))
    ya = ya.reshape(bsz, seq, A_WIDTH) * jax.nn.silu(a_g)
    yb = dsa_attention(apply_rope(heads(b_q, B_HEADS), cos, sin), rope1(b_k), b_v,
                       apply_rope(heads(i_q, B_IDX_HEADS), cos, sin), rope1(i_k), i_w)
    yb = yb.reshape(bsz, seq, B_WIDTH) * jax.nn.silu(b_g)
    yc = dilated_mixture(apply_rope(heads(c_q, C_HEADS), cos, sin),
                         apply_rope(heads(c_k, C_HEADS), cos, sin),
                         heads(c_v, C_HEADS))
    yc = yc.reshape(bsz, seq, C_WIDTH) * jax.nn.silu(c_g)
    gates = jax.nn.sigmoid(m_g).reshape(bsz, seq, N_BRANCH, D_MODEL)
    merged = (gates[:, :, 0] * (ya @ w_br_a)
              + gates[:, :, 1] * (yb @ w_br_b)
              + gates[:, :, 2] * (yc @ w_br_c))
    return x + merged @ w_out


def setup_inputs(seed: int = 0) -> dict:
    key = jax.random.key(seed)
    ks = jax.random.split(key, 8)
    x = jax.random.normal(ks[0], (BATCH, SEQ, D_MODEL), jnp.float32)
    norm_g = 1.0 + 0.02 * jax.random.normal(ks[1], (DEPTH, D_MODEL), jnp.float32)
    w_in = jax.random.normal(ks[2], (DEPTH, D_MODEL, IN_WIDTH), jnp.float32) * D_MODEL ** -0.5
    w_br_a = jax.random.normal(ks[3], (DEPTH, A_WIDTH, D_MODEL), jnp.float32) * A_WIDTH ** -0.5
    w_br_b = jax.random.normal(ks[4], (DEPTH, B_WIDTH, D_MODEL), jnp.float32) * B_WIDTH ** -0.5
    w_br_c = jax.random.normal(ks[5], (DEPTH, C_WIDTH, D_MODEL), jnp.float32) * C_WIDTH ** -0.5
    w_out = jax.random.normal(ks[6], (DEPTH, D_MODEL, D_MODEL), jnp.float32) * D_MODEL ** -0.5
    final_norm_g = 1.0 + 0.02 * jax.random.normal(ks[7], (D_MODEL,), jnp.float32)
    return {'x': x, 'norm_g': norm_g, 'w_in': w_in, 'w_br_a': w_br_a, 'w_br_b': w_br_b,
            'w_br_c': w_br_c, 'w_out': w_out, 'final_norm_g': final_norm_g}


def reference(x, norm_g, w_in, w_br_a, w_br_b, w_br_c, w_out, final_norm_g):
    cos, sin = rope_tables(x.shape[1])
    for layer in range(DEPTH):
        x = hybrid_layer(x, norm_g[layer], w_in[layer], w_br_a[layer], w_br_b[layer],
                         w_br_c[layer], w_out[layer], cos, sin)
    return rmsnorm(x, final_norm_g)
```

```python
import numpy as np
import ml_dtypes
import concourse.bass as bass
import concourse.mybir as mybir
from concourse.bass_utils import run_bass_kernel_spmd

F32 = mybir.dt.float32
BF16 = mybir.dt.bfloat16
AF = mybir.ActivationFunctionType
ALU = mybir.AluOpType
AX = mybir.AxisListType

S = 2048
D = 1024
NT = 16
NEG = -30000.0
NBIS = 14

_splits = (384, 384, 384, 384, 384, 64, 64, 384, 256, 64, 4, 768, 768, 768, 256, 3072)
_names = ("a_q", "a_k", "a_v", "a_g", "b_q", "b_k", "b_v", "b_g", "i_q", "i_k", "i_w",
          "c_q", "c_k", "c_v", "c_g", "m_g")
_off = {}
_o = 0
for _n, _s in zip(_names, _splits):
    _off[_n] = _o
    _o += _s


def _rot(cols):
    cols = np.asarray(cols).reshape(-1, 2, 32)
    return cols[:, ::-1, :].reshape(-1)


def _tile_cols():
    t = []
    rng = lambda n, a, b: np.arange(_off[n] + a, _off[n] + b)
    for p in range(3):
        c = rng("a_q", 128 * p, 128 * p + 128); t.append((f"AQ{p}", c)); t.append((f"AQR{p}", _rot(c)))
        c = rng("a_k", 128 * p, 128 * p + 128); t.append((f"AK{p}", c)); t.append((f"AKR{p}", _rot(c)))
        t.append((f"AG{p}", rng("a_g", 128 * p, 128 * p + 128)))
        t.append((f"AV{p}", rng("a_v", 128 * p, 128 * p + 128)))
    for p in range(3):
        c = rng("b_q", 128 * p, 128 * p + 128); t.append((f"BQ{p}", c)); t.append((f"BQR{p}", _rot(c)))
        t.append((f"BG{p}", rng("b_g", 128 * p, 128 * p + 128)))
    c = np.concatenate([rng("b_k", 0, 64), rng("b_k", 0, 64)]); t.append(("BK", c)); t.append(("BKR", _rot(c)))
    c = np.concatenate([rng("i_k", 0, 64), rng("i_k", 0, 64)]); t.append(("IK", c)); t.append(("IKR", _rot(c)))
    for p in range(2):
        c = rng("i_q", 128 * p, 128 * p + 128); t.append((f"IQ{p}", c)); t.append((f"IQR{p}", _rot(c)))
    c = np.concatenate([rng("b_v", 0, 64), rng("i_w", 0, 4), rng("i_w", 0, 4).repeat(15)]); t.append(("BV", c))
    for g in range(3):
        for p in range(2):
            c = rng("c_q", 256 * g + 128 * p, 256 * g + 128 * p + 128); t.append((f"CQ{g}{p}", c)); t.append((f"CQR{g}{p}", _rot(c)))
            c = rng("c_k", 256 * g + 128 * p, 256 * g + 128 * p + 128); t.append((f"CK{g}{p}", c)); t.append((f"CKR{g}{p}", _rot(c)))
            t.append((f"CV{g}{p}", rng("c_v", 256 * g + 128 * p, 256 * g + 128 * p + 128)))
    for p in range(2):
        t.append((f"CG{p}", rng("c_g", 128 * p, 128 * p + 128)))
    for j in range(24):
        t.append((f"MG{j}", rng("m_g", 128 * j, 128 * j + 128)))
    return t


_TILES = _tile_cols()
_TIDX = {n: i for i, (n, _) in enumerate(_TILES)}
NWT = len(_TILES)
_XIDX = {}
for _i, _n in enumerate([f"WA{c}" for c in range(3)] + [f"WB{c}" for c in range(3)] +
                        [f"WC{c}" for c in range(2)] + [f"WO{c}" for c in range(8)]):
    _XIDX[_n] = NWT + _i
NWALL = NWT + 16


def _pack_weights(w_in, w_br_a, w_br_b, w_br_c, w_out):
    L = w_in.shape[0]
    out = np.empty((L, NWALL, 128, 1024), np.float32)
    allc = np.concatenate([c for _, c in _TILES])
    for l in range(L):
        g = w_in[l][:, allc]
        g = g.reshape(8, 128, NWT, 128).transpose(2, 1, 0, 3)
        out[l, :NWT] = g.reshape(NWT, 128, 1024)
        out[l, NWT:NWT + 3] = w_br_a[l].reshape(3, 128, 1024)
        out[l, NWT + 3:NWT + 6] = w_br_b[l].reshape(3, 128, 1024)
        out[l, NWT + 6:NWT + 8] = w_br_c[l].reshape(2, 128, 1024)
        out[l, NWT + 8:NWT + 16] = w_out[l].reshape(8, 128, 1024)
    return out


CF_COS = 0
CF_SIN = CF_COS + S
CF_NEGTRI = CF_SIN + S
CF_AGM = CF_NEGTRI + 128
CF_AVAL = CF_AGM + 128
CF_AOWN = CF_AVAL + 128
CF_POW2 = CF_AOWN + 128
CF_N = CF_POW2 + 32
CB_ID = 0
CB_MOWN = CB_ID + 128
CB_MPREV = CB_MOWN + 128
CB_ONEHOT = CB_MPREV + 128
CB_ONES = CB_ONEHOT + S
CB_N = CB_ONES + 128


def _consts():
    cf = np.zeros((128, CF_N), np.float32)
    inv = 1.0 / (10000.0 ** (np.arange(0, 64, 2, dtype=np.float32) / 64.0))
    ang = np.arange(S, dtype=np.float32)[None, :] * inv[:, None].astype(np.float32)
    cos = np.cos(ang).astype(np.float32)
    sin = np.sin(ang).astype(np.float32)
    for p in range(128):
        j = p % 32
        cf[p, CF_COS:CF_COS + S] = cos[j]
        cf[p, CF_SIN:CF_SIN + S] = sin[j] * (-1.0 if (p % 64) < 32 else 1.0)
    t = np.arange(128)[:, None]
    s_ = np.arange(128)[None, :]
    cf[:, CF_NEGTRI:CF_NEGTRI + 128] = np.where(s_ <= t, 0.0, -1e30)
    for qt in range(16):
        own = qt // 2
        for n in range(8):
            cf[:, CF_AGM + qt * 8 + n] = 0.0 if n < own else -1e30
            cf[:, CF_AVAL + qt * 8 + n] = 1.0 if n < own else 0.0
            cf[:, CF_AOWN + qt * 8 + n] = 0.0 if n == own else NEG
    for k in range(32):
        cf[:, CF_POW2 + k] = 2.0 ** (-k)
    cb = np.zeros((128, CB_N), np.float32)
    cb[:, CB_ID:CB_ID + 128] = np.eye(128)
    cb[:, CB_MOWN:CB_MOWN + 128] = (t <= s_)
    cb[:, CB_MPREV:CB_MPREV + 128] = (t >= s_)
    for n in range(8):
        cb[n, CB_ONEHOT + 256 * n:CB_ONEHOT + 256 * n + 256] = 1.0
    cb[:, CB_ONES:CB_ONES + 128] = 1.0
    return cf, cb.astype(ml_dtypes.bfloat16)


class Sched:
    ENG = ("pe", "act", "dve", "pool", "sp")

    def __init__(self):
        self.q = {e: [] for e in self.ENG}
        self.cnt = {e: 0 for e in self.ENG}
        self.seen = {e: {} for e in self.ENG}
        self.lastw = {}
        self.readers = {}
        self.dcnt = {}

    def _need(self, eng, reads, writes):
        need = {}

        def add(dep, raw):
            de, dc = dep
            if de == eng and (eng == "pe" or eng == "sp" or not raw):
                return
            if need.get(de, 0) < dc:
                need[de] = dc
        for r in reads:
            w = self.lastw.get(r)
            if w is not None:
                add(w, True)
        for w_ in writes:
            w = self.lastw.get(w_)
            if w is not None:
                add(w, False)
            for de, dc in self.readers.get(w_, {}).items():
                add((de, dc), False)
        waits = []
        for de, dc in need.items():
            if self.seen[eng].get(de, 0) < dc:
                self.seen[eng][de] = dc
                waits.append((de, dc))
        return waits

    def _record(self, tag, reads, writes):
        for r in reads:
            d = self.readers.setdefault(r, {})
            if d.get(tag[0], 0) < tag[1]:
                d[tag[0]] = tag[1]
        for w_ in writes:
            self.lastw[w_] = tag
            self.readers[w_] = {}

    def op(self, eng, fn, reads=(), writes=(), inc=True):
        waits = self._need(eng, reads, writes)
        tag = (eng, self.cnt[eng] + 1)
        if inc:
            self.cnt[eng] += 1
        self.q[eng].append((waits, fn, ("E", eng) if inc else None))
        self._record(tag, reads, writes)

    def dma(self, key, fn, reads=(), writes=(), eng="sp"):
        waits = self._need(eng, reads, writes)
        de = ("dma", key)
        self.dcnt[de] = self.dcnt.get(de, 0) + 16
        self.q[eng].append((waits, fn, ("D", de)))
        self._record((de, self.dcnt[de]), reads, writes)

    def barrier(self):
        tgt = {e: self.cnt[e] for e in ("pe", "act", "dve", "pool")}
        tgt.update(self.dcnt)
        for e in self.ENG:
            waits = []
            for de, dc in tgt.items():
                if de == e or dc == 0:
                    continue
                if self.seen[e].get(de, 0) < dc:
                    self.seen[e][de] = dc
                    waits.append((de, dc))
            if waits:
                self.q[e].append((waits, None, None))

    def final_wait(self, eng, dma_keys):
        waits = [(("dma", k), self.dcnt[("dma", k)]) for k in dma_keys if ("dma", k) in self.dcnt]
        self.q[eng].append((waits, None, None))


def _build(nseq, layers_all=(0, 1), final_norm=True, taps=()):
    nc = bass.Bass("TRN2", target_bir_lowering=False)
    x_d = nc.dram_tensor("x", [nseq, S, D], F32, kind="ExternalInput").ap()
    wt_d = nc.dram_tensor("wt", [2, NWALL, 128, 1024], F32, kind="ExternalInput").ap()
    gv_d = nc.dram_tensor("gv", [3, D], F32, kind="ExternalInput").ap()
    cf_d = nc.dram_tensor("cf", [128, CF_N], F32, kind="ExternalInput").ap()
    cb_d = nc.dram_tensor("cb", [128, CB_N], BF16, kind="ExternalInput").ap()
    out_d = nc.dram_tensor("out", [nseq, S, D], F32, kind="ExternalOutput").ap()
    xs_d = nc.dram_tensor("xscr", [nseq, S, D], F32).ap()
    tap_d = {}
    for name, shape, dt in taps:
        tap_d[name] = nc.dram_tensor("tap_" + name, list(shape), dt, kind="ExternalOutput").ap()

    sc = Sched()
    sb = {}

    def alloc(name, shape, dt):
        t = nc.alloc_sbuf_tensor(name, list(shape), dt) if False else None
        return t

    from contextlib import ExitStack
    es = ExitStack()

    def SB(name, shape, dt):
        t = es.enter_context(nc.sbuf_tensor("sb_" + name, list(shape), dt))
        sb[name] = t
        return t

    def PS(name, shape, dt):
        return es.enter_context(nc.psum_tensor(name, list(shape), dt))

    cf = SB("cf", [128, CF_N], F32)
    cb = SB("cb", [128, CB_N], BF16)
    hT = SB("hT", [128, 8, S], BF16)
    yT = SB("yT", [128, 8, S], BF16)
    gbc = SB("gbc", [128, 2, D], F32)
    wst = SB("wst", [128, 2, 1024], F32)
    wbf = SB("wbf", [128, 4, 1024], BF16)
    xt = SB("xt", [128, 2, D], F32)
    hn = SB("hn", [128, 2, D], BF16)
    st = SB("st", [128, 64], F32)
    scr = SB("scr", [128, 36864], BF16)
    ps = [PS(f"ps{i}", [128, 512], F32) for i in range(8)]

    def scr_view(off_bytes, shape, dt):
        n = int(np.prod(shape))
        if dt == F32:
            assert off_bytes % 4 == 0
            v = scr[:, off_bytes // 2: off_bytes // 2 + 2 * n].bitcast(F32)
        else:
            v = scr[:, off_bytes // 2: off_bytes // 2 + n]
        if len(shape) == 2:
            v = v.rearrange("p (a b) -> p a b", b=shape[1])
        elif len(shape) == 3:
            v = v.rearrange("p (a b c) -> p a b c", b=shape[1], c=shape[2])
        return v

    ident = cb[:, CB_ID:CB_ID + 128]

    def mm(out, lhsT, rhs, start, stop, reads, writes, inc=None):
        if inc is None:
            inc = stop
        sc.op("pe", ("matmul", dict(out=out, lhsT=lhsT, rhs=rhs, start=start, stop=stop, skip_group_check=True)),
              reads=reads, writes=writes, inc=inc)

    def tr(out, in_, reads, writes, inc=True):
        sc.op("pe", ("transpose", dict(out=out, in_=in_, identity=ident[:in_.shape[0], :in_.shape[0]])), reads=reads, writes=writes, inc=inc)

    sc.dma("c0", ("dma_start", dict(out=cf[:], in_=cf_d[:, :])), writes=["cf"])
    sc.dma("c1", ("dma_start", dict(out=cb[:], in_=cb_d[:, :])), writes=["cb"])

    wstate = {"n": 0, "g": 0}

    def load_w(layer, idx, dest=None, dest_key=None):
        i = wstate["n"]; wstate["n"] += 1
        s = i % 2
        sc.dma(("w", s), ("dma_start", dict(out=wst[:, s, :], in_=wt_d[layer, idx, :, :])), writes=[("wst", s)])
        if dest is None:
            b = i % 4
            dest = wbf[:, b, :]
            dest_key = ("wbf", b)
        sc.op("pool", ("tensor_copy", dict(out=dest, in_=wst[:, s, :])), reads=[("wst", s)], writes=[dest_key])
        return dest, dest_key

    def load_g(slot, row):
        src = gv_d[row:row + 1, :].partition_broadcast(128) if False else None
        from concourse.ap import AP
        src = AP(gv_d.tensor, row * D, [[0, 128], [1, D]])
        sc.dma(("g", slot), ("dma_start", dict(out=gbc[:, slot, :], in_=src)), writes=[("gbc", slot)])

    def phase_norm(si, layer, src_d):
        load_g(layer % 2, layer)
        pst = ps[7][:, 0:512].bitcast(BF16)
        for t in range(NT):
            s = t % 2
            sc.dma(("x", s), ("dma_start", dict(out=xt[:, s, :], in_=src_d[si, t * 128:(t + 1) * 128, :])),
                   writes=[("xt", s)])
            ss = st[:, 2 * s:2 * s + 1]
            rs = st[:, 2 * s + 1:2 * s + 2]
            sc.op("act", ("activation", dict(out=hn[:, s, :], in_=xt[:, s, :], func=AF.Square, accum_out=ss)),
                  reads=[("xt", s)], writes=[("hn", s), ("st", s)])
            sc.op("dve", ("tensor_scalar", dict(out=rs, in0=ss, scalar1=1.0 / D, scalar2=1e-6, op0=ALU.mult, op1=ALU.add)),
                  reads=[("st", s)], writes=[("st", s)])
            sc.op("act", ("activation", dict(out=rs, in_=rs, func=AF.Sqrt)), reads=[("st", s)], writes=[("st", s)])
            sc.op("dve", ("reciprocal", dict(out=rs, in_=rs)), reads=[("st", s)], writes=[("st", s)])
            sc.op("dve", ("scalar_tensor_tensor", dict(out=hn[:, s, :], in0=xt[:, s, :], scalar=rs, in1=gbc[:, layer % 2, :],
                                                                     op0=ALU.mult, op1=ALU.mult)),
                  reads=[("xt", s), ("st", s), ("gbc", layer % 2)], writes=[("hn", s)])
            for c in range(8):
                tr(pst[:, c * 128:(c + 1) * 128], hn[:, s, c * 128:(c + 1) * 128], reads=[("hn", s), "cb"], writes=[("ps", 7)], inc=(c == 7))
            sc.op("act", ("copy", dict(out=hT[:, :, t * 128:(t + 1) * 128], in_=pst.rearrange("p (c n) -> p c n", n=128))),
                  reads=[("ps", 7)], writes=[("hT", t // 4)])

    ppp = {"i": 0}

    def proj_fm(layer, name, rope, evac):
        w1, k1 = load_w(layer, _TIDX[name])
        w1 = w1.rearrange("p (c n) -> p c n", n=128)
        if rope:
            w2, k2 = load_w(layer, _TIDX[name[:2] + "R" + name[2:]])
            w2 = w2.rearrange("p (c n) -> p c n", n=128)
        for c4 in range(4):
            i = ppp["i"]; ppp["i"] += 1
            b1 = i % 2
            b2 = 2 + i % 2
            for c in range(8):
                mm(ps[b1][:, :], w1[:, c, :], hT[:, c, c4 * 512:(c4 + 1) * 512], c == 0, c == 7,
                   reads=[k1, ("hT", c4)], writes=[("ps", b1)])
            if rope:
                for c in range(8):
                    mm(ps[b2][:, :], w2[:, c, :], hT[:, c, c4 * 512:(c4 + 1) * 512], c == 0, c == 7,
                       reads=[k2, ("hT", c4)], writes=[("ps", b2)])
                evac(c4, b1, b2)
            else:
                evac(c4, b1)

    rt = SB("rt", [128, 2, 2, 512], F32)

    def rope_evac(dst_fn, dst_keys_fn, split_heads):
        st_ = {"i": 0}

        def ev(c4, b1, b2):
            j = st_["i"] % 2; st_["i"] += 1
            t1 = rt[:, j, 0, :]
            t2 = rt[:, j, 1, :]
            cs = cf[:, CF_COS + c4 * 512:CF_COS + (c4 + 1) * 512]
            sn = cf[:, CF_SIN + c4 * 512:CF_SIN + (c4 + 1) * 512]
            sc.op("dve", ("tensor_tensor", dict(out=t1, in0=ps[b1][:, :], in1=cs, op=ALU.mult)),
                  reads=[("ps", b1), "cf"], writes=[("rt", j, 0)])
            sc.op("dve", ("tensor_tensor", dict(out=t2, in0=ps[b2][:, :], in1=sn, op=ALU.mult)),
                  reads=[("ps", b2), "cf"], writes=[("rt", j, 1)])
            if split_heads:
                for h in range(2):
                    d = dst_fn(c4, h)
                    sc.op("pool", ("tensor_tensor", dict(out=d, in0=t1[64 * h:64 * h + 64, :], in1=t2[64 * h:64 * h + 64, :], op=ALU.add)),
                          reads=[("rt", j, 0), ("rt", j, 1)], writes=dst_keys_fn(c4, h))
            else:
                d = dst_fn(c4, None)
                sc.op("pool", ("tensor_tensor", dict(out=d, in0=t1, in1=t2, op=ALU.add)),
                      reads=[("rt", j, 0), ("rt", j, 1)], writes=dst_keys_fn(c4, None))
        return ev

    def silu_evac(ychunk):
        def ev(c4, b1):
            sc.op("act", ("activation", dict(out=yT[:, ychunk, c4 * 512:(c4 + 1) * 512], in_=ps[b1][:, :], func=AF.Silu)),
                  reads=[("ps", b1)], writes=[("yT", ychunk, c4)])
        return ev

    def proj_tm(layer, name, nblk, tok_fn, evac):
        w1, k1 = load_w(layer, _TIDX[name])
        w1 = w1.rearrange("p (c n) -> p c n", n=128)
        for g4 in range(0, nblk, 4):
            i = ppp["i"]; ppp["i"] += 1
            b1 = i % 2
            for j in range(4):
                blk = g4 + j
                for c in range(8):
                    mm(ps[b1][:, j * 128:(j + 1) * 128], tok_fn(c, blk), w1[:, c, :], (c == 0 and j == 0), c == 7,
                       reads=[k1] + [("hT", q) for q in range(4)], writes=[("ps", b1)], inc=(c == 7 and j == 3))
            evac(g4, b1)

    def mixer_A(layer):
        qa = scr_view(0, [2, S], BF16)
        ka = scr_view(8192, [2, S], BF16)
        va = scr_view(16384, [16, 256], BF16)
        km = scr_view(24576, [2, 8], BF16)
        kmf = scr_view(24576 + 64, [2, 8], F32)
        gt = scr_view(24576 + 256, [128], F32)
        cmp_ = scr_view(24576 + 1024, [128, 8], F32)
        rk = scr_view(24576 + 1024 + 4096, [128], F32)
        nb = scr_view(24576 + 1024 + 4096 + 512, [128], BF16)
        et = scr_view(32768, [2, 512], BF16)
        rd = scr_view(32768 + 2048, [512], F32)
        rn = scr_view(32768 + 4096, [512], F32)
        for e_ in range(2):
            sc.op("pool", ("tensor_copy", dict(out=ka[64:72, e_, :], in_=cb[0:8, CB_ONEHOT:CB_ONEHOT + S])),
                  reads=["cb"], writes=[("ka", e_, c4) for c4 in range(4)])
        for h in range(2):
            sc.op("pool", ("tensor_copy", dict(out=va[:, :, 128 * h + 64:128 * h + 128],
                                                       in_=cb[:, CB_ONES:CB_ONES + 64].unsqueeze(1).to_broadcast([128, 16, 64]))),
                  reads=["cb"], writes=[("va1", h)])
        for p in range(3):
            proj_fm(layer, f"AQ{p}", True, rope_evac(lambda c4, h: qa[0:64, h, c4 * 512:(c4 + 1) * 512],
                                                     lambda c4, h: [("qa", h, c4)], True))
            proj_fm(layer, f"AK{p}", True, rope_evac(lambda c4, h: ka[0:64, h, c4 * 512:(c4 + 1) * 512],
                                                     lambda c4, h: [("ka", h, c4)], True))
            proj_fm(layer, f"AG{p}", False, silu_evac(p))

            def vev(g4, b1):
                for h in range(2):
                    sc.op("act", ("copy", dict(out=va[:, g4:g4 + 4, 128 * h:128 * h + 64],
                                                       in_=ps[b1][:, :].rearrange("p (j n) -> p j n", n=128)[:, :, 64 * h:64 * h + 64])),
                          reads=[("ps", b1)], writes=[("va", h, g4)])
            proj_tm(layer, f"AV{p}", 16, lambda c, blk: hT[:, c, blk * 128:(blk + 1) * 128], vev)
            for h in range(2):
                allk = [("ka", h, c4) for c4 in range(4)]
                allq = [("qa", h, c4) for c4 in range(4)]
                sc.op("dve", ("tensor_reduce", dict(out=kmf[0:64, h, :], in_=ka[0:64, h, :].rearrange("p (n b) -> p n b", b=256),
                                                             axis=AX.X, op=ALU.add)), reads=allk, writes=[("kmf", h)])
                sc.op("dve", ("tensor_copy", dict(out=km[0:64, h, :], in_=kmf[0:64, h, :])), reads=[("kmf", h)], writes=[("km", h)])
                gp = ps[4][:, 0:128]
                for qt in range(16):
                    mm(gp[:, qt * 8:(qt + 1) * 8], qa[0:64, h, qt * 128:(qt + 1) * 128], km[0:64, h, :], True, True,
                       reads=allq + [("km", h)], writes=[("ps", 4)], inc=(qt == 15))
                sc.op("dve", ("tensor_tensor", dict(out=gt, in0=gp, in1=cf[:, CF_AGM:CF_AGM + 128], op=ALU.add)),
                      reads=[("ps", 4), "cf"], writes=["gt"])
                g3 = gt.rearrange("p (q n) -> p q n", n=8)
                sc.op("dve", ("tensor_tensor", dict(out=cmp_.rearrange("p (q n) m -> p q n m", n=8),
                                                       in0=g3.unsqueeze(2).to_broadcast([128, 16, 8, 8]),
                                                       in1=g3.unsqueeze(3).to_broadcast([128, 16, 8, 8]), op=ALU.is_gt)),
                      reads=["gt"], writes=["cmp"])
                sc.op("dve", ("tensor_reduce", dict(out=rk, in_=cmp_, axis=AX.X, op=ALU.add)), reads=["cmp"], writes=["rk"])
                sc.op("dve", ("tensor_scalar", dict(out=rk, in0=rk, scalar1=2.5, scalar2=None, op0=ALU.is_lt)), reads=["rk"], writes=["rk"])
                sc.op("dve", ("tensor_tensor", dict(out=rk, in0=rk, in1=cf[:, CF_AVAL:CF_AVAL + 128], op=ALU.mult)), reads=["rk", "cf"], writes=["rk"])
                sc.op("dve", ("scalar_tensor_tensor", dict(out=rk, in0=rk, scalar=-NEG, in1=cf[:, CF_AOWN:CF_AOWN + 128],
                                                              op0=ALU.mult, op1=ALU.add)), reads=["rk", "cf"], writes=["rk"])
                sc.op("dve", ("tensor_copy", dict(out=nb, in_=rk)), reads=["rk"], writes=["nb"])
                tp = ps[5][:, 0:512].bitcast(BF16)
                for half in range(2):
                    for q8 in range(8):
                        qt = half * 8 + q8
                        tr(tp[0:8, q8 * 128:(q8 + 1) * 128], nb[:, qt * 8:(qt + 1) * 8], reads=["nb", "cb"], writes=[("ps", 5)], inc=(q8 == 7))
                    sc.op("act", ("copy", dict(out=qa[64:72, h, half * 1024:(half + 1) * 1024], in_=tp[0:8, :])),
                          reads=[("ps", 5)], writes=[("qa", h, 2 * half), ("qa", h, 2 * half + 1)])
            ei = 0
            for h in range(2):
                hh = 2 * p + h
                for c4 in range(4):
                    ob = 6 + (c4 % 2)
                    nkt = 4 * c4 + 4
                    for kt in range(nkt):
                        q0 = max(kt * 128, c4 * 512)
                        q1 = (c4 + 1) * 512
                        n = q1 - q0
                        sb_ = 4 + (ei % 2)
                        ej = ei % 2
                        ei += 1
                        mm(ps[sb_][:, 0:n], ka[0:72, h, kt * 128:(kt + 1) * 128], qa[0:72, h, q0:q1], True, True,
                           reads=[("ka", h, kt // 4), ("qa", h, c4)], writes=[("ps", sb_)])
                        sc.op("act", ("activation", dict(out=et[:, ej, 0:n], in_=ps[sb_][:, 0:n], func=AF.Exp, scale=0.125)),
                              reads=[("ps", sb_)], writes=[("et", ej)])
                        if q0 == kt * 128:
                            sc.op("dve", ("tensor_tensor", dict(out=et[:, ej, 0:128], in0=et[:, ej, 0:128],
                                                                          in1=cb[:, CB_MOWN:CB_MOWN + 128], op=ALU.mult)),
                                  reads=[("et", ej), "cb"], writes=[("et", ej)])
                        mm(ps[ob][:, q0 - c4 * 512:512], va[:, kt, 128 * h:128 * h + 128], et[:, ej, 0:n], kt == 0, kt == nkt - 1,
                           reads=[("va", h, (kt // 4) * 4), ("va1", h), ("et", ej)], writes=[("ps", ob)], inc=True)
                    sc.op("dve", ("reciprocal", dict(out=rd[64:128, :], in_=ps[ob][64:128, :])), reads=[("ps", ob)], writes=["rd"])
                    sc.op("dve", ("tensor_tensor", dict(out=rn[64 * h:64 * h + 64, :], in0=ps[ob][0:64, :], in1=rd[64:128, :], op=ALU.mult)),
                          reads=[("ps", ob), "rd"], writes=["rd0"])
                    ydst = yT[64 * h:64 * h + 64, p, c4 * 512:(c4 + 1) * 512]
                    sc.op("pool", ("tensor_tensor", dict(out=ydst, in0=ydst, in1=rn[64 * h:64 * h + 64, :], op=ALU.mult)),
                          reads=["rd0", ("yT", p, c4)], writes=[("yT", p, c4)])

    def mixer_B(layer):
        qb = scr_view(0, [3, S], BF16)
        kb = scr_view(12288, [S], BF16)
        ikb = scr_view(16384, [S], BF16)
        iqb = scr_view(20480, [2, S], BF16)
        vb = scr_view(28672, [16, 128], BF16)
        iw = scr_view(32768, [16, 4], F32)
        acc = scr_view(33024, [2, S], F32)
        tmp = scr_view(49408, [2, 512], F32)
        mk = scr_view(53504, [S], BF16)
        mt = scr_view(57600, [2, 16, 128], BF16)
        et = scr_view(65792, [2, 384], BF16)
        pt = scr_view(67328, [2, 384], BF16)
        rd = scr_view(68864, [384], F32)
        bs = scr_view(70400, [64], F32)
        rn = scr_view(70656, [384], F32)
        sc.op("pool", ("tensor_copy", dict(out=vb[:, :, 64:128], in_=cb[:, CB_ONES:CB_ONES + 64].unsqueeze(1).to_broadcast([128, 16, 64]))),
              reads=["cb"], writes=["vb1"])
        for p in range(3):
            proj_fm(layer, f"BQ{p}", True, rope_evac(lambda c4, h, p=p: qb[:, p, c4 * 512:(c4 + 1) * 512],
                                                     lambda c4, h, p=p: [("qb", c4)], False))
            proj_fm(layer, f"BG{p}", False, silu_evac(3 + p))
        proj_fm(layer, "BK", True, rope_evac(lambda c4, h: kb[:, c4 * 512:(c4 + 1) * 512], lambda c4, h: [("kb", c4)], False))
        proj_fm(layer, "IK", True, rope_evac(lambda c4, h: ikb[:, c4 * 512:(c4 + 1) * 512], lambda c4, h: [("ikb", c4)], False))
        for p in range(2):
            proj_fm(layer, f"IQ{p}", True, rope_evac(lambda c4, h, p=p: iqb[:, p, c4 * 512:(c4 + 1) * 512],
                                                     lambda c4, h, p=p: [("iqb", c4)], False))

        def vev(g4, b1):
            v3 = ps[b1][:, :].rearrange("p (j n) -> p j n", n=128)
            sc.op("act", ("copy", dict(out=vb[:, g4:g4 + 4, 0:64], in_=v3[:, :, 0:64])), reads=[("ps", b1)], writes=[("vb", g4)])
            sc.op("act", ("copy", dict(out=iw[:, g4:g4 + 4, :], in_=v3[:, :, 64:68])), reads=[("ps", b1)], writes=["iw"])
        proj_tm(layer, "BV", 16, lambda c, blk: hT[:, c, blk * 128:(blk + 1) * 128], vev)

        li = 0
        ei = 0
        for qt in range(16):
            N = 128 * (qt + 1)
            a = qt % 2
            qs = slice(qt * 128, (qt + 1) * 128)
            for j in range((N + 511) // 512):
                k0 = j * 512
                n = min(512, N - k0)
                for h in range(4):
                    e_ = h % 2
                    lb = li % 2
                    li += 1
                    mm(ps[lb][:, 0:n], iqb[64 * e_:64 * e_ + 64, h // 2, qs], ikb[64 * e_:64 * e_ + 64, k0:k0 + n], True, True,
                       reads=[("iqb", qt // 4), ("ikb", j)], writes=[("ps", lb)])
                    if h == 0:
                        sc.op("dve", ("tensor_scalar", dict(out=acc[:, a, k0:k0 + n], in0=ps[lb][:, 0:n], scalar1=0.0,
                                                                                   scalar2=iw[:, qt, 0:1], op0=ALU.max, op1=ALU.mult)),
                              reads=[("ps", lb), "iw"], writes=[("acc", a)])
                    else:
                        tj = li % 2
                        sc.op("dve", ("tensor_scalar", dict(out=tmp[:, tj, 0:n], in0=ps[lb][:, 0:n], scalar1=0.0,
                                                                                        scalar2=iw[:, qt, h:h + 1], op0=ALU.max, op1=ALU.mult)),
                              reads=[("ps", lb), "iw"], writes=[("tmp", tj)])
                        sc.op("pool", ("tensor_tensor", dict(out=acc[:, a, k0:k0 + n], in0=acc[:, a, k0:k0 + n],
                                                                                    in1=tmp[:, tj, 0:n], op=ALU.add)),
                              reads=[("acc", a), ("tmp", tj)], writes=[("acc", a)])
            sc.op("pool", ("tensor_tensor", dict(out=acc[:, a, qs], in0=acc[:, a, qs], in1=cf[:, CF_NEGTRI:CF_NEGTRI + 128], op=ALU.add)),
                  reads=[("acc", a), "cf"], writes=[("acc", a)])
            thr = bs[:, 0:1]
            if qt < 2:
                sc.op("dve", ("memset", dict(ap=thr, constant=-1e29)), writes=["thr"])
            else:
                hi = bs[:, 1:2]; lo = bs[:, 2:3]; cnt = bs[:, 3:4]; tt = bs[:, 4:5]
                W = bs[:, 8:8 + NBIS + 2]
                junk = tmp.rearrange("p a b -> p (a b)")
                sc.op("dve", ("tensor_reduce", dict(out=hi, in_=acc[:, a, 0:N], axis=AX.X, op=ALU.max)), reads=[("acc", a)], writes=["bs_hi"])
                sc.op("dve", ("tensor_reduce", dict(out=lo, in_=acc[:, a, 0:N - 128], axis=AX.X, op=ALU.min)), reads=[("acc", a)], writes=["bs_lo"])
                sc.op("dve", ("tensor_tensor", dict(out=tt, in0=hi, in1=lo, op=ALU.subtract)), reads=["bs_hi", "bs_lo"], writes=["bs_tt"])
                sc.op("dve", ("tensor_scalar", dict(out=W, in0=cf[:, CF_POW2:CF_POW2 + NBIS + 2], scalar1=tt, scalar2=None, op0=ALU.mult)),
                      reads=["bs_tt", "cf"], writes=["bs_W"])
                sc.op("dve", ("tensor_tensor", dict(out=thr, in0=lo, in1=W[:, 1:2], op=ALU.add)), reads=["bs_lo", "bs_W"], writes=["thr"])
                for k in range(NBIS):
                    sc.op("dve", ("tensor_scalar", dict(out=junk[:, 0:N] if N <= 1024 else acc[:, 1 - a, 0:N], in0=acc[:, a, 0:N], scalar1=thr, scalar2=0.0,
                                                           op0=ALU.is_ge, op1=ALU.add, accum_out=cnt)),
                          reads=[("acc", a), "thr"], writes=["bs_cnt", ("tmp", 0), ("tmp", 1)] + ([("acc", 1 - a)] if N > 1024 else []))
                    sc.op("dve", ("tensor_scalar", dict(out=tt, in0=cnt, scalar1=255.5, scalar2=0.5, op0=ALU.is_ge, op1=ALU.subtract)),
                          reads=["bs_cnt"], writes=["bs_tt"])
                    sc.op("dve", ("scalar_tensor_tensor", dict(out=thr, in0=tt, scalar=W[:, k + 1:k + 2], in1=thr, op0=ALU.mult, op1=ALU.add)),
                          reads=["bs_tt", "bs_W", "thr"], writes=["thr"])
                sc.op("dve", ("tensor_tensor", dict(out=thr, in0=thr, in1=W[:, NBIS + 1:NBIS + 2], op=ALU.subtract)), reads=["thr", "bs_W"], writes=["thr"])
            sc.op("dve", ("tensor_scalar", dict(out=mk[:, 0:N], in0=acc[:, a, 0:N], scalar1=thr, scalar2=None, op0=ALU.is_ge)),
                  reads=[("acc", a), "thr"], writes=["mk"])
            m = qt % 2
            tp = [ps[2][:, 0:512].bitcast(BF16), ps[3][:, 0:512].bitcast(BF16)]
            for kt in range(qt + 1):
                tr(tp[kt // 8][:, (kt % 8) * 128:(kt % 8 + 1) * 128], mk[:, kt * 128:(kt + 1) * 128], reads=["mk", "cb"], writes=[("ps", 2 + kt // 8)],
                   inc=(kt == qt or kt == 7))
            for half in range((qt // 8) + 1):
                nk = min(8, qt + 1 - 8 * half)
                sc.op("act", ("copy", dict(out=mt[:, m, 8 * half:8 * half + nk, :],
                                                               in_=tp[half][:, 0:nk * 128].rearrange("p (k n) -> p k n", n=128))),
                      reads=[("ps", 2 + half)], writes=[("mt", m)])
            for kt in range(qt + 1):
                for e_ in range(2):
                    sb_ = 4 + (ei % 2)
                    ej = ei % 2
                    ei += 1
                    ob = 6 + e_
                    mm(ps[sb_][:, 0:384], kb[64 * e_:64 * e_ + 64, kt * 128:(kt + 1) * 128], qb[64 * e_:64 * e_ + 64, :, qs], True, True,
                       reads=[("kb", kt // 4), ("qb", qt // 4)], writes=[("ps", sb_)])
                    sc.op("act", ("activation", dict(out=et[:, ej, :], in_=ps[sb_][:, 0:384], func=AF.Exp, scale=0.125)),
                          reads=[("ps", sb_)], writes=[("et", ej)])
                    sc.op("dve", ("tensor_tensor", dict(out=pt[:, ej, :].rearrange("p (h n) -> p h n", n=128),
                                                                         in0=et[:, ej, :].rearrange("p (h n) -> p h n", n=128),
                                                                         in1=mt[:, m, kt, :].unsqueeze(1).to_broadcast([128, 3, 128]), op=ALU.mult)),
                          reads=[("et", ej), ("mt", m)], writes=[("pt", ej)])
                    mm(ps[ob][:, 0:384], vb[:, kt, :], pt[:, ej, :], kt == 0, kt == qt,
                       reads=[("vb", (kt // 4) * 4), "vb1", ("pt", ej)], writes=[("ps", ob)], inc=True)
            for e_ in range(2):
                ob = 6 + e_
                sc.op("dve", ("reciprocal", dict(out=rd[64:128, :], in_=ps[ob][64:128, 0:384])), reads=[("ps", ob)], writes=["rd"])
                sc.op("dve", ("tensor_tensor", dict(out=rn[64 * e_:64 * e_ + 64, :], in0=ps[ob][0:64, 0:384], in1=rd[64:128, :], op=ALU.mult)),
                      reads=[("ps", ob), "rd"], writes=["rd0"])
                ydst = yT[64 * e_:64 * e_ + 64, 3:6, qs]
                sc.op("pool", ("tensor_tensor", dict(out=ydst, in0=ydst, in1=rn[64 * e_:64 * e_ + 64, :].rearrange("p (h n) -> p h n", n=128), op=ALU.mult)),
                      reads=["rd0"] + [("yT", 3 + p, qt // 4) for p in range(3)], writes=[("yT", 3 + p, qt // 4) for p in range(3)])

    def mixer_C(layer):
        qc = scr_view(0, [2, S], BF16)
        kc = scr_view(8192, [2, S], BF16)
        vc = scr_view(16384, [16, 512], BF16)
        ac = scr_view(32768, [4, S], F32)
        et = scr_view(65536, [2, 256], BF16)
        rd = scr_view(65536 + 1024, [512], F32)
        rn = scr_view(65536 + 3072, [512], F32)
        sc.op("pool", ("tensor_copy", dict(out=vc.rearrange("p b (h c) -> p b h c", c=128)[:, :, :, 64:128],
                                              in_=cb[:, CB_ONES:CB_ONES + 64].unsqueeze(1).unsqueeze(1).to_broadcast([128, 16, 4, 64]))),
              reads=["cb"], writes=["vc1"])
        for p in range(2):
            proj_fm(layer, f"CG{p}", False, silu_evac(6 + p))
        ei = 0
        first_g = [True]
        for g, dil in enumerate((1, 4, 16)):
            if g not in getattr(_build, 'cgroups', (0, 1, 2)):
                continue
            if g != min(getattr(_build, 'cgroups', (0, 1, 2))):
                first_g[0] = False
            nblk = S // dil // 128

            def toks(r, m, cnt=128):
                st0 = r + dil * 128 * m
                return slice(st0, st0 + dil * (cnt - 1) + 1, dil)
            for p in range(2):
                proj_fm(layer, f"CQ{g}{p}", True, rope_evac(lambda c4, h, p=p: qc[:, p, c4 * 512:(c4 + 1) * 512], lambda c4, h, p=p: [("qc", p, c4)], False))
                proj_fm(layer, f"CK{g}{p}", True, rope_evac(lambda c4, h, p=p: kc[:, p, c4 * 512:(c4 + 1) * 512], lambda c4, h, p=p: [("kc", p, c4)], False))

                def vev(g4, b1, p=p):
                    v4 = ps[b1][:, :].rearrange("p (j h c) -> p j h c", h=2, c=64)
                    sc.op("act", ("copy", dict(out=vc.rearrange("p b (h c) -> p b h c", c=128)[:, g4:g4 + 4, 2 * p:2 * p + 2, 0:64], in_=v4)),
                          reads=[("ps", b1)], writes=[("vc", p, g4)])
                proj_tm(layer, f"CV{g}{p}", 16, lambda c, blk: hT[:, c, toks(blk // nblk, blk % nblk)], vev)
            for j in range(4):
                p, e_ = j // 2, j % 2
                pr = slice(64 * e_, 64 * e_ + 64)
                started = [False] * 4
                allq = [("qc", p, c4) for c4 in range(4)]
                allk = [("kc", p, c4) for c4 in range(4)]
                nmm = 0
                for r in range(dil):
                    for m in range(nblk):
                        blk = r * nblk + m
                        nq = 256 if m + 1 < nblk else 128
                        sb_ = 4 + (ei % 2)
                        ej = ei % 2
                        ei += 1
                        mm(ps[sb_][:, 0:nq], kc[pr, p, toks(r, m)], qc[pr, p, toks(r, m, nq)], True, True,
                           reads=allk + allq, writes=[("ps", sb_)])
                        sc.op("act", ("activation", dict(out=et[:, ej, 0:nq], in_=ps[sb_][:, 0:nq], func=AF.Exp, scale=0.125)),
                              reads=[("ps", sb_)], writes=[("et", ej)])
                        sc.op("dve", ("tensor_tensor", dict(out=et[:, ej, 0:nq], in0=et[:, ej, 0:nq], in1=cb[:, CB_MOWN:CB_MOWN + nq], op=ALU.mult)),
                              reads=[("et", ej), "cb"], writes=[("et", ej)])
                        pieces = []
                        for part in range(nq // 128):
                            t0 = r + dil * 128 * (m + part)
                            if dil <= 4:
                                pieces.append((part * 128, 128, t0))
                            else:
                                for q4 in range(4):
                                    pieces.append((part * 128 + 32 * q4, 32, t0 + dil * 32 * q4))
                        for (c0, cn, t0) in pieces:
                            bnk = t0 // 512
                            o0 = t0 % 512
                            mm(ps[bnk][:, o0:o0 + dil * (cn - 1) + 1:dil], vc[:, blk, 128 * j:128 * j + 128], et[:, ej, c0:c0 + cn],
                               not started[bnk], False, reads=[("vc", p, (blk // 4) * 4), "vc1", ("et", ej)],
                               writes=[("ps", bnk)], inc=True)
                            started[bnk] = True
                for c4 in range(4):
                    dst = ac[:, j, c4 * 512:(c4 + 1) * 512]
                    if first_g[0]:
                        sc.op("act", ("copy", dict(out=dst, in_=ps[c4][:, :])), reads=[("ps", c4)], writes=[("ac", j, c4)])
                    else:
                        sc.op("dve", ("tensor_tensor", dict(out=dst, in0=ps[c4][:, :], in1=dst, op=ALU.add)),
                              reads=[("ps", c4), ("ac", j, c4)], writes=[("ac", j, c4)])
        for j in range(4):
            p, e_ = j // 2, j % 2
            for c4 in range(4):
                cs = slice(c4 * 512, (c4 + 1) * 512)
                sc.op("dve", ("reciprocal", dict(out=rd[64:128, :], in_=ac[64:128, j, cs])), reads=[("ac", j, c4)], writes=["rd"])
                sc.op("act", ("copy", dict(out=rn[64:128, :], in_=ac[0:64, j, cs])), reads=[("ac", j, c4)], writes=[("rn", 1)])
                sc.op("dve", ("tensor_tensor", dict(out=rn[64 * e_:64 * e_ + 64, :], in0=rn[64:128, :], in1=rd[64:128, :], op=ALU.mult)),
                      reads=[("rn", 1), "rd"], writes=[("rn", e_)])
                ydst = yT[64 * e_:64 * e_ + 64, 6 + p, cs]
                sc.op("pool", ("tensor_tensor", dict(out=ydst, in0=ydst, in1=rn[64 * e_:64 * e_ + 64, :], op=ALU.mult)),
                      reads=[("rn", e_), ("yT", 6 + p, c4)], writes=[("yT", 6 + p, c4)])

    def phase_final(si, layer, src_d, dst_d, last):
        mg = scr_view(0, [8, S], BF16)
        wbr = scr_view(32768, [8, D], BF16)
        wo = scr_view(49152, [8, D], BF16)
        gs = scr_view(65536, [3, 512], BF16)
        tm = scr_view(65536 + 3072, [512], F32)
        for c in range(8):
            nm = (f"WA{c}" if c < 3 else f"WB{c - 3}" if c < 6 else f"WC{c - 6}")
            load_w(layer, _XIDX[nm], dest=wbr[:, c, :], dest_key=("wbr", c))
        for c in range(8):
            load_w(layer, _XIDX[f"WO{c}"], dest=wo[:, c, :], dest_key=("wo", c))
        if last:
            load_g(0, 2)
        kch = ((0, 3), (3, 6), (6, 8))
        for dt_ in range(8):
            gw = []
            for br in range(3):
                w_, k_ = load_w(layer, _TIDX[f"MG{8 * br + dt_}"])
                gw.append((w_.rearrange("p (c n) -> p c n", n=128), k_))
            for c4 in range(4):
                cs = slice(c4 * 512, (c4 + 1) * 512)
                for br in range(3):
                    for c in range(8):
                        mm(ps[br][:, :], gw[br][0][:, c, :], hT[:, c, cs], c == 0, c == 7, reads=[gw[br][1], ("hT", c4)], writes=[("ps", br)])
                    sc.op("act", ("activation", dict(out=gs[:, br, :], in_=ps[br][:, :], func=AF.Sigmoid)), reads=[("ps", br)], writes=[("gs", br)])
                for br in range(3):
                    a0, a1 = kch[br]
                    for c in range(a0, a1):
                        mm(ps[3 + br][:, :], wbr[:, c, dt_ * 128:(dt_ + 1) * 128], yT[:, c, cs], c == a0, c == a1 - 1,
                           reads=[("wbr", c), ("yT", c, c4)], writes=[("ps", 3 + br)])
                sc.op("dve", ("tensor_tensor", dict(out=tm, in0=ps[3][:, :], in1=gs[:, 0, :], op=ALU.mult)), reads=[("ps", 3), ("gs", 0)], writes=["tm"])
                sc.op("dve", ("tensor_tensor", dict(out=gs[:, 1, :], in0=ps[4][:, :], in1=gs[:, 1, :], op=ALU.mult)), reads=[("ps", 4), ("gs", 1)], writes=[("gs", 1)])
                sc.op("dve", ("tensor_tensor", dict(out=gs[:, 2, :], in0=ps[5][:, :], in1=gs[:, 2, :], op=ALU.mult)), reads=[("ps", 5), ("gs", 2)], writes=[("gs", 2)])
                sc.op("pool", ("tensor_tensor", dict(out=tm, in0=tm, in1=gs[:, 1, :], op=ALU.add)), reads=["tm", ("gs", 1)], writes=["tm"])
                sc.op("pool", ("tensor_tensor", dict(out=mg[:, dt_, cs], in0=tm, in1=gs[:, 2, :], op=ALU.add)),
                      reads=["tm", ("gs", 2)], writes=[("mg", c4)])
        for t in range(NT):
            s = t % 2
            ts = slice(t * 128, (t + 1) * 128)
            sc.dma(("x", s), ("dma_start", dict(out=xt[:, s, :], in_=src_d[si, ts, :])), writes=[("xt", s)])
            for hf in range(2):
                b = 6 + hf
                for c in range(8):
                    mm(ps[b][:, :], mg[:, c, ts], wo[:, c, hf * 512:(hf + 1) * 512], c == 0, c == 7,
                       reads=[("mg", t // 4), ("wo", c)], writes=[("ps", b)])
                sc.op("dve", ("tensor_tensor", dict(out=xt[:, s, hf * 512:(hf + 1) * 512], in0=ps[b][:, :],
                                                                        in1=xt[:, s, hf * 512:(hf + 1) * 512], op=ALU.add)),
                      reads=[("ps", b), ("xt", s)], writes=[("xt", s)])
            if last:
                ss = st[:, 8 + 2 * s:8 + 2 * s + 1]
                rs = st[:, 8 + 2 * s + 1:8 + 2 * s + 2]
                sc.op("act", ("activation", dict(out=hn[:, s, :], in_=xt[:, s, :], func=AF.Square, accum_out=ss)),
                      reads=[("xt", s)], writes=[("hn", s), ("st", s)])
                sc.op("dve", ("tensor_scalar", dict(out=rs, in0=ss, scalar1=1.0 / D, scalar2=1e-6, op0=ALU.mult, op1=ALU.add)),
                      reads=[("st", s)], writes=[("st", s)])
                sc.op("act", ("activation", dict(out=rs, in_=rs, func=AF.Sqrt)), reads=[("st", s)], writes=[("st", s)])
                sc.op("dve", ("reciprocal", dict(out=rs, in_=rs)), reads=[("st", s)], writes=[("st", s)])
                sc.op("dve", ("scalar_tensor_tensor", dict(out=xt[:, s, :], in0=xt[:, s, :], scalar=rs, in1=gbc[:, 0, :],
                                                                         op0=ALU.mult, op1=ALU.mult)),
                      reads=[("xt", s), ("st", s), ("gbc", 0)], writes=[("xt", s)])
            sc.dma(("o", s), ("dma_start", dict(out=dst_d[si, ts, :], in_=xt[:, s, :])), reads=[("xt", s)], writes=[("dram", si, t)])

    enabled = set(getattr(_build, "enabled", ("A", "B", "C")))
    for si in range(nseq):
        for li_, layer in enumerate(layers_all):
            first = (li_ == 0)
            last = (li_ == len(layers_all) - 1)
            src = x_d if first else xs_d
            dst = out_d if last else xs_d
            phase_norm(si, layer, src)
            if "A" in enabled:
                sc.barrier()
                mixer_A(layer)
            if "B" in enabled:
                sc.barrier()
                mixer_B(layer)
            if "C" in enabled:
                sc.barrier()
                mixer_C(layer)
            sc.barrier()
            for name, shape, dt in taps:
                if name == f"yT{layer}" and si == 0:
                    sc.dma(("tap", name), ("dma_start", dict(out=tap_d[name][:, :, :], in_=yT[:])),
                           reads=[("yT", c, c4) for c in range(8) for c4 in range(4)])
                if name == f"hT{layer}" and si == 0:
                    sc.dma(("tap", name), ("dma_start", dict(out=tap_d[name][:, :, :], in_=hT[:])),
                           reads=[("hT", c4) for c4 in range(4)])
            phase_final(si, layer, src, dst, last and final_norm)
            sc.barrier()
    sc.final_wait("sp", [("o", 0), ("o", 1)] + [("tap", n) for n, _, _ in taps])

    sems = {}
    for e in Sched.ENG:
        sems[e] = es.enter_context(nc.semaphore("s_" + e))
    for de in sc.dcnt:
        sems[de] = es.enter_context(nc.semaphore("d%d" % len(sems)))
    block = es.enter_context(nc.Block())

    def replay(engname):
        def run(eng):
            for waits, fn, inc in sc.q[engname]:
                for de, dc in waits:
                    eng.wait_ge(sems[de], dc)
                if fn is None:
                    continue
                ins = getattr(eng, fn[0])(**fn[1])
                if inc is not None:
                    if inc[0] == "E":
                        ins.then_inc(sems[inc[1]], 1)
                    else:
                        ins.then_inc(sems[inc[1]], 16)
        return run
    block.tensor(replay("pe"))
    block.scalar(replay("act"))
    block.vector(replay("dve"))
    block.gpsimd(replay("pool"))
    block.sync(replay("sp"))
    es.close()
    return nc


_CACHE = {}


def kernel(x, norm_g, w_in, w_br_a, w_br_b, w_br_c, w_out, final_norm_g):
    x = np.ascontiguousarray(np.asarray(x, np.float32))
    ncores = 8
    nseq = x.shape[0] // ncores
    wt = _pack_weights(np.asarray(w_in, np.float32), np.asarray(w_br_a, np.float32), np.asarray(w_br_b, np.float32),
                       np.asarray(w_br_c, np.float32), np.asarray(w_out, np.float32))
    gv = np.concatenate([np.asarray(norm_g, np.float32), np.asarray(final_norm_g, np.float32)[None, :]], axis=0)
    cf, cb = _consts()
    nc = _build(nseq)
    in_maps = [{"x": x[i * nseq:(i + 1) * nseq], "wt": wt, "gv": gv, "cf": cf, "cb": cb} for i in range(ncores)]
    res = run_bass_kernel_spmd(nc, in_maps, core_ids=list(range(ncores)))
    return np.concatenate([r["out"] for r in res.results], axis=0)
```

```python
import numpy as np
import ml_dtypes
import concourse.bass as bass
import concourse.mybir as mybir
from concourse.bass_utils import run_bass_kernel_spmd

F32 = mybir.dt.float32
BF16 = mybir.dt.bfloat16
AF = mybir.ActivationFunctionType
ALU = mybir.AluOpType
AX = mybir.AxisListType

S = 2048
D = 1024
NT = 16
NEG = -30000.0
NBIS = 12
ANNOTATE = False

_splits = (384, 384, 384, 384, 384, 64, 64, 384, 256, 64, 4, 768, 768, 768, 256, 3072)
_names = ("a_q", "a_k", "a_v", "a_g", "b_q", "b_k", "b_v", "b_g", "i_q", "i_k", "i_w",
          "c_q", "c_k", "c_v", "c_g", "m_g")
_off = {}
_o = 0
for _n, _s in zip(_names, _splits):
    _off[_n] = _o
    _o += _s


def _rot(cols):
    cols = np.asarray(cols).reshape(-1, 2, 32)
    return cols[:, ::-1, :].reshape(-1)


def _tile_cols():
    t = []
    rng = lambda n, a, b: np.arange(_off[n] + a, _off[n] + b)
    for p in range(3):
        c = rng("a_q", 128 * p, 128 * p + 128); t.append((f"AQ{p}", c)); t.append((f"AQR{p}", _rot(c)))
        c = rng("a_k", 128 * p, 128 * p + 128); t.append((f"AK{p}", c)); t.append((f"AKR{p}", _rot(c)))
        t.append((f"AG{p}", rng("a_g", 128 * p, 128 * p + 128)))
        t.append((f"AV{p}", rng("a_v", 128 * p, 128 * p + 128)))
    for p in range(3):
        c = rng("b_q", 128 * p, 128 * p + 128); t.append((f"BQ{p}", c)); t.append((f"BQR{p}", _rot(c)))
        t.append((f"BG{p}", rng("b_g", 128 * p, 128 * p + 128)))
    c = np.concatenate([rng("b_k", 0, 64), rng("b_k", 0, 64)]); t.append(("BK", c)); t.append(("BKR", _rot(c)))
    c = np.concatenate([rng("i_k", 0, 64), rng("i_k", 0, 64)]); t.append(("IK", c)); t.append(("IKR", _rot(c)))
    for p in range(2):
        c = rng("i_q", 128 * p, 128 * p + 128); t.append((f"IQ{p}", c)); t.append((f"IQR{p}", _rot(c)))
    c = np.concatenate([rng("b_v", 0, 64), rng("i_w", 0, 4), rng("i_w", 0, 4).repeat(15)]); t.append(("BV", c))
    for g in range(3):
        for p in range(2):
            c = rng("c_q", 256 * g + 128 * p, 256 * g + 128 * p + 128); t.append((f"CQ{g}{p}", c)); t.append((f"CQR{g}{p}", _rot(c)))
            c = rng("c_k", 256 * g + 128 * p, 256 * g + 128 * p + 128); t.append((f"CK{g}{p}", c)); t.append((f"CKR{g}{p}", _rot(c)))
            t.append((f"CV{g}{p}", rng("c_v", 256 * g + 128 * p, 256 * g + 128 * p + 128)))
    for p in range(2):
        t.append((f"CG{p}", rng("c_g", 128 * p, 128 * p + 128)))
    for j in range(24):
        t.append((f"MG{j}", rng("m_g", 128 * j, 128 * j + 128)))
    return t


_TILES = _tile_cols()
_TIDX = {n: i for i, (n, _) in enumerate(_TILES)}
NWT = len(_TILES)
_XIDX = {}
for _i, _n in enumerate([f"WA{c}" for c in range(3)] + [f"WB{c}" for c in range(3)] +
                        [f"WC{c}" for c in range(2)] + [f"WO{c}" for c in range(8)]):
    _XIDX[_n] = NWT + _i
NWALL = NWT + 16


def _pack_weights(w_in, w_br_a, w_br_b, w_br_c, w_out):
    L = w_in.shape[0]
    out = np.empty((L, NWALL, 128, 1024), np.float32)
    allc = np.concatenate([c for _, c in _TILES])
    for l in range(L):
        g = w_in[l][:, allc]
        g = g.reshape(8, 128, NWT, 128).transpose(2, 1, 0, 3)
        out[l, :NWT] = g.reshape(NWT, 128, 1024)
        out[l, NWT:NWT + 3] = w_br_a[l].reshape(3, 128, 1024)
        out[l, NWT + 3:NWT + 6] = w_br_b[l].reshape(3, 128, 1024)
        out[l, NWT + 6:NWT + 8] = w_br_c[l].reshape(2, 128, 1024)
        out[l, NWT + 8:NWT + 16] = w_out[l].reshape(8, 128, 1024)
    return out


CF_COS = 0
CF_SIN = CF_COS + S
CF_NEGTRI = CF_SIN + S
CF_AGM = CF_NEGTRI + 128
CF_AVAL = CF_AGM + 128
CF_AOWN = CF_AVAL + 128
CF_POW2 = CF_AOWN + 128
CF_N = CF_POW2 + 32
CB_ID = 0
CB_MOWN = CB_ID + 128
CB_MPREV = CB_MOWN + 128
CB_ONEHOT = CB_MPREV + 128
CB_ONES = CB_ONEHOT + S
CB_N = CB_ONES + 128


def _consts():
    cf = np.zeros((128, CF_N), np.float32)
    inv = 1.0 / (10000.0 ** (np.arange(0, 64, 2, dtype=np.float32) / 64.0))
    ang = np.arange(S, dtype=np.float32)[None, :] * inv[:, None].astype(np.float32)
    cos = np.cos(ang).astype(np.float32)
    sin = np.sin(ang).astype(np.float32)
    for p in range(128):
        j = p % 32
        cf[p, CF_COS:CF_COS + S] = cos[j]
        cf[p, CF_SIN:CF_SIN + S] = sin[j] * (-1.0 if (p % 64) < 32 else 1.0)
    t = np.arange(128)[:, None]
    s_ = np.arange(128)[None, :]
    cf[:, CF_NEGTRI:CF_NEGTRI + 128] = np.where(s_ <= t, 0.0, -1e30)
    for qt in range(16):
        own = qt // 2
        for n in range(8):
            cf[:, CF_AGM + qt * 8 + n] = 0.0 if n < own else -1e30
            cf[:, CF_AVAL + qt * 8 + n] = 1.0 if n < own else 0.0
            cf[:, CF_AOWN + qt * 8 + n] = 0.0 if n == own else NEG
    for k in range(32):
        cf[:, CF_POW2 + k] = 2.0 ** (-k)
    cb = np.zeros((128, CB_N), np.float32)
    cb[:, CB_ID:CB_ID + 128] = np.eye(128)
    cb[:, CB_MOWN:CB_MOWN + 128] = (t <= s_)
    cb[:, CB_MPREV:CB_MPREV + 128] = (t >= s_)
    for n in range(8):
        cb[n, CB_ONEHOT + 256 * n:CB_ONEHOT + 256 * n + 256] = 1.0
    cb[:, CB_ONES:CB_ONES + 128] = 1.0
    return cf, cb.astype(ml_dtypes.bfloat16)


class Sched:
    ENG = ("pe", "act", "dve", "pool", "sp")

    def __init__(self):
        self.q = {e: [] for e in self.ENG}
        self.cnt = {e: 0 for e in self.ENG}
        self.seen = {e: {} for e in self.ENG}
        self.lastw = {}
        self.readers = {}
        self.dcnt = {}
        self.phase = "init"

    def _need(self, eng, reads, writes):
        need = {}

        def add(dep, raw):
            de, dc = dep
            if de == eng and (eng == "pe" or eng == "sp"):
                return
            if need.get(de, 0) < dc:
                need[de] = dc
        for r in reads:
            w = self.lastw.get(r)
            if w is not None:
                add(w, True)
        for w_ in writes:
            w = self.lastw.get(w_)
            if w is not None:
                add(w, False)
            for de, dc in self.readers.get(w_, {}).items():
                add((de, dc), False)
        waits = []
        for de, dc in need.items():
            if self.seen[eng].get(de, 0) < dc:
                self.seen[eng][de] = dc
                waits.append((de, dc))
        return waits

    def _record(self, tag, reads, writes):
        for r in reads:
            d = self.readers.setdefault(r, {})
            if d.get(tag[0], 0) < tag[1]:
                d[tag[0]] = tag[1]
        for w_ in writes:
            self.lastw[w_] = tag
            self.readers[w_] = {}

    def op(self, eng, fn, reads=(), writes=(), inc=True):
        waits = self._need(eng, reads, writes)
        tag = (eng, self.cnt[eng] + 1)
        if inc:
            self.cnt[eng] += 1
        self.q[eng].append((waits, fn, ("E", eng) if inc else None, self.phase))
        self._record(tag, reads, writes)

    def dma(self, key, fn, reads=(), writes=(), eng="sp"):
        waits = self._need(eng, reads, writes)
        de = ("dma", key)
        self.dcnt[de] = self.dcnt.get(de, 0) + 16
        self.q[eng].append((waits, fn, ("D", de), self.phase))
        self._record((de, self.dcnt[de]), reads, writes)

    def barrier(self):
        tgt = {e: self.cnt[e] for e in ("pe", "act", "dve", "pool")}
        tgt.update(self.dcnt)
        for e in self.ENG:
            waits = []
            for de, dc in tgt.items():
                if de == e or dc == 0:
                    continue
                if self.seen[e].get(de, 0) < dc:
                    self.seen[e][de] = dc
                    waits.append((de, dc))
            if waits:
                self.q[e].append((waits, None, None, None))

    def final_wait(self, eng, dma_keys):
        waits = [(("dma", k), self.dcnt[("dma", k)]) for k in dma_keys if ("dma", k) in self.dcnt]
        self.q[eng].append((waits, None, None, None))


def _build(nseq, layers_all=(0, 1), final_norm=True, taps=()):
    nc = bass.Bass("TRN2", target_bir_lowering=False)
    x_d = nc.dram_tensor("x", [nseq, S, D], F32, kind="ExternalInput").ap()
    wt_d = nc.dram_tensor("wt", [2, NWALL, 128, 1024], F32, kind="ExternalInput").ap()
    gv_d = nc.dram_tensor("gv", [3, D], F32, kind="ExternalInput").ap()
    cf_d = nc.dram_tensor("cf", [128, CF_N], F32, kind="ExternalInput").ap()
    cb_d = nc.dram_tensor("cb", [128, CB_N], BF16, kind="ExternalInput").ap()
    out_d = nc.dram_tensor("out", [nseq, S, D], F32, kind="ExternalOutput").ap()
    xs_d = nc.dram_tensor("xscr", [nseq, S, D], F32).ap()
    tap_d = {}
    for name, shape, dt in taps:
        tap_d[name] = nc.dram_tensor("tap_" + name, list(shape), dt, kind="ExternalOutput").ap()

    sc = Sched()
    sb = {}

    def alloc(name, shape, dt):
        t = nc.alloc_sbuf_tensor(name, list(shape), dt) if False else None
        return t

    from contextlib import ExitStack
    es = ExitStack()

    def SB(name, shape, dt):
        t = es.enter_context(nc.sbuf_tensor("sb_" + name, list(shape), dt))
        sb[name] = t
        return t

    def PS(name, shape, dt):
        return es.enter_context(nc.psum_tensor(name, list(shape), dt))

    cf = SB("cf", [128, CF_N], F32)
    cb = SB("cb", [128, CB_N], BF16)
    hT = SB("hT", [128, 8, S], BF16)
    yT = SB("yT", [128, 8, S], BF16)
    gbc = SB("gbc", [128, 2, D], F32)
    wbf = SB("wbf", [128, 8, 1024], BF16)
    xt = SB("xt", [128, 2, D], F32)
    hn = SB("hn", [128, 2, D], BF16)
    st = SB("st", [128, 64], F32)
    scr = SB("scr", [128, 36864], BF16)
    ps = [PS(f"ps{i}", [128, 512], F32) for i in range(8)]

    def scr_view(off_bytes, shape, dt):
        n = int(np.prod(shape))
        if dt == F32:
            assert off_bytes % 4 == 0
            v = scr[:, off_bytes // 2: off_bytes // 2 + 2 * n].bitcast(F32)
        else:
            v = scr[:, off_bytes // 2: off_bytes // 2 + n]
        if len(shape) == 2:
            v = v.rearrange("p (a b) -> p a b", b=shape[1])
        elif len(shape) == 3:
            v = v.rearrange("p (a b c) -> p a b c", b=shape[1], c=shape[2])
        return v

    ident = cb[:, CB_ID:CB_ID + 128]

    def mm(out, lhsT, rhs, start, stop, reads, writes, inc=None):
        if inc is None:
            inc = stop
        sc.op("pe", ("matmul", dict(out=out, lhsT=lhsT, rhs=rhs, start=start, stop=stop, skip_group_check=True)),
              reads=reads, writes=writes, inc=inc)

    def tr(out, in_, reads, writes, inc=True):
        sc.op("pe", ("transpose", dict(out=out, in_=in_, identity=ident[:in_.shape[0], :in_.shape[0]])), reads=reads, writes=writes, inc=inc)


    NSLOT = 8
    WDEPTH = 4
    wstate = {"n": 0, "ring": 0, "rec": True, "issued": 0, "xd": 0}
    wplan = []

    def _issue_w(ent):
        layer, idx, dest, dest_key, skey = ent
        sc.dma(skey, ("dma_start", dict(out=dest, in_=wt_d[layer, idx, :, :])), writes=[dest_key], eng="pool")

    def load_w(layer, idx, dest=None, dest_key=None):
        n = wstate["n"]; wstate["n"] += 1
        if wstate["rec"]:
            if dest is None:
                b = wstate["ring"] % NSLOT; wstate["ring"] += 1
                dest = wbf[:, b, :]
                dest_key = ("wbf", b)
                skey = ("w", b)
            else:
                skey = ("wx", wstate["xd"] % 16); wstate["xd"] += 1
            wplan.append((layer, idx, dest, dest_key, skey))
            return dest, dest_key
        while wstate["issued"] < len(wplan) and wstate["issued"] <= n + WDEPTH:
            ent = wplan[wstate["issued"]]
            if ent[4][0] == "wx" and wstate["issued"] > n:
                break
            _issue_w(ent)
            wstate["issued"] += 1
        ent = wplan[n]
        return ent[2], ent[3]

    def load_g(slot, row):
        src = gv_d[row:row + 1, :].partition_broadcast(128) if False else None
        from concourse.ap import AP
        src = AP(gv_d.tensor, row * D, [[0, 128], [1, D]])
        sc.dma(("g", slot), ("dma_start", dict(out=gbc[:, slot, :], in_=src)), writes=[("gbc", slot)])

    def phase_norm(si, layer, src_d):
        load_g(layer % 2, layer)
        pst = ps[7][:, 0:512].bitcast(BF16)
        for t in range(NT):
            s = t % 2
            sc.dma(("x", s), ("dma_start", dict(out=xt[:, s, :], in_=src_d[si, t * 128:(t + 1) * 128, :])),
                   writes=[("xt", s)])
            ss = st[:, 2 * s:2 * s + 1]
            rs = st[:, 2 * s + 1:2 * s + 2]
            sc.op("act", ("activation", dict(out=hn[:, s, :], in_=xt[:, s, :], func=AF.Square, accum_out=ss)),
                  reads=[("xt", s)], writes=[("hn", s), ("st", s)])
            sc.op("dve", ("tensor_scalar", dict(out=rs, in0=ss, scalar1=1.0 / D, scalar2=1e-6, op0=ALU.mult, op1=ALU.add)),
                  reads=[("st", s)], writes=[("st", s)])
            sc.op("act", ("activation", dict(out=rs, in_=rs, func=AF.Sqrt)), reads=[("st", s)], writes=[("st", s)])
            sc.op("dve", ("reciprocal", dict(out=rs, in_=rs)), reads=[("st", s)], writes=[("st", s)])
            sc.op("dve", ("scalar_tensor_tensor", dict(out=hn[:, s, :], in0=xt[:, s, :], scalar=rs, in1=gbc[:, layer % 2, :],
                                                                     op0=ALU.mult, op1=ALU.mult)),
                  reads=[("xt", s), ("st", s), ("gbc", layer % 2)], writes=[("hn", s)])
            for c in range(8):
                tr(pst[:, c * 128:(c + 1) * 128], hn[:, s, c * 128:(c + 1) * 128], reads=[("hn", s), "cb"], writes=[("ps", 7)], inc=(c == 7))
            sc.op("act", ("copy", dict(out=hT[:, :, t * 128:(t + 1) * 128], in_=pst.rearrange("p (c n) -> p c n", n=128))),
                  reads=[("ps", 7)], writes=[("hT", t // 4)])

    ppp = {"i": 0}

    def proj_fm(layer, name, rope, evac):
        w1, k1 = load_w(layer, _TIDX[name])
        w1 = w1.rearrange("p (c n) -> p c n", n=128)
        if rope:
            w2, k2 = load_w(layer, _TIDX[name[:2] + "R" + name[2:]])
            w2 = w2.rearrange("p (c n) -> p c n", n=128)
        for c4 in range(4):
            i = ppp["i"]; ppp["i"] += 1
            b1 = i % 2
            b2 = 2 + i % 2
            for c in range(8):
                mm(ps[b1][:, :], w1[:, c, :], hT[:, c, c4 * 512:(c4 + 1) * 512], c == 0, c == 7,
                   reads=[k1, ("hT", c4)], writes=[("ps", b1)])
            if rope:
                for c in range(8):
                    mm(ps[b2][:, :], w2[:, c, :], hT[:, c, c4 * 512:(c4 + 1) * 512], c == 0, c == 7,
                       reads=[k2, ("hT", c4)], writes=[("ps", b2)])
                evac(c4, b1, b2)
            else:
                evac(c4, b1)

    rt = SB("rt", [128, 2, 2, 512], F32)

    def rope_evac(dst_fn, dst_keys_fn, split_heads):
        st_ = {"i": 0}

        def ev(c4, b1, b2):
            j = st_["i"] % 2; st_["i"] += 1
            t1 = rt[:, j, 0, :]
            t2 = rt[:, j, 1, :]
            cs = cf[:, CF_COS + c4 * 512:CF_COS + (c4 + 1) * 512]
            sn = cf[:, CF_SIN + c4 * 512:CF_SIN + (c4 + 1) * 512]
            sc.op("dve", ("tensor_tensor", dict(out=t1, in0=ps[b1][:, :], in1=cs, op=ALU.mult)),
                  reads=[("ps", b1), "cf"], writes=[("rt", j, 0)])
            sc.op("dve", ("tensor_tensor", dict(out=t2, in0=ps[b2][:, :], in1=sn, op=ALU.mult)),
                  reads=[("ps", b2), "cf"], writes=[("rt", j, 1)])
            if split_heads:
                for h in range(2):
                    d = dst_fn(c4, h)
                    sc.op("pool", ("tensor_tensor", dict(out=d, in0=t1[64 * h:64 * h + 64, :], in1=t2[64 * h:64 * h + 64, :], op=ALU.add)),
                          reads=[("rt", j, 0), ("rt", j, 1)], writes=dst_keys_fn(c4, h))
            else:
                d = dst_fn(c4, None)
                sc.op("pool", ("tensor_tensor", dict(out=d, in0=t1, in1=t2, op=ALU.add)),
                      reads=[("rt", j, 0), ("rt", j, 1)], writes=dst_keys_fn(c4, None))
        return ev

    def silu_evac(ychunk):
        def ev(c4, b1):
            sc.op("act", ("activation", dict(out=yT[:, ychunk, c4 * 512:(c4 + 1) * 512], in_=ps[b1][:, :], func=AF.Silu)),
                  reads=[("ps", b1)], writes=[("yT", ychunk, c4)])
        return ev

    def proj_tm(layer, name, nblk, tok_fn, evac):
        w1, k1 = load_w(layer, _TIDX[name])
        w1 = w1.rearrange("p (c n) -> p c n", n=128)
        for g4 in range(0, nblk, 4):
            i = ppp["i"]; ppp["i"] += 1
            b1 = i % 2
            for j in range(4):
                blk = g4 + j
                for c in range(8):
                    mm(ps[b1][:, j * 128:(j + 1) * 128], tok_fn(c, blk), w1[:, c, :], (c == 0 and j == 0), c == 7,
                       reads=[k1] + [("hT", q) for q in range(4)], writes=[("ps", b1)], inc=(c == 7 and j == 3))
            evac(g4, b1)

    def mixer_A(layer):
        qa = scr_view(0, [2, S], BF16)
        ka = scr_view(8192, [2, S], BF16)
        va = scr_view(16384, [16, 256], BF16)
        km = scr_view(24576, [2, 8], BF16)
        kmf = scr_view(24576 + 64, [2, 8], F32)
        gt = scr_view(24576 + 256, [128], F32)
        cmp_ = scr_view(24576 + 1024, [128, 8], F32)
        rk = scr_view(24576 + 1024 + 4096, [128], F32)
        nb = scr_view(24576 + 1024 + 4096 + 512, [128], BF16)
        et = scr_view(32768, [2, 512], BF16)
        rd = scr_view(32768 + 2048, [512], F32)
        rn = scr_view(32768 + 4096, [512], F32)
        for e_ in range(2):
            sc.op("pool", ("tensor_copy", dict(out=ka[64:72, e_, :], in_=cb[0:8, CB_ONEHOT:CB_ONEHOT + S])),
                  reads=["cb"], writes=[("ka", e_, c4) for c4 in range(4)])
        for h in range(2):
            sc.op("pool", ("tensor_copy", dict(out=va[:, :, 128 * h + 64:128 * h + 128],
                                                       in_=cb[:, CB_ONES:CB_ONES + 64].unsqueeze(1).to_broadcast([128, 16, 64]))),
                  reads=["cb"], writes=[("va1", h)])
        for p in range(3):
            proj_fm(layer, f"AQ{p}", True, rope_evac(lambda c4, h: qa[0:64, h, c4 * 512:(c4 + 1) * 512],
                                                     lambda c4, h: [("qa", h, c4)], True))
            proj_fm(layer, f"AK{p}", True, rope_evac(lambda c4, h: ka[0:64, h, c4 * 512:(c4 + 1) * 512],
                                                     lambda c4, h: [("ka", h, c4)], True))
            proj_fm(layer, f"AG{p}", False, silu_evac(p))

            def vev(g4, b1):
                for h in range(2):
                    sc.op("act", ("copy", dict(out=va[:, g4:g4 + 4, 128 * h:128 * h + 64],
                                                       in_=ps[b1][:, :].rearrange("p (j n) -> p j n", n=128)[:, :, 64 * h:64 * h + 64])),
                          reads=[("ps", b1)], writes=[("va", h, g4)])
            proj_tm(layer, f"AV{p}", 16, lambda c, blk: hT[:, c, blk * 128:(blk + 1) * 128], vev)
            for h in range(2):
                allk = [("ka", h, c4) for c4 in range(4)]
                allq = [("qa", h, c4) for c4 in range(4)]
                sc.op("dve", ("tensor_reduce", dict(out=kmf[0:64, h, :], in_=ka[0:64, h, :].rearrange("p (n b) -> p n b", b=256),
                                                             axis=AX.X, op=ALU.add)), reads=allk, writes=[("kmf", h)])
                sc.op("dve", ("tensor_copy", dict(out=km[0:64, h, :], in_=kmf[0:64, h, :])), reads=[("kmf", h)], writes=[("km", h)])
                gp = ps[4][:, 0:128]
                for qt in range(16):
                    mm(gp[:, qt * 8:(qt + 1) * 8], qa[0:64, h, qt * 128:(qt + 1) * 128], km[0:64, h, :], True, True,
                       reads=allq + [("km", h)], writes=[("ps", 4)], inc=(qt == 15))
                sc.op("dve", ("tensor_tensor", dict(out=gt, in0=gp, in1=cf[:, CF_AGM:CF_AGM + 128], op=ALU.add)),
                      reads=[("ps", 4), "cf"], writes=["gt"])
                g3 = gt.rearrange("p (q n) -> p q n", n=8)
                sc.op("dve", ("tensor_tensor", dict(out=cmp_.rearrange("p (q n) m -> p q n m", n=8),
                                                       in0=g3.unsqueeze(2).to_broadcast([128, 16, 8, 8]),
                                                       in1=g3.unsqueeze(3).to_broadcast([128, 16, 8, 8]), op=ALU.is_gt)),
                      reads=["gt"], writes=["cmp"])
                sc.op("dve", ("tensor_reduce", dict(out=rk, in_=cmp_, axis=AX.X, op=ALU.add)), reads=["cmp"], writes=["rk"])
                sc.op("dve", ("tensor_scalar", dict(out=rk, in0=rk, scalar1=2.5, scalar2=None, op0=ALU.is_lt)), reads=["rk"], writes=["rk"])
                sc.op("dve", ("tensor_tensor", dict(out=rk, in0=rk, in1=cf[:, CF_AVAL:CF_AVAL + 128], op=ALU.mult)), reads=["rk", "cf"], writes=["rk"])
                sc.op("dve", ("scalar_tensor_tensor", dict(out=rk, in0=rk, scalar=-NEG, in1=cf[:, CF_AOWN:CF_AOWN + 128],
                                                              op0=ALU.mult, op1=ALU.add)), reads=["rk", "cf"], writes=["rk"])
                sc.op("dve", ("tensor_copy", dict(out=nb, in_=rk)), reads=["rk"], writes=["nb"])
                tp = ps[5][:, 0:512].bitcast(BF16)
                for half in range(2):
                    for q8 in range(8):
                        qt = half * 8 + q8
                        tr(tp[0:8, q8 * 128:(q8 + 1) * 128], nb[:, qt * 8:(qt + 1) * 8], reads=["nb", "cb"], writes=[("ps", 5)], inc=(q8 == 7))
                    sc.op("act", ("copy", dict(out=qa[64:72, h, half * 1024:(half + 1) * 1024], in_=tp[0:8, :])),
                          reads=[("ps", 5)], writes=[("qa", h, 2 * half), ("qa", h, 2 * half + 1)])
            ei = 0
            for h in range(2):
                hh = 2 * p + h
                for c4 in range(4):
                    ob = 6 + (c4 % 2)
                    nkt = 4 * c4 + 4
                    for kt in range(nkt):
                        q0 = max(kt * 128, c4 * 512)
                        q1 = (c4 + 1) * 512
                        n = q1 - q0
                        sb_ = 4 + (ei % 2)
                        ej = ei % 2
                        ei += 1
                        mm(ps[sb_][:, 0:n], ka[0:72, h, kt * 128:(kt + 1) * 128], qa[0:72, h, q0:q1], True, True,
                           reads=[("ka", h, kt // 4), ("qa", h, c4)], writes=[("ps", sb_)])
                        sc.op("act", ("activation", dict(out=et[:, ej, 0:n], in_=ps[sb_][:, 0:n], func=AF.Exp, scale=0.125)),
                              reads=[("ps", sb_)], writes=[("et", ej)])
                        if q0 == kt * 128:
                            sc.op("dve", ("tensor_tensor", dict(out=et[:, ej, 0:128], in0=et[:, ej, 0:128],
                                                                          in1=cb[:, CB_MOWN:CB_MOWN + 128], op=ALU.mult)),
                                  reads=[("et", ej), "cb"], writes=[("et", ej)])
                        mm(ps[ob][:, q0 - c4 * 512:512], va[:, kt, 128 * h:128 * h + 128], et[:, ej, 0:n], kt == 0, kt == nkt - 1,
                           reads=[("va", h, (kt // 4) * 4), ("va1", h), ("et", ej)], writes=[("ps", ob)], inc=True)
                    sc.op("act", ("activation", dict(out=rd[64:128, :], in_=ps[ob][64:128, :], func=AF.Ln)), reads=[("ps", ob)], writes=["rd"])
                    sc.op("act", ("activation", dict(out=rd[64:128, :], in_=rd[64:128, :], func=AF.Exp, scale=-1.0)), reads=["rd"], writes=["rd"])
                    sc.op("dve", ("tensor_tensor", dict(out=rn[64 * h:64 * h + 64, :], in0=ps[ob][0:64, :], in1=rd[64:128, :], op=ALU.mult)),
                          reads=[("ps", ob), "rd"], writes=["rd0"])
                    ydst = yT[64 * h:64 * h + 64, p, c4 * 512:(c4 + 1) * 512]
                    sc.op("pool", ("tensor_tensor", dict(out=ydst, in0=ydst, in1=rn[64 * h:64 * h + 64, :], op=ALU.mult)),
                          reads=["rd0", ("yT", p, c4)], writes=[("yT", p, c4)])

    def mixer_B(layer):
        qb = scr_view(0, [3, S], BF16)
        kb = scr_view(12288, [S], BF16)
        ikb = scr_view(16384, [S], BF16)
        iqb = scr_view(20480, [2, S], BF16)
        vb = scr_view(28672, [16, 128], BF16)
        iw = scr_view(32768, [16, 4], F32)
        acc = scr_view(33024, [2, S], F32)
        tmp = scr_view(49408, [2, 512], F32)
        mk = scr_view(53504, [S], BF16)
        mt = scr_view(57600, [2, 16, 128], BF16)
        et = scr_view(65792, [2, 384], BF16)
        pt = scr_view(67328, [2, 384], BF16)
        rd = scr_view(68864, [384], F32)
        bs = scr_view(70400, [64], F32)
        rn = scr_view(70656, [384], F32)
        sc.op("pool", ("tensor_copy", dict(out=vb[:, :, 64:128], in_=cb[:, CB_ONES:CB_ONES + 64].unsqueeze(1).to_broadcast([128, 16, 64]))),
              reads=["cb"], writes=["vb1"])
        for p in range(3):
            proj_fm(layer, f"BQ{p}", True, rope_evac(lambda c4, h, p=p: qb[:, p, c4 * 512:(c4 + 1) * 512],
                                                     lambda c4, h, p=p: [("qb", c4)], False))
            proj_fm(layer, f"BG{p}", False, silu_evac(3 + p))
        proj_fm(layer, "BK", True, rope_evac(lambda c4, h: kb[:, c4 * 512:(c4 + 1) * 512], lambda c4, h: [("kb", c4)], False))
        proj_fm(layer, "IK", True, rope_evac(lambda c4, h: ikb[:, c4 * 512:(c4 + 1) * 512], lambda c4, h: [("ikb", c4)], False))
        for p in range(2):
            proj_fm(layer, f"IQ{p}", True, rope_evac(lambda c4, h, p=p: iqb[:, p, c4 * 512:(c4 + 1) * 512],
                                                     lambda c4, h, p=p: [("iqb", c4)], False))

        def vev(g4, b1):
            v3 = ps[b1][:, :].rearrange("p (j n) -> p j n", n=128)
            sc.op("act", ("copy", dict(out=vb[:, g4:g4 + 4, 0:64], in_=v3[:, :, 0:64])), reads=[("ps", b1)], writes=[("vb", g4)])
            sc.op("act", ("copy", dict(out=iw[:, g4:g4 + 4, :], in_=v3[:, :, 64:68])), reads=[("ps", b1)], writes=["iw"])
        proj_tm(layer, "BV", 16, lambda c, blk: hT[:, c, blk * 128:(blk + 1) * 128], vev)

        li = 0
        ei = 0
        for qt in range(16):
            N = 128 * (qt + 1)
            a = qt % 2
            qs = slice(qt * 128, (qt + 1) * 128)
            for j in range((N + 511) // 512):
                k0 = j * 512
                n = min(512, N - k0)
                for h in range(4):
                    e_ = h % 2
                    lb = li % 2
                    li += 1
                    mm(ps[lb][:, 0:n], iqb[64 * e_:64 * e_ + 64, h // 2, qs], ikb[64 * e_:64 * e_ + 64, k0:k0 + n], True, True,
                       reads=[("iqb", qt // 4), ("ikb", j)], writes=[("ps", lb)])
                    if h == 0:
                        sc.op("dve", ("tensor_scalar", dict(out=acc[:, a, k0:k0 + n], in0=ps[lb][:, 0:n], scalar1=0.0,
                                                                                   scalar2=iw[:, qt, 0:1], op0=ALU.max, op1=ALU.mult)),
                              reads=[("ps", lb), "iw"], writes=[("acc", a)])
                    else:
                        tj = li % 2
                        sc.op("dve", ("tensor_scalar", dict(out=tmp[:, tj, 0:n], in0=ps[lb][:, 0:n], scalar1=0.0,
                                                                                        scalar2=iw[:, qt, h:h + 1], op0=ALU.max, op1=ALU.mult)),
                              reads=[("ps", lb), "iw"], writes=[("tmp", tj)])
                        sc.op("pool", ("tensor_tensor", dict(out=acc[:, a, k0:k0 + n], in0=acc[:, a, k0:k0 + n],
                                                                                    in1=tmp[:, tj, 0:n], op=ALU.add)),
                              reads=[("acc", a), ("tmp", tj)], writes=[("acc", a)])
            sc.op("pool", ("tensor_tensor", dict(out=acc[:, a, qs], in0=acc[:, a, qs], in1=cf[:, CF_NEGTRI:CF_NEGTRI + 128], op=ALU.add)),
                  reads=[("acc", a), "cf"], writes=[("acc", a)])
            thr = bs[:, 0:1]
            if qt < 2:
                sc.op("dve", ("memset", dict(ap=thr, constant=-1e29)), writes=["thr"])
            else:
                hi = bs[:, 1:2]; lo = bs[:, 2:3]; cnt = bs[:, 3:4]; tt = bs[:, 4:5]
                W = bs[:, 8:8 + NBIS + 2]
                junk = tmp.rearrange("p a b -> p (a b)")
                sc.op("dve", ("tensor_reduce", dict(out=hi, in_=acc[:, a, 0:N], axis=AX.X, op=ALU.max)), reads=[("acc", a)], writes=["bs_hi"])
                sc.op("dve", ("tensor_reduce", dict(out=lo, in_=acc[:, a, 0:N - 128], axis=AX.X, op=ALU.min)), reads=[("acc", a)], writes=["bs_lo"])
                sc.op("dve", ("tensor_tensor", dict(out=tt, in0=hi, in1=lo, op=ALU.subtract)), reads=["bs_hi", "bs_lo"], writes=["bs_tt"])
                sc.op("dve", ("tensor_scalar", dict(out=W, in0=cf[:, CF_POW2:CF_POW2 + NBIS + 2], scalar1=tt, scalar2=None, op0=ALU.mult)),
                      reads=["bs_tt", "cf"], writes=["bs_W"])
                sc.op("dve", ("tensor_tensor", dict(out=thr, in0=lo, in1=W[:, 1:2], op=ALU.add)), reads=["bs_lo", "bs_W"], writes=["thr"])
                for k in range(NBIS):
                    sc.op("dve", ("tensor_scalar", dict(out=junk[:, 0:N] if N <= 1024 else acc[:, 1 - a, 0:N], in0=acc[:, a, 0:N], scalar1=thr, scalar2=0.0,
                                                           op0=ALU.is_ge, op1=ALU.add, accum_out=cnt)),
                          reads=[("acc", a), "thr"], writes=["bs_cnt", ("tmp", 0), ("tmp", 1)] + ([("acc", 1 - a)] if N > 1024 else []))
                    sc.op("dve", ("tensor_scalar", dict(out=tt, in0=cnt, scalar1=255.5, scalar2=0.5, op0=ALU.is_ge, op1=ALU.subtract)),
                          reads=["bs_cnt"], writes=["bs_tt"])
                    sc.op("dve", ("scalar_tensor_tensor", dict(out=thr, in0=tt, scalar=W[:, k + 1:k + 2], in1=thr, op0=ALU.mult, op1=ALU.add)),
                          reads=["bs_tt", "bs_W", "thr"], writes=["thr"])
                sc.op("dve", ("tensor_tensor", dict(out=thr, in0=thr, in1=W[:, NBIS + 1:NBIS + 2], op=ALU.subtract)), reads=["thr", "bs_W"], writes=["thr"])
            sc.op("dve", ("tensor_scalar", dict(out=mk[:, 0:N], in0=acc[:, a, 0:N], scalar1=thr, scalar2=None, op0=ALU.is_ge)),
                  reads=[("acc", a), "thr"], writes=["mk"])
            m = qt % 2
            tp = [ps[2][:, 0:512].bitcast(BF16), ps[3][:, 0:512].bitcast(BF16)]
            for kt in range(qt + 1):
                tr(tp[kt // 8][:, (kt % 8) * 128:(kt % 8 + 1) * 128], mk[:, kt * 128:(kt + 1) * 128], reads=["mk", "cb"], writes=[("ps", 2 + kt // 8)],
                   inc=(kt == qt or kt == 7))
            for half in range((qt // 8) + 1):
                nk = min(8, qt + 1 - 8 * half)
                sc.op("act", ("copy", dict(out=mt[:, m, 8 * half:8 * half + nk, :],
                                                               in_=tp[half][:, 0:nk * 128].rearrange("p (k n) -> p k n", n=128))),
                      reads=[("ps", 2 + half)], writes=[("mt", m)])
            for kt in range(qt + 1):
                for e_ in range(2):
                    sb_ = 4 + (ei % 2)
                    ej = ei % 2
                    ei += 1
                    ob = 6 + e_
                    mm(ps[sb_][:, 0:384], kb[64 * e_:64 * e_ + 64, kt * 128:(kt + 1) * 128], qb[64 * e_:64 * e_ + 64, :, qs], True, True,
                       reads=[("kb", kt // 4), ("qb", qt // 4)], writes=[("ps", sb_)])
                    sc.op("act", ("activation", dict(out=et[:, ej, :], in_=ps[sb_][:, 0:384], func=AF.Exp, scale=0.125)),
                          reads=[("ps", sb_)], writes=[("et", ej)])
                    sc.op("dve", ("tensor_tensor", dict(out=pt[:, ej, :].rearrange("p (h n) -> p h n", n=128),
                                                                         in0=et[:, ej, :].rearrange("p (h n) -> p h n", n=128),
                                                                         in1=mt[:, m, kt, :].unsqueeze(1).to_broadcast([128, 3, 128]), op=ALU.mult)),
                          reads=[("et", ej), ("mt", m)], writes=[("pt", ej)])
                    mm(ps[ob][:, 0:384], vb[:, kt, :], pt[:, ej, :], kt == 0, kt == qt,
                       reads=[("vb", (kt // 4) * 4), "vb1", ("pt", ej)], writes=[("ps", ob)], inc=True)
            for e_ in range(2):
                ob = 6 + e_
                sc.op("act", ("activation", dict(out=rd[64:128, :], in_=ps[ob][64:128, 0:384], func=AF.Ln)), reads=[("ps", ob)], writes=["rd"])
                sc.op("act", ("activation", dict(out=rd[64:128, :], in_=rd[64:128, :], func=AF.Exp, scale=-1.0)), reads=["rd"], writes=["rd"])
                sc.op("dve", ("tensor_tensor", dict(out=rn[64 * e_:64 * e_ + 64, :], in0=ps[ob][0:64, 0:384], in1=rd[64:128, :], op=ALU.mult)),
                      reads=[("ps", ob), "rd"], writes=["rd0"])
                ydst = yT[64 * e_:64 * e_ + 64, 3:6, qs]
                sc.op("pool", ("tensor_tensor", dict(out=ydst, in0=ydst, in1=rn[64 * e_:64 * e_ + 64, :].rearrange("p (h n) -> p h n", n=128), op=ALU.mult)),
                      reads=["rd0"] + [("yT", 3 + p, qt // 4) for p in range(3)], writes=[("yT", 3 + p, qt // 4) for p in range(3)])

    def mixer_C(layer):
        qc = scr_view(0, [2, S], BF16)
        kc = scr_view(8192, [2, S], BF16)
        vc = scr_view(16384, [16, 512], BF16)
        ac = scr_view(32768, [4, S], F32)
        et = scr_view(65536, [2, 256], BF16)
        rd = scr_view(65536 + 1024, [512], F32)
        rn = scr_view(65536 + 3072, [512], F32)
        sc.op("pool", ("tensor_copy", dict(out=vc.rearrange("p b (h c) -> p b h c", c=128)[:, :, :, 64:128],
                                              in_=cb[:, CB_ONES:CB_ONES + 64].unsqueeze(1).unsqueeze(1).to_broadcast([128, 16, 4, 64]))),
              reads=["cb"], writes=["vc1"])
        for p in range(2):
            proj_fm(layer, f"CG{p}", False, silu_evac(6 + p))
        ei = 0
        first_g = [True]
        for g, dil in enumerate((1, 4, 16)):
            if g not in getattr(_build, 'cgroups', (0, 1, 2)):
                continue
            if g != min(getattr(_build, 'cgroups', (0, 1, 2))):
                first_g[0] = False
            nblk = S // dil // 128

            def toks(r, m, cnt=128):
                st0 = r + dil * 128 * m
                return slice(st0, st0 + dil * (cnt - 1) + 1, dil)
            for p in range(2):
                proj_fm(layer, f"CQ{g}{p}", True, rope_evac(lambda c4, h, p=p: qc[:, p, c4 * 512:(c4 + 1) * 512], lambda c4, h, p=p: [("qc", p, c4)], False))
                proj_fm(layer, f"CK{g}{p}", True, rope_evac(lambda c4, h, p=p: kc[:, p, c4 * 512:(c4 + 1) * 512], lambda c4, h, p=p: [("kc", p, c4)], False))

                def vev(g4, b1, p=p):
                    v4 = ps[b1][:, :].rearrange("p (j h c) -> p j h c", h=2, c=64)
                    sc.op("act", ("copy", dict(out=vc.rearrange("p b (h c) -> p b h c", c=128)[:, g4:g4 + 4, 2 * p:2 * p + 2, 0:64], in_=v4)),
                          reads=[("ps", b1)], writes=[("vc", p, g4)])
                proj_tm(layer, f"CV{g}{p}", 16, lambda c, blk: hT[:, c, toks(blk // nblk, blk % nblk)], vev)
            for j in range(4):
                p, e_ = j // 2, j % 2
                pr = slice(64 * e_, 64 * e_ + 64)
                started = [False] * 4
                allq = [("qc", p, c4) for c4 in range(4)]
                allk = [("kc", p, c4) for c4 in range(4)]
                nmm = 0
                for r in range(dil):
                    for m in range(nblk):
                        blk = r * nblk + m
                        nq = 256 if m + 1 < nblk else 128
                        sb_ = 4 + (ei % 2)
                        ej = ei % 2
                        ei += 1
                        mm(ps[sb_][:, 0:nq], kc[pr, p, toks(r, m)], qc[pr, p, toks(r, m, nq)], True, True,
                           reads=allk + allq, writes=[("ps", sb_)])
                        sc.op("act", ("activation", dict(out=et[:, ej, 0:nq], in_=ps[sb_][:, 0:nq], func=AF.Exp, scale=0.125)),
                              reads=[("ps", sb_)], writes=[("et", ej)])
                        sc.op("dve", ("tensor_tensor", dict(out=et[:, ej, 0:nq], in0=et[:, ej, 0:nq], in1=cb[:, CB_MOWN:CB_MOWN + nq], op=ALU.mult)),
                              reads=[("et", ej), "cb"], writes=[("et", ej)])
                        pieces = []
                        for part in range(nq // 128):
                            t0 = r + dil * 128 * (m + part)
                            if dil <= 4:
                                pieces.append((part * 128, 128, t0))
                            else:
                                for q4 in range(4):
                                    pieces.append((part * 128 + 32 * q4, 32, t0 + dil * 32 * q4))
                        for (c0, cn, t0) in pieces:
                            bnk = t0 // 512
                            o0 = t0 % 512
                            mm(ps[bnk][:, o0:o0 + dil * (cn - 1) + 1:dil], vc[:, blk, 128 * j:128 * j + 128], et[:, ej, c0:c0 + cn],
                               not started[bnk], False, reads=[("vc", p, (blk // 4) * 4), "vc1", ("et", ej)],
                               writes=[("ps", bnk)], inc=True)
                            started[bnk] = True
                for c4 in range(4):
                    dst = ac[:, j, c4 * 512:(c4 + 1) * 512]
                    if first_g[0]:
                        sc.op("act", ("copy", dict(out=dst, in_=ps[c4][:, :])), reads=[("ps", c4)], writes=[("ac", j, c4)])
                    else:
                        sc.op("dve", ("tensor_tensor", dict(out=dst, in0=ps[c4][:, :], in1=dst, op=ALU.add)),
                              reads=[("ps", c4), ("ac", j, c4)], writes=[("ac", j, c4)])
        for j in range(4):
            p, e_ = j // 2, j % 2
            for c4 in range(4):
                cs = slice(c4 * 512, (c4 + 1) * 512)
                sc.op("act", ("activation", dict(out=rd[64:128, :], in_=ac[64:128, j, cs], func=AF.Ln)), reads=[("ac", j, c4)], writes=["rd"])
                sc.op("act", ("activation", dict(out=rd[64:128, :], in_=rd[64:128, :], func=AF.Exp, scale=-1.0)), reads=["rd"], writes=["rd"])
                sc.op("act", ("copy", dict(out=rn[64:128, :], in_=ac[0:64, j, cs])), reads=[("ac", j, c4)], writes=[("rn", 1)])
                sc.op("dve", ("tensor_tensor", dict(out=rn[64 * e_:64 * e_ + 64, :], in0=rn[64:128, :], in1=rd[64:128, :], op=ALU.mult)),
                      reads=[("rn", 1), "rd"], writes=[("rn", e_)])
                ydst = yT[64 * e_:64 * e_ + 64, 6 + p, cs]
                sc.op("pool", ("tensor_tensor", dict(out=ydst, in0=ydst, in1=rn[64 * e_:64 * e_ + 64, :], op=ALU.mult)),
                      reads=[("rn", e_), ("yT", 6 + p, c4)], writes=[("yT", 6 + p, c4)])

    def phase_final(si, layer, src_d, dst_d, last):
        mg = scr_view(0, [8, S], BF16)
        wbr = scr_view(32768, [8, D], BF16)
        wo = scr_view(49152, [8, D], BF16)
        gs = scr_view(65536, [3, 512], BF16)
        tm = scr_view(65536 + 3072, [512], F32)
        for c in range(8):
            nm = (f"WA{c}" if c < 3 else f"WB{c - 3}" if c < 6 else f"WC{c - 6}")
            load_w(layer, _XIDX[nm], dest=wbr[:, c, :], dest_key=("wbr", c))
        for c in range(8):
            load_w(layer, _XIDX[f"WO{c}"], dest=wo[:, c, :], dest_key=("wo", c))
        if last:
            load_g(0, 2)
        kch = ((0, 3), (3, 6), (6, 8))
        for dt_ in range(8):
            gw = []
            for br in range(3):
                w_, k_ = load_w(layer, _TIDX[f"MG{8 * br + dt_}"])
                gw.append((w_.rearrange("p (c n) -> p c n", n=128), k_))
            for c4 in range(4):
                cs = slice(c4 * 512, (c4 + 1) * 512)
                for br in range(3):
                    for c in range(8):
                        mm(ps[br][:, :], gw[br][0][:, c, :], hT[:, c, cs], c == 0, c == 7, reads=[gw[br][1], ("hT", c4)], writes=[("ps", br)])
                    sc.op("act", ("activation", dict(out=gs[:, br, :], in_=ps[br][:, :], func=AF.Sigmoid)), reads=[("ps", br)], writes=[("gs", br)])
                for br in range(3):
                    a0, a1 = kch[br]
                    for c in range(a0, a1):
                        mm(ps[3 + br][:, :], wbr[:, c, dt_ * 128:(dt_ + 1) * 128], yT[:, c, cs], c == a0, c == a1 - 1,
                           reads=[("wbr", c), ("yT", c, c4)], writes=[("ps", 3 + br)])
                sc.op("dve", ("tensor_tensor", dict(out=tm, in0=ps[3][:, :], in1=gs[:, 0, :], op=ALU.mult)), reads=[("ps", 3), ("gs", 0)], writes=["tm"])
                sc.op("dve", ("tensor_tensor", dict(out=gs[:, 1, :], in0=ps[4][:, :], in1=gs[:, 1, :], op=ALU.mult)), reads=[("ps", 4), ("gs", 1)], writes=[("gs", 1)])
                sc.op("dve", ("tensor_tensor", dict(out=gs[:, 2, :], in0=ps[5][:, :], in1=gs[:, 2, :], op=ALU.mult)), reads=[("ps", 5), ("gs", 2)], writes=[("gs", 2)])
                sc.op("pool", ("tensor_tensor", dict(out=tm, in0=tm, in1=gs[:, 1, :], op=ALU.add)), reads=["tm", ("gs", 1)], writes=["tm"])
                sc.op("pool", ("tensor_tensor", dict(out=mg[:, dt_, cs], in0=tm, in1=gs[:, 2, :], op=ALU.add)),
                      reads=["tm", ("gs", 2)], writes=[("mg", c4)])
        for t in range(NT):
            s = t % 2
            ts = slice(t * 128, (t + 1) * 128)
            sc.dma(("x", s), ("dma_start", dict(out=xt[:, s, :], in_=src_d[si, ts, :])), writes=[("xt", s)])
            for hf in range(2):
                b = 6 + hf
                for c in range(8):
                    mm(ps[b][:, :], mg[:, c, ts], wo[:, c, hf * 512:(hf + 1) * 512], c == 0, c == 7,
                       reads=[("mg", t // 4), ("wo", c)], writes=[("ps", b)])
                sc.op("dve", ("tensor_tensor", dict(out=xt[:, s, hf * 512:(hf + 1) * 512], in0=ps[b][:, :],
                                                                        in1=xt[:, s, hf * 512:(hf + 1) * 512], op=ALU.add)),
                      reads=[("ps", b), ("xt", s)], writes=[("xt", s)])
            if last:
                ss = st[:, 8 + 2 * s:8 + 2 * s + 1]
                rs = st[:, 8 + 2 * s + 1:8 + 2 * s + 2]
                sc.op("act", ("activation", dict(out=hn[:, s, :], in_=xt[:, s, :], func=AF.Square, accum_out=ss)),
                      reads=[("xt", s)], writes=[("hn", s), ("st", s)])
                sc.op("dve", ("tensor_scalar", dict(out=rs, in0=ss, scalar1=1.0 / D, scalar2=1e-6, op0=ALU.mult, op1=ALU.add)),
                      reads=[("st", s)], writes=[("st", s)])
                sc.op("act", ("activation", dict(out=rs, in_=rs, func=AF.Sqrt)), reads=[("st", s)], writes=[("st", s)])
                sc.op("dve", ("reciprocal", dict(out=rs, in_=rs)), reads=[("st", s)], writes=[("st", s)])
                sc.op("dve", ("scalar_tensor_tensor", dict(out=xt[:, s, :], in0=xt[:, s, :], scalar=rs, in1=gbc[:, 0, :],
                                                                         op0=ALU.mult, op1=ALU.mult)),
                      reads=[("xt", s), ("st", s), ("gbc", 0)], writes=[("xt", s)])
            sc.dma(("o", s), ("dma_start", dict(out=dst_d[si, ts, :], in_=xt[:, s, :])), reads=[("xt", s)], writes=[("dram", si, t)])

    enabled = set(getattr(_build, "enabled", ("A", "B", "C")))

    def program():
        sc.dma("c0", ("dma_start", dict(out=cf[:], in_=cf_d[:, :])), writes=["cf"])
        sc.dma("c1", ("dma_start", dict(out=cb[:], in_=cb_d[:, :])), writes=["cb"])
        for si in range(nseq):
            for li_, layer in enumerate(layers_all):
                first = (li_ == 0)
                last = (li_ == len(layers_all) - 1)
                src = x_d if first else xs_d
                dst = out_d if last else xs_d
                sc.phase = "norm"
                phase_norm(si, layer, src)
                if "A" in enabled:
                    sc.barrier()
                    sc.phase = "A"
                    mixer_A(layer)
                if "B" in enabled:
                    sc.barrier()
                    sc.phase = "B"
                    mixer_B(layer)
                if "C" in enabled:
                    sc.barrier()
                    sc.phase = "C"
                    mixer_C(layer)
                sc.barrier()
                sc.phase = "final"
                for name, shape, dt in taps:
                    if name == f"yT{layer}" and si == 0:
                        sc.dma(("tap", name), ("dma_start", dict(out=tap_d[name][:, :, :], in_=yT[:])),
                               reads=[("yT", c, c4) for c in range(8) for c4 in range(4)])
                    if name == f"hT{layer}" and si == 0:
                        sc.dma(("tap", name), ("dma_start", dict(out=tap_d[name][:, :, :], in_=hT[:])),
                               reads=[("hT", c4) for c4 in range(4)])
                phase_final(si, layer, src, dst, last and final_norm)
                sc.barrier()
        sc.final_wait("sp", [("o", 0), ("o", 1)] + [("tap", n) for n, _, _ in taps])


    program()
    sc = Sched()
    wstate.update(n=0, rec=False, issued=0)
    ppp["i"] = 0
    program()

    waited = {e: set() for e in ("pe", "act", "dve", "pool")}
    for e in Sched.ENG:
        for waits, fn, inc, ph in sc.q[e]:
            for de, dc in waits:
                if de in waited:
                    waited[de].add(dc)
    remap = {e: {c: i + 1 for i, c in enumerate(sorted(waited[e]))} for e in waited}
    for e in waited:
        c = 0
        newq = []
        for waits, fn, inc, ph in sc.q[e]:
            if inc is not None and inc[0] == "E":
                c += 1
                if c not in remap[e]:
                    inc = None
            newq.append((waits, fn, inc, ph))
        sc.q[e] = newq
    for e in Sched.ENG:
        sc.q[e] = [([(de, remap[de][dc]) if de in remap else (de, dc) for de, dc in waits], fn, inc, ph)
                   for waits, fn, inc, ph in sc.q[e]]

    sems = {}
    for e in Sched.ENG:
        sems[e] = es.enter_context(nc.semaphore("s_" + e))
    for de in sc.dcnt:
        sems[de] = es.enter_context(nc.semaphore("d%d" % len(sems)))
    block = es.enter_context(nc.Block())

    def replay(engname):
        def run(eng):
            for waits, fn, inc, ph in sc.q[engname]:
                for de, dc in waits:
                    eng.wait_ge(sems[de], dc)
                if fn is None:
                    continue
                ins = getattr(eng, fn[0])(**fn[1])
                if ANNOTATE:
                    ins.annotate(ph)
                if inc is not None:
                    if inc[0] == "E":
                        ins.then_inc(sems[inc[1]], 1)
                    else:
                        ins.then_inc(sems[inc[1]], 16)
        return run
    block.tensor(replay("pe"))
    block.scalar(replay("act"))
    block.vector(replay("dve"))
    block.gpsimd(replay("pool"))
    block.sync(replay("sp"))
    es.close()
    return nc


_CACHE = {}


def kernel(x, norm_g, w_in, w_br_a, w_br_b, w_br_c, w_out, final_norm_g):
    x = np.ascontiguousarray(np.asarray(x, np.float32))
    ncores = 8
    nseq = x.shape[0] // ncores
    wt = _pack_weights(np.asarray(w_in, np.float32), np.asarray(w_br_a, np.float32), np.asarray(w_br_b, np.float32),
                       np.asarray(w_br_c, np.float32), np.asarray(w_out, np.float32))
    gv = np.concatenate([np.asarray(norm_g, np.float32), np.asarray(final_norm_g, np.float32)[None, :]], axis=0)
    cf, cb = _consts()
    nc = _build(nseq)
    in_maps = [{"x": x[i * nseq:(i + 1) * nseq], "wt": wt, "gv": gv, "cf": cf, "cb": cb} for i in range(ncores)]
    res = run_bass_kernel_spmd(nc, in_maps, core_ids=list(range(ncores)))
    return np.concatenate([r["out"] for r in res.results], axis=0)
```

```python
import numpy as np
import ml_dtypes
import concourse.bass as bass
import concourse.mybir as mybir
from concourse.bass_utils import run_bass_kernel_spmd

F32 = mybir.dt.float32
BF16 = mybir.dt.bfloat16
AF = mybir.ActivationFunctionType
ALU = mybir.AluOpType
AX = mybir.AxisListType

S = 2048
D = 1024
NT = 16
NEG = -30000.0
NBIS = 12
ANNOTATE = False

_splits = (384, 384, 384, 384, 384, 64, 64, 384, 256, 64, 4, 768, 768, 768, 256, 3072)
_names = ("a_q", "a_k", "a_v", "a_g", "b_q", "b_k", "b_v", "b_g", "i_q", "i_k", "i_w",
          "c_q", "c_k", "c_v", "c_g", "m_g")
_off = {}
_o = 0
for _n, _s in zip(_names, _splits):
    _off[_n] = _o
    _o += _s


def _rot(cols):
    cols = np.asarray(cols).reshape(-1, 2, 32)
    return cols[:, ::-1, :].reshape(-1)


def _tile_cols():
    t = []
    rng = lambda n, a, b: np.arange(_off[n] + a, _off[n] + b)
    for p in range(3):
        c = rng("a_q", 128 * p, 128 * p + 128); t.append((f"AQ{p}", c)); t.append((f"AQR{p}", _rot(c)))
        c = rng("a_k", 128 * p, 128 * p + 128); t.append((f"AK{p}", c)); t.append((f"AKR{p}", _rot(c)))
        t.append((f"AG{p}", rng("a_g", 128 * p, 128 * p + 128)))
        t.append((f"AV{p}", rng("a_v", 128 * p, 128 * p + 128)))
    for p in range(3):
        c = rng("b_q", 128 * p, 128 * p + 128); t.append((f"BQ{p}", c)); t.append((f"BQR{p}", _rot(c)))
        t.append((f"BG{p}", rng("b_g", 128 * p, 128 * p + 128)))
    c = np.concatenate([rng("b_k", 0, 64), rng("b_k", 0, 64)]); t.append(("BK", c)); t.append(("BKR", _rot(c)))
    c = np.concatenate([rng("i_k", 0, 64), rng("i_k", 0, 64)]); t.append(("IK", c)); t.append(("IKR", _rot(c)))
    for p in range(2):
        c = rng("i_q", 128 * p, 128 * p + 128); t.append((f"IQ{p}", c)); t.append((f"IQR{p}", _rot(c)))
    c = np.concatenate([rng("b_v", 0, 64), rng("i_w", 0, 4), rng("i_w", 0, 4).repeat(15)]); t.append(("BV", c))
    for g in range(3):
        for p in range(2):
            c = rng("c_q", 256 * g + 128 * p, 256 * g + 128 * p + 128); t.append((f"CQ{g}{p}", c)); t.append((f"CQR{g}{p}", _rot(c)))
            c = rng("c_k", 256 * g + 128 * p, 256 * g + 128 * p + 128); t.append((f"CK{g}{p}", c)); t.append((f"CKR{g}{p}", _rot(c)))
            t.append((f"CV{g}{p}", rng("c_v", 256 * g + 128 * p, 256 * g + 128 * p + 128)))
    for p in range(2):
        t.append((f"CG{p}", rng("c_g", 128 * p, 128 * p + 128)))
    for j in range(24):
        t.append((f"MG{j}", rng("m_g", 128 * j, 128 * j + 128)))
    return t


_TILES = _tile_cols()
_TIDX = {n: i for i, (n, _) in enumerate(_TILES)}
NWT = len(_TILES)
_XIDX = {}
for _i, _n in enumerate([f"WA{c}" for c in range(3)] + [f"WB{c}" for c in range(3)] +
                        [f"WC{c}" for c in range(2)] + [f"WO{c}" for c in range(8)]):
    _XIDX[_n] = NWT + _i
NWALL = NWT + 16


def _pack_weights(w_in, w_br_a, w_br_b, w_br_c, w_out):
    L = w_in.shape[0]
    out = np.empty((L, NWALL, 128, 1024), np.float32)
    allc = np.concatenate([c for _, c in _TILES])
    for l in range(L):
        g = w_in[l][:, allc]
        g = g.reshape(8, 128, NWT, 128).transpose(2, 1, 0, 3)
        out[l, :NWT] = g.reshape(NWT, 128, 1024)
        out[l, NWT:NWT + 3] = w_br_a[l].reshape(3, 128, 1024)
        out[l, NWT + 3:NWT + 6] = w_br_b[l].reshape(3, 128, 1024)
        out[l, NWT + 6:NWT + 8] = w_br_c[l].reshape(2, 128, 1024)
        out[l, NWT + 8:NWT + 16] = w_out[l].reshape(8, 128, 1024)
    return out


CF_COS = 0
CF_SIN = CF_COS + S
CF_NEGTRI = CF_SIN + S
CF_AGM = CF_NEGTRI + 128
CF_AVAL = CF_AGM + 128
CF_AOWN = CF_AVAL + 128
CF_POW2 = CF_AOWN + 128
CF_N = CF_POW2 + 32
CB_ID = 0
CB_MOWN = CB_ID + 128
CB_MPREV = CB_MOWN + 128
CB_ONEHOT = CB_MPREV + 128
CB_ONES = CB_ONEHOT + S
CB_N = CB_ONES + 128


def _consts():
    cf = np.zeros((128, CF_N), np.float32)
    inv = 1.0 / (10000.0 ** (np.arange(0, 64, 2, dtype=np.float32) / 64.0))
    ang = np.arange(S, dtype=np.float32)[None, :] * inv[:, None].astype(np.float32)
    cos = np.cos(ang).astype(np.float32)
    sin = np.sin(ang).astype(np.float32)
    for p in range(128):
        j = p % 32
        cf[p, CF_COS:CF_COS + S] = cos[j]
        cf[p, CF_SIN:CF_SIN + S] = sin[j] * (-1.0 if (p % 64) < 32 else 1.0)
    t = np.arange(128)[:, None]
    s_ = np.arange(128)[None, :]
    cf[:, CF_NEGTRI:CF_NEGTRI + 128] = np.where(s_ <= t, 0.0, -1e30)
    for qt in range(16):
        own = qt // 2
        for n in range(8):
            cf[:, CF_AGM + qt * 8 + n] = 0.0 if n < own else -1e30
            cf[:, CF_AVAL + qt * 8 + n] = 1.0 if n < own else 0.0
            cf[:, CF_AOWN + qt * 8 + n] = 0.0 if n == own else NEG
    for k in range(32):
        cf[:, CF_POW2 + k] = 2.0 ** (-k)
    cb = np.zeros((128, CB_N), np.float32)
    cb[:, CB_ID:CB_ID + 128] = np.eye(128)
    cb[:, CB_MOWN:CB_MOWN + 128] = (t <= s_)
    cb[:, CB_MPREV:CB_MPREV + 128] = (t >= s_)
    for n in range(8):
        cb[n, CB_ONEHOT + 256 * n:CB_ONEHOT + 256 * n + 256] = 1.0
    cb[:, CB_ONES:CB_ONES + 128] = 1.0
    return cf, cb.astype(ml_dtypes.bfloat16)


class Sched:
    ENG = ("pe", "act", "dve", "pool", "sp")

    def __init__(self):
        self.q = {e: [] for e in self.ENG}
        self.cnt = {e: 0 for e in self.ENG}
        self.seen = {e: {} for e in self.ENG}
        self.lastw = {}
        self.readers = {}
        self.dcnt = {}
        self.phase = "init"

    def _need(self, eng, reads, writes):
        need = {}

        def add(dep, raw):
            de, dc = dep
            if de == eng and (eng == "pe" or eng == "sp"):
                return
            if need.get(de, 0) < dc:
                need[de] = dc
        for r in reads:
            w = self.lastw.get(r)
            if w is not None:
                add(w, True)
        for w_ in writes:
            w = self.lastw.get(w_)
            if w is not None:
                add(w, False)
            for de, dc in self.readers.get(w_, {}).items():
                add((de, dc), False)
        waits = []
        for de, dc in need.items():
            if self.seen[eng].get(de, 0) < dc:
                self.seen[eng][de] = dc
                waits.append((de, dc))
        return waits

    def _record(self, tag, reads, writes):
        for r in reads:
            d = self.readers.setdefault(r, {})
            if d.get(tag[0], 0) < tag[1]:
                d[tag[0]] = tag[1]
        for w_ in writes:
            self.lastw[w_] = tag
            self.readers[w_] = {}

    def op(self, eng, fn, reads=(), writes=(), inc=True):
        waits = self._need(eng, reads, writes)
        tag = (eng, self.cnt[eng] + 1)
        if inc:
            self.cnt[eng] += 1
        self.q[eng].append((waits, fn, ("E", eng) if inc else None, self.phase))
        self._record(tag, reads, writes)

    def dma(self, key, fn, reads=(), writes=(), eng="sp"):
        waits = self._need(eng, reads, writes)
        de = ("dma", key)
        self.dcnt[de] = self.dcnt.get(de, 0) + 16
        self.q[eng].append((waits, fn, ("D", de), self.phase))
        self._record((de, self.dcnt[de]), reads, writes)

    def barrier(self):
        tgt = {e: self.cnt[e] for e in ("pe", "act", "dve", "pool")}
        tgt.update(self.dcnt)
        for e in self.ENG:
            waits = []
            for de, dc in tgt.items():
                if de == e or dc == 0:
                    continue
                if self.seen[e].get(de, 0) < dc:
                    self.seen[e][de] = dc
                    waits.append((de, dc))
            if waits:
                self.q[e].append((waits, None, None, None))

    def final_wait(self, eng, dma_keys):
        waits = [(("dma", k), self.dcnt[("dma", k)]) for k in dma_keys if ("dma", k) in self.dcnt]
        self.q[eng].append((waits, None, None, None))


def _build(nseq, layers_all=(0, 1), final_norm=True, taps=()):
    nc = bass.Bass("TRN2", target_bir_lowering=False)
    x_d = nc.dram_tensor("x", [nseq, S, D], F32, kind="ExternalInput").ap()
    wt_d = nc.dram_tensor("wt", [2, NWALL, 128, 1024], F32, kind="ExternalInput").ap()
    gv_d = nc.dram_tensor("gv", [3, D], F32, kind="ExternalInput").ap()
    cf_d = nc.dram_tensor("cf", [128, CF_N], F32, kind="ExternalInput").ap()
    cb_d = nc.dram_tensor("cb", [128, CB_N], BF16, kind="ExternalInput").ap()
    out_d = nc.dram_tensor("out", [nseq, S, D], F32, kind="ExternalOutput").ap()
    xs_d = nc.dram_tensor("xscr", [nseq, S, D], F32).ap()
    tap_d = {}
    for name, shape, dt in taps:
        tap_d[name] = nc.dram_tensor("tap_" + name, list(shape), dt, kind="ExternalOutput").ap()

    sc = Sched()
    sb = {}

    def alloc(name, shape, dt):
        t = nc.alloc_sbuf_tensor(name, list(shape), dt) if False else None
        return t

    from contextlib import ExitStack
    es = ExitStack()

    def SB(name, shape, dt):
        t = es.enter_context(nc.sbuf_tensor("sb_" + name, list(shape), dt))
        sb[name] = t
        return t

    def PS(name, shape, dt):
        return es.enter_context(nc.psum_tensor(name, list(shape), dt))

    cf = SB("cf", [128, CF_N], F32)
    cb = SB("cb", [128, CB_N], BF16)
    hT = SB("hT", [128, 8, S], BF16)
    yT = SB("yT", [128, 8, S], BF16)
    gbc = SB("gbc", [128, 2, D], F32)
    wbf = SB("wbf", [128, 8, 1024], BF16)
    xt = SB("xt", [128, 2, D], F32)
    hn = SB("hn", [128, 2, D], BF16)
    st = SB("st", [128, 64], F32)
    scr = SB("scr", [128, 36864], BF16)
    ps = [PS(f"ps{i}", [128, 512], F32) for i in range(8)]

    def scr_view(off_bytes, shape, dt):
        n = int(np.prod(shape))
        if dt == F32:
            assert off_bytes % 4 == 0
            v = scr[:, off_bytes // 2: off_bytes // 2 + 2 * n].bitcast(F32)
        else:
            v = scr[:, off_bytes // 2: off_bytes // 2 + n]
        if len(shape) == 2:
            v = v.rearrange("p (a b) -> p a b", b=shape[1])
        elif len(shape) == 3:
            v = v.rearrange("p (a b c) -> p a b c", b=shape[1], c=shape[2])
        return v

    ident = cb[:, CB_ID:CB_ID + 128]

    def mm(out, lhsT, rhs, start, stop, reads, writes, inc=None):
        if inc is None:
            inc = stop
        sc.op("pe", ("matmul", dict(out=out, lhsT=lhsT, rhs=rhs, start=start, stop=stop, skip_group_check=True)),
              reads=reads, writes=writes, inc=inc)

    def tr(out, in_, reads, writes, inc=True):
        sc.op("pe", ("transpose", dict(out=out, in_=in_, identity=ident[:in_.shape[0], :in_.shape[0]])), reads=reads, writes=writes, inc=inc)


    NSLOT = 8
    WDEPTH = 4
    wstate = {"n": 0, "ring": 0, "rec": True, "issued": 0, "xd": 0}
    wplan = []

    def _issue_w(ent):
        layer, idx, dest, dest_key, skey = ent
        sc.dma(skey, ("dma_start", dict(out=dest, in_=wt_d[layer, idx, :, :])), writes=[dest_key], eng="pool")

    def load_w(layer, idx, dest=None, dest_key=None):
        n = wstate["n"]; wstate["n"] += 1
        if wstate["rec"]:
            if dest is None:
                b = wstate["ring"] % NSLOT; wstate["ring"] += 1
                dest = wbf[:, b, :]
                dest_key = ("wbf", b)
                skey = ("w", b)
            else:
                skey = ("wx", wstate["xd"] % 16); wstate["xd"] += 1
            wplan.append((layer, idx, dest, dest_key, skey))
            return dest, dest_key
        while wstate["issued"] < len(wplan) and wstate["issued"] <= n + WDEPTH:
            ent = wplan[wstate["issued"]]
            if ent[4][0] == "wx" and wstate["issued"] > n:
                break
            _issue_w(ent)
            wstate["issued"] += 1
        ent = wplan[n]
        return ent[2], ent[3]

    def load_g(slot, row):
        src = gv_d[row:row + 1, :].partition_broadcast(128) if False else None
        from concourse.ap import AP
        src = AP(gv_d.tensor, row * D, [[0, 128], [1, D]])
        sc.dma(("g", slot), ("dma_start", dict(out=gbc[:, slot, :], in_=src)), writes=[("gbc", slot)])

    def phase_norm(si, layer, src_d):
        load_g(layer % 2, layer)
        pst = ps[7][:, 0:512].bitcast(BF16)
        for t in range(NT):
            s = t % 2
            sc.dma(("x", s), ("dma_start", dict(out=xt[:, s, :], in_=src_d[si, t * 128:(t + 1) * 128, :])),
                   writes=[("xt", s)])
            ss = st[:, 2 * s:2 * s + 1]
            rs = st[:, 2 * s + 1:2 * s + 2]
            sc.op("act", ("activation", dict(out=hn[:, s, :], in_=xt[:, s, :], func=AF.Square, accum_out=ss)),
                  reads=[("xt", s)], writes=[("hn", s), ("st", s)])
            sc.op("dve", ("tensor_scalar", dict(out=rs, in0=ss, scalar1=1.0 / D, scalar2=1e-6, op0=ALU.mult, op1=ALU.add)),
                  reads=[("st", s)], writes=[("st", s)])
            sc.op("act", ("activation", dict(out=rs, in_=rs, func=AF.Sqrt)), reads=[("st", s)], writes=[("st", s)])
            sc.op("dve", ("reciprocal", dict(out=rs, in_=rs)), reads=[("st", s)], writes=[("st", s)])
            sc.op("dve", ("scalar_tensor_tensor", dict(out=hn[:, s, :], in0=xt[:, s, :], scalar=rs, in1=gbc[:, layer % 2, :],
                                                                     op0=ALU.mult, op1=ALU.mult)),
                  reads=[("xt", s), ("st", s), ("gbc", layer % 2)], writes=[("hn", s)])
            for c in range(8):
                tr(pst[:, c * 128:(c + 1) * 128], hn[:, s, c * 128:(c + 1) * 128], reads=[("hn", s), "cb"], writes=[("ps", 7)], inc=(c == 7))
            sc.op("act", ("copy", dict(out=hT[:, :, t * 128:(t + 1) * 128], in_=pst.rearrange("p (c n) -> p c n", n=128))),
                  reads=[("ps", 7)], writes=[("hT", t // 4)])

    def run(units):
        for u in units:
            u()

    def run_merged(*lists):
        lists = [l for l in lists if l]
        idx = [0] * len(lists)
        while True:
            cand = [(idx[i] / len(l), i) for i, l in enumerate(lists) if idx[i] < len(l)]
            if not cand:
                break
            i = min(cand)[1]
            lists[i][idx[i]]()
            idx[i] += 1

    def skew(units, items, fa, fb, others):
        pend = None
        for it in items:
            if it[0] == "t":
                st_ = {}
                a_ = (lambda it=it, st_=st_: fa(st_, *it[1:]))
                if pend is None:
                    units.append(a_)
                else:
                    units.append(lambda a_=a_, b_=pend: (a_(), b_()))
                pend = (lambda it=it, st_=st_: fb(st_, *it[1:]))
            else:
                if pend is not None:
                    units.append(pend)
                    pend = None
                units.append(lambda it=it: others[it[0]](*it[1:]))
        if pend is not None:
            units.append(pend)

    ppp = {"i": 0}

    def proj_fm(layer, name, rope, evac, banks=((0, 1), (2, 3))):
        stt = {}

        def u(c4):
            if c4 == 0:
                w1, k1 = load_w(layer, _TIDX[name])
                stt["w1"] = (w1.rearrange("p (c n) -> p c n", n=128), k1)
                if rope:
                    w2, k2 = load_w(layer, _TIDX[name[:2] + "R" + name[2:]])
                    stt["w2"] = (w2.rearrange("p (c n) -> p c n", n=128), k2)
            w1, k1 = stt["w1"]
            i = ppp["i"]; ppp["i"] += 1
            b1 = banks[0][i % len(banks[0])]
            b2 = banks[1][i % len(banks[1])]
            for c in range(8):
                mm(ps[b1][:, :], w1[:, c, :], hT[:, c, c4 * 512:(c4 + 1) * 512], c == 0, c == 7,
                   reads=[k1, ("hT", c4)], writes=[("ps", b1)])
            if rope:
                w2, k2 = stt["w2"]
                for c in range(8):
                    mm(ps[b2][:, :], w2[:, c, :], hT[:, c, c4 * 512:(c4 + 1) * 512], c == 0, c == 7,
                       reads=[k2, ("hT", c4)], writes=[("ps", b2)])
                evac(c4, b1, b2)
            else:
                evac(c4, b1)
        return [(lambda c4=c4: u(c4)) for c4 in range(4)]

    rt = SB("rt", [128, 2, 2, 512], F32)
    rtc = {"i": 0}

    def rope_evac(dst_fn, dst_keys_fn, split_heads):
        def ev(c4, b1, b2):
            j = rtc["i"] % 2; rtc["i"] += 1
            t1 = rt[:, j, 0, :]
            t2 = rt[:, j, 1, :]
            cs = cf[:, CF_COS + c4 * 512:CF_COS + (c4 + 1) * 512]
            sn = cf[:, CF_SIN + c4 * 512:CF_SIN + (c4 + 1) * 512]
            sc.op("dve", ("tensor_tensor", dict(out=t1, in0=ps[b1][:, :], in1=cs, op=ALU.mult)),
                  reads=[("ps", b1), "cf"], writes=[("rt", j, 0)])
            sc.op("dve", ("tensor_tensor", dict(out=t2, in0=ps[b2][:, :], in1=sn, op=ALU.mult)),
                  reads=[("ps", b2), "cf"], writes=[("rt", j, 1)])
            if split_heads:
                for h in range(2):
                    d = dst_fn(c4, h)
                    sc.op("pool", ("tensor_tensor", dict(out=d, in0=t1[64 * h:64 * h + 64, :], in1=t2[64 * h:64 * h + 64, :], op=ALU.add)),
                          reads=[("rt", j, 0), ("rt", j, 1)], writes=dst_keys_fn(c4, h))
            else:
                d = dst_fn(c4, None)
                sc.op("pool", ("tensor_tensor", dict(out=d, in0=t1, in1=t2, op=ALU.add)),
                      reads=[("rt", j, 0), ("rt", j, 1)], writes=dst_keys_fn(c4, None))
        return ev

    def silu_evac(ychunk):
        def ev(c4, b1):
            sc.op("act", ("activation", dict(out=yT[:, ychunk, c4 * 512:(c4 + 1) * 512], in_=ps[b1][:, :], func=AF.Silu)),
                  reads=[("ps", b1)], writes=[("yT", ychunk, c4)])
        return ev

    def proj_tm(layer, name, nblk, tok_fn, evac, banks=(0, 1)):
        stt = {}

        def u(g4):
            if g4 == 0:
                w1, k1 = load_w(layer, _TIDX[name])
                stt["w1"] = (w1.rearrange("p (c n) -> p c n", n=128), k1)
            w1, k1 = stt["w1"]
            i = ppp["i"]; ppp["i"] += 1
            b1 = banks[i % len(banks)]
            for j in range(4):
                blk = g4 + j
                for c in range(8):
                    mm(ps[b1][:, j * 128:(j + 1) * 128], tok_fn(c, blk), w1[:, c, :], (c == 0 and j == 0), c == 7,
                       reads=[k1] + [("hT", q) for q in range(4)], writes=[("ps", b1)], inc=(c == 7 and j == 3))
            evac(g4, b1)
        return [(lambda g4=g4: u(g4)) for g4 in range(0, nblk, 4)]

    def mixer_A(layer):
        QA = [scr_view(0, [2, S], BF16), scr_view(8192, [2, S], BF16)]
        KA = [scr_view(16384, [2, S], BF16), scr_view(24576, [2, S], BF16)]
        VA = [scr_view(32768, [16, 256], BF16), scr_view(40960, [16, 256], BF16)]
        o0 = 49152
        km = scr_view(o0, [2, 8], BF16)
        kmf = scr_view(o0 + 64, [2, 8], F32)
        gt = scr_view(o0 + 256, [128], F32)
        cmp_ = scr_view(o0 + 1024, [128, 8], F32)
        rk = scr_view(o0 + 1024 + 4096, [128], F32)
        nb = scr_view(o0 + 1024 + 4096 + 512, [128], BF16)
        et = scr_view(57344, [2, 512], BF16)
        rd = scr_view(57344 + 2048, [512], F32)
        rn = scr_view(57344 + 4096, [512], F32)
        est = {"i": 0}

        def proj_units(p):
            bs_ = p % 2
            qa, ka, va = QA[bs_], KA[bs_], VA[bs_]
            units = []
            if p < 2:
                def init():
                    for e_ in range(2):
                        sc.op("pool", ("tensor_copy", dict(out=ka[64:72, e_, :], in_=cb[0:8, CB_ONEHOT:CB_ONEHOT + S])),
                              reads=["cb"], writes=[("ka", bs_, e_, c4) for c4 in range(4)])
                    for h in range(2):
                        sc.op("pool", ("tensor_copy", dict(out=va[:, :, 128 * h + 64:128 * h + 128],
                                                           in_=cb[:, CB_ONES:CB_ONES + 64].unsqueeze(1).to_broadcast([128, 16, 64]))),
                              reads=["cb"], writes=[("va1", bs_, h)])
                units.append(init)
            units += proj_fm(layer, f"AQ{p}", True, rope_evac(lambda c4, h: qa[0:64, h, c4 * 512:(c4 + 1) * 512],
                                                              lambda c4, h: [("qa", bs_, h, c4)], True))
            units += proj_fm(layer, f"AK{p}", True, rope_evac(lambda c4, h: ka[0:64, h, c4 * 512:(c4 + 1) * 512],
                                                              lambda c4, h: [("ka", bs_, h, c4)], True))
            units += proj_fm(layer, f"AG{p}", False, silu_evac(p))

            def vev(g4, b1):
                for h in range(2):
                    sc.op("act", ("copy", dict(out=va[:, g4:g4 + 4, 128 * h:128 * h + 64],
                                               in_=ps[b1][:, :].rearrange("p (j n) -> p j n", n=128)[:, :, 64 * h:64 * h + 64])),
                          reads=[("ps", b1)], writes=[("va", bs_, h, g4)])
            units += proj_tm(layer, f"AV{p}", 16, lambda c, blk: hT[:, c, blk * 128:(blk + 1) * 128], vev)
            return units

        def attn_units(p):
            bs_ = p % 2
            qa, ka, va = QA[bs_], KA[bs_], VA[bs_]
            units = []

            def gate(h):
                allk = [("ka", bs_, h, c4) for c4 in range(4)]
                allq = [("qa", bs_, h, c4) for c4 in range(4)]
                sc.op("dve", ("tensor_reduce", dict(out=kmf[0:64, h, :], in_=ka[0:64, h, :].rearrange("p (n b) -> p n b", b=256),
                                                    axis=AX.X, op=ALU.add)), reads=allk, writes=[("kmf", h)])
                sc.op("dve", ("tensor_copy", dict(out=km[0:64, h, :], in_=kmf[0:64, h, :])), reads=[("kmf", h)], writes=[("km", h)])
                gp = ps[4][:, 0:128]
                for qt in range(16):
                    mm(gp[:, qt * 8:(qt + 1) * 8], qa[0:64, h, qt * 128:(qt + 1) * 128], km[0:64, h, :], True, True,
                       reads=allq + [("km", h)], writes=[("ps", 4)], inc=(qt == 15))
                sc.op("dve", ("tensor_tensor", dict(out=gt, in0=gp, in1=cf[:, CF_AGM:CF_AGM + 128], op=ALU.add)),
                      reads=[("ps", 4), "cf"], writes=["gt"])
                g3 = gt.rearrange("p (q n) -> p q n", n=8)
                sc.op("dve", ("tensor_tensor", dict(out=cmp_.rearrange("p (q n) m -> p q n m", n=8),
                                                    in0=g3.unsqueeze(2).to_broadcast([128, 16, 8, 8]),
                                                    in1=g3.unsqueeze(3).to_broadcast([128, 16, 8, 8]), op=ALU.is_gt)),
                      reads=["gt"], writes=["cmp"])
                sc.op("dve", ("tensor_reduce", dict(out=rk, in_=cmp_, axis=AX.X, op=ALU.add)), reads=["cmp"], writes=["rk"])
                sc.op("dve", ("tensor_scalar", dict(out=rk, in0=rk, scalar1=2.5, scalar2=None, op0=ALU.is_lt)), reads=["rk"], writes=["rk"])
                sc.op("dve", ("tensor_tensor", dict(out=rk, in0=rk, in1=cf[:, CF_AVAL:CF_AVAL + 128], op=ALU.mult)), reads=["rk", "cf"], writes=["rk"])
                sc.op("dve", ("scalar_tensor_tensor", dict(out=rk, in0=rk, scalar=-NEG, in1=cf[:, CF_AOWN:CF_AOWN + 128],
                                                           op0=ALU.mult, op1=ALU.add)), reads=["rk", "cf"], writes=["rk"])
                sc.op("dve", ("tensor_copy", dict(out=nb, in_=rk)), reads=["rk"], writes=["nb"])
                tp = ps[5][:, 0:512].bitcast(BF16)
                for half in range(2):
                    for q8 in range(8):
                        qt = half * 8 + q8
                        tr(tp[0:8, q8 * 128:(q8 + 1) * 128], nb[:, qt * 8:(qt + 1) * 8], reads=["nb", "cb"], writes=[("ps", 5)], inc=(q8 == 7))
                    sc.op("act", ("copy", dict(out=qa[64:72, h, half * 1024:(half + 1) * 1024], in_=tp[0:8, :])),
                          reads=[("ps", 5)], writes=[("qa", bs_, h, 2 * half), ("qa", bs_, h, 2 * half + 1)])

            def tile_a(st_, h, c4, kt, nkt):
                q0 = max(kt * 128, c4 * 512)
                q1 = (c4 + 1) * 512
                n = q1 - q0
                ei = est["i"]; est["i"] += 1
                sb_ = 4 + (ei % 2)
                ej = ei % 2
                st_.update(q0=q0, n=n, ej=ej)
                mm(ps[sb_][:, 0:n], ka[0:72, h, kt * 128:(kt + 1) * 128], qa[0:72, h, q0:q1], True, True,
                   reads=[("ka", bs_, h, kt // 4), ("qa", bs_, h, c4)], writes=[("ps", sb_)])
                sc.op("act", ("activation", dict(out=et[:, ej, 0:n], in_=ps[sb_][:, 0:n], func=AF.Exp, scale=0.125)),
                      reads=[("ps", sb_)], writes=[("et", ej)])
                if q0 == kt * 128:
                    sc.op("dve", ("tensor_tensor", dict(out=et[:, ej, 0:128], in0=et[:, ej, 0:128],
                                                        in1=cb[:, CB_MOWN:CB_MOWN + 128], op=ALU.mult)),
                          reads=[("et", ej), "cb"], writes=[("et", ej)])

            def tile_b(st_, h, c4, kt, nkt):
                ob = 6 + (c4 % 2)
                q0, n, ej = st_["q0"], st_["n"], st_["ej"]
                mm(ps[ob][:, q0 - c4 * 512:512], va[:, kt, 128 * h:128 * h + 128], et[:, ej, 0:n], kt == 0, kt == nkt - 1,
                   reads=[("va", bs_, h, (kt // 4) * 4), ("va1", bs_, h), ("et", ej)], writes=[("ps", ob)], inc=True)

            def norm(h, c4):
                ob = 6 + (c4 % 2)
                sc.op("act", ("activation", dict(out=rd[64:128, :], in_=ps[ob][64:128, :], func=AF.Ln)), reads=[("ps", ob)], writes=["rd"])
                sc.op("act", ("activation", dict(out=rd[64:128, :], in_=rd[64:128, :], func=AF.Exp, scale=-1.0)), reads=["rd"], writes=["rd"])
                sc.op("dve", ("tensor_tensor", dict(out=rn[64 * h:64 * h + 64, :], in0=ps[ob][0:64, :], in1=rd[64:128, :], op=ALU.mult)),
                      reads=[("ps", ob), "rd"], writes=["rd0"])
                ydst = yT[64 * h:64 * h + 64, p, c4 * 512:(c4 + 1) * 512]
                sc.op("pool", ("tensor_tensor", dict(out=ydst, in0=ydst, in1=rn[64 * h:64 * h + 64, :], op=ALU.mult)),
                      reads=["rd0", ("yT", p, c4)], writes=[("yT", p, c4)])

            items = []
            for h in range(2):
                items.append(("g", h))
                for c4 in range(4):
                    nkt = 4 * c4 + 4
                    for kt in range(nkt):
                        items.append(("t", h, c4, kt, nkt))
                    items.append(("n", h, c4))
            skew(units, items, tile_a, tile_b, {"g": gate, "n": norm})
            return units

        run(proj_units(0))
        for p in range(3):
            run_merged(attn_units(p), proj_units(p + 1) if p < 2 else [])

    def mixer_B(layer):
        qb = scr_view(0, [3, S], BF16)
        kb = scr_view(12288, [S], BF16)
        ikb = scr_view(16384, [S], BF16)
        iqb = scr_view(20480, [2, S], BF16)
        vb = scr_view(28672, [16, 128], BF16)
        iw = scr_view(32768, [16, 4], F32)
        acc = scr_view(33024, [2, S], F32)
        tmp = scr_view(49408, [2, 512], F32)
        mk = scr_view(53504, [S], BF16)
        mt = scr_view(57600, [2, 16, 128], BF16)
        et = scr_view(65792, [2, 384], BF16)
        pt = scr_view(67328, [2, 384], BF16)
        rd = scr_view(68864, [384], F32)
        bs = scr_view(70400, [64], F32)
        rn = scr_view(70656, [384], F32)
        sc.op("pool", ("tensor_copy", dict(out=vb[:, :, 64:128], in_=cb[:, CB_ONES:CB_ONES + 64].unsqueeze(1).to_broadcast([128, 16, 64]))),
              reads=["cb"], writes=["vb1"])
        pu = []
        for p in range(3):
            pu += proj_fm(layer, f"BQ{p}", True, rope_evac(lambda c4, h, p=p: qb[:, p, c4 * 512:(c4 + 1) * 512],
                                                           lambda c4, h, p=p: [("qb", c4)], False))
            pu += proj_fm(layer, f"BG{p}", False, silu_evac(3 + p))
        pu += proj_fm(layer, "BK", True, rope_evac(lambda c4, h: kb[:, c4 * 512:(c4 + 1) * 512], lambda c4, h: [("kb", c4)], False))
        pu += proj_fm(layer, "IK", True, rope_evac(lambda c4, h: ikb[:, c4 * 512:(c4 + 1) * 512], lambda c4, h: [("ikb", c4)], False))
        for p in range(2):
            pu += proj_fm(layer, f"IQ{p}", True, rope_evac(lambda c4, h, p=p: iqb[:, p, c4 * 512:(c4 + 1) * 512],
                                                           lambda c4, h, p=p: [("iqb", c4)], False))

        def vev(g4, b1):
            v3 = ps[b1][:, :].rearrange("p (j n) -> p j n", n=128)
            sc.op("act", ("copy", dict(out=vb[:, g4:g4 + 4, 0:64], in_=v3[:, :, 0:64])), reads=[("ps", b1)], writes=[("vb", g4)])
            sc.op("act", ("copy", dict(out=iw[:, g4:g4 + 4, :], in_=v3[:, :, 64:68])), reads=[("ps", b1)], writes=["iw"])
        pu += proj_tm(layer, "BV", 16, lambda c, blk: hT[:, c, blk * 128:(blk + 1) * 128], vev)
        run(pu)
        cst = {"li": 0, "ei": 0}
        thr = bs[:, 0:1]; hi = bs[:, 1:2]; lo = bs[:, 2:3]; cnt = bs[:, 3:4]; tt = bs[:, 4:5]
        W = bs[:, 8:8 + NBIS + 2]
        junk = tmp.rearrange("p a b -> p (a b)")
        tpb = [ps[2][:, 0:512].bitcast(BF16), ps[3][:, 0:512].bitcast(BF16)]

        def stage1(qt):
            N = 128 * (qt + 1)
            a = qt % 2
            m = qt % 2
            qs = slice(qt * 128, (qt + 1) * 128)
            units = []

            def idx(j, h):
                k0 = j * 512
                n = min(512, N - k0)
                e_ = h % 2
                lb = cst["li"] % 2
                cst["li"] += 1
                mm(ps[lb][:, 0:n], iqb[64 * e_:64 * e_ + 64, h // 2, qs], ikb[64 * e_:64 * e_ + 64, k0:k0 + n], True, True,
                   reads=[("iqb", qt // 4), ("ikb", j)], writes=[("ps", lb)])
                if h == 0:
                    sc.op("dve", ("tensor_scalar", dict(out=acc[:, a, k0:k0 + n], in0=ps[lb][:, 0:n], scalar1=0.0,
                                                        scalar2=iw[:, qt, 0:1], op0=ALU.max, op1=ALU.mult)),
                          reads=[("ps", lb), "iw"], writes=[("acc", a)])
                else:
                    tj = cst["li"] % 2
                    sc.op("dve", ("tensor_scalar", dict(out=tmp[:, tj, 0:n], in0=ps[lb][:, 0:n], scalar1=0.0,
                                                        scalar2=iw[:, qt, h:h + 1], op0=ALU.max, op1=ALU.mult)),
                          reads=[("ps", lb), "iw"], writes=[("tmp", tj)])
                    sc.op("pool", ("tensor_tensor", dict(out=acc[:, a, k0:k0 + n], in0=acc[:, a, k0:k0 + n],
                                                         in1=tmp[:, tj, 0:n], op=ALU.add)),
                          reads=[("acc", a), ("tmp", tj)], writes=[("acc", a)])
            for j in range((N + 511) // 512):
                for h in range(4):
                    units.append(lambda j=j, h=h: idx(j, h))

            def bis_init():
                sc.op("pool", ("tensor_tensor", dict(out=acc[:, a, qs], in0=acc[:, a, qs], in1=cf[:, CF_NEGTRI:CF_NEGTRI + 128], op=ALU.add)),
                      reads=[("acc", a), "cf"], writes=[("acc", a)])
                if qt < 2:
                    sc.op("dve", ("memset", dict(ap=thr, constant=-1e29)), writes=["thr"])
                    return
                sc.op("dve", ("tensor_reduce", dict(out=hi, in_=acc[:, a, 0:N], axis=AX.X, op=ALU.max)), reads=[("acc", a)], writes=["bs_hi"])
                sc.op("dve", ("tensor_reduce", dict(out=lo, in_=acc[:, a, 0:N - 128], axis=AX.X, op=ALU.min)), reads=[("acc", a)], writes=["bs_lo"])
                sc.op("dve", ("tensor_tensor", dict(out=tt, in0=hi, in1=lo, op=ALU.subtract)), reads=["bs_hi", "bs_lo"], writes=["bs_tt"])
                sc.op("dve", ("tensor_scalar", dict(out=W, in0=cf[:, CF_POW2:CF_POW2 + NBIS + 2], scalar1=tt, scalar2=None, op0=ALU.mult)),
                      reads=["bs_tt", "cf"], writes=["bs_W"])
                sc.op("dve", ("tensor_tensor", dict(out=thr, in0=lo, in1=W[:, 1:2], op=ALU.add)), reads=["bs_lo", "bs_W"], writes=["thr"])
            units.append(bis_init)

            def bis(k):
                big = N > 1024
                sc.op("dve", ("tensor_scalar", dict(out=acc[:, 1 - a, 0:N] if big else junk[:, 0:N], in0=acc[:, a, 0:N], scalar1=thr, scalar2=0.0,
                                                    op0=ALU.is_ge, op1=ALU.add, accum_out=cnt)),
                      reads=[("acc", a), "thr"], writes=["bs_cnt"] + ([("acc", 1 - a)] if big else [("tmp", 0), ("tmp", 1)]))
                sc.op("dve", ("tensor_scalar", dict(out=tt, in0=cnt, scalar1=255.5, scalar2=0.5, op0=ALU.is_ge, op1=ALU.subtract)),
                      reads=["bs_cnt"], writes=["bs_tt"])
                sc.op("dve", ("scalar_tensor_tensor", dict(out=thr, in0=tt, scalar=W[:, k + 1:k + 2], in1=thr, op0=ALU.mult, op1=ALU.add)),
                      reads=["bs_tt", "bs_W", "thr"], writes=["thr"])
            if qt >= 2:
                for k in range(NBIS):
                    units.append(lambda k=k: bis(k))

            def fin():
                if qt >= 2:
                    sc.op("dve", ("tensor_tensor", dict(out=thr, in0=thr, in1=W[:, NBIS + 1:NBIS + 2], op=ALU.subtract)), reads=["thr", "bs_W"], writes=["thr"])
                sc.op("dve", ("tensor_scalar", dict(out=mk[:, 0:N], in0=acc[:, a, 0:N], scalar1=thr, scalar2=None, op0=ALU.is_ge)),
                      reads=[("acc", a), "thr"], writes=["mk"])
                for kt in range(qt + 1):
                    tr(tpb[kt // 8][:, (kt % 8) * 128:(kt % 8 + 1) * 128], mk[:, kt * 128:(kt + 1) * 128], reads=["mk", "cb"], writes=[("ps", 2 + kt // 8)],
                       inc=(kt == qt or kt == 7))
                for half in range((qt // 8) + 1):
                    nk = min(8, qt + 1 - 8 * half)
                    sc.op("act", ("copy", dict(out=mt[:, m, 8 * half:8 * half + nk, :],
                                               in_=tpb[half][:, 0:nk * 128].rearrange("p (k n) -> p k n", n=128))),
                          reads=[("ps", 2 + half)], writes=[("mt", m)])
            units.append(fin)
            return units

        def stage2(qt):
            m = qt % 2
            qs = slice(qt * 128, (qt + 1) * 128)
            units = []

            def tile_a(st_, kt, e_):
                ei = cst["ei"]; cst["ei"] += 1
                sb_ = 4 + (ei % 2)
                ej = ei % 2
                st_.update(ej=ej)
                mm(ps[sb_][:, 0:384], kb[64 * e_:64 * e_ + 64, kt * 128:(kt + 1) * 128], qb[64 * e_:64 * e_ + 64, :, qs], True, True,
                   reads=[("kb", kt // 4), ("qb", qt // 4)], writes=[("ps", sb_)])
                sc.op("act", ("activation", dict(out=et[:, ej, :], in_=ps[sb_][:, 0:384], func=AF.Exp, scale=0.125)),
                      reads=[("ps", sb_)], writes=[("et", ej)])
                sc.op("dve", ("tensor_tensor", dict(out=pt[:, ej, :].rearrange("p (h n) -> p h n", n=128),
                                                    in0=et[:, ej, :].rearrange("p (h n) -> p h n", n=128),
                                                    in1=mt[:, m, kt, :].unsqueeze(1).to_broadcast([128, 3, 128]), op=ALU.mult)),
                      reads=[("et", ej), ("mt", m)], writes=[("pt", ej)])

            def tile_b(st_, kt, e_):
                ob = 6 + e_
                ej = st_["ej"]
                mm(ps[ob][:, 0:384], vb[:, kt, :], pt[:, ej, :], kt == 0, kt == qt,
                   reads=[("vb", (kt // 4) * 4), "vb1", ("pt", ej)], writes=[("ps", ob)], inc=True)
            items = [("t", kt, e_) for kt in range(qt + 1) for e_ in range(2)]

            def norm(e_):
                ob = 6 + e_
                sc.op("act", ("activation", dict(out=rd[64:128, :], in_=ps[ob][64:128, 0:384], func=AF.Ln)), reads=[("ps", ob)], writes=["rd"])
                sc.op("act", ("activation", dict(out=rd[64:128, :], in_=rd[64:128, :], func=AF.Exp, scale=-1.0)), reads=["rd"], writes=["rd"])
                sc.op("dve", ("tensor_tensor", dict(out=rn[64 * e_:64 * e_ + 64, :], in0=ps[ob][0:64, 0:384], in1=rd[64:128, :], op=ALU.mult)),
                      reads=[("ps", ob), "rd"], writes=["rd0"])
                ydst = yT[64 * e_:64 * e_ + 64, 3:6, qs]
                sc.op("pool", ("tensor_tensor", dict(out=ydst, in0=ydst, in1=rn[64 * e_:64 * e_ + 64, :].rearrange("p (h n) -> p h n", n=128), op=ALU.mult)),
                      reads=["rd0"] + [("yT", 3 + p, qt // 4) for p in range(3)], writes=[("yT", 3 + p, qt // 4) for p in range(3)])
            items += [("n", 0), ("n", 1)]
            skew(units, items, tile_a, tile_b, {"n": norm})
            return units

        run(stage1(0))
        for qt in range(16):
            run_merged(stage2(qt), stage1(qt + 1) if qt < 15 else [])

    def mixer_C(layer):
        qc = scr_view(0, [2, S], BF16)
        kc = scr_view(8192, [2, S], BF16)
        vc = scr_view(16384, [16, 512], BF16)
        ac = scr_view(32768, [4, S], F32)
        et = scr_view(65536, [2, 256], BF16)
        rd = scr_view(65536 + 1024, [512], F32)
        rn = scr_view(65536 + 3072, [512], F32)
        sc.op("pool", ("tensor_copy", dict(out=vc.rearrange("p b (h c) -> p b h c", c=128)[:, :, :, 64:128],
                                           in_=cb[:, CB_ONES:CB_ONES + 64].unsqueeze(1).unsqueeze(1).to_broadcast([128, 16, 4, 64]))),
              reads=["cb"], writes=["vc1"])
        gu = []
        for p in range(2):
            gu += proj_fm(layer, f"CG{p}", False, silu_evac(6 + p))
        run(gu)
        groups = [g for g in range(3) if g in getattr(_build, 'cgroups', (0, 1, 2))]
        dils = (1, 4, 16)
        cst = {"ei": 0}

        def toks_of(dil):
            def toks(r, m, cnt=128):
                st0 = r + dil * 128 * m
                return slice(st0, st0 + dil * (cnt - 1) + 1, dil)
            return toks

        def proj_units(g, p, pbanks):
            dil = dils[g]
            nblk = S // dil // 128
            toks = toks_of(dil)
            units = []
            units += proj_fm(layer, f"CQ{g}{p}", True, rope_evac(lambda c4, h: qc[:, p, c4 * 512:(c4 + 1) * 512], lambda c4, h: [("qc", p, c4)], False),
                             banks=pbanks)
            units += proj_fm(layer, f"CK{g}{p}", True, rope_evac(lambda c4, h: kc[:, p, c4 * 512:(c4 + 1) * 512], lambda c4, h: [("kc", p, c4)], False),
                             banks=pbanks)

            def vev(g4, b1):
                v4 = ps[b1][:, :].rearrange("p (j h c) -> p j h c", h=2, c=64)
                sc.op("act", ("copy", dict(out=vc.rearrange("p b (h c) -> p b h c", c=128)[:, g4:g4 + 4, 2 * p:2 * p + 2, 0:64], in_=v4)),
                      reads=[("ps", b1)], writes=[("vc", p, g4)])
            units += proj_tm(layer, f"CV{g}{p}", 16, lambda c, blk: hT[:, c, toks(blk // nblk, blk % nblk)], vev,
                             banks=(pbanks[0][0], pbanks[1][0]) if len(pbanks[0]) == 1 else (0, 1))
            return units

        def attn_units(g, j, first):
            dil = dils[g]
            nblk = S // dil // 128
            toks = toks_of(dil)
            p, e_ = j // 2, j % 2
            pr = slice(64 * e_, 64 * e_ + 64)
            allq = [("qc", p, c4) for c4 in range(4)]
            allk = [("kc", p, c4) for c4 in range(4)]
            started = [False] * 4
            units = []

            def blk_a(st_, r, m):
                nq = 256 if m + 1 < nblk else 128
                ei = cst["ei"]; cst["ei"] += 1
                sb_ = 4 + (ei % 2)
                ej = ei % 2
                st_.update(nq=nq, ej=ej)
                mm(ps[sb_][:, 0:nq], kc[pr, p, toks(r, m)], qc[pr, p, toks(r, m, nq)], True, True,
                   reads=allk + allq, writes=[("ps", sb_)])
                sc.op("act", ("activation", dict(out=et[:, ej, 0:nq], in_=ps[sb_][:, 0:nq], func=AF.Exp, scale=0.125)),
                      reads=[("ps", sb_)], writes=[("et", ej)])
                sc.op("dve", ("tensor_tensor", dict(out=et[:, ej, 0:nq], in0=et[:, ej, 0:nq], in1=cb[:, CB_MOWN:CB_MOWN + nq], op=ALU.mult)),
                      reads=[("et", ej), "cb"], writes=[("et", ej)])

            def blk_b(st_, r, m):
                blk = r * nblk + m
                nq, ej = st_["nq"], st_["ej"]
                pieces = []
                for part in range(nq // 128):
                    t0 = r + dil * 128 * (m + part)
                    if dil <= 4:
                        pieces.append((part * 128, 128, t0))
                    else:
                        for q4 in range(4):
                            pieces.append((part * 128 + 32 * q4, 32, t0 + dil * 32 * q4))
                for pi, (c0, cn, t0) in enumerate(pieces):
                    bnk = t0 // 512
                    o0 = t0 % 512
                    mm(ps[bnk][:, o0:o0 + dil * (cn - 1) + 1:dil], vc[:, blk, 128 * j:128 * j + 128], et[:, ej, c0:c0 + cn],
                       not started[bnk], False, reads=[("vc", p, (blk // 4) * 4), "vc1", ("et", ej)],
                       writes=[("ps", bnk)], inc=(pi == len(pieces) - 1))
                    started[bnk] = True
            items = [("t", r, m) for r in range(dil) for m in range(nblk)] + [("e",)]

            def evac():
                for c4 in range(4):
                    dst = ac[:, j, c4 * 512:(c4 + 1) * 512]
                    if first:
                        sc.op("act", ("copy", dict(out=dst, in_=ps[c4][:, :])), reads=[("ps", c4)], writes=[("ac", j, c4)])
                    else:
                        sc.op("dve", ("tensor_tensor", dict(out=dst, in0=ps[c4][:, :], in1=dst, op=ALU.add)),
                              reads=[("ps", c4), ("ac", j, c4)], writes=[("ac", j, c4)])
            skew(units, items, blk_a, blk_b, {"e": evac})
            return units

        PB = ((6,), (7,))
        seq = []
        for gi, g in enumerate(groups):
            seq.append((g, 0)); seq.append((g, 1))
        run(proj_units(seq[0][0], seq[0][1], ((0, 1), (2, 3))))
        for i, (g, p) in enumerate(seq):
            nxt = proj_units(seq[i + 1][0], seq[i + 1][1], PB) if i + 1 < len(seq) else []
            first = (g == groups[0])
            run_merged(attn_units(g, 2 * p, first) + attn_units(g, 2 * p + 1, first), nxt)
        for j in range(4):
            p, e_ = j // 2, j % 2
            for c4 in range(4):
                cs = slice(c4 * 512, (c4 + 1) * 512)
                sc.op("act", ("activation", dict(out=rd[64:128, :], in_=ac[64:128, j, cs], func=AF.Ln)), reads=[("ac", j, c4)], writes=["rd"])
                sc.op("act", ("activation", dict(out=rd[64:128, :], in_=rd[64:128, :], func=AF.Exp, scale=-1.0)), reads=["rd"], writes=["rd"])
                sc.op("act", ("copy", dict(out=rn[64:128, :], in_=ac[0:64, j, cs])), reads=[("ac", j, c4)], writes=[("rn", 1)])
                sc.op("dve", ("tensor_tensor", dict(out=rn[64 * e_:64 * e_ + 64, :], in0=rn[64:128, :], in1=rd[64:128, :], op=ALU.mult)),
                      reads=[("rn", 1), "rd"], writes=[("rn", e_)])
                ydst = yT[64 * e_:64 * e_ + 64, 6 + p, cs]
                sc.op("pool", ("tensor_tensor", dict(out=ydst, in0=ydst, in1=rn[64 * e_:64 * e_ + 64, :], op=ALU.mult)),
                      reads=[("rn", e_), ("yT", 6 + p, c4)], writes=[("yT", 6 + p, c4)])

    def phase_final(si, layer, src_d, dst_d, last):
        mg = scr_view(0, [8, S], BF16)
        wbr = scr_view(32768, [8, D], BF16)
        wo = scr_view(49152, [8, D], BF16)
        gs = scr_view(65536, [3, 512], BF16)
        tm = scr_view(65536 + 3072, [512], F32)
        for c in range(8):
            nm = (f"WA{c}" if c < 3 else f"WB{c - 3}" if c < 6 else f"WC{c - 6}")
            load_w(layer, _XIDX[nm], dest=wbr[:, c, :], dest_key=("wbr", c))
        for c in range(8):
            load_w(layer, _XIDX[f"WO{c}"], dest=wo[:, c, :], dest_key=("wo", c))
        if last:
            load_g(0, 2)
        kch = ((0, 3), (3, 6), (6, 8))
        for dt_ in range(8):
            gw = []
            for br in range(3):
                w_, k_ = load_w(layer, _TIDX[f"MG{8 * br + dt_}"])
                gw.append((w_.rearrange("p (c n) -> p c n", n=128), k_))
            for c4 in range(4):
                cs = slice(c4 * 512, (c4 + 1) * 512)
                for br in range(3):
                    for c in range(8):
                        mm(ps[br][:, :], gw[br][0][:, c, :], hT[:, c, cs], c == 0, c == 7, reads=[gw[br][1], ("hT", c4)], writes=[("ps", br)])
                    sc.op("act", ("activation", dict(out=gs[:, br, :], in_=ps[br][:, :], func=AF.Sigmoid)), reads=[("ps", br)], writes=[("gs", br)])
                for br in range(3):
                    a0, a1 = kch[br]
                    for c in range(a0, a1):
                        mm(ps[3 + br][:, :], wbr[:, c, dt_ * 128:(dt_ + 1) * 128], yT[:, c, cs], c == a0, c == a1 - 1,
                           reads=[("wbr", c), ("yT", c, c4)], writes=[("ps", 3 + br)])
                sc.op("dve", ("tensor_tensor", dict(out=tm, in0=ps[3][:, :], in1=gs[:, 0, :], op=ALU.mult)), reads=[("ps", 3), ("gs", 0)], writes=["tm"])
                sc.op("dve", ("tensor_tensor", dict(out=gs[:, 1, :], in0=ps[4][:, :], in1=gs[:, 1, :], op=ALU.mult)), reads=[("ps", 4), ("gs", 1)], writes=[("gs", 1)])
                sc.op("dve", ("tensor_tensor", dict(out=gs[:, 2, :], in0=ps[5][:, :], in1=gs[:, 2, :], op=ALU.mult)), reads=[("ps", 5), ("gs", 2)], writes=[("gs", 2)])
                sc.op("pool", ("tensor_tensor", dict(out=tm, in0=tm, in1=gs[:, 1, :], op=ALU.add)), reads=["tm", ("gs", 1)], writes=["tm"])
                sc.op("pool", ("tensor_tensor", dict(out=mg[:, dt_, cs], in0=tm, in1=gs[:, 2, :], op=ALU.add)),
                      reads=["tm", ("gs", 2)], writes=[("mg", c4)])
        for t in range(NT):
            s = t % 2
            ts = slice(t * 128, (t + 1) * 128)
            sc.dma(("x", s), ("dma_start", dict(out=xt[:, s, :], in_=src_d[si, ts, :])), writes=[("xt", s)])
            for hf in range(2):
                b = 6 + hf
                for c in range(8):
                    mm(ps[b][:, :], mg[:, c, ts], wo[:, c, hf * 512:(hf + 1) * 512], c == 0, c == 7,
                       reads=[("mg", t // 4), ("wo", c)], writes=[("ps", b)])
                sc.op("dve", ("tensor_tensor", dict(out=xt[:, s, hf * 512:(hf + 1) * 512], in0=ps[b][:, :],
                                                                        in1=xt[:, s, hf * 512:(hf + 1) * 512], op=ALU.add)),
                      reads=[("ps", b), ("xt", s)], writes=[("xt", s)])
            if last:
                ss = st[:, 8 + 2 * s:8 + 2 * s + 1]
                rs = st[:, 8 + 2 * s + 1:8 + 2 * s + 2]
                sc.op("act", ("activation", dict(out=hn[:, s, :], in_=xt[:, s, :], func=AF.Square, accum_out=ss)),
                      reads=[("xt", s)], writes=[("hn", s), ("st", s)])
                sc.op("dve", ("tensor_scalar", dict(out=rs, in0=ss, scalar1=1.0 / D, scalar2=1e-6, op0=ALU.mult, op1=ALU.add)),
                      reads=[("st", s)], writes=[("st", s)])
                sc.op("act", ("activation", dict(out=rs, in_=rs, func=AF.Sqrt)), reads=[("st", s)], writes=[("st", s)])
                sc.op("dve", ("reciprocal", dict(out=rs, in_=rs)), reads=[("st", s)], writes=[("st", s)])
                sc.op("dve", ("scalar_tensor_tensor", dict(out=xt[:, s, :], in0=xt[:, s, :], scalar=rs, in1=gbc[:, 0, :],
                                                                         op0=ALU.mult, op1=ALU.mult)),
                      reads=[("xt", s), ("st", s), ("gbc", 0)], writes=[("xt", s)])
            sc.dma(("o", s), ("dma_start", dict(out=dst_d[si, ts, :], in_=xt[:, s, :])), reads=[("xt", s)], writes=[("dram", si, t)])

    enabled = set(getattr(_build, "enabled", ("A", "B", "C")))

    def program():
        sc.dma("c0", ("dma_start", dict(out=cf[:], in_=cf_d[:, :])), writes=["cf"])
        sc.dma("c1", ("dma_start", dict(out=cb[:], in_=cb_d[:, :])), writes=["cb"])
        for si in range(nseq):
            for li_, layer in enumerate(layers_all):
                first = (li_ == 0)
                last = (li_ == len(layers_all) - 1)
                src = x_d if first else xs_d
                dst = out_d if last else xs_d
                sc.phase = "norm"
                phase_norm(si, layer, src)
                if "A" in enabled:
                    sc.barrier()
                    sc.phase = "A"
                    mixer_A(layer)
                if "B" in enabled:
                    sc.barrier()
                    sc.phase = "B"
                    mixer_B(layer)
                if "C" in enabled:
                    sc.barrier()
                    sc.phase = "C"
                    mixer_C(layer)
                sc.barrier()
                sc.phase = "final"
                for name, shape, dt in taps:
                    if name == f"yT{layer}" and si == 0:
                        sc.dma(("tap", name), ("dma_start", dict(out=tap_d[name][:, :, :], in_=yT[:])),
                               reads=[("yT", c, c4) for c in range(8) for c4 in range(4)])
                    if name == f"hT{layer}" and si == 0:
                        sc.dma(("tap", name), ("dma_start", dict(out=tap_d[name][:, :, :], in_=hT[:])),
                               reads=[("hT", c4) for c4 in range(4)])
                phase_final(si, layer, src, dst, last and final_norm)
                sc.barrier()
        sc.final_wait("sp", [("o", 0), ("o", 1)] + [("tap", n) for n, _, _ in taps])


    program()
    sc = Sched()
    wstate.update(n=0, rec=False, issued=0)
    ppp["i"] = 0
    rtc["i"] = 0
    program()

    waited = {e: set() for e in ("pe", "act", "dve", "pool")}
    for e in Sched.ENG:
        for waits, fn, inc, ph in sc.q[e]:
            for de, dc in waits:
                if de in waited:
                    waited[de].add(dc)
    remap = {e: {c: i + 1 for i, c in enumerate(sorted(waited[e]))} for e in waited}
    for e in waited:
        c = 0
        newq = []
        for waits, fn, inc, ph in sc.q[e]:
            if inc is not None and inc[0] == "E":
                c += 1
                if c not in remap[e]:
                    inc = None
            newq.append((waits, fn, inc, ph))
        sc.q[e] = newq
    for e in Sched.ENG:
        sc.q[e] = [([(de, remap[de][dc]) if de in remap else (de, dc) for de, dc in waits], fn, inc, ph)
                   for waits, fn, inc, ph in sc.q[e]]

    sems = {}
    for e in Sched.ENG:
        sems[e] = es.enter_context(nc.semaphore("s_" + e))
    for de in sc.dcnt:
        sems[de] = es.enter_context(nc.semaphore("d%d" % len(sems)))
    block = es.enter_context(nc.Block())

    def replay(engname):
        def run(eng):
            for waits, fn, inc, ph in sc.q[engname]:
                for de, dc in waits:
                    eng.wait_ge(sems[de], dc)
                if fn is None:
                    continue
                ins = getattr(eng, fn[0])(**fn[1])
                if ANNOTATE:
                    ins.annotate(ph)
                if inc is not None:
                    if inc[0] == "E":
                        ins.then_inc(sems[inc[1]], 1)
                    else:
                        ins.then_inc(sems[inc[1]], 16)
        return run
    block.tensor(replay("pe"))
    block.scalar(replay("act"))
    block.vector(replay("dve"))
    block.gpsimd(replay("pool"))
    block.sync(replay("sp"))
    es.close()
    return nc


_CACHE = {}


def kernel(x, norm_g, w_in, w_br_a, w_br_b, w_br_c, w_out, final_norm_g):
    x = np.ascontiguousarray(np.asarray(x, np.float32))
    ncores = 8
    nseq = x.shape[0] // ncores
    wt = _pack_weights(np.asarray(w_in, np.float32), np.asarray(w_br_a, np.float32), np.asarray(w_br_b, np.float32),
                       np.asarray(w_br_c, np.float32), np.asarray(w_out, np.float32))
    gv = np.concatenate([np.asarray(norm_g, np.float32), np.asarray(final_norm_g, np.float32)[None, :]], axis=0)
    cf, cb = _consts()
    nc = _build(nseq)
    in_maps = [{"x": x[i * nseq:(i + 1) * nseq], "wt": wt, "gv": gv, "cf": cf, "cb": cb} for i in range(ncores)]
    res = run_bass_kernel_spmd(nc, in_maps, core_ids=list(range(ncores)))
    return np.concatenate([r["out"] for r in res.results], axis=0)
```

```python
import numpy as np
import ml_dtypes
import concourse.bass as bass
import concourse.mybir as mybir
from concourse.bass_utils import run_bass_kernel_spmd

F32 = mybir.dt.float32
BF16 = mybir.dt.bfloat16
AF = mybir.ActivationFunctionType
ALU = mybir.AluOpType
AX = mybir.AxisListType

S = 2048
D = 1024
NT = 16
NEG = -30000.0
NBIS = 12
ANNOTATE = False

_splits = (384, 384, 384, 384, 384, 64, 64, 384, 256, 64, 4, 768, 768, 768, 256, 3072)
_names = ("a_q", "a_k", "a_v", "a_g", "b_q", "b_k", "b_v", "b_g", "i_q", "i_k", "i_w",
          "c_q", "c_k", "c_v", "c_g", "m_g")
_off = {}
_o = 0
for _n, _s in zip(_names, _splits):
    _off[_n] = _o
    _o += _s


def _rot(cols):
    cols = np.asarray(cols).reshape(-1, 2, 32)
    return cols[:, ::-1, :].reshape(-1)


def _tile_cols():
    t = []
    rng = lambda n, a, b: np.arange(_off[n] + a, _off[n] + b)
    for p in range(3):
        c = rng("a_q", 128 * p, 128 * p + 128); t.append((f"AQ{p}", c)); t.append((f"AQR{p}", _rot(c)))
        c = rng("a_k", 128 * p, 128 * p + 128); t.append((f"AK{p}", c)); t.append((f"AKR{p}", _rot(c)))
        t.append((f"AG{p}", rng("a_g", 128 * p, 128 * p + 128)))
        t.append((f"AV{p}", rng("a_v", 128 * p, 128 * p + 128)))
    for p in range(3):
        c = rng("b_q", 128 * p, 128 * p + 128); t.append((f"BQ{p}", c)); t.append((f"BQR{p}", _rot(c)))
        t.append((f"BG{p}", rng("b_g", 128 * p, 128 * p + 128)))
    c = np.concatenate([rng("b_k", 0, 64), rng("b_k", 0, 64)]); t.append(("BK", c)); t.append(("BKR", _rot(c)))
    c = np.concatenate([rng("i_k", 0, 64), rng("i_k", 0, 64)]); t.append(("IK", c)); t.append(("IKR", _rot(c)))
    for p in range(2):
        c = rng("i_q", 128 * p, 128 * p + 128); t.append((f"IQ{p}", c)); t.append((f"IQR{p}", _rot(c)))
    c = np.concatenate([rng("b_v", 0, 64), rng("i_w", 0, 4), rng("i_w", 0, 4).repeat(15)]); t.append(("BV", c))
    for g in range(3):
        for p in range(2):
            c = rng("c_q", 256 * g + 128 * p, 256 * g + 128 * p + 128); t.append((f"CQ{g}{p}", c)); t.append((f"CQR{g}{p}", _rot(c)))
            c = rng("c_k", 256 * g + 128 * p, 256 * g + 128 * p + 128); t.append((f"CK{g}{p}", c)); t.append((f"CKR{g}{p}", _rot(c)))
            t.append((f"CV{g}{p}", rng("c_v", 256 * g + 128 * p, 256 * g + 128 * p + 128)))
    for p in range(2):
        t.append((f"CG{p}", rng("c_g", 128 * p, 128 * p + 128)))
    for j in range(24):
        t.append((f"MG{j}", rng("m_g", 128 * j, 128 * j + 128)))
    return t


_TILES = _tile_cols()
_TIDX = {n: i for i, (n, _) in enumerate(_TILES)}
NWT = len(_TILES)
_XIDX = {}
for _i, _n in enumerate([f"WA{c}" for c in range(3)] + [f"WB{c}" for c in range(3)] +
                        [f"WC{c}" for c in range(2)] + [f"WO{c}" for c in range(8)]):
    _XIDX[_n] = NWT + _i
NWALL = NWT + 16


def _pack_weights(w_in, w_br_a, w_br_b, w_br_c, w_out):
    L = w_in.shape[0]
    out = np.empty((L, NWALL, 128, 1024), np.float32)
    allc = np.concatenate([c for _, c in _TILES])
    for l in range(L):
        g = w_in[l][:, allc]
        g = g.reshape(8, 128, NWT, 128).transpose(2, 1, 0, 3)
        out[l, :NWT] = g.reshape(NWT, 128, 1024)
        out[l, NWT:NWT + 3] = w_br_a[l].reshape(3, 128, 1024)
        out[l, NWT + 3:NWT + 6] = w_br_b[l].reshape(3, 128, 1024)
        out[l, NWT + 6:NWT + 8] = w_br_c[l].reshape(2, 128, 1024)
        out[l, NWT + 8:NWT + 16] = w_out[l].reshape(8, 128, 1024)
    return out


CF_COS = 0
CF_SIN = CF_COS + S
CF_NEGTRI = CF_SIN + S
CF_AGM = CF_NEGTRI + 128
CF_AVAL = CF_AGM + 128
CF_AOWN = CF_AVAL + 128
CF_POW2 = CF_AOWN + 128
CF_N = CF_POW2 + 32
CB_ID = 0
CB_MOWN = CB_ID + 128
CB_MPREV = CB_MOWN + 128
CB_ONEHOT = CB_MPREV + 128
CB_ONES = CB_ONEHOT + S
CB_N = CB_ONES + 128


def _consts():
    cf = np.zeros((128, CF_N), np.float32)
    inv = 1.0 / (10000.0 ** (np.arange(0, 64, 2, dtype=np.float32) / 64.0))
    ang = np.arange(S, dtype=np.float32)[None, :] * inv[:, None].astype(np.float32)
    cos = np.cos(ang).astype(np.float32)
    sin = np.sin(ang).astype(np.float32)
    for p in range(128):
        j = p % 32
        cf[p, CF_COS:CF_COS + S] = cos[j]
        cf[p, CF_SIN:CF_SIN + S] = sin[j] * (-1.0 if (p % 64) < 32 else 1.0)
    t = np.arange(128)[:, None]
    s_ = np.arange(128)[None, :]
    cf[:, CF_NEGTRI:CF_NEGTRI + 128] = np.where(s_ <= t, 0.0, -1e30)
    for qt in range(16):
        own = qt // 2
        for n in range(8):
            cf[:, CF_AGM + qt * 8 + n] = 0.0 if n < own else -1e30
            cf[:, CF_AVAL + qt * 8 + n] = 1.0 if n < own else 0.0
            cf[:, CF_AOWN + qt * 8 + n] = 0.0 if n == own else NEG
    for k in range(32):
        cf[:, CF_POW2 + k] = 2.0 ** (-k)
    cb = np.zeros((128, CB_N), np.float32)
    cb[:, CB_ID:CB_ID + 128] = np.eye(128)
    cb[:, CB_MOWN:CB_MOWN + 128] = (t <= s_)
    cb[:, CB_MPREV:CB_MPREV + 128] = (t >= s_)
    for n in range(8):
        cb[n, CB_ONEHOT + 256 * n:CB_ONEHOT + 256 * n + 256] = 1.0
    cb[:, CB_ONES:CB_ONES + 128] = 1.0
    return cf, cb.astype(ml_dtypes.bfloat16)


class Sched:
    ENG = ("pe", "act", "dve", "pool", "sp")

    def __init__(self):
        self.q = {e: [] for e in self.ENG}
        self.cnt = {e: 0 for e in self.ENG}
        self.seen = {e: {} for e in self.ENG}
        self.lastw = {}
        self.readers = {}
        self.dcnt = {}
        self.phase = "init"

    def _need(self, eng, reads, writes):
        need = {}

        def add(dep, raw):
            de, dc = dep
            if de == eng and (eng == "pe" or eng == "sp"):
                return
            if need.get(de, 0) < dc:
                need[de] = dc
        for r in reads:
            w = self.lastw.get(r)
            if w is not None:
                add(w, True)
        for w_ in writes:
            w = self.lastw.get(w_)
            if w is not None:
                add(w, False)
            for de, dc in self.readers.get(w_, {}).items():
                add((de, dc), False)
        waits = []
        for de, dc in need.items():
            if self.seen[eng].get(de, 0) < dc:
                self.seen[eng][de] = dc
                waits.append((de, dc))
        return waits

    def _record(self, tag, reads, writes):
        for r in reads:
            d = self.readers.setdefault(r, {})
            if d.get(tag[0], 0) < tag[1]:
                d[tag[0]] = tag[1]
        for w_ in writes:
            self.lastw[w_] = tag
            self.readers[w_] = {}

    def op(self, eng, fn, reads=(), writes=(), inc=True):
        waits = self._need(eng, reads, writes)
        tag = (eng, self.cnt[eng] + 1)
        if inc:
            self.cnt[eng] += 1
        self.q[eng].append((waits, fn, ("E", eng) if inc else None, self.phase))
        self._record(tag, reads, writes)

    def dma(self, key, fn, reads=(), writes=(), eng="sp"):
        waits = self._need(eng, reads, writes)
        de = ("dma", key)
        self.dcnt[de] = self.dcnt.get(de, 0) + 16
        self.q[eng].append((waits, fn, ("D", de), self.phase))
        self._record((de, self.dcnt[de]), reads, writes)

    def barrier(self):
        tgt = {e: self.cnt[e] for e in ("pe", "act", "dve", "pool")}
        tgt.update(self.dcnt)
        for e in self.ENG:
            waits = []
            for de, dc in tgt.items():
                if de == e or dc == 0:
                    continue
                if self.seen[e].get(de, 0) < dc:
                    self.seen[e][de] = dc
                    waits.append((de, dc))
            if waits:
                self.q[e].append((waits, None, None, None))

    def final_wait(self, eng, dma_keys):
        waits = [(("dma", k), self.dcnt[("dma", k)]) for k in dma_keys if ("dma", k) in self.dcnt]
        self.q[eng].append((waits, None, None, None))


def _build(nseq, layers_all=(0, 1), final_norm=True, taps=()):
    nc = bass.Bass("TRN2", target_bir_lowering=False)
    x_d = nc.dram_tensor("x", [nseq, S, D], F32, kind="ExternalInput").ap()
    wt_d = nc.dram_tensor("wt", [2, NWALL, 128, 1024], F32, kind="ExternalInput").ap()
    gv_d = nc.dram_tensor("gv", [3, D], F32, kind="ExternalInput").ap()
    cf_d = nc.dram_tensor("cf", [128, CF_N], F32, kind="ExternalInput").ap()
    cb_d = nc.dram_tensor("cb", [128, CB_N], BF16, kind="ExternalInput").ap()
    out_d = nc.dram_tensor("out", [nseq, S, D], F32, kind="ExternalOutput").ap()
    xs_d = nc.dram_tensor("xscr", [nseq, S, D], F32).ap()
    tap_d = {}
    for name, shape, dt in taps:
        tap_d[name] = nc.dram_tensor("tap_" + name, list(shape), dt, kind="ExternalOutput").ap()

    sc = Sched()
    sb = {}

    def alloc(name, shape, dt):
        t = nc.alloc_sbuf_tensor(name, list(shape), dt) if False else None
        return t

    from contextlib import ExitStack
    es = ExitStack()

    def SB(name, shape, dt):
        t = es.enter_context(nc.sbuf_tensor("sb_" + name, list(shape), dt))
        sb[name] = t
        return t

    def PS(name, shape, dt):
        return es.enter_context(nc.psum_tensor(name, list(shape), dt))

    cf = SB("cf", [128, CF_N], F32)
    cb = SB("cb", [128, CB_N], BF16)
    hT = SB("hT", [128, 8, S], BF16)
    yT = SB("yT", [128, 8, S], BF16)
    gbc = SB("gbc", [128, 2, D], F32)
    wbf = SB("wbf", [128, 8, 1024], BF16)
    xt = SB("xt", [128, 2, D], F32)
    hn = SB("hn", [128, 2, D], BF16)
    st = SB("st", [128, 64], F32)
    scr = SB("scr", [128, 36864], BF16)
    ps = [PS(f"ps{i}", [128, 512], F32) for i in range(8)]

    def scr_view(off_bytes, shape, dt):
        n = int(np.prod(shape))
        if dt == F32:
            assert off_bytes % 4 == 0
            v = scr[:, off_bytes // 2: off_bytes // 2 + 2 * n].bitcast(F32)
        else:
            v = scr[:, off_bytes // 2: off_bytes // 2 + n]
        if len(shape) == 2:
            v = v.rearrange("p (a b) -> p a b", b=shape[1])
        elif len(shape) == 3:
            v = v.rearrange("p (a b c) -> p a b c", b=shape[1], c=shape[2])
        return v

    ident = cb[:, CB_ID:CB_ID + 128]

    def mm(out, lhsT, rhs, start, stop, reads, writes, inc=None):
        if inc is None:
            inc = stop
        sc.op("pe", ("matmul", dict(out=out, lhsT=lhsT, rhs=rhs, start=start, stop=stop, skip_group_check=True)),
              reads=reads, writes=writes, inc=inc)

    def tr(out, in_, reads, writes, inc=True):
        sc.op("pe", ("transpose", dict(out=out, in_=in_, identity=ident[:in_.shape[0], :in_.shape[0]])), reads=reads, writes=writes, inc=inc)


    NSLOT = 8
    WDEPTH = 4
    wstate = {"n": 0, "ring": 0, "rec": True, "issued": 0, "xd": 0}
    wplan = []

    def _issue_w(ent):
        layer, idx, dest, dest_key, skey = ent
        sc.dma(skey, ("dma_start", dict(out=dest, in_=wt_d[layer, idx, :, :])), writes=[dest_key], eng="pool")

    def load_w(layer, idx, dest=None, dest_key=None):
        n = wstate["n"]; wstate["n"] += 1
        if wstate["rec"]:
            if dest is None:
                b = wstate["ring"] % NSLOT; wstate["ring"] += 1
                dest = wbf[:, b, :]
                dest_key = ("wbf", b)
                skey = ("w", b)
            else:
                skey = ("wx", wstate["xd"] % 16); wstate["xd"] += 1
            wplan.append((layer, idx, dest, dest_key, skey))
            return dest, dest_key
        while wstate["issued"] < len(wplan) and wstate["issued"] <= n + WDEPTH:
            ent = wplan[wstate["issued"]]
            if ent[4][0] == "wx" and wstate["issued"] > n:
                break
            _issue_w(ent)
            wstate["issued"] += 1
        ent = wplan[n]
        return ent[2], ent[3]

    def load_g(slot, row):
        src = gv_d[row:row + 1, :].partition_broadcast(128) if False else None
        from concourse.ap import AP
        src = AP(gv_d.tensor, row * D, [[0, 128], [1, D]])
        sc.dma(("g", slot), ("dma_start", dict(out=gbc[:, slot, :], in_=src)), writes=[("gbc", slot)])

    def phase_norm(si, layer, src_d):
        load_g(layer % 2, layer)
        pst = ps[7][:, 0:512].bitcast(BF16)
        def stats(t):
            s = t % 2
            sc.dma(("x", s), ("dma_start", dict(out=xt[:, s, :], in_=src_d[si, t * 128:(t + 1) * 128, :])),
                   writes=[("xt", s)])
            ss = st[:, 2 * s:2 * s + 1]
            rs = st[:, 2 * s + 1:2 * s + 2]
            sc.op("act", ("activation", dict(out=hn[:, s, :], in_=xt[:, s, :], func=AF.Square, accum_out=ss)),
                  reads=[("xt", s)], writes=[("hn", s), ("st", s)])
            sc.op("dve", ("tensor_scalar", dict(out=rs, in0=ss, scalar1=1.0 / D, scalar2=1e-6, op0=ALU.mult, op1=ALU.add)),
                  reads=[("st", s)], writes=[("st", s)])
            sc.op("act", ("activation", dict(out=rs, in_=rs, func=AF.Sqrt)), reads=[("st", s)], writes=[("st", s)])
            sc.op("dve", ("reciprocal", dict(out=rs, in_=rs)), reads=[("st", s)], writes=[("st", s)])
            sc.op("dve", ("scalar_tensor_tensor", dict(out=hn[:, s, :], in0=xt[:, s, :], scalar=rs, in1=gbc[:, layer % 2, :],
                                                                     op0=ALU.mult, op1=ALU.mult)),
                  reads=[("xt", s), ("st", s), ("gbc", layer % 2)], writes=[("hn", s)])

        def trans(t):
            s = t % 2
            for c in range(8):
                tr(pst[:, c * 128:(c + 1) * 128], hn[:, s, c * 128:(c + 1) * 128], reads=[("hn", s), "cb"], writes=[("ps", 7)], inc=(c == 7))
            sc.op("act", ("copy", dict(out=hT[:, :, t * 128:(t + 1) * 128], in_=pst.rearrange("p (c n) -> p c n", n=128))),
                  reads=[("ps", 7)], writes=[("hT", t // 4)])
        stats(0)
        for t in range(NT):
            if t + 1 < NT:
                stats(t + 1)
            trans(t)

    def run(units):
        for u in units:
            u()

    def run_merged(*lists):
        lists = [l for l in lists if l]
        idx = [0] * len(lists)
        while True:
            cand = [(idx[i] / len(l), i) for i, l in enumerate(lists) if idx[i] < len(l)]
            if not cand:
                break
            i = min(cand)[1]
            lists[i][idx[i]]()
            idx[i] += 1

    def skew(units, items, fa, fb, others):
        pend = None
        for it in items:
            if it[0] == "t":
                st_ = {}
                a_ = (lambda it=it, st_=st_: fa(st_, *it[1:]))
                if pend is None:
                    units.append(a_)
                else:
                    units.append(lambda a_=a_, b_=pend: (a_(), b_()))
                pend = (lambda it=it, st_=st_: fb(st_, *it[1:]))
            else:
                if pend is not None:
                    units.append(pend)
                    pend = None
                units.append(lambda it=it: others[it[0]](*it[1:]))
        if pend is not None:
            units.append(pend)

    ppp = {"i": 0}

    def proj_fm(layer, name, rope, evac, banks=((0, 1), (2, 3))):
        stt = {}

        def u(c4):
            if c4 == 0:
                w1, k1 = load_w(layer, _TIDX[name])
                stt["w1"] = (w1.rearrange("p (c n) -> p c n", n=128), k1)
                if rope:
                    w2, k2 = load_w(layer, _TIDX[name[:2] + "R" + name[2:]])
                    stt["w2"] = (w2.rearrange("p (c n) -> p c n", n=128), k2)
            w1, k1 = stt["w1"]
            i = ppp["i"]; ppp["i"] += 1
            b1 = banks[0][i % len(banks[0])]
            b2 = banks[1][i % len(banks[1])]
            for c in range(8):
                mm(ps[b1][:, :], w1[:, c, :], hT[:, c, c4 * 512:(c4 + 1) * 512], c == 0, c == 7,
                   reads=[k1, ("hT", c4)], writes=[("ps", b1)])
            if rope:
                w2, k2 = stt["w2"]
                for c in range(8):
                    mm(ps[b2][:, :], w2[:, c, :], hT[:, c, c4 * 512:(c4 + 1) * 512], c == 0, c == 7,
                       reads=[k2, ("hT", c4)], writes=[("ps", b2)])
                evac(c4, b1, b2)
            else:
                evac(c4, b1)
        return [(lambda c4=c4: u(c4)) for c4 in range(4)]

    rt = SB("rt", [128, 2, 2, 512], F32)
    rtc = {"i": 0}

    def rope_evac(dst_fn, dst_keys_fn, split_heads):
        def ev(c4, b1, b2):
            j = rtc["i"] % 2; rtc["i"] += 1
            t1 = rt[:, j, 0, :]
            t2 = rt[:, j, 1, :]
            cs = cf[:, CF_COS + c4 * 512:CF_COS + (c4 + 1) * 512]
            sn = cf[:, CF_SIN + c4 * 512:CF_SIN + (c4 + 1) * 512]
            sc.op("dve", ("tensor_tensor", dict(out=t1, in0=ps[b1][:, :], in1=cs, op=ALU.mult)),
                  reads=[("ps", b1), "cf"], writes=[("rt", j, 0)])
            sc.op("dve", ("tensor_tensor", dict(out=t2, in0=ps[b2][:, :], in1=sn, op=ALU.mult)),
                  reads=[("ps", b2), "cf"], writes=[("rt", j, 1)])
            if split_heads:
                for h in range(2):
                    d = dst_fn(c4, h)
                    sc.op("pool", ("tensor_tensor", dict(out=d, in0=t1[64 * h:64 * h + 64, :], in1=t2[64 * h:64 * h + 64, :], op=ALU.add)),
                          reads=[("rt", j, 0), ("rt", j, 1)], writes=dst_keys_fn(c4, h))
            else:
                d = dst_fn(c4, None)
                sc.op("pool", ("tensor_tensor", dict(out=d, in0=t1, in1=t2, op=ALU.add)),
                      reads=[("rt", j, 0), ("rt", j, 1)], writes=dst_keys_fn(c4, None))
        return ev

    def silu_evac(ychunk):
        def ev(c4, b1):
            sc.op("act", ("activation", dict(out=yT[:, ychunk, c4 * 512:(c4 + 1) * 512], in_=ps[b1][:, :], func=AF.Silu)),
                  reads=[("ps", b1)], writes=[("yT", ychunk, c4)])
        return ev

    def proj_tm(layer, name, nblk, tok_fn, evac, banks=(0, 1)):
        stt = {}

        def u(g4):
            if g4 == 0:
                w1, k1 = load_w(layer, _TIDX[name])
                stt["w1"] = (w1.rearrange("p (c n) -> p c n", n=128), k1)
            w1, k1 = stt["w1"]
            i = ppp["i"]; ppp["i"] += 1
            b1 = banks[i % len(banks)]
            for j in range(4):
                blk = g4 + j
                for c in range(8):
                    mm(ps[b1][:, j * 128:(j + 1) * 128], tok_fn(c, blk), w1[:, c, :], (c == 0 and j == 0), c == 7,
                       reads=[k1] + [("hT", q) for q in range(4)], writes=[("ps", b1)], inc=(c == 7 and j == 3))
            evac(g4, b1)
        return [(lambda g4=g4: u(g4)) for g4 in range(0, nblk, 4)]

    def mixer_A(layer):
        QA = [scr_view(0, [2, S], BF16), scr_view(8192, [2, S], BF16)]
        KA = [scr_view(16384, [2, S], BF16), scr_view(24576, [2, S], BF16)]
        VA = [scr_view(32768, [16, 256], BF16), scr_view(40960, [16, 256], BF16)]
        o0 = 49152
        km = scr_view(o0, [2, 8], BF16)
        kmf = scr_view(o0 + 64, [2, 8], F32)
        gt = scr_view(o0 + 256, [128], F32)
        cmp_ = scr_view(o0 + 1024, [128, 8], F32)
        rk = scr_view(o0 + 1024 + 4096, [128], F32)
        nb = scr_view(o0 + 1024 + 4096 + 512, [128], BF16)
        et = scr_view(57344, [2, 512], BF16)
        rd = scr_view(57344 + 2048, [512], F32)
        rn = scr_view(57344 + 4096, [512], F32)
        est = {"i": 0}

        def proj_units(p):
            bs_ = p % 2
            qa, ka, va = QA[bs_], KA[bs_], VA[bs_]
            units = []
            if p < 2:
                def init():
                    for e_ in range(2):
                        sc.op("pool", ("tensor_copy", dict(out=ka[64:72, e_, :], in_=cb[0:8, CB_ONEHOT:CB_ONEHOT + S])),
                              reads=["cb"], writes=[("ka", bs_, e_, c4) for c4 in range(4)])
                    for h in range(2):
                        sc.op("pool", ("tensor_copy", dict(out=va[:, :, 128 * h + 64:128 * h + 128],
                                                           in_=cb[:, CB_ONES:CB_ONES + 64].unsqueeze(1).to_broadcast([128, 16, 64]))),
                              reads=["cb"], writes=[("va1", bs_, h)])
                units.append(init)
            units += proj_fm(layer, f"AQ{p}", True, rope_evac(lambda c4, h: qa[0:64, h, c4 * 512:(c4 + 1) * 512],
                                                              lambda c4, h: [("qa", bs_, h, c4)], True))
            units += proj_fm(layer, f"AK{p}", True, rope_evac(lambda c4, h: ka[0:64, h, c4 * 512:(c4 + 1) * 512],
                                                              lambda c4, h: [("ka", bs_, h, c4)], True))
            units += proj_fm(layer, f"AG{p}", False, silu_evac(p))

            def vev(g4, b1):
                for h in range(2):
                    sc.op("act", ("copy", dict(out=va[:, g4:g4 + 4, 128 * h:128 * h + 64],
                                               in_=ps[b1][:, :].rearrange("p (j n) -> p j n", n=128)[:, :, 64 * h:64 * h + 64])),
                          reads=[("ps", b1)], writes=[("va", bs_, h, g4)])
            units += proj_tm(layer, f"AV{p}", 16, lambda c, blk: hT[:, c, blk * 128:(blk + 1) * 128], vev)
            return units

        def attn_units(p):
            bs_ = p % 2
            qa, ka, va = QA[bs_], KA[bs_], VA[bs_]
            units = []

            def gate(h):
                allk = [("ka", bs_, h, c4) for c4 in range(4)]
                allq = [("qa", bs_, h, c4) for c4 in range(4)]
                sc.op("dve", ("tensor_reduce", dict(out=kmf[0:64, h, :], in_=ka[0:64, h, :].rearrange("p (n b) -> p n b", b=256),
                                                    axis=AX.X, op=ALU.add)), reads=allk, writes=[("kmf", h)])
                sc.op("dve", ("tensor_copy", dict(out=km[0:64, h, :], in_=kmf[0:64, h, :])), reads=[("kmf", h)], writes=[("km", h)])
                gp = ps[4][:, 0:128]
                for qt in range(16):
                    mm(gp[:, qt * 8:(qt + 1) * 8], qa[0:64, h, qt * 128:(qt + 1) * 128], km[0:64, h, :], True, True,
                       reads=allq + [("km", h)], writes=[("ps", 4)], inc=(qt == 15))
                sc.op("dve", ("tensor_tensor", dict(out=gt, in0=gp, in1=cf[:, CF_AGM:CF_AGM + 128], op=ALU.add)),
                      reads=[("ps", 4), "cf"], writes=["gt"])
                g3 = gt.rearrange("p (q n) -> p q n", n=8)
                sc.op("dve", ("tensor_tensor", dict(out=cmp_.rearrange("p (q n) m -> p q n m", n=8),
                                                    in0=g3.unsqueeze(2).to_broadcast([128, 16, 8, 8]),
                                                    in1=g3.unsqueeze(3).to_broadcast([128, 16, 8, 8]), op=ALU.is_gt)),
                      reads=["gt"], writes=["cmp"])
                sc.op("dve", ("tensor_reduce", dict(out=rk, in_=cmp_, axis=AX.X, op=ALU.add)), reads=["cmp"], writes=["rk"])
                sc.op("dve", ("tensor_scalar", dict(out=rk, in0=rk, scalar1=2.5, scalar2=None, op0=ALU.is_lt)), reads=["rk"], writes=["rk"])
                sc.op("dve", ("tensor_tensor", dict(out=rk, in0=rk, in1=cf[:, CF_AVAL:CF_AVAL + 128], op=ALU.mult)), reads=["rk", "cf"], writes=["rk"])
                sc.op("dve", ("scalar_tensor_tensor", dict(out=rk, in0=rk, scalar=-NEG, in1=cf[:, CF_AOWN:CF_AOWN + 128],
                                                           op0=ALU.mult, op1=ALU.add)), reads=["rk", "cf"], writes=["rk"])
                sc.op("dve", ("tensor_copy", dict(out=nb, in_=rk)), reads=["rk"], writes=["nb"])
                tp = ps[5][:, 0:512].bitcast(BF16)
                for half in range(2):
                    for q8 in range(8):
                        qt = half * 8 + q8
                        tr(tp[0:8, q8 * 128:(q8 + 1) * 128], nb[:, qt * 8:(qt + 1) * 8], reads=["nb", "cb"], writes=[("ps", 5)], inc=(q8 == 7))
                    sc.op("act", ("copy", dict(out=qa[64:72, h, half * 1024:(half + 1) * 1024], in_=tp[0:8, :])),
                          reads=[("ps", 5)], writes=[("qa", bs_, h, 2 * half), ("qa", bs_, h, 2 * half + 1)])

            def tile_a(st_, h, c4, kt, nkt):
                q0 = max(kt * 128, c4 * 512)
                q1 = (c4 + 1) * 512
                n = q1 - q0
                ei = est["i"]; est["i"] += 1
                sb_ = 4 + (ei % 2)
                ej = ei % 2
                st_.update(q0=q0, n=n, ej=ej)
                mm(ps[sb_][:, 0:n], ka[0:72, h, kt * 128:(kt + 1) * 128], qa[0:72, h, q0:q1], True, True,
                   reads=[("ka", bs_, h, kt // 4), ("qa", bs_, h, c4)], writes=[("ps", sb_)])
                sc.op("act", ("activation", dict(out=et[:, ej, 0:n], in_=ps[sb_][:, 0:n], func=AF.Exp, scale=0.125)),
                      reads=[("ps", sb_)], writes=[("et", ej)])
                if q0 == kt * 128:
                    sc.op("dve", ("tensor_tensor", dict(out=et[:, ej, 0:128], in0=et[:, ej, 0:128],
                                                        in1=cb[:, CB_MOWN:CB_MOWN + 128], op=ALU.mult)),
                          reads=[("et", ej), "cb"], writes=[("et", ej)])

            def tile_b(st_, h, c4, kt, nkt):
                ob = 6 + (c4 % 2)
                q0, n, ej = st_["q0"], st_["n"], st_["ej"]
                mm(ps[ob][:, q0 - c4 * 512:512], va[:, kt, 128 * h:128 * h + 128], et[:, ej, 0:n], kt == 0, kt == nkt - 1,
                   reads=[("va", bs_, h, (kt // 4) * 4), ("va1", bs_, h), ("et", ej)], writes=[("ps", ob)], inc=True)

            def norm(h, c4):
                ob = 6 + (c4 % 2)
                sc.op("act", ("activation", dict(out=rd[64:128, :], in_=ps[ob][64:128, :], func=AF.Ln)), reads=[("ps", ob)], writes=["rd"])
                sc.op("act", ("activation", dict(out=rd[64:128, :], in_=rd[64:128, :], func=AF.Exp, scale=-1.0)), reads=["rd"], writes=["rd"])
                sc.op("dve", ("tensor_tensor", dict(out=rn[64 * h:64 * h + 64, :], in0=ps[ob][0:64, :], in1=rd[64:128, :], op=ALU.mult)),
                      reads=[("ps", ob), "rd"], writes=["rd0"])
                ydst = yT[64 * h:64 * h + 64, p, c4 * 512:(c4 + 1) * 512]
                sc.op("pool", ("tensor_tensor", dict(out=ydst, in0=ydst, in1=rn[64 * h:64 * h + 64, :], op=ALU.mult)),
                      reads=["rd0", ("yT", p, c4)], writes=[("yT", p, c4)])

            items = []
            for h in range(2):
                items.append(("g", h))
                for c4 in range(4):
                    nkt = 4 * c4 + 4
                    for kt in range(nkt):
                        items.append(("t", h, c4, kt, nkt))
                    items.append(("n", h, c4))
            skew(units, items, tile_a, tile_b, {"g": gate, "n": norm})
            return units

        run(proj_units(0))
        for p in range(3):
            run_merged(attn_units(p), proj_units(p + 1) if p < 2 else [])

    def mixer_B(layer):
        qb = scr_view(0, [3, S], BF16)
        kb = scr_view(12288, [S], BF16)
        ikb = scr_view(16384, [S], BF16)
        iqb = scr_view(20480, [2, S], BF16)
        vb = scr_view(28672, [16, 128], BF16)
        iw = scr_view(32768, [16, 4], F32)
        acc = scr_view(33024, [2, S], F32)
        tmp = scr_view(49408, [2, 512], F32)
        mk = scr_view(53504, [S], BF16)
        mt = scr_view(57600, [2, 16, 128], BF16)
        et = scr_view(65792, [2, 384], BF16)
        pt = scr_view(67328, [2, 384], BF16)
        rd = scr_view(68864, [384], F32)
        bs = scr_view(70400, [64], F32)
        rn = scr_view(70656, [384], F32)
        sc.op("pool", ("tensor_copy", dict(out=vb[:, :, 64:128], in_=cb[:, CB_ONES:CB_ONES + 64].unsqueeze(1).to_broadcast([128, 16, 64]))),
              reads=["cb"], writes=["vb1"])
        pu = []
        for p in range(3):
            pu += proj_fm(layer, f"BQ{p}", True, rope_evac(lambda c4, h, p=p: qb[:, p, c4 * 512:(c4 + 1) * 512],
                                                           lambda c4, h, p=p: [("qb", c4)], False))
            pu += proj_fm(layer, f"BG{p}", False, silu_evac(3 + p))
        pu += proj_fm(layer, "BK", True, rope_evac(lambda c4, h: kb[:, c4 * 512:(c4 + 1) * 512], lambda c4, h: [("kb", c4)], False))
        pu += proj_fm(layer, "IK", True, rope_evac(lambda c4, h: ikb[:, c4 * 512:(c4 + 1) * 512], lambda c4, h: [("ikb", c4)], False))
        for p in range(2):
            pu += proj_fm(layer, f"IQ{p}", True, rope_evac(lambda c4, h, p=p: iqb[:, p, c4 * 512:(c4 + 1) * 512],
                                                           lambda c4, h, p=p: [("iqb", c4)], False))

        def vev(g4, b1):
            v3 = ps[b1][:, :].rearrange("p (j n) -> p j n", n=128)
            sc.op("act", ("copy", dict(out=vb[:, g4:g4 + 4, 0:64], in_=v3[:, :, 0:64])), reads=[("ps", b1)], writes=[("vb", g4)])
            sc.op("act", ("copy", dict(out=iw[:, g4:g4 + 4, :], in_=v3[:, :, 64:68])), reads=[("ps", b1)], writes=["iw"])
        pu += proj_tm(layer, "BV", 16, lambda c, blk: hT[:, c, blk * 128:(blk + 1) * 128], vev)
        run(pu)
        cst = {"li": 0, "ei": 0}
        thr = bs[:, 0:1]; hi = bs[:, 1:2]; lo = bs[:, 2:3]; cnt = bs[:, 3:4]; tt = bs[:, 4:5]; nthr = bs[:, 5:6]
        W = bs[:, 8:8 + NBIS + 2]
        junk = tmp.rearrange("p a b -> p (a b)")
        tpb = [ps[2][:, 0:512].bitcast(BF16), ps[3][:, 0:512].bitcast(BF16)]

        def stage1(qt):
            N = 128 * (qt + 1)
            a = qt % 2
            m = qt % 2
            qs = slice(qt * 128, (qt + 1) * 128)
            units = []

            def idx(j, h):
                k0 = j * 512
                n = min(512, N - k0)
                e_ = h % 2
                lb = cst["li"] % 2
                cst["li"] += 1
                mm(ps[lb][:, 0:n], iqb[64 * e_:64 * e_ + 64, h // 2, qs], ikb[64 * e_:64 * e_ + 64, k0:k0 + n], True, True,
                   reads=[("iqb", qt // 4), ("ikb", j)], writes=[("ps", lb)])
                if h == 0:
                    sc.op("dve", ("tensor_scalar", dict(out=acc[:, a, k0:k0 + n], in0=ps[lb][:, 0:n], scalar1=0.0,
                                                        scalar2=iw[:, qt, 0:1], op0=ALU.max, op1=ALU.mult)),
                          reads=[("ps", lb), "iw"], writes=[("acc", a)])
                else:
                    tj = cst["li"] % 2
                    sc.op("dve", ("tensor_scalar", dict(out=tmp[:, tj, 0:n], in0=ps[lb][:, 0:n], scalar1=0.0,
                                                        scalar2=iw[:, qt, h:h + 1], op0=ALU.max, op1=ALU.mult)),
                          reads=[("ps", lb), "iw"], writes=[("tmp", tj)])
                    sc.op("pool", ("tensor_tensor", dict(out=acc[:, a, k0:k0 + n], in0=acc[:, a, k0:k0 + n],
                                                         in1=tmp[:, tj, 0:n], op=ALU.add)),
                          reads=[("acc", a), ("tmp", tj)], writes=[("acc", a)])
            for j in range((N + 511) // 512):
                for h in range(4):
                    units.append(lambda j=j, h=h: idx(j, h))

            def bis_init():
                sc.op("pool", ("tensor_tensor", dict(out=acc[:, a, qs], in0=acc[:, a, qs], in1=cf[:, CF_NEGTRI:CF_NEGTRI + 128], op=ALU.add)),
                      reads=[("acc", a), "cf"], writes=[("acc", a)])
                if qt < 2:
                    sc.op("dve", ("memset", dict(ap=thr, constant=-1e29)), writes=["thr"])
                    return
                sc.op("dve", ("tensor_reduce", dict(out=hi, in_=acc[:, a, 0:N], axis=AX.X, op=ALU.max)), reads=[("acc", a)], writes=["bs_hi"])
                sc.op("dve", ("tensor_reduce", dict(out=lo, in_=acc[:, a, 0:N - 128], axis=AX.X, op=ALU.min)), reads=[("acc", a)], writes=["bs_lo"])
                sc.op("dve", ("tensor_tensor", dict(out=tt, in0=hi, in1=lo, op=ALU.subtract)), reads=["bs_hi", "bs_lo"], writes=["bs_tt"])
                sc.op("dve", ("tensor_scalar", dict(out=W, in0=cf[:, CF_POW2:CF_POW2 + NBIS + 2], scalar1=tt, scalar2=None, op0=ALU.mult)),
                      reads=["bs_tt", "cf"], writes=["bs_W"])
                sc.op("dve", ("scalar_tensor_tensor", dict(out=nthr, in0=lo, scalar=-1.0, in1=W[:, 1:2], op0=ALU.mult, op1=ALU.subtract)),
                      reads=["bs_lo", "bs_W"], writes=["nthr"])
            units.append(bis_init)

            def bis(k):
                sc.op("act", ("activation", dict(out=mk[:, 0:N], in_=acc[:, a, 0:N], func=AF.Sign, bias=nthr, scale=1.0, accum_out=cnt)),
                      reads=[("acc", a), "nthr"], writes=["bs_cnt", "mk"])
                sc.op("dve", ("tensor_scalar", dict(out=tt, in0=cnt, scalar1=511.0 - N, scalar2=0.5, op0=ALU.is_lt, op1=ALU.subtract)),
                      reads=["bs_cnt"], writes=["bs_tt"])
                sc.op("dve", ("scalar_tensor_tensor", dict(out=nthr, in0=tt, scalar=W[:, k + 1:k + 2], in1=nthr, op0=ALU.mult, op1=ALU.add)),
                      reads=["bs_tt", "bs_W", "nthr"], writes=["nthr"])
            if qt >= 2:
                for k in range(NBIS):
                    units.append(lambda k=k: bis(k))

            def fin():
                if qt >= 2:
                    sc.op("dve", ("scalar_tensor_tensor", dict(out=thr, in0=nthr, scalar=-1.0, in1=W[:, NBIS + 1:NBIS + 2], op0=ALU.mult, op1=ALU.subtract)),
                          reads=["nthr", "bs_W"], writes=["thr"])
                sc.op("dve", ("tensor_scalar", dict(out=mk[:, 0:N], in0=acc[:, a, 0:N], scalar1=thr, scalar2=None, op0=ALU.is_ge)),
                      reads=[("acc", a), "thr"], writes=["mk"])
                for kt in range(qt + 1):
                    tr(tpb[kt // 8][:, (kt % 8) * 128:(kt % 8 + 1) * 128], mk[:, kt * 128:(kt + 1) * 128], reads=["mk", "cb"], writes=[("ps", 2 + kt // 8)],
                       inc=(kt == qt or kt == 7))
                for half in range((qt // 8) + 1):
                    nk = min(8, qt + 1 - 8 * half)
                    sc.op("act", ("copy", dict(out=mt[:, m, 8 * half:8 * half + nk, :],
                                               in_=tpb[half][:, 0:nk * 128].rearrange("p (k n) -> p k n", n=128))),
                          reads=[("ps", 2 + half)], writes=[("mt", m)])
            units.append(fin)
            return units

        def stage2(qt):
            m = qt % 2
            qs = slice(qt * 128, (qt + 1) * 128)
            units = []

            def tile_a(st_, kt, e_):
                ei = cst["ei"]; cst["ei"] += 1
                sb_ = 4 + (ei % 2)
                ej = ei % 2
                st_.update(ej=ej)
                mm(ps[sb_][:, 0:384], kb[64 * e_:64 * e_ + 64, kt * 128:(kt + 1) * 128], qb[64 * e_:64 * e_ + 64, :, qs], True, True,
                   reads=[("kb", kt // 4), ("qb", qt // 4)], writes=[("ps", sb_)])
                sc.op("act", ("activation", dict(out=et[:, ej, :], in_=ps[sb_][:, 0:384], func=AF.Exp, scale=0.125)),
                      reads=[("ps", sb_)], writes=[("et", ej)])
                sc.op("dve", ("tensor_tensor", dict(out=pt[:, ej, :].rearrange("p (h n) -> p h n", n=128),
                                                    in0=et[:, ej, :].rearrange("p (h n) -> p h n", n=128),
                                                    in1=mt[:, m, kt, :].unsqueeze(1).to_broadcast([128, 3, 128]), op=ALU.mult)),
                      reads=[("et", ej), ("mt", m)], writes=[("pt", ej)])

            def tile_b(st_, kt, e_):
                ob = 6 + e_
                ej = st_["ej"]
                mm(ps[ob][:, 0:384], vb[:, kt, :], pt[:, ej, :], kt == 0, kt == qt,
                   reads=[("vb", (kt // 4) * 4), "vb1", ("pt", ej)], writes=[("ps", ob)], inc=True)
            items = [("t", kt, e_) for kt in range(qt + 1) for e_ in range(2)]

            def norm(e_):
                ob = 6 + e_
                sc.op("act", ("activation", dict(out=rd[64:128, :], in_=ps[ob][64:128, 0:384], func=AF.Ln)), reads=[("ps", ob)], writes=["rd"])
                sc.op("act", ("activation", dict(out=rd[64:128, :], in_=rd[64:128, :], func=AF.Exp, scale=-1.0)), reads=["rd"], writes=["rd"])
                sc.op("dve", ("tensor_tensor", dict(out=rn[64 * e_:64 * e_ + 64, :], in0=ps[ob][0:64, 0:384], in1=rd[64:128, :], op=ALU.mult)),
                      reads=[("ps", ob), "rd"], writes=["rd0"])
                ydst = yT[64 * e_:64 * e_ + 64, 3:6, qs]
                sc.op("pool", ("tensor_tensor", dict(out=ydst, in0=ydst, in1=rn[64 * e_:64 * e_ + 64, :].rearrange("p (h n) -> p h n", n=128), op=ALU.mult)),
                      reads=["rd0"] + [("yT", 3 + p, qt // 4) for p in range(3)], writes=[("yT", 3 + p, qt // 4) for p in range(3)])
            items += [("n", 0), ("n", 1)]
            skew(units, items, tile_a, tile_b, {"n": norm})
            return units

        run(stage1(0))
        for qt in range(16):
            run_merged(stage2(qt), stage1(qt + 1) if qt < 15 else [])

    def mixer_C(layer):
        qc = scr_view(0, [2, S], BF16)
        kc = scr_view(8192, [2, S], BF16)
        vc = scr_view(16384, [16, 512], BF16)
        ac = scr_view(32768, [4, S], F32)
        et = scr_view(65536, [2, 256], BF16)
        rd = scr_view(65536 + 1024, [512], F32)
        rn = scr_view(65536 + 3072, [512], F32)
        sc.op("pool", ("tensor_copy", dict(out=vc.rearrange("p b (h c) -> p b h c", c=128)[:, :, :, 64:128],
                                           in_=cb[:, CB_ONES:CB_ONES + 64].unsqueeze(1).unsqueeze(1).to_broadcast([128, 16, 4, 64]))),
              reads=["cb"], writes=["vc1"])
        gu = []
        for p in range(2):
            gu += proj_fm(layer, f"CG{p}", False, silu_evac(6 + p))
        run(gu)
        groups = [g for g in range(3) if g in getattr(_build, 'cgroups', (0, 1, 2))]
        dils = (1, 4, 16)
        cst = {"ei": 0}

        def toks_of(dil):
            def toks(r, m, cnt=128):
                st0 = r + dil * 128 * m
                return slice(st0, st0 + dil * (cnt - 1) + 1, dil)
            return toks

        def proj_units(g, p, pbanks):
            dil = dils[g]
            nblk = S // dil // 128
            toks = toks_of(dil)
            units = []
            units += proj_fm(layer, f"CQ{g}{p}", True, rope_evac(lambda c4, h: qc[:, p, c4 * 512:(c4 + 1) * 512], lambda c4, h: [("qc", p, c4)], False),
                             banks=pbanks)
            units += proj_fm(layer, f"CK{g}{p}", True, rope_evac(lambda c4, h: kc[:, p, c4 * 512:(c4 + 1) * 512], lambda c4, h: [("kc", p, c4)], False),
                             banks=pbanks)

            def vev(g4, b1):
                v4 = ps[b1][:, :].rearrange("p (j h c) -> p j h c", h=2, c=64)
                sc.op("act", ("copy", dict(out=vc.rearrange("p b (h c) -> p b h c", c=128)[:, g4:g4 + 4, 2 * p:2 * p + 2, 0:64], in_=v4)),
                      reads=[("ps", b1)], writes=[("vc", p, g4)])
            units += proj_tm(layer, f"CV{g}{p}", 16, lambda c, blk: hT[:, c, toks(blk // nblk, blk % nblk)], vev,
                             banks=(pbanks[0][0], pbanks[1][0]) if len(pbanks[0]) == 1 else (0, 1))
            return units

        def attn_units(g, j, first):
            dil = dils[g]
            nblk = S // dil // 128
            toks = toks_of(dil)
            p, e_ = j // 2, j % 2
            pr = slice(64 * e_, 64 * e_ + 64)
            allq = [("qc", p, c4) for c4 in range(4)]
            allk = [("kc", p, c4) for c4 in range(4)]
            started = [False] * 4
            units = []

            def blk_a(st_, r, m):
                nq = 256 if m + 1 < nblk else 128
                ei = cst["ei"]; cst["ei"] += 1
                sb_ = 4 + (ei % 2)
                ej = ei % 2
                st_.update(nq=nq, ej=ej)
                mm(ps[sb_][:, 0:nq], kc[pr, p, toks(r, m)], qc[pr, p, toks(r, m, nq)], True, True,
                   reads=allk + allq, writes=[("ps", sb_)])
                sc.op("act", ("activation", dict(out=et[:, ej, 0:nq], in_=ps[sb_][:, 0:nq], func=AF.Exp, scale=0.125)),
                      reads=[("ps", sb_)], writes=[("et", ej)])
                sc.op("dve", ("tensor_tensor", dict(out=et[:, ej, 0:nq], in0=et[:, ej, 0:nq], in1=cb[:, CB_MOWN:CB_MOWN + nq], op=ALU.mult)),
                      reads=[("et", ej), "cb"], writes=[("et", ej)])

            def blk_b(st_, r, m):
                blk = r * nblk + m
                nq, ej = st_["nq"], st_["ej"]
                pieces = []
                for part in range(nq // 128):
                    t0 = r + dil * 128 * (m + part)
                    if dil <= 4:
                        pieces.append((part * 128, 128, t0))
                    else:
                        for q4 in range(4):
                            pieces.append((part * 128 + 32 * q4, 32, t0 + dil * 32 * q4))
                for pi, (c0, cn, t0) in enumerate(pieces):
                    bnk = t0 // 512
                    o0 = t0 % 512
                    mm(ps[bnk][:, o0:o0 + dil * (cn - 1) + 1:dil], vc[:, blk, 128 * j:128 * j + 128], et[:, ej, c0:c0 + cn],
                       not started[bnk], False, reads=[("vc", p, (blk // 4) * 4), "vc1", ("et", ej)],
                       writes=[("ps", bnk)], inc=(pi == len(pieces) - 1))
                    started[bnk] = True
            items = [("t", r, m) for r in range(dil) for m in range(nblk)] + [("e",)]

            def evac():
                for c4 in range(4):
                    dst = ac[:, j, c4 * 512:(c4 + 1) * 512]
                    if first:
                        sc.op("act", ("copy", dict(out=dst, in_=ps[c4][:, :])), reads=[("ps", c4)], writes=[("ac", j, c4)])
                    else:
                        sc.op("dve", ("tensor_tensor", dict(out=dst, in0=ps[c4][:, :], in1=dst, op=ALU.add)),
                              reads=[("ps", c4), ("ac", j, c4)], writes=[("ac", j, c4)])
            skew(units, items, blk_a, blk_b, {"e": evac})
            return units

        PB = ((6,), (7,))
        seq = []
        for gi, g in enumerate(groups):
            seq.append((g, 0)); seq.append((g, 1))
        run(proj_units(seq[0][0], seq[0][1], ((0, 1), (2, 3))))
        for i, (g, p) in enumerate(seq):
            nxt = proj_units(seq[i + 1][0], seq[i + 1][1], PB) if i + 1 < len(seq) else []
            first = (g == groups[0])
            run_merged(attn_units(g, 2 * p, first) + attn_units(g, 2 * p + 1, first), nxt)
        for j in range(4):
            p, e_ = j // 2, j % 2
            for c4 in range(4):
                cs = slice(c4 * 512, (c4 + 1) * 512)
                sc.op("act", ("activation", dict(out=rd[64:128, :], in_=ac[64:128, j, cs], func=AF.Ln)), reads=[("ac", j, c4)], writes=["rd"])
                sc.op("act", ("activation", dict(out=rd[64:128, :], in_=rd[64:128, :], func=AF.Exp, scale=-1.0)), reads=["rd"], writes=["rd"])
                sc.op("act", ("copy", dict(out=rn[64:128, :], in_=ac[0:64, j, cs])), reads=[("ac", j, c4)], writes=[("rn", 1)])
                sc.op("dve", ("tensor_tensor", dict(out=rn[64 * e_:64 * e_ + 64, :], in0=rn[64:128, :], in1=rd[64:128, :], op=ALU.mult)),
                      reads=[("rn", 1), "rd"], writes=[("rn", e_)])
                ydst = yT[64 * e_:64 * e_ + 64, 6 + p, cs]
                sc.op("pool", ("tensor_tensor", dict(out=ydst, in0=ydst, in1=rn[64 * e_:64 * e_ + 64, :], op=ALU.mult)),
                      reads=[("rn", e_), ("yT", 6 + p, c4)], writes=[("yT", 6 + p, c4)])

    def phase_final(si, layer, src_d, dst_d, last):
        mg = scr_view(0, [8, S], BF16)
        wbr = scr_view(32768, [8, D], BF16)
        wo = scr_view(49152, [8, D], BF16)
        gs = scr_view(65536, [3, 512], BF16)
        tm = scr_view(65536 + 3072, [512], F32)
        for c in range(8):
            nm = (f"WA{c}" if c < 3 else f"WB{c - 3}" if c < 6 else f"WC{c - 6}")
            load_w(layer, _XIDX[nm], dest=wbr[:, c, :], dest_key=("wbr", c))
        for c in range(8):
            load_w(layer, _XIDX[f"WO{c}"], dest=wo[:, c, :], dest_key=("wo", c))
        if last:
            load_g(0, 2)
        kch = ((0, 3), (3, 6), (6, 8))
        for dt_ in range(8):
            gw = []
            for br in range(3):
                w_, k_ = load_w(layer, _TIDX[f"MG{8 * br + dt_}"])
                gw.append((w_.rearrange("p (c n) -> p c n", n=128), k_))
            for c4 in range(4):
                cs = slice(c4 * 512, (c4 + 1) * 512)
                for br in range(3):
                    for c in range(8):
                        mm(ps[br][:, :], gw[br][0][:, c, :], hT[:, c, cs], c == 0, c == 7, reads=[gw[br][1], ("hT", c4)], writes=[("ps", br)])
                    sc.op("act", ("activation", dict(out=gs[:, br, :], in_=ps[br][:, :], func=AF.Sigmoid)), reads=[("ps", br)], writes=[("gs", br)])
                for br in range(3):
                    a0, a1 = kch[br]
                    for c in range(a0, a1):
                        mm(ps[3 + br][:, :], wbr[:, c, dt_ * 128:(dt_ + 1) * 128], yT[:, c, cs], c == a0, c == a1 - 1,
                           reads=[("wbr", c), ("yT", c, c4)], writes=[("ps", 3 + br)])
                sc.op("dve", ("tensor_tensor", dict(out=tm, in0=ps[3][:, :], in1=gs[:, 0, :], op=ALU.mult)), reads=[("ps", 3), ("gs", 0)], writes=["tm"])
                sc.op("dve", ("tensor_tensor", dict(out=gs[:, 1, :], in0=ps[4][:, :], in1=gs[:, 1, :], op=ALU.mult)), reads=[("ps", 4), ("gs", 1)], writes=[("gs", 1)])
                sc.op("dve", ("tensor_tensor", dict(out=gs[:, 2, :], in0=ps[5][:, :], in1=gs[:, 2, :], op=ALU.mult)), reads=[("ps", 5), ("gs", 2)], writes=[("gs", 2)])
                sc.op("pool", ("tensor_tensor", dict(out=tm, in0=tm, in1=gs[:, 1, :], op=ALU.add)), reads=["tm", ("gs", 1)], writes=["tm"])
                sc.op("pool", ("tensor_tensor", dict(out=mg[:, dt_, cs], in0=tm, in1=gs[:, 2, :], op=ALU.add)),
                      reads=["tm", ("gs", 2)], writes=[("mg", c4)])
        for t in range(NT):
            s = t % 2
            ts = slice(t * 128, (t + 1) * 128)
            sc.dma(("x", s), ("dma_start", dict(out=xt[:, s, :], in_=src_d[si, ts, :])), writes=[("xt", s)])
            for hf in range(2):
                b = 6 + hf
                for c in range(8):
                    mm(ps[b][:, :], mg[:, c, ts], wo[:, c, hf * 512:(hf + 1) * 512], c == 0, c == 7,
                       reads=[("mg", t // 4), ("wo", c)], writes=[("ps", b)])
                sc.op("dve", ("tensor_tensor", dict(out=xt[:, s, hf * 512:(hf + 1) * 512], in0=ps[b][:, :],
                                                                        in1=xt[:, s, hf * 512:(hf + 1) * 512], op=ALU.add)),
                      reads=[("ps", b), ("xt", s)], writes=[("xt", s)])
            if last:
                ss = st[:, 8 + 2 * s:8 + 2 * s + 1]
                rs = st[:, 8 + 2 * s + 1:8 + 2 * s + 2]
                sc.op("act", ("activation", dict(out=hn[:, s, :], in_=xt[:, s, :], func=AF.Square, accum_out=ss)),
                      reads=[("xt", s)], writes=[("hn", s), ("st", s)])
                sc.op("dve", ("tensor_scalar", dict(out=rs, in0=ss, scalar1=1.0 / D, scalar2=1e-6, op0=ALU.mult, op1=ALU.add)),
                      reads=[("st", s)], writes=[("st", s)])
                sc.op("act", ("activation", dict(out=rs, in_=rs, func=AF.Sqrt)), reads=[("st", s)], writes=[("st", s)])
                sc.op("dve", ("reciprocal", dict(out=rs, in_=rs)), reads=[("st", s)], writes=[("st", s)])
                sc.op("dve", ("scalar_tensor_tensor", dict(out=xt[:, s, :], in0=xt[:, s, :], scalar=rs, in1=gbc[:, 0, :],
                                                                         op0=ALU.mult, op1=ALU.mult)),
                      reads=[("xt", s), ("st", s), ("gbc", 0)], writes=[("xt", s)])
            sc.dma(("o", s), ("dma_start", dict(out=dst_d[si, ts, :], in_=xt[:, s, :])), reads=[("xt", s)], writes=[("dram", si, t)])

    enabled = set(getattr(_build, "enabled", ("A", "B", "C")))

    def program():
        sc.dma("c0", ("dma_start", dict(out=cf[:], in_=cf_d[:, :])), writes=["cf"])
        sc.dma("c1", ("dma_start", dict(out=cb[:], in_=cb_d[:, :])), writes=["cb"])
        for si in range(nseq):
            for li_, layer in enumerate(layers_all):
                first = (li_ == 0)
                last = (li_ == len(layers_all) - 1)
                src = x_d if first else xs_d
                dst = out_d if last else xs_d
                sc.phase = "norm"
                phase_norm(si, layer, src)
                if "A" in enabled:
                    sc.barrier()
                    sc.phase = "A"
                    mixer_A(layer)
                if "B" in enabled:
                    sc.barrier()
                    sc.phase = "B"
                    mixer_B(layer)
                if "C" in enabled:
                    sc.barrier()
                    sc.phase = "C"
                    mixer_C(layer)
                sc.barrier()
                sc.phase = "final"
                for name, shape, dt in taps:
                    if name == f"yT{layer}" and si == 0:
                        sc.dma(("tap", name), ("dma_start", dict(out=tap_d[name][:, :, :], in_=yT[:])),
                               reads=[("yT", c, c4) for c in range(8) for c4 in range(4)])
                    if name == f"hT{layer}" and si == 0:
                        sc.dma(("tap", name), ("dma_start", dict(out=tap_d[name][:, :, :], in_=hT[:])),
                               reads=[("hT", c4) for c4 in range(4)])
                phase_final(si, layer, src, dst, last and final_norm)
                sc.barrier()
        sc.final_wait("sp", [("o", 0), ("o", 1)] + [("tap", n) for n, _, _ in taps])


    program()
    sc = Sched()
    wstate.update(n=0, rec=False, issued=0)
    ppp["i"] = 0
    rtc["i"] = 0
    program()

    waited = {e: set() for e in ("pe", "act", "dve", "pool")}
    for e in Sched.ENG:
        for waits, fn, inc, ph in sc.q[e]:
            for de, dc in waits:
                if de in waited:
                    waited[de].add(dc)
    remap = {e: {c: i + 1 for i, c in enumerate(sorted(waited[e]))} for e in waited}
    for e in waited:
        c = 0
        newq = []
        for waits, fn, inc, ph in sc.q[e]:
            if inc is not None and inc[0] == "E":
                c += 1
                if c not in remap[e]:
                    inc = None
            newq.append((waits, fn, inc, ph))
        sc.q[e] = newq
    for e in Sched.ENG:
        sc.q[e] = [([(de, remap[de][dc]) if de in remap else (de, dc) for de, dc in waits], fn, inc, ph)
                   for waits, fn, inc, ph in sc.q[e]]

    sems = {}
    for e in Sched.ENG:
        sems[e] = es.enter_context(nc.semaphore("s_" + e))
    for de in sc.dcnt:
        sems[de] = es.enter_context(nc.semaphore("d%d" % len(sems)))
    block = es.enter_context(nc.Block())

    def replay(engname):
        def run(eng):
            for waits, fn, inc, ph in sc.q[engname]:
                for de, dc in waits:
                    eng.wait_ge(sems[de], dc)
                if fn is None:
                    continue
                ins = getattr(eng, fn[0])(**fn[1])
                if ANNOTATE:
                    ins.annotate(ph)
                if inc is not None:
                    if inc[0] == "E":
                        ins.then_inc(sems[inc[1]], 1)
                    else:
                        ins.then_inc(sems[inc[1]], 16)
        return run
    block.tensor(replay("pe"))
    block.scalar(replay("act"))
    block.vector(replay("dve"))
    block.gpsimd(replay("pool"))
    block.sync(replay("sp"))
    es.close()
    return nc


_CACHE = {}


def kernel(x, norm_g, w_in, w_br_a, w_br_b, w_br_c, w_out, final_norm_g):
    x = np.ascontiguousarray(np.asarray(x, np.float32))
    ncores = 8
    nseq = x.shape[0] // ncores
    wt = _pack_weights(np.asarray(w_in, np.float32), np.asarray(w_br_a, np.float32), np.asarray(w_br_b, np.float32),
                       np.asarray(w_br_c, np.float32), np.asarray(w_out, np.float32))
    gv = np.concatenate([np.asarray(norm_g, np.float32), np.asarray(final_norm_g, np.float32)[None, :]], axis=0)
    cf, cb = _consts()
    nc = _build(nseq)
    in_maps = [{"x": x[i * nseq:(i + 1) * nseq], "wt": wt, "gv": gv, "cf": cf, "cb": cb} for i in range(ncores)]
    res = run_bass_kernel_spmd(nc, in_maps, core_ids=list(range(ncores)))
    return np.concatenate([r["out"] for r in res.results], axis=0)
```

```python
import numpy as np
import ml_dtypes
import concourse.bass as bass
import concourse.mybir as mybir
from concourse.bass_utils import run_bass_kernel_spmd

F32 = mybir.dt.float32
BF16 = mybir.dt.bfloat16
AF = mybir.ActivationFunctionType
ALU = mybir.AluOpType
AX = mybir.AxisListType

S = 2048
D = 1024
NT = 16
NEG = -30000.0
NBIS = 12
ANNOTATE = False

_splits = (384, 384, 384, 384, 384, 64, 64, 384, 256, 64, 4, 768, 768, 768, 256, 3072)
_names = ("a_q", "a_k", "a_v", "a_g", "b_q", "b_k", "b_v", "b_g", "i_q", "i_k", "i_w",
          "c_q", "c_k", "c_v", "c_g", "m_g")
_off = {}
_o = 0
for _n, _s in zip(_names, _splits):
    _off[_n] = _o
    _o += _s


def _rot(cols):
    cols = np.asarray(cols).reshape(-1, 2, 32)
    return cols[:, ::-1, :].reshape(-1)


def _tile_cols():
    t = []
    rng = lambda n, a, b: np.arange(_off[n] + a, _off[n] + b)
    for p in range(3):
        c = rng("a_q", 128 * p, 128 * p + 128); t.append((f"AQ{p}", c)); t.append((f"AQR{p}", _rot(c)))
        c = rng("a_k", 128 * p, 128 * p + 128); t.append((f"AK{p}", c)); t.append((f"AKR{p}", _rot(c)))
        t.append((f"AG{p}", rng("a_g", 128 * p, 128 * p + 128)))
        t.append((f"AV{p}", rng("a_v", 128 * p, 128 * p + 128)))
    for p in range(3):
        c = rng("b_q", 128 * p, 128 * p + 128); t.append((f"BQ{p}", c)); t.append((f"BQR{p}", _rot(c)))
        t.append((f"BG{p}", rng("b_g", 128 * p, 128 * p + 128)))
    c = np.concatenate([rng("b_k", 0, 64), rng("b_k", 0, 64)]); t.append(("BK", c)); t.append(("BKR", _rot(c)))
    c = np.concatenate([rng("i_k", 0, 64), rng("i_k", 0, 64)]); t.append(("IK", c)); t.append(("IKR", _rot(c)))
    for p in range(2):
        c = rng("i_q", 128 * p, 128 * p + 128); t.append((f"IQ{p}", c)); t.append((f"IQR{p}", _rot(c)))
    c = np.concatenate([rng("b_v", 0, 64), rng("i_w", 0, 4), rng("i_w", 0, 4).repeat(15)]); t.append(("BV", c))
    for g in range(3):
        for p in range(2):
            c = rng("c_q", 256 * g + 128 * p, 256 * g + 128 * p + 128); t.append((f"CQ{g}{p}", c)); t.append((f"CQR{g}{p}", _rot(c)))
            c = rng("c_k", 256 * g + 128 * p, 256 * g + 128 * p + 128); t.append((f"CK{g}{p}", c)); t.append((f"CKR{g}{p}", _rot(c)))
            t.append((f"CV{g}{p}", rng("c_v", 256 * g + 128 * p, 256 * g + 128 * p + 128)))
    for p in range(2):
        t.append((f"CG{p}", rng("c_g", 128 * p, 128 * p + 128)))
    for j in range(24):
        t.append((f"MG{j}", rng("m_g", 128 * j, 128 * j + 128)))
    return t


_TILES = _tile_cols()
_TIDX = {n: i for i, (n, _) in enumerate(_TILES)}
NWT = len(_TILES)
_XIDX = {}
for _i, _n in enumerate([f"WA{c}" for c in range(3)] + [f"WB{c}" for c in range(3)] +
                        [f"WC{c}" for c in range(2)] + [f"WO{c}" for c in range(8)]):
    _XIDX[_n] = NWT + _i
NWALL = NWT + 16


def _pack_weights(w_in, w_br_a, w_br_b, w_br_c, w_out):
    L = w_in.shape[0]
    out = np.empty((L, NWALL, 128, 1024), np.float32)
    allc = np.concatenate([c for _, c in _TILES])
    for l in range(L):
        g = w_in[l][:, allc]
        g = g.reshape(8, 128, NWT, 128).transpose(2, 1, 0, 3)
        out[l, :NWT] = g.reshape(NWT, 128, 1024)
        out[l, NWT:NWT + 3] = w_br_a[l].reshape(3, 128, 1024)
        out[l, NWT + 3:NWT + 6] = w_br_b[l].reshape(3, 128, 1024)
        out[l, NWT + 6:NWT + 8] = w_br_c[l].reshape(2, 128, 1024)
        out[l, NWT + 8:NWT + 16] = w_out[l].reshape(8, 128, 1024)
    return out


CF_COS = 0
CF_SIN = CF_COS + S
CF_NEGTRI = CF_SIN + S
CF_AGM = CF_NEGTRI + 128
CF_AVAL = CF_AGM + 128
CF_AOWN = CF_AVAL + 128
CF_POW2 = CF_AOWN + 128
CF_N = CF_POW2 + 32
CB_ID = 0
CB_MOWN = CB_ID + 128
CB_MPREV = CB_MOWN + 128
CB_ONEHOT = CB_MPREV + 128
CB_ONES = CB_ONEHOT + S
CB_PERM = CB_ONES + 128
CB_N = CB_PERM + 128


def _consts():
    cf = np.zeros((128, CF_N), np.float32)
    inv = 1.0 / (10000.0 ** (np.arange(0, 64, 2, dtype=np.float32) / 64.0))
    ang = np.arange(S, dtype=np.float32)[None, :] * inv[:, None].astype(np.float32)
    cos = np.cos(ang).astype(np.float32)
    sin = np.sin(ang).astype(np.float32)
    for p in range(128):
        j = p % 32
        cf[p, CF_COS:CF_COS + S] = cos[j]
        cf[p, CF_SIN:CF_SIN + S] = sin[j] * (-1.0 if (p % 64) < 32 else 1.0)
    t = np.arange(128)[:, None]
    s_ = np.arange(128)[None, :]
    cf[:, CF_NEGTRI:CF_NEGTRI + 128] = np.where(s_ <= t, 0.0, -1e30)
    for qt in range(16):
        own = qt // 2
        for n in range(8):
            cf[:, CF_AGM + qt * 8 + n] = 0.0 if n < own else -1e30
            cf[:, CF_AVAL + qt * 8 + n] = 1.0 if n < own else 0.0
            cf[:, CF_AOWN + qt * 8 + n] = 0.0 if n == own else NEG
    for k in range(32):
        cf[:, CF_POW2 + k] = 2.0 ** (-k)
    cb = np.zeros((128, CB_N), np.float32)
    cb[:, CB_ID:CB_ID + 128] = np.eye(128)
    cb[:, CB_MOWN:CB_MOWN + 128] = (t <= s_)
    cb[:, CB_MPREV:CB_MPREV + 128] = (t >= s_)
    for n in range(8):
        cb[n, CB_ONEHOT + 256 * n:CB_ONEHOT + 256 * n + 256] = 1.0
    cb[:, CB_ONES:CB_ONES + 128] = 1.0
    for po in range(128):
        cb[(po // 64) * 64 + ((po % 64) + 32) % 64, CB_PERM + po] = 1.0
    return cf, cb.astype(ml_dtypes.bfloat16)


class Sched:
    ENG = ("pe", "act", "dve", "pool", "sp")

    def __init__(self):
        self.q = {e: [] for e in self.ENG}
        self.cnt = {e: 0 for e in self.ENG}
        self.seen = {e: {} for e in self.ENG}
        self.lastw = {}
        self.readers = {}
        self.dcnt = {}
        self.phase = "init"

    def _need(self, eng, reads, writes):
        need = {}

        def add(dep, raw):
            de, dc = dep
            if de == eng and (eng == "pe" or eng == "sp"):
                return
            if need.get(de, 0) < dc:
                need[de] = dc
        for r in reads:
            w = self.lastw.get(r)
            if w is not None:
                add(w, True)
        for w_ in writes:
            w = self.lastw.get(w_)
            if w is not None:
                add(w, False)
            for de, dc in self.readers.get(w_, {}).items():
                add((de, dc), False)
        waits = []
        for de, dc in need.items():
            if self.seen[eng].get(de, 0) < dc:
                self.seen[eng][de] = dc
                waits.append((de, dc))
        return waits

    def _record(self, tag, reads, writes):
        for r in reads:
            d = self.readers.setdefault(r, {})
            if d.get(tag[0], 0) < tag[1]:
                d[tag[0]] = tag[1]
        for w_ in writes:
            self.lastw[w_] = tag
            self.readers[w_] = {}

    def op(self, eng, fn, reads=(), writes=(), inc=True):
        waits = self._need(eng, reads, writes)
        tag = (eng, self.cnt[eng] + 1)
        if inc:
            self.cnt[eng] += 1
        self.q[eng].append((waits, fn, ("E", eng) if inc else None, self.phase))
        self._record(tag, reads, writes)

    def dma(self, key, fn, reads=(), writes=(), eng="sp"):
        waits = self._need(eng, reads, writes)
        de = ("dma", key)
        self.dcnt[de] = self.dcnt.get(de, 0) + 16
        self.q[eng].append((waits, fn, ("D", de), self.phase))
        self._record((de, self.dcnt[de]), reads, writes)

    def barrier(self):
        tgt = {e: self.cnt[e] for e in ("pe", "act", "dve", "pool")}
        tgt.update(self.dcnt)
        for e in self.ENG:
            waits = []
            for de, dc in tgt.items():
                if de == e or dc == 0:
                    continue
                if self.seen[e].get(de, 0) < dc:
                    self.seen[e][de] = dc
                    waits.append((de, dc))
            if waits:
                self.q[e].append((waits, None, None, None))

    def final_wait(self, eng, dma_keys):
        waits = [(("dma", k), self.dcnt[("dma", k)]) for k in dma_keys if ("dma", k) in self.dcnt]
        self.q[eng].append((waits, None, None, None))


def _build(nseq, layers_all=(0, 1), final_norm=True, taps=()):
    nc = bass.Bass("TRN2", target_bir_lowering=False)
    x_d = nc.dram_tensor("x", [nseq, S, D], F32, kind="ExternalInput").ap()
    wt_d = nc.dram_tensor("wt", [2, NWALL, 128, 1024], F32, kind="ExternalInput").ap()
    gv_d = nc.dram_tensor("gv", [3, D], F32, kind="ExternalInput").ap()
    cf_d = nc.dram_tensor("cf", [128, CF_N], F32, kind="ExternalInput").ap()
    cb_d = nc.dram_tensor("cb", [128, CB_N], BF16, kind="ExternalInput").ap()
    out_d = nc.dram_tensor("out", [nseq, S, D], F32, kind="ExternalOutput").ap()
    xs_d = nc.dram_tensor("xscr", [nseq, S, D], F32).ap()
    tap_d = {}
    for name, shape, dt in taps:
        tap_d[name] = nc.dram_tensor("tap_" + name, list(shape), dt, kind="ExternalOutput").ap()

    sc = Sched()
    sb = {}

    def alloc(name, shape, dt):
        t = nc.alloc_sbuf_tensor(name, list(shape), dt) if False else None
        return t

    from contextlib import ExitStack
    es = ExitStack()

    def SB(name, shape, dt):
        t = es.enter_context(nc.sbuf_tensor("sb_" + name, list(shape), dt))
        sb[name] = t
        return t

    def PS(name, shape, dt):
        return es.enter_context(nc.psum_tensor(name, list(shape), dt))

    cf = SB("cf", [128, CF_N], F32)
    cb = SB("cb", [128, CB_N], BF16)
    hT = SB("hT", [128, 8, S], BF16)
    yT = SB("yT", [128, 8, S], BF16)
    gbc = SB("gbc", [128, 2, D], F32)
    wbf = SB("wbf", [128, 8, 1024], BF16)
    xt = SB("xt", [128, 2, D], F32)
    hn = SB("hn", [128, 2, D], BF16)
    st = SB("st", [128, 64], F32)
    scr = SB("scr", [128, 36864], BF16)
    ps = [PS(f"ps{i}", [128, 512], F32) for i in range(8)]

    def scr_view(off_bytes, shape, dt):
        n = int(np.prod(shape))
        if dt == F32:
            assert off_bytes % 4 == 0
            v = scr[:, off_bytes // 2: off_bytes // 2 + 2 * n].bitcast(F32)
        else:
            v = scr[:, off_bytes // 2: off_bytes // 2 + n]
        if len(shape) == 2:
            v = v.rearrange("p (a b) -> p a b", b=shape[1])
        elif len(shape) == 3:
            v = v.rearrange("p (a b c) -> p a b c", b=shape[1], c=shape[2])
        return v

    ident = cb[:, CB_ID:CB_ID + 128]

    def mm(out, lhsT, rhs, start, stop, reads, writes, inc=None):
        if inc is None:
            inc = stop
        sc.op("pe", ("matmul", dict(out=out, lhsT=lhsT, rhs=rhs, start=start, stop=stop, skip_group_check=True)),
              reads=reads, writes=writes, inc=inc)

    def tr(out, in_, reads, writes, inc=True):
        sc.op("pe", ("transpose", dict(out=out, in_=in_, identity=ident[:in_.shape[0], :in_.shape[0]])), reads=reads, writes=writes, inc=inc)


    NSLOT = 8
    WDEPTH = 4
    wstate = {"n": 0, "ring": 0, "rec": True, "issued": 0, "xd": 0}
    wplan = []

    def _issue_w(ent):
        layer, idx, dest, dest_key, skey = ent
        sc.dma(skey, ("dma_start", dict(out=dest, in_=wt_d[layer, idx, :, :])), writes=[dest_key], eng="pool")

    def load_w(layer, idx, dest=None, dest_key=None):
        n = wstate["n"]; wstate["n"] += 1
        if wstate["rec"]:
            if dest is None:
                b = wstate["ring"] % NSLOT; wstate["ring"] += 1
                dest = wbf[:, b, :]
                dest_key = ("wbf", b)
                skey = ("w", b)
            else:
                skey = ("wx", wstate["xd"] % 16); wstate["xd"] += 1
            wplan.append((layer, idx, dest, dest_key, skey))
            return dest, dest_key
        while wstate["issued"] < len(wplan) and wstate["issued"] <= n + WDEPTH:
            ent = wplan[wstate["issued"]]
            if ent[4][0] == "wx" and wstate["issued"] > n:
                break
            _issue_w(ent)
            wstate["issued"] += 1
        ent = wplan[n]
        return ent[2], ent[3]

    def load_g(slot, row):
        src = gv_d[row:row + 1, :].partition_broadcast(128) if False else None
        from concourse.ap import AP
        src = AP(gv_d.tensor, row * D, [[0, 128], [1, D]])
        sc.dma(("g", slot), ("dma_start", dict(out=gbc[:, slot, :], in_=src)), writes=[("gbc", slot)])

    def phase_norm(si, layer, src_d):
        load_g(layer % 2, layer)
        pst = ps[7][:, 0:512].bitcast(BF16)
        def stats(t):
            s = t % 2
            sc.dma(("x", s), ("dma_start", dict(out=xt[:, s, :], in_=src_d[si, t * 128:(t + 1) * 128, :])),
                   writes=[("xt", s)])
            ss = st[:, 2 * s:2 * s + 1]
            rs = st[:, 2 * s + 1:2 * s + 2]
            sc.op("act", ("activation", dict(out=hn[:, s, :], in_=xt[:, s, :], func=AF.Square, accum_out=ss)),
                  reads=[("xt", s)], writes=[("hn", s), ("st", s)])
            sc.op("dve", ("tensor_scalar", dict(out=rs, in0=ss, scalar1=1.0 / D, scalar2=1e-6, op0=ALU.mult, op1=ALU.add)),
                  reads=[("st", s)], writes=[("st", s)])
            sc.op("act", ("activation", dict(out=rs, in_=rs, func=AF.Sqrt)), reads=[("st", s)], writes=[("st", s)])
            sc.op("dve", ("reciprocal", dict(out=rs, in_=rs)), reads=[("st", s)], writes=[("st", s)])
            sc.op("dve", ("scalar_tensor_tensor", dict(out=hn[:, s, :], in0=xt[:, s, :], scalar=rs, in1=gbc[:, layer % 2, :],
                                                                     op0=ALU.mult, op1=ALU.mult)),
                  reads=[("xt", s), ("st", s), ("gbc", layer % 2)], writes=[("hn", s)])

        def trans(t):
            s = t % 2
            for c in range(8):
                tr(pst[:, c * 128:(c + 1) * 128], hn[:, s, c * 128:(c + 1) * 128], reads=[("hn", s), "cb"], writes=[("ps", 7)], inc=(c == 7))
            sc.op("act", ("copy", dict(out=hT[:, :, t * 128:(t + 1) * 128], in_=pst.rearrange("p (c n) -> p c n", n=128))),
                  reads=[("ps", 7)], writes=[("hT", t // 4)])
        stats(0)
        for t in range(NT):
            if t + 1 < NT:
                stats(t + 1)
            trans(t)

    def run(units):
        for u in units:
            u()

    def run_merged(*lists):
        lists = [l for l in lists if l]
        idx = [0] * len(lists)
        while True:
            cand = [(idx[i] / len(l), i) for i, l in enumerate(lists) if idx[i] < len(l)]
            if not cand:
                break
            i = min(cand)[1]
            lists[i][idx[i]]()
            idx[i] += 1

    def skew(units, items, fa, fb, others):
        pend = None
        for it in items:
            if it[0] == "t":
                st_ = {}
                a_ = (lambda it=it, st_=st_: fa(st_, *it[1:]))
                if pend is None:
                    units.append(a_)
                else:
                    units.append(lambda a_=a_, b_=pend: (a_(), b_()))
                pend = (lambda it=it, st_=st_: fb(st_, *it[1:]))
            else:
                if pend is not None:
                    units.append(pend)
                    pend = None
                units.append(lambda it=it: others[it[0]](*it[1:]))
        if pend is not None:
            units.append(pend)

    ppp = {"i": 0}

    qsb = SB("qsb", [128, 2, 512], BF16)
    qsc = {"i": 0}

    def proj_fm(layer, name, rope, evac, banks=((0, 1), (2, 3))):
        stt = {"pend": None}

        def rot_evac(c4, j):
            i = ppp["i"]; ppp["i"] += 1
            b2 = banks[1][i % len(banks[1])]
            mm(ps[b2][:, :], cb[:, CB_PERM:CB_PERM + 128], qsb[:, j, :], True, True, reads=[("qsb", j), "cb"], writes=[("ps", b2)])
            evac(c4, j, b2)

        def u(c4):
            if c4 == 0:
                w1, k1 = load_w(layer, _TIDX[name])
                stt["w1"] = (w1.rearrange("p (c n) -> p c n", n=128), k1)
            w1, k1 = stt["w1"]
            i = ppp["i"]; ppp["i"] += 1
            b1 = banks[0][i % len(banks[0])]
            for c in range(8):
                mm(ps[b1][:, :], w1[:, c, :], hT[:, c, c4 * 512:(c4 + 1) * 512], c == 0, c == 7,
                   reads=[k1, ("hT", c4)], writes=[("ps", b1)])
            if rope:
                j = qsc["i"] % 2; qsc["i"] += 1
                sc.op("act", ("copy", dict(out=qsb[:, j, :], in_=ps[b1][:, :])), reads=[("ps", b1)], writes=[("qsb", j)])
                if stt["pend"] is not None:
                    rot_evac(*stt["pend"])
                stt["pend"] = (c4, j)
                if c4 == 3:
                    rot_evac(*stt["pend"])
                    stt["pend"] = None
            else:
                evac(c4, b1)
        return [(lambda c4=c4: u(c4)) for c4 in range(4)]

    rt = SB("rt", [128, 2, 2, 512], F32)
    rtc = {"i": 0}

    def rope_evac(dst_fn, dst_keys_fn, split_heads):
        def ev(c4, jq, b2):
            j = rtc["i"] % 2; rtc["i"] += 1
            t1 = rt[:, j, 0, :]
            t2 = rt[:, j, 1, :]
            cs = cf[:, CF_COS + c4 * 512:CF_COS + (c4 + 1) * 512]
            sn = cf[:, CF_SIN + c4 * 512:CF_SIN + (c4 + 1) * 512]
            sc.op("dve", ("tensor_tensor", dict(out=t1, in0=qsb[:, jq, :], in1=cs, op=ALU.mult)),
                  reads=[("qsb", jq), "cf"], writes=[("rt", j, 0)])
            sc.op("dve", ("tensor_tensor", dict(out=t2, in0=ps[b2][:, :], in1=sn, op=ALU.mult)),
                  reads=[("ps", b2), "cf"], writes=[("rt", j, 1)])
            if split_heads:
                for h in range(2):
                    d = dst_fn(c4, h)
                    sc.op("pool", ("tensor_tensor", dict(out=d, in0=t1[64 * h:64 * h + 64, :], in1=t2[64 * h:64 * h + 64, :], op=ALU.add)),
                          reads=[("rt", j, 0), ("rt", j, 1)], writes=dst_keys_fn(c4, h))
            else:
                d = dst_fn(c4, None)
                sc.op("pool", ("tensor_tensor", dict(out=d, in0=t1, in1=t2, op=ALU.add)),
                      reads=[("rt", j, 0), ("rt", j, 1)], writes=dst_keys_fn(c4, None))
        return ev

    def silu_evac(ychunk):
        def ev(c4, b1):
            sc.op("act", ("activation", dict(out=yT[:, ychunk, c4 * 512:(c4 + 1) * 512], in_=ps[b1][:, :], func=AF.Silu)),
                  reads=[("ps", b1)], writes=[("yT", ychunk, c4)])
        return ev

    def proj_tm(layer, name, nblk, tok_fn, evac, banks=(0, 1)):
        stt = {}

        def u(g4):
            if g4 == 0:
                w1, k1 = load_w(layer, _TIDX[name])
                stt["w1"] = (w1.rearrange("p (c n) -> p c n", n=128), k1)
            w1, k1 = stt["w1"]
            i = ppp["i"]; ppp["i"] += 1
            b1 = banks[i % len(banks)]
            for j in range(4):
                blk = g4 + j
                for c in range(8):
                    mm(ps[b1][:, j * 128:(j + 1) * 128], tok_fn(c, blk), w1[:, c, :], (c == 0 and j == 0), c == 7,
                       reads=[k1] + [("hT", q) for q in range(4)], writes=[("ps", b1)], inc=(c == 7 and j == 3))
            evac(g4, b1)
        return [(lambda g4=g4: u(g4)) for g4 in range(0, nblk, 4)]

    def mixer_A(layer):
        QA = [scr_view(0, [2, S], BF16), scr_view(8192, [2, S], BF16)]
        KA = [scr_view(16384, [2, S], BF16), scr_view(24576, [2, S], BF16)]
        VA = [scr_view(32768, [16, 256], BF16), scr_view(40960, [16, 256], BF16)]
        o0 = 49152
        km = scr_view(o0, [2, 8], BF16)
        kmf = scr_view(o0 + 64, [2, 8], F32)
        gt = scr_view(o0 + 256, [128], F32)
        cmp_ = scr_view(o0 + 1024, [128, 8], F32)
        rk = scr_view(o0 + 1024 + 4096, [128], F32)
        nb = scr_view(o0 + 1024 + 4096 + 512, [128], BF16)
        et = scr_view(57344, [2, 512], BF16)
        rd = scr_view(57344 + 2048, [512], F32)
        rn = scr_view(57344 + 4096, [512], F32)
        est = {"i": 0}

        def proj_units(p):
            bs_ = p % 2
            qa, ka, va = QA[bs_], KA[bs_], VA[bs_]
            units = []
            if p < 2:
                def init():
                    for e_ in range(2):
                        sc.op("pool", ("tensor_copy", dict(out=ka[64:72, e_, :], in_=cb[0:8, CB_ONEHOT:CB_ONEHOT + S])),
                              reads=["cb"], writes=[("ka", bs_, e_, c4) for c4 in range(4)])
                    for h in range(2):
                        sc.op("pool", ("tensor_copy", dict(out=va[:, :, 128 * h + 64:128 * h + 128],
                                                           in_=cb[:, CB_ONES:CB_ONES + 64].unsqueeze(1).to_broadcast([128, 16, 64]))),
                              reads=["cb"], writes=[("va1", bs_, h)])
                units.append(init)
            units += proj_fm(layer, f"AQ{p}", True, rope_evac(lambda c4, h: qa[0:64, h, c4 * 512:(c4 + 1) * 512],
                                                              lambda c4, h: [("qa", bs_, h, c4)], True))
            units += proj_fm(layer, f"AK{p}", True, rope_evac(lambda c4, h: ka[0:64, h, c4 * 512:(c4 + 1) * 512],
                                                              lambda c4, h: [("ka", bs_, h, c4)], True))
            units += proj_fm(layer, f"AG{p}", False, silu_evac(p))

            def vev(g4, b1):
                for h in range(2):
                    sc.op("act", ("copy", dict(out=va[:, g4:g4 + 4, 128 * h:128 * h + 64],
                                               in_=ps[b1][:, :].rearrange("p (j n) -> p j n", n=128)[:, :, 64 * h:64 * h + 64])),
                          reads=[("ps", b1)], writes=[("va", bs_, h, g4)])
            units += proj_tm(layer, f"AV{p}", 16, lambda c, blk: hT[:, c, blk * 128:(blk + 1) * 128], vev)
            return units

        def attn_units(p):
            bs_ = p % 2
            qa, ka, va = QA[bs_], KA[bs_], VA[bs_]
            units = []

            def gate(h):
                allk = [("ka", bs_, h, c4) for c4 in range(4)]
                allq = [("qa", bs_, h, c4) for c4 in range(4)]
                sc.op("dve", ("tensor_reduce", dict(out=kmf[0:64, h, :], in_=ka[0:64, h, :].rearrange("p (n b) -> p n b", b=256),
                                                    axis=AX.X, op=ALU.add)), reads=allk, writes=[("kmf", h)])
                sc.op("dve", ("tensor_copy", dict(out=km[0:64, h, :], in_=kmf[0:64, h, :])), reads=[("kmf", h)], writes=[("km", h)])
                gp = ps[4][:, 0:128]
                for qt in range(16):
                    mm(gp[:, qt * 8:(qt + 1) * 8], qa[0:64, h, qt * 128:(qt + 1) * 128], km[0:64, h, :], True, True,
                       reads=allq + [("km", h)], writes=[("ps", 4)], inc=(qt == 15))
                sc.op("dve", ("tensor_tensor", dict(out=gt, in0=gp, in1=cf[:, CF_AGM:CF_AGM + 128], op=ALU.add)),
                      reads=[("ps", 4), "cf"], writes=["gt"])
                g3 = gt.rearrange("p (q n) -> p q n", n=8)
                sc.op("dve", ("tensor_tensor", dict(out=cmp_.rearrange("p (q n) m -> p q n m", n=8),
                                                    in0=g3.unsqueeze(2).to_broadcast([128, 16, 8, 8]),
                                                    in1=g3.unsqueeze(3).to_broadcast([128, 16, 8, 8]), op=ALU.is_gt)),
                      reads=["gt"], writes=["cmp"])
                sc.op("dve", ("tensor_reduce", dict(out=rk, in_=cmp_, axis=AX.X, op=ALU.add)), reads=["cmp"], writes=["rk"])
                sc.op("dve", ("tensor_scalar", dict(out=rk, in0=rk, scalar1=2.5, scalar2=None, op0=ALU.is_lt)), reads=["rk"], writes=["rk"])
                sc.op("dve", ("tensor_tensor", dict(out=rk, in0=rk, in1=cf[:, CF_AVAL:CF_AVAL + 128], op=ALU.mult)), reads=["rk", "cf"], writes=["rk"])
                sc.op("dve", ("scalar_tensor_tensor", dict(out=rk, in0=rk, scalar=-NEG, in1=cf[:, CF_AOWN:CF_AOWN + 128],
                                                           op0=ALU.mult, op1=ALU.add)), reads=["rk", "cf"], writes=["rk"])
                sc.op("dve", ("tensor_copy", dict(out=nb, in_=rk)), reads=["rk"], writes=["nb"])
                tp = ps[5][:, 0:512].bitcast(BF16)
                for half in range(2):
                    for q8 in range(8):
                        qt = half * 8 + q8
                        tr(tp[0:8, q8 * 128:(q8 + 1) * 128], nb[:, qt * 8:(qt + 1) * 8], reads=["nb", "cb"], writes=[("ps", 5)], inc=(q8 == 7))
                    sc.op("act", ("copy", dict(out=qa[64:72, h, half * 1024:(half + 1) * 1024], in_=tp[0:8, :])),
                          reads=[("ps", 5)], writes=[("qa", bs_, h, 2 * half), ("qa", bs_, h, 2 * half + 1)])

            def tile_a(st_, h, c4, kt, nkt):
                q0 = max(kt * 128, c4 * 512)
                q1 = (c4 + 1) * 512
                n = q1 - q0
                ei = est["i"]; est["i"] += 1
                sb_ = 4 + (ei % 2)
                ej = ei % 2
                st_.update(q0=q0, n=n, ej=ej)
                mm(ps[sb_][:, 0:n], ka[0:72, h, kt * 128:(kt + 1) * 128], qa[0:72, h, q0:q1], True, True,
                   reads=[("ka", bs_, h, kt // 4), ("qa", bs_, h, c4)], writes=[("ps", sb_)])
                sc.op("act", ("activation", dict(out=et[:, ej, 0:n], in_=ps[sb_][:, 0:n], func=AF.Exp, scale=0.125)),
                      reads=[("ps", sb_)], writes=[("et", ej)])
                if q0 == kt * 128:
                    sc.op("dve", ("tensor_tensor", dict(out=et[:, ej, 0:128], in0=et[:, ej, 0:128],
                                                        in1=cb[:, CB_MOWN:CB_MOWN + 128], op=ALU.mult)),
                          reads=[("et", ej), "cb"], writes=[("et", ej)])

            def tile_b(st_, h, c4, kt, nkt):
                ob = 6 + (c4 % 2)
                q0, n, ej = st_["q0"], st_["n"], st_["ej"]
                mm(ps[ob][:, q0 - c4 * 512:512], va[:, kt, 128 * h:128 * h + 128], et[:, ej, 0:n], kt == 0, kt == nkt - 1,
                   reads=[("va", bs_, h, (kt // 4) * 4), ("va1", bs_, h), ("et", ej)], writes=[("ps", ob)], inc=True)

            def norm(h, c4):
                ob = 6 + (c4 % 2)
                sc.op("act", ("activation", dict(out=rd[64:128, :], in_=ps[ob][64:128, :], func=AF.Ln)), reads=[("ps", ob)], writes=["rd"])
                sc.op("act", ("activation", dict(out=rd[64:128, :], in_=rd[64:128, :], func=AF.Exp, scale=-1.0)), reads=["rd"], writes=["rd"])
                sc.op("dve", ("tensor_tensor", dict(out=rn[64 * h:64 * h + 64, :], in0=ps[ob][0:64, :], in1=rd[64:128, :], op=ALU.mult)),
                      reads=[("ps", ob), "rd"], writes=["rd0"])
                ydst = yT[64 * h:64 * h + 64, p, c4 * 512:(c4 + 1) * 512]
                sc.op("pool", ("tensor_tensor", dict(out=ydst, in0=ydst, in1=rn[64 * h:64 * h + 64, :], op=ALU.mult)),
                      reads=["rd0", ("yT", p, c4)], writes=[("yT", p, c4)])

            items = []
            for h in range(2):
                items.append(("g", h))
                for c4 in range(4):
                    nkt = 4 * c4 + 4
                    for kt in range(nkt):
                        items.append(("t", h, c4, kt, nkt))
                    items.append(("n", h, c4))
            skew(units, items, tile_a, tile_b, {"g": gate, "n": norm})
            return units

        run(proj_units(0))
        for p in range(3):
            run_merged(attn_units(p), proj_units(p + 1) if p < 2 else [])

    def mixer_B(layer):
        qb = scr_view(0, [3, S], BF16)
        kb = scr_view(12288, [S], BF16)
        ikb = scr_view(16384, [S], BF16)
        iqb = scr_view(20480, [2, S], BF16)
        vb = scr_view(28672, [16, 128], BF16)
        iw = scr_view(32768, [16, 4], F32)
        acc = scr_view(33024, [2, S], F32)
        tmp = scr_view(49408, [2, 512], F32)
        mk = scr_view(53504, [S], BF16)
        mt = scr_view(57600, [2, 16, 128], BF16)
        et = scr_view(65792, [2, 384], BF16)
        pt = scr_view(67328, [2, 384], BF16)
        rd = scr_view(68864, [384], F32)
        bs = scr_view(70400, [64], F32)
        rn = scr_view(70656, [384], F32)
        sc.op("pool", ("tensor_copy", dict(out=vb[:, :, 64:128], in_=cb[:, CB_ONES:CB_ONES + 64].unsqueeze(1).to_broadcast([128, 16, 64]))),
              reads=["cb"], writes=["vb1"])
        pu = []
        pu += proj_fm(layer, "IK", True, rope_evac(lambda c4, h: ikb[:, c4 * 512:(c4 + 1) * 512], lambda c4, h: [("ikb", c4)], False))
        for p in range(2):
            pu += proj_fm(layer, f"IQ{p}", True, rope_evac(lambda c4, h, p=p: iqb[:, p, c4 * 512:(c4 + 1) * 512],
                                                           lambda c4, h, p=p: [("iqb", c4)], False))

        def vev(g4, b1):
            v3 = ps[b1][:, :].rearrange("p (j n) -> p j n", n=128)
            sc.op("act", ("copy", dict(out=vb[:, g4:g4 + 4, 0:64], in_=v3[:, :, 0:64])), reads=[("ps", b1)], writes=[("vb", g4)])
            sc.op("act", ("copy", dict(out=iw[:, g4:g4 + 4, :], in_=v3[:, :, 64:68])), reads=[("ps", b1)], writes=["iw"])
        pu += proj_tm(layer, "BV", 16, lambda c, blk: hT[:, c, blk * 128:(blk + 1) * 128], vev)
        run(pu)
        PB = ((6,), (7,))
        pu2 = []
        for p in range(3):
            pu2 += proj_fm(layer, f"BQ{p}", True, rope_evac(lambda c4, h, p=p: qb[:, p, c4 * 512:(c4 + 1) * 512],
                                                            lambda c4, h, p=p: [("qb", c4)], False), banks=PB)
            pu2 += proj_fm(layer, f"BG{p}", False, silu_evac(3 + p), banks=PB)
        pu2 += proj_fm(layer, "BK", True, rope_evac(lambda c4, h: kb[:, c4 * 512:(c4 + 1) * 512], lambda c4, h: [("kb", c4)], False), banks=PB)
        cst = {"li": 0, "ei": 0}
        mkb = [mk, hn[:, :, :].rearrange("p a b -> p (a b)")]
        xtb = xt[:, :, :].rearrange("p a b -> p (a b)").bitcast(BF16).rearrange("p (i k n) -> p i k n", i=2, n=128)
        mtb = [mt[:, 0, :, :], mt[:, 1, :, :], xtb[:, 0, :, :], xtb[:, 1, :, :]]

        def stage1(qt):
            N = 128 * (qt + 1)
            a = qt % 2
            par = qt % 2
            m4 = qt % 4
            qs = slice(qt * 128, (qt + 1) * 128)
            mk_ = mkb[par]
            mt_ = mtb[m4]
            o = 32 * par
            thr = bs[:, o:o + 1]; hi = bs[:, o + 1:o + 2]; lo = bs[:, o + 2:o + 3]; cnt = bs[:, o + 3:o + 4]
            tt = bs[:, o + 4:o + 5]; nthr = bs[:, o + 5:o + 6]
            W = bs[:, o + 8:o + 8 + NBIS + 2]
            K = lambda nm: (nm, par)
            tpb = ps[2 + par][:, 0:512].bitcast(BF16)
            lb = par
            tj = par
            units = []

            def idx(j, h):
                k0 = j * 512
                n = min(512, N - k0)
                e_ = h % 2
                mm(ps[lb][:, 0:n], iqb[64 * e_:64 * e_ + 64, h // 2, qs], ikb[64 * e_:64 * e_ + 64, k0:k0 + n], True, True,
                   reads=[("iqb", qt // 4), ("ikb", j)], writes=[("ps", lb)])
                if h == 0:
                    sc.op("dve", ("tensor_scalar", dict(out=acc[:, a, k0:k0 + n], in0=ps[lb][:, 0:n], scalar1=0.0,
                                                        scalar2=iw[:, qt, 0:1], op0=ALU.max, op1=ALU.mult)),
                          reads=[("ps", lb), "iw"], writes=[("acc", a)])
                else:
                    sc.op("dve", ("tensor_scalar", dict(out=tmp[:, tj, 0:n], in0=ps[lb][:, 0:n], scalar1=0.0,
                                                        scalar2=iw[:, qt, h:h + 1], op0=ALU.max, op1=ALU.mult)),
                          reads=[("ps", lb), "iw"], writes=[("tmp", tj)])
                    sc.op("pool", ("tensor_tensor", dict(out=acc[:, a, k0:k0 + n], in0=acc[:, a, k0:k0 + n],
                                                         in1=tmp[:, tj, 0:n], op=ALU.add)),
                          reads=[("acc", a), ("tmp", tj)], writes=[("acc", a)])
            for j in range((N + 511) // 512):
                for h in range(4):
                    units.append(lambda j=j, h=h: idx(j, h))

            def bis_init():
                sc.op("pool", ("tensor_tensor", dict(out=acc[:, a, qs], in0=acc[:, a, qs], in1=cf[:, CF_NEGTRI:CF_NEGTRI + 128], op=ALU.add)),
                      reads=[("acc", a), "cf"], writes=[("acc", a)])
                if qt < 2:
                    sc.op("dve", ("memset", dict(ap=thr, constant=-1e29)), writes=[K("thr")])
                    return
                sc.op("dve", ("tensor_reduce", dict(out=hi, in_=acc[:, a, 0:N], axis=AX.X, op=ALU.max)), reads=[("acc", a)], writes=[K("bs_hi")])
                sc.op("dve", ("tensor_reduce", dict(out=lo, in_=acc[:, a, 0:N - 128], axis=AX.X, op=ALU.min)), reads=[("acc", a)], writes=[K("bs_lo")])
                sc.op("dve", ("tensor_tensor", dict(out=tt, in0=lo, in1=hi, op=ALU.subtract)), reads=[K("bs_hi"), K("bs_lo")], writes=[K("bs_tt")])
                sc.op("dve", ("tensor_scalar", dict(out=W, in0=cf[:, CF_POW2:CF_POW2 + NBIS + 2], scalar1=tt, scalar2=None, op0=ALU.mult)),
                      reads=[K("bs_tt"), "cf"], writes=[K("bs_W")])
                sc.op("dve", ("tensor_tensor", dict(out=nthr, in0=W[:, 1:2], in1=lo, op=ALU.subtract)),
                      reads=[K("bs_lo"), K("bs_W")], writes=[K("nthr")])
            units.append(bis_init)

            def bis(k):
                sc.op("act", ("activation", dict(out=mk_[:, 0:N], in_=acc[:, a, 0:N], func=AF.Sign, bias=nthr, scale=1.0, accum_out=cnt)),
                      reads=[("acc", a), K("nthr")], writes=[K("bs_cnt"), K("mk")])
                sc.op("act", ("activation", dict(out=tt, in_=cnt, func=AF.Sign, bias=float(N - 511), scale=1.0)),
                      reads=[K("bs_cnt")], writes=[K("bs_tt")])
                sc.op("act", ("activation", dict(out=nthr, in_=tt, func=AF.Identity, bias=nthr, scale=W[:, k + 2:k + 3])),
                      reads=[K("bs_tt"), K("bs_W"), K("nthr")], writes=[K("nthr")])
            if qt >= 2:
                for k in range(NBIS):
                    units.append(lambda k=k: bis(k))

            def fin():
                if qt >= 2:
                    sc.op("dve", ("scalar_tensor_tensor", dict(out=thr, in0=nthr, scalar=-1.0, in1=W[:, NBIS + 1:NBIS + 2], op0=ALU.mult, op1=ALU.add)),
                          reads=[K("nthr"), K("bs_W")], writes=[K("thr")])
                sc.op("dve", ("tensor_scalar", dict(out=mk_[:, 0:N], in0=acc[:, a, 0:N], scalar1=thr, scalar2=None, op0=ALU.is_ge)),
                      reads=[("acc", a), K("thr")], writes=[K("mk")])
                for half in range((qt // 8) + 1):
                    nk = min(8, qt + 1 - 8 * half)
                    for i in range(nk):
                        kt = 8 * half + i
                        tr(tpb[:, i * 128:(i + 1) * 128], mk_[:, kt * 128:(kt + 1) * 128], reads=[K("mk"), "cb"], writes=[("ps", 2 + par)],
                           inc=(i == nk - 1))
                    sc.op("act", ("copy", dict(out=mt_[:, 8 * half:8 * half + nk, :],
                                               in_=tpb[:, 0:nk * 128].rearrange("p (k n) -> p k n", n=128))),
                          reads=[("ps", 2 + par)], writes=[("mt", m4)])
            units.append(fin)
            return units

        def stage2(qt):
            m = qt % 4
            mt_ = mtb[m]
            qs = slice(qt * 128, (qt + 1) * 128)
            units = []

            def tile_a(st_, kt, e_):
                ei = cst["ei"]; cst["ei"] += 1
                sb_ = 4 + (ei % 2)
                ej = ei % 2
                st_.update(ej=ej)
                mm(ps[sb_][:, 0:384], kb[64 * e_:64 * e_ + 64, kt * 128:(kt + 1) * 128], qb[64 * e_:64 * e_ + 64, :, qs], True, True,
                   reads=[("kb", kt // 4), ("qb", qt // 4)], writes=[("ps", sb_)])
                sc.op("act", ("activation", dict(out=et[:, ej, :], in_=ps[sb_][:, 0:384], func=AF.Exp, scale=0.125)),
                      reads=[("ps", sb_)], writes=[("et", ej)])
                sc.op("dve", ("tensor_tensor", dict(out=pt[:, ej, :].rearrange("p (h n) -> p h n", n=128),
                                                    in0=et[:, ej, :].rearrange("p (h n) -> p h n", n=128),
                                                    in1=mt_[:, kt, :].unsqueeze(1).to_broadcast([128, 3, 128]), op=ALU.mult)),
                      reads=[("et", ej), ("mt", m)], writes=[("pt", ej)])

            def tile_b(st_, kt, e_):
                ob = 6 + e_
                ej = st_["ej"]
                mm(ps[ob][:, 0:384], vb[:, kt, :], pt[:, ej, :], kt == 0, kt == qt,
                   reads=[("vb", (kt // 4) * 4), "vb1", ("pt", ej)], writes=[("ps", ob)], inc=True)
            items = [("t", kt, e_) for kt in range(qt + 1) for e_ in range(2)]

            def norm(e_):
                ob = 6 + e_
                sc.op("act", ("activation", dict(out=rd[64:128, :], in_=ps[ob][64:128, 0:384], func=AF.Ln)), reads=[("ps", ob)], writes=["rd"])
                sc.op("act", ("activation", dict(out=rd[64:128, :], in_=rd[64:128, :], func=AF.Exp, scale=-1.0)), reads=["rd"], writes=["rd"])
                sc.op("dve", ("tensor_tensor", dict(out=rn[64 * e_:64 * e_ + 64, :], in0=ps[ob][0:64, 0:384], in1=rd[64:128, :], op=ALU.mult)),
                      reads=[("ps", ob), "rd"], writes=["rd0"])
                ydst = yT[64 * e_:64 * e_ + 64, 3:6, qs]
                sc.op("pool", ("tensor_tensor", dict(out=ydst, in0=ydst, in1=rn[64 * e_:64 * e_ + 64, :].rearrange("p (h n) -> p h n", n=128), op=ALU.mult)),
                      reads=["rd0"] + [("yT", 3 + p, qt // 4) for p in range(3)], writes=[("yT", 3 + p, qt // 4) for p in range(3)])
            items += [("n", 0), ("n", 1)]
            skew(units, items, tile_a, tile_b, {"n": norm})
            return units

        run_merged(stage1(0), stage1(1), pu2)
        for P in range(8):
            nx = [stage1(2 * P + 2), stage1(2 * P + 3)] if P < 7 else []
            run_merged(stage2(2 * P) + stage2(2 * P + 1), *nx)

    def mixer_C(layer):
        qc = scr_view(0, [2, S], BF16)
        kc = scr_view(8192, [2, S], BF16)
        vc = scr_view(16384, [16, 512], BF16)
        ac = scr_view(32768, [4, S], F32)
        et = scr_view(65536, [2, 256], BF16)
        rd = scr_view(65536 + 1024, [512], F32)
        rn = scr_view(65536 + 3072, [512], F32)
        sc.op("pool", ("tensor_copy", dict(out=vc.rearrange("p b (h c) -> p b h c", c=128)[:, :, :, 64:128],
                                           in_=cb[:, CB_ONES:CB_ONES + 64].unsqueeze(1).unsqueeze(1).to_broadcast([128, 16, 4, 64]))),
              reads=["cb"], writes=["vc1"])
        gu = []
        for p in range(2):
            gu += proj_fm(layer, f"CG{p}", False, silu_evac(6 + p))
        run(gu)
        groups = [g for g in range(3) if g in getattr(_build, 'cgroups', (0, 1, 2))]
        dils = (1, 4, 16)
        cst = {"ei": 0}

        def toks_of(dil):
            def toks(r, m, cnt=128):
                st0 = r + dil * 128 * m
                return slice(st0, st0 + dil * (cnt - 1) + 1, dil)
            return toks

        def proj_units(g, p, pbanks):
            dil = dils[g]
            nblk = S // dil // 128
            toks = toks_of(dil)
            units = []
            units += proj_fm(layer, f"CQ{g}{p}", True, rope_evac(lambda c4, h: qc[:, p, c4 * 512:(c4 + 1) * 512], lambda c4, h: [("qc", p, c4)], False),
                             banks=pbanks)
            units += proj_fm(layer, f"CK{g}{p}", True, rope_evac(lambda c4, h: kc[:, p, c4 * 512:(c4 + 1) * 512], lambda c4, h: [("kc", p, c4)], False),
                             banks=pbanks)

            def vev(g4, b1):
                v4 = ps[b1][:, :].rearrange("p (j h c) -> p j h c", h=2, c=64)
                sc.op("act", ("copy", dict(out=vc.rearrange("p b (h c) -> p b h c", c=128)[:, g4:g4 + 4, 2 * p:2 * p + 2, 0:64], in_=v4)),
                      reads=[("ps", b1)], writes=[("vc", p, g4)])
            units += proj_tm(layer, f"CV{g}{p}", 16, lambda c, blk: hT[:, c, toks(blk // nblk, blk % nblk)], vev,
                             banks=(pbanks[0][0], pbanks[1][0]) if len(pbanks[0]) == 1 else (0, 1))
            return units

        def attn_units(g, j, first):
            dil = dils[g]
            nblk = S // dil // 128
            toks = toks_of(dil)
            p, e_ = j // 2, j % 2
            pr = slice(64 * e_, 64 * e_ + 64)
            allq = [("qc", p, c4) for c4 in range(4)]
            allk = [("kc", p, c4) for c4 in range(4)]
            started = [False] * 4
            units = []

            def blk_a(st_, r, m):
                nq = 256 if m + 1 < nblk else 128
                ei = cst["ei"]; cst["ei"] += 1
                sb_ = 4 + (ei % 2)
                ej = ei % 2
                st_.update(nq=nq, ej=ej)
                mm(ps[sb_][:, 0:nq], kc[pr, p, toks(r, m)], qc[pr, p, toks(r, m, nq)], True, True,
                   reads=allk + allq, writes=[("ps", sb_)])
                sc.op("act", ("activation", dict(out=et[:, ej, 0:nq], in_=ps[sb_][:, 0:nq], func=AF.Exp, scale=0.125)),
                      reads=[("ps", sb_)], writes=[("et", ej)])
                sc.op("dve", ("tensor_tensor", dict(out=et[:, ej, 0:nq], in0=et[:, ej, 0:nq], in1=cb[:, CB_MOWN:CB_MOWN + nq], op=ALU.mult)),
                      reads=[("et", ej), "cb"], writes=[("et", ej)])

            def blk_b(st_, r, m):
                blk = r * nblk + m
                nq, ej = st_["nq"], st_["ej"]
                pieces = []
                for part in range(nq // 128):
                    t0 = r + dil * 128 * (m + part)
                    if dil <= 4:
                        pieces.append((part * 128, 128, t0))
                    else:
                        for q4 in range(4):
                            pieces.append((part * 128 + 32 * q4, 32, t0 + dil * 32 * q4))
                for pi, (c0, cn, t0) in enumerate(pieces):
                    bnk = t0 // 512
                    o0 = t0 % 512
                    mm(ps[bnk][:, o0:o0 + dil * (cn - 1) + 1:dil], vc[:, blk, 128 * j:128 * j + 128], et[:, ej, c0:c0 + cn],
                       not started[bnk], False, reads=[("vc", p, (blk // 4) * 4), "vc1", ("et", ej)],
                       writes=[("ps", bnk)], inc=(pi == len(pieces) - 1))
                    started[bnk] = True
            items = [("t", r, m) for r in range(dil) for m in range(nblk)] + [("e",)]

            def evac():
                for c4 in range(4):
                    dst = ac[:, j, c4 * 512:(c4 + 1) * 512]
                    if first:
                        sc.op("act", ("copy", dict(out=dst, in_=ps[c4][:, :])), reads=[("ps", c4)], writes=[("ac", j, c4)])
                    else:
                        sc.op("dve", ("tensor_tensor", dict(out=dst, in0=ps[c4][:, :], in1=dst, op=ALU.add)),
                              reads=[("ps", c4), ("ac", j, c4)], writes=[("ac", j, c4)])
            skew(units, items, blk_a, blk_b, {"e": evac})
            return units

        PB = ((6,), (7,))
        seq = []
        for gi, g in enumerate(groups):
            seq.append((g, 0)); seq.append((g, 1))
        run(proj_units(seq[0][0], seq[0][1], ((0, 1), (2, 3))))
        for i, (g, p) in enumerate(seq):
            nxt = proj_units(seq[i + 1][0], seq[i + 1][1], PB) if i + 1 < len(seq) else []
            first = (g == groups[0])
            run_merged(attn_units(g, 2 * p, first) + attn_units(g, 2 * p + 1, first), nxt)
        for j in range(4):
            p, e_ = j // 2, j % 2
            for c4 in range(4):
                cs = slice(c4 * 512, (c4 + 1) * 512)
                sc.op("act", ("activation", dict(out=rd[64:128, :], in_=ac[64:128, j, cs], func=AF.Ln)), reads=[("ac", j, c4)], writes=["rd"])
                sc.op("act", ("activation", dict(out=rd[64:128, :], in_=rd[64:128, :], func=AF.Exp, scale=-1.0)), reads=["rd"], writes=["rd"])
                sc.op("act", ("copy", dict(out=rn[64:128, :], in_=ac[0:64, j, cs])), reads=[("ac", j, c4)], writes=[("rn", 1)])
                sc.op("dve", ("tensor_tensor", dict(out=rn[64 * e_:64 * e_ + 64, :], in0=rn[64:128, :], in1=rd[64:128, :], op=ALU.mult)),
                      reads=[("rn", 1), "rd"], writes=[("rn", e_)])
                ydst = yT[64 * e_:64 * e_ + 64, 6 + p, cs]
                sc.op("pool", ("tensor_tensor", dict(out=ydst, in0=ydst, in1=rn[64 * e_:64 * e_ + 64, :], op=ALU.mult)),
                      reads=[("rn", e_), ("yT", 6 + p, c4)], writes=[("yT", 6 + p, c4)])

    def phase_final(si, layer, src_d, dst_d, last):
        mg = scr_view(0, [8, S], BF16)
        wbr = scr_view(32768, [8, D], BF16)
        wo = scr_view(49152, [8, D], BF16)
        gs = scr_view(65536, [3, 512], BF16)
        tm = scr_view(65536 + 3072, [512], F32)
        for c in range(8):
            nm = (f"WA{c}" if c < 3 else f"WB{c - 3}" if c < 6 else f"WC{c - 6}")
            load_w(layer, _XIDX[nm], dest=wbr[:, c, :], dest_key=("wbr", c))
        for c in range(8):
            load_w(layer, _XIDX[f"WO{c}"], dest=wo[:, c, :], dest_key=("wo", c))
        if last:
            load_g(0, 2)
        kch = ((0, 3), (3, 6), (6, 8))
        for dt_ in range(8):
            gw = []
            for br in range(3):
                w_, k_ = load_w(layer, _TIDX[f"MG{8 * br + dt_}"])
                gw.append((w_.rearrange("p (c n) -> p c n", n=128), k_))
            for c4 in range(4):
                cs = slice(c4 * 512, (c4 + 1) * 512)
                for br in range(3):
                    for c in range(8):
                        mm(ps[br][:, :], gw[br][0][:, c, :], hT[:, c, cs], c == 0, c == 7, reads=[gw[br][1], ("hT", c4)], writes=[("ps", br)])
                    sc.op("act", ("activation", dict(out=gs[:, br, :], in_=ps[br][:, :], func=AF.Sigmoid)), reads=[("ps", br)], writes=[("gs", br)])
                for br in range(3):
                    a0, a1 = kch[br]
                    for c in range(a0, a1):
                        mm(ps[3 + br][:, :], wbr[:, c, dt_ * 128:(dt_ + 1) * 128], yT[:, c, cs], c == a0, c == a1 - 1,
                           reads=[("wbr", c), ("yT", c, c4)], writes=[("ps", 3 + br)])
                sc.op("dve", ("tensor_tensor", dict(out=tm, in0=ps[3][:, :], in1=gs[:, 0, :], op=ALU.mult)), reads=[("ps", 3), ("gs", 0)], writes=["tm"])
                sc.op("dve", ("tensor_tensor", dict(out=gs[:, 1, :], in0=ps[4][:, :], in1=gs[:, 1, :], op=ALU.mult)), reads=[("ps", 4), ("gs", 1)], writes=[("gs", 1)])
                sc.op("dve", ("tensor_tensor", dict(out=gs[:, 2, :], in0=ps[5][:, :], in1=gs[:, 2, :], op=ALU.mult)), reads=[("ps", 5), ("gs", 2)], writes=[("gs", 2)])
                sc.op("pool", ("tensor_tensor", dict(out=tm, in0=tm, in1=gs[:, 1, :], op=ALU.add)), reads=["tm", ("gs", 1)], writes=["tm"])
                sc.op("pool", ("tensor_tensor", dict(out=mg[:, dt_, cs], in0=tm, in1=gs[:, 2, :], op=ALU.add)),
                      reads=["tm", ("gs", 2)], writes=[("mg", c4)])
        for t in range(NT):
            s = t % 2
            ts = slice(t * 128, (t + 1) * 128)
            sc.dma(("x", s), ("dma_start", dict(out=xt[:, s, :], in_=src_d[si, ts, :])), writes=[("xt", s)])
            for hf in range(2):
                b = 6 + hf
                for c in range(8):
                    mm(ps[b][:, :], mg[:, c, ts], wo[:, c, hf * 512:(hf + 1) * 512], c == 0, c == 7,
                       reads=[("mg", t // 4), ("wo", c)], writes=[("ps", b)])
                sc.op("dve", ("tensor_tensor", dict(out=xt[:, s, hf * 512:(hf + 1) * 512], in0=ps[b][:, :],
                                                                        in1=xt[:, s, hf * 512:(hf + 1) * 512], op=ALU.add)),
                      reads=[("ps", b), ("xt", s)], writes=[("xt", s)])
            if last:
                ss = st[:, 8 + 2 * s:8 + 2 * s + 1]
                rs = st[:, 8 + 2 * s + 1:8 + 2 * s + 2]
                sc.op("act", ("activation", dict(out=hn[:, s, :], in_=xt[:, s, :], func=AF.Square, accum_out=ss)),
                      reads=[("xt", s)], writes=[("hn", s), ("st", s)])
                sc.op("dve", ("tensor_scalar", dict(out=rs, in0=ss, scalar1=1.0 / D, scalar2=1e-6, op0=ALU.mult, op1=ALU.add)),
                      reads=[("st", s)], writes=[("st", s)])
                sc.op("act", ("activation", dict(out=rs, in_=rs, func=AF.Sqrt)), reads=[("st", s)], writes=[("st", s)])
                sc.op("dve", ("reciprocal", dict(out=rs, in_=rs)), reads=[("st", s)], writes=[("st", s)])
                sc.op("dve", ("scalar_tensor_tensor", dict(out=xt[:, s, :], in0=xt[:, s, :], scalar=rs, in1=gbc[:, 0, :],
                                                                         op0=ALU.mult, op1=ALU.mult)),
                      reads=[("xt", s), ("st", s), ("gbc", 0)], writes=[("xt", s)])
            sc.dma(("o", s), ("dma_start", dict(out=dst_d[si, ts, :], in_=xt[:, s, :])), reads=[("xt", s)], writes=[("dram", si, t)])

    enabled = set(getattr(_build, "enabled", ("A", "B", "C")))

    def program():
        sc.dma("c0", ("dma_start", dict(out=cf[:], in_=cf_d[:, :])), writes=["cf"])
        sc.dma("c1", ("dma_start", dict(out=cb[:], in_=cb_d[:, :])), writes=["cb"])
        for si in range(nseq):
            for li_, layer in enumerate(layers_all):
                first = (li_ == 0)
                last = (li_ == len(layers_all) - 1)
                src = x_d if first else xs_d
                dst = out_d if last else xs_d
                sc.phase = "norm"
                phase_norm(si, layer, src)
                if "A" in enabled:
                    sc.barrier()
                    sc.phase = "A"
                    mixer_A(layer)
                if "B" in enabled:
                    sc.barrier()
                    sc.phase = "B"
                    mixer_B(layer)
                if "C" in enabled:
                    sc.barrier()
                    sc.phase = "C"
                    mixer_C(layer)
                sc.barrier()
                sc.phase = "final"
                for name, shape, dt in taps:
                    if name == f"yT{layer}" and si == 0:
                        sc.dma(("tap", name), ("dma_start", dict(out=tap_d[name][:, :, :], in_=yT[:])),
                               reads=[("yT", c, c4) for c in range(8) for c4 in range(4)])
                    if name == f"hT{layer}" and si == 0:
                        sc.dma(("tap", name), ("dma_start", dict(out=tap_d[name][:, :, :], in_=hT[:])),
                               reads=[("hT", c4) for c4 in range(4)])
                phase_final(si, layer, src, dst, last and final_norm)
                sc.barrier()
        sc.final_wait("sp", [("o", 0), ("o", 1)] + [("tap", n) for n, _, _ in taps])


    program()
    sc = Sched()
    wstate.update(n=0, rec=False, issued=0)
    ppp["i"] = 0
    rtc["i"] = 0
    qsc["i"] = 0
    program()

    waited = {e: set() for e in ("pe", "act", "dve", "pool")}
    for e in Sched.ENG:
        for waits, fn, inc, ph in sc.q[e]:
            for de, dc in waits:
                if de in waited:
                    waited[de].add(dc)
    remap = {e: {c: i + 1 for i, c in enumerate(sorted(waited[e]))} for e in waited}
    for e in waited:
        c = 0
        newq = []
        for waits, fn, inc, ph in sc.q[e]:
            if inc is not None and inc[0] == "E":
                c += 1
                if c not in remap[e]:
                    inc = None
            newq.append((waits, fn, inc, ph))
        sc.q[e] = newq
    for e in Sched.ENG:
        sc.q[e] = [([(de, remap[de][dc]) if de in remap else (de, dc) for de, dc in waits], fn, inc, ph)
                   for waits, fn, inc, ph in sc.q[e]]

    sems = {}
    for e in Sched.ENG:
        sems[e] = es.enter_context(nc.semaphore("s_" + e))
    for de in sc.dcnt:
        sems[de] = es.enter_context(nc.semaphore("d%d" % len(sems)))
    block = es.enter_context(nc.Block())

    def replay(engname):
        def run(eng):
            for waits, fn, inc, ph in sc.q[engname]:
                for de, dc in waits:
                    eng.wait_ge(sems[de], dc)
                if fn is None:
                    continue
                ins = getattr(eng, fn[0])(**fn[1])
                if ANNOTATE:
                    ins.annotate(ph)
                if inc is not None:
                    if inc[0] == "E":
                        ins.then_inc(sems[inc[1]], 1)
                    else:
                        ins.then_inc(sems[inc[1]], 16)
        return run
    block.tensor(replay("pe"))
    block.scalar(replay("act"))
    block.vector(replay("dve"))
    block.gpsimd(replay("pool"))
    block.sync(replay("sp"))
    es.close()
    return nc


_CACHE = {}


def kernel(x, norm_g, w_in, w_br_a, w_br_b, w_br_c, w_out, final_norm_g):
    x = np.ascontiguousarray(np.asarray(x, np.float32))
    ncores = 8
    nseq = x.shape[0] // ncores
    wt = _pack_weights(np.asarray(w_in, np.float32), np.asarray(w_br_a, np.float32), np.asarray(w_br_b, np.float32),
                       np.asarray(w_br_c, np.float32), np.asarray(w_out, np.float32))
    gv = np.concatenate([np.asarray(norm_g, np.float32), np.asarray(final_norm_g, np.float32)[None, :]], axis=0)
    cf, cb = _consts()
    nc = _build(nseq)
    in_maps = [{"x": x[i * nseq:(i + 1) * nseq], "wt": wt, "gv": gv, "cf": cf, "cb": cb} for i in range(ncores)]
    res = run_bass_kernel_spmd(nc, in_maps, core_ids=list(range(ncores)))
    return np.concatenate([r["out"] for r in res.results], axis=0)
```

```python
import numpy as np
import ml_dtypes
import concourse.bass as bass
import concourse.mybir as mybir
from concourse.bass_utils import run_bass_kernel_spmd

F32 = mybir.dt.float32
BF16 = mybir.dt.bfloat16
AF = mybir.ActivationFunctionType
ALU = mybir.AluOpType
AX = mybir.AxisListType

S = 2048
D = 1024
NT = 16
NEG = -30000.0
NBIS = 12
ANNOTATE = False

_splits = (384, 384, 384, 384, 384, 64, 64, 384, 256, 64, 4, 768, 768, 768, 256, 3072)
_names = ("a_q", "a_k", "a_v", "a_g", "b_q", "b_k", "b_v", "b_g", "i_q", "i_k", "i_w",
          "c_q", "c_k", "c_v", "c_g", "m_g")
_off = {}
_o = 0
for _n, _s in zip(_names, _splits):
    _off[_n] = _o
    _o += _s


def _rot(cols):
    cols = np.asarray(cols).reshape(-1, 2, 32)
    return cols[:, ::-1, :].reshape(-1)


def _tile_cols():
    t = []
    rng = lambda n, a, b: np.arange(_off[n] + a, _off[n] + b)
    for p in range(3):
        c = rng("a_q", 128 * p, 128 * p + 128); t.append((f"AQ{p}", c)); t.append((f"AQR{p}", _rot(c)))
        c = rng("a_k", 128 * p, 128 * p + 128); t.append((f"AK{p}", c)); t.append((f"AKR{p}", _rot(c)))
        t.append((f"AG{p}", rng("a_g", 128 * p, 128 * p + 128)))
        t.append((f"AV{p}", rng("a_v", 128 * p, 128 * p + 128)))
    for p in range(3):
        c = rng("b_q", 128 * p, 128 * p + 128); t.append((f"BQ{p}", c)); t.append((f"BQR{p}", _rot(c)))
        t.append((f"BG{p}", rng("b_g", 128 * p, 128 * p + 128)))
    c = np.concatenate([rng("b_k", 0, 64), rng("b_k", 0, 64)]); t.append(("BK", c)); t.append(("BKR", _rot(c)))
    c = np.concatenate([rng("i_k", 0, 64), rng("i_k", 0, 64)]); t.append(("IK", c)); t.append(("IKR", _rot(c)))
    for p in range(2):
        c = rng("i_q", 128 * p, 128 * p + 128); t.append((f"IQ{p}", c)); t.append((f"IQR{p}", _rot(c)))
    c = np.concatenate([rng("b_v", 0, 64), rng("i_w", 0, 4), rng("i_w", 0, 4).repeat(15)]); t.append(("BV", c))
    for g in range(3):
        for p in range(2):
            c = rng("c_q", 256 * g + 128 * p, 256 * g + 128 * p + 128); t.append((f"CQ{g}{p}", c)); t.append((f"CQR{g}{p}", _rot(c)))
            c = rng("c_k", 256 * g + 128 * p, 256 * g + 128 * p + 128); t.append((f"CK{g}{p}", c)); t.append((f"CKR{g}{p}", _rot(c)))
            t.append((f"CV{g}{p}", rng("c_v", 256 * g + 128 * p, 256 * g + 128 * p + 128)))
    for p in range(2):
        t.append((f"CG{p}", rng("c_g", 128 * p, 128 * p + 128)))
    for j in range(24):
        t.append((f"MG{j}", rng("m_g", 128 * j, 128 * j + 128)))
    return t


_TILES = _tile_cols()
_TIDX = {n: i for i, (n, _) in enumerate(_TILES)}
NWT = len(_TILES)
_XIDX = {}
for _i, _n in enumerate([f"WA{c}" for c in range(3)] + [f"WB{c}" for c in range(3)] +
                        [f"WC{c}" for c in range(2)] + [f"WO{c}" for c in range(8)]):
    _XIDX[_n] = NWT + _i
NWALL = NWT + 16


def _pack_weights(w_in, w_br_a, w_br_b, w_br_c, w_out):
    L = w_in.shape[0]
    out = np.empty((L, NWALL, 128, 1024), np.float32)
    allc = np.concatenate([c for _, c in _TILES])
    for l in range(L):
        g = w_in[l][:, allc]
        g = g.reshape(8, 128, NWT, 128).transpose(2, 1, 0, 3)
        out[l, :NWT] = g.reshape(NWT, 128, 1024)
        out[l, NWT:NWT + 3] = w_br_a[l].reshape(3, 128, 1024)
        out[l, NWT + 3:NWT + 6] = w_br_b[l].reshape(3, 128, 1024)
        out[l, NWT + 6:NWT + 8] = w_br_c[l].reshape(2, 128, 1024)
        out[l, NWT + 8:NWT + 16] = w_out[l].reshape(8, 128, 1024)
    return out


CF_COS = 0
CF_SIN = CF_COS + S
CF_NEGTRI = CF_SIN + S
CF_AGM = CF_NEGTRI + 128
CF_AVAL = CF_AGM + 128
CF_AOWN = CF_AVAL + 128
CF_POW2 = CF_AOWN + 128
CF_N = CF_POW2 + 32
CB_ID = 0
CB_MOWN = CB_ID + 128
CB_MPREV = CB_MOWN + 128
CB_ONEHOT = CB_MPREV + 128
CB_ONES = CB_ONEHOT + S
CB_PERM = CB_ONES + 128
CB_N = CB_PERM + 128


def _consts():
    cf = np.zeros((128, CF_N), np.float32)
    inv = 1.0 / (10000.0 ** (np.arange(0, 64, 2, dtype=np.float32) / 64.0))
    ang = np.arange(S, dtype=np.float32)[None, :] * inv[:, None].astype(np.float32)
    cos = np.cos(ang).astype(np.float32)
    sin = np.sin(ang).astype(np.float32)
    for p in range(128):
        j = p % 32
        cf[p, CF_COS:CF_COS + S] = cos[j]
        cf[p, CF_SIN:CF_SIN + S] = sin[j] * (-1.0 if (p % 64) < 32 else 1.0)
    t = np.arange(128)[:, None]
    s_ = np.arange(128)[None, :]
    cf[:, CF_NEGTRI:CF_NEGTRI + 128] = np.where(s_ <= t, 0.0, -1e30)
    for qt in range(16):
        own = qt // 2
        for n in range(8):
            cf[:, CF_AGM + qt * 8 + n] = 0.0 if n < own else -1e30
            cf[:, CF_AVAL + qt * 8 + n] = 1.0 if n < own else 0.0
            cf[:, CF_AOWN + qt * 8 + n] = 0.0 if n == own else NEG
    for k in range(32):
        cf[:, CF_POW2 + k] = 2.0 ** (-k)
    cb = np.zeros((128, CB_N), np.float32)
    cb[:, CB_ID:CB_ID + 128] = np.eye(128)
    cb[:, CB_MOWN:CB_MOWN + 128] = (t <= s_)
    cb[:, CB_MPREV:CB_MPREV + 128] = (t >= s_)
    for n in range(8):
        cb[n, CB_ONEHOT + 256 * n:CB_ONEHOT + 256 * n + 256] = 1.0
    cb[:, CB_ONES:CB_ONES + 128] = 1.0
    for po in range(128):
        cb[(po // 64) * 64 + ((po % 64) + 32) % 64, CB_PERM + po] = 1.0
    return cf, cb.astype(ml_dtypes.bfloat16)


class Sched:
    ENG = ("pe", "act", "dve", "pool", "sp")

    def __init__(self):
        self.q = {e: [] for e in self.ENG}
        self.cnt = {e: 0 for e in self.ENG}
        self.seen = {e: {} for e in self.ENG}
        self.lastw = {}
        self.readers = {}
        self.dcnt = {}
        self.phase = "init"

    def _need(self, eng, reads, writes):
        need = {}

        def add(dep, raw):
            de, dc = dep
            if de == eng and (eng == "pe" or eng == "sp"):
                return
            if need.get(de, 0) < dc:
                need[de] = dc
        for r in reads:
            w = self.lastw.get(r)
            if w is not None:
                add(w, True)
        for w_ in writes:
            w = self.lastw.get(w_)
            if w is not None:
                add(w, False)
            for de, dc in self.readers.get(w_, {}).items():
                add((de, dc), False)
        waits = []
        for de, dc in need.items():
            if self.seen[eng].get(de, 0) < dc:
                self.seen[eng][de] = dc
                waits.append((de, dc))
        return waits

    def _record(self, tag, reads, writes):
        for r in reads:
            d = self.readers.setdefault(r, {})
            if d.get(tag[0], 0) < tag[1]:
                d[tag[0]] = tag[1]
        for w_ in writes:
            self.lastw[w_] = tag
            self.readers[w_] = {}

    def op(self, eng, fn, reads=(), writes=(), inc=True):
        waits = self._need(eng, reads, writes)
        tag = (eng, self.cnt[eng] + 1)
        if inc:
            self.cnt[eng] += 1
        self.q[eng].append((waits, fn, ("E", eng) if inc else None, self.phase))
        self._record(tag, reads, writes)

    def dma(self, key, fn, reads=(), writes=(), eng="sp"):
        waits = self._need(eng, reads, writes)
        de = ("dma", key)
        self.dcnt[de] = self.dcnt.get(de, 0) + 16
        self.q[eng].append((waits, fn, ("D", de), self.phase))
        self._record((de, self.dcnt[de]), reads, writes)

    def barrier(self):
        tgt = {e: self.cnt[e] for e in ("pe", "act", "dve", "pool")}
        tgt.update(self.dcnt)
        for e in self.ENG:
            waits = []
            for de, dc in tgt.items():
                if de == e or dc == 0:
                    continue
                if self.seen[e].get(de, 0) < dc:
                    self.seen[e][de] = dc
                    waits.append((de, dc))
            if waits:
                self.q[e].append((waits, None, None, None))

    def final_wait(self, eng, dma_keys):
        waits = [(("dma", k), self.dcnt[("dma", k)]) for k in dma_keys if ("dma", k) in self.dcnt]
        self.q[eng].append((waits, None, None, None))


def _build(nseq, layers_all=(0, 1), final_norm=True, taps=()):
    nc = bass.Bass("TRN2", target_bir_lowering=False)
    x_d = nc.dram_tensor("x", [nseq, S, D], F32, kind="ExternalInput").ap()
    wt_d = nc.dram_tensor("wt", [2, NWALL, 128, 1024], F32, kind="ExternalInput").ap()
    gv_d = nc.dram_tensor("gv", [3, D], F32, kind="ExternalInput").ap()
    cf_d = nc.dram_tensor("cf", [128, CF_N], F32, kind="ExternalInput").ap()
    cb_d = nc.dram_tensor("cb", [128, CB_N], BF16, kind="ExternalInput").ap()
    out_d = nc.dram_tensor("out", [nseq, S, D], F32, kind="ExternalOutput").ap()
    xs_d = nc.dram_tensor("xscr", [nseq, S, D], F32).ap()
    tap_d = {}
    for name, shape, dt in taps:
        tap_d[name] = nc.dram_tensor("tap_" + name, list(shape), dt, kind="ExternalOutput").ap()

    sc = Sched()
    sb = {}

    def alloc(name, shape, dt):
        t = nc.alloc_sbuf_tensor(name, list(shape), dt) if False else None
        return t

    from contextlib import ExitStack
    es = ExitStack()

    def SB(name, shape, dt):
        t = es.enter_context(nc.sbuf_tensor("sb_" + name, list(shape), dt))
        sb[name] = t
        return t

    def PS(name, shape, dt):
        return es.enter_context(nc.psum_tensor(name, list(shape), dt))

    cf = SB("cf", [128, CF_N], F32)
    cb = SB("cb", [128, CB_N], BF16)
    hT = SB("hT", [128, 8, S], BF16)
    yT = SB("yT", [128, 8, S], BF16)
    gbc = SB("gbc", [128, 2, D], F32)
    wbf = SB("wbf", [128, 8, 1024], BF16)
    xt = SB("xt", [128, 2, D], F32)
    hn = SB("hn", [128, 2, D], BF16)
    st = SB("st", [128, 64], F32)
    scr = SB("scr", [128, 36864], BF16)
    ps = [PS(f"ps{i}", [128, 512], F32) for i in range(8)]

    def scr_view(off_bytes, shape, dt):
        n = int(np.prod(shape))
        if dt == F32:
            assert off_bytes % 4 == 0
            v = scr[:, off_bytes // 2: off_bytes // 2 + 2 * n].bitcast(F32)
        else:
            v = scr[:, off_bytes // 2: off_bytes // 2 + n]
        if len(shape) == 2:
            v = v.rearrange("p (a b) -> p a b", b=shape[1])
        elif len(shape) == 3:
            v = v.rearrange("p (a b c) -> p a b c", b=shape[1], c=shape[2])
        return v

    ident = cb[:, CB_ID:CB_ID + 128]

    def mm(out, lhsT, rhs, start, stop, reads, writes, inc=None):
        if inc is None:
            inc = stop
        sc.op("pe", ("matmul", dict(out=out, lhsT=lhsT, rhs=rhs, start=start, stop=stop, skip_group_check=True)),
              reads=reads, writes=writes, inc=inc)

    def tr(out, in_, reads, writes, inc=True):
        sc.op("pe", ("transpose", dict(out=out, in_=in_, identity=ident[:in_.shape[0], :in_.shape[0]])), reads=reads, writes=writes, inc=inc)


    NSLOT = 8
    WDEPTH = 4
    wstate = {"n": 0, "ring": 0, "rec": True, "issued": 0, "xd": 0}
    wplan = []

    def _issue_w(ent):
        layer, idx, dest, dest_key, skey = ent
        sc.dma(skey, ("dma_start", dict(out=dest, in_=wt_d[layer, idx, :, :])), writes=[dest_key], eng="pool")

    def load_w(layer, idx, dest=None, dest_key=None):
        n = wstate["n"]; wstate["n"] += 1
        if wstate["rec"]:
            if dest is None:
                b = wstate["ring"] % NSLOT; wstate["ring"] += 1
                dest = wbf[:, b, :]
                dest_key = ("wbf", b)
                skey = ("w", b)
            else:
                skey = ("wx", wstate["xd"] % 16); wstate["xd"] += 1
            wplan.append((layer, idx, dest, dest_key, skey))
            return dest, dest_key
        while wstate["issued"] < len(wplan) and wstate["issued"] <= n + WDEPTH:
            ent = wplan[wstate["issued"]]
            if ent[4][0] == "wx" and wstate["issued"] > n:
                break
            _issue_w(ent)
            wstate["issued"] += 1
        ent = wplan[n]
        return ent[2], ent[3]

    def load_g(slot, row):
        src = gv_d[row:row + 1, :].partition_broadcast(128) if False else None
        from concourse.ap import AP
        src = AP(gv_d.tensor, row * D, [[0, 128], [1, D]])
        sc.dma(("g", slot), ("dma_start", dict(out=gbc[:, slot, :], in_=src)), writes=[("gbc", slot)])

    def phase_norm(si, layer, src_d):
        load_g(layer % 2, layer)
        pst = ps[7][:, 0:512].bitcast(BF16)
        def stats(t):
            s = t % 2
            sc.dma(("x", s), ("dma_start", dict(out=xt[:, s, :], in_=src_d[si, t * 128:(t + 1) * 128, :])),
                   reads=[("dram", si, t)], writes=[("xt", s)])
            ss = st[:, 2 * s:2 * s + 1]
            rs = st[:, 2 * s + 1:2 * s + 2]
            sc.op("act", ("activation", dict(out=hn[:, s, :], in_=xt[:, s, :], func=AF.Square, accum_out=ss)),
                  reads=[("xt", s)], writes=[("hn", s), ("st", s)])
            sc.op("dve", ("tensor_scalar", dict(out=rs, in0=ss, scalar1=1.0 / D, scalar2=1e-6, op0=ALU.mult, op1=ALU.add)),
                  reads=[("st", s)], writes=[("st", s)])
            sc.op("act", ("activation", dict(out=rs, in_=rs, func=AF.Sqrt)), reads=[("st", s)], writes=[("st", s)])
            sc.op("dve", ("reciprocal", dict(out=rs, in_=rs)), reads=[("st", s)], writes=[("st", s)])
            sc.op("dve", ("scalar_tensor_tensor", dict(out=hn[:, s, :], in0=xt[:, s, :], scalar=rs, in1=gbc[:, layer % 2, :],
                                                                     op0=ALU.mult, op1=ALU.mult)),
                  reads=[("xt", s), ("st", s), ("gbc", layer % 2)], writes=[("hn", s)])

        def trans(t):
            s = t % 2
            for c in range(8):
                tr(pst[:, c * 128:(c + 1) * 128], hn[:, s, c * 128:(c + 1) * 128], reads=[("hn", s), "cb"], writes=[("ps", 7)], inc=(c == 7))
            sc.op("act", ("copy", dict(out=hT[:, :, t * 128:(t + 1) * 128], in_=pst.rearrange("p (c n) -> p c n", n=128))),
                  reads=[("ps", 7)], writes=[("hT", t // 4)])
        stats(0)
        for t in range(NT):
            if t + 1 < NT:
                stats(t + 1)
            trans(t)

    def run(units):
        for u in units:
            u()

    def run_merged(*lists):
        lists = [l for l in lists if l]
        idx = [0] * len(lists)
        while True:
            cand = [(idx[i] / len(l), i) for i, l in enumerate(lists) if idx[i] < len(l)]
            if not cand:
                break
            i = min(cand)[1]
            lists[i][idx[i]]()
            idx[i] += 1

    def skew(units, items, fa, fb, others):
        pend = None
        for it in items:
            if it[0] == "t":
                st_ = {}
                a_ = (lambda it=it, st_=st_: fa(st_, *it[1:]))
                if pend is None:
                    units.append(a_)
                else:
                    units.append(lambda a_=a_, b_=pend: (a_(), b_()))
                pend = (lambda it=it, st_=st_: fb(st_, *it[1:]))
            else:
                if pend is not None:
                    units.append(pend)
                    pend = None
                units.append(lambda it=it: others[it[0]](*it[1:]))
        if pend is not None:
            units.append(pend)

    ppp = {"i": 0}

    qsb = SB("qsb", [128, 2, 512], BF16)
    qsc = {"i": 0}

    def proj_fm(layer, name, rope, evac, banks=((0, 1), (2, 3))):
        stt = {"pend": None}

        def rot_evac(c4, j):
            i = ppp["i"]; ppp["i"] += 1
            b2 = banks[1][i % len(banks[1])]
            mm(ps[b2][:, :], cb[:, CB_PERM:CB_PERM + 128], qsb[:, j, :], True, True, reads=[("qsb", j), "cb"], writes=[("ps", b2)])
            evac(c4, j, b2)

        def u(c4):
            if c4 == 0:
                w1, k1 = load_w(layer, _TIDX[name])
                stt["w1"] = (w1.rearrange("p (c n) -> p c n", n=128), k1)
            w1, k1 = stt["w1"]
            i = ppp["i"]; ppp["i"] += 1
            b1 = banks[0][i % len(banks[0])]
            for c in range(8):
                mm(ps[b1][:, :], w1[:, c, :], hT[:, c, c4 * 512:(c4 + 1) * 512], c == 0, c == 7,
                   reads=[k1, ("hT", c4)], writes=[("ps", b1)])
            if rope:
                j = qsc["i"] % 2; qsc["i"] += 1
                sc.op("act", ("copy", dict(out=qsb[:, j, :], in_=ps[b1][:, :])), reads=[("ps", b1)], writes=[("qsb", j)])
                if stt["pend"] is not None:
                    rot_evac(*stt["pend"])
                stt["pend"] = (c4, j)
                if c4 == 3:
                    rot_evac(*stt["pend"])
                    stt["pend"] = None
            else:
                evac(c4, b1)
        return [(lambda c4=c4: u(c4)) for c4 in range(4)]

    rt = SB("rt", [128, 2, 2, 512], F32)
    rtc = {"i": 0}

    def rope_evac(dst_fn, dst_keys_fn, split_heads):
        def ev(c4, jq, b2):
            j = rtc["i"] % 2; rtc["i"] += 1
            t1 = rt[:, j, 0, :]
            t2 = rt[:, j, 1, :]
            cs = cf[:, CF_COS + c4 * 512:CF_COS + (c4 + 1) * 512]
            sn = cf[:, CF_SIN + c4 * 512:CF_SIN + (c4 + 1) * 512]
            sc.op("dve", ("tensor_tensor", dict(out=t1, in0=qsb[:, jq, :], in1=cs, op=ALU.mult)),
                  reads=[("qsb", jq), "cf"], writes=[("rt", j, 0)])
            sc.op("dve", ("tensor_tensor", dict(out=t2, in0=ps[b2][:, :], in1=sn, op=ALU.mult)),
                  reads=[("ps", b2), "cf"], writes=[("rt", j, 1)])
            if split_heads:
                for h in range(2):
                    d = dst_fn(c4, h)
                    sc.op("pool", ("tensor_tensor", dict(out=d, in0=t1[64 * h:64 * h + 64, :], in1=t2[64 * h:64 * h + 64, :], op=ALU.add)),
                          reads=[("rt", j, 0), ("rt", j, 1)], writes=dst_keys_fn(c4, h))
            else:
                d = dst_fn(c4, None)
                sc.op("pool", ("tensor_tensor", dict(out=d, in0=t1, in1=t2, op=ALU.add)),
                      reads=[("rt", j, 0), ("rt", j, 1)], writes=dst_keys_fn(c4, None))
        return ev

    def silu_evac(ychunk):
        def ev(c4, b1):
            sc.op("act", ("activation", dict(out=yT[:, ychunk, c4 * 512:(c4 + 1) * 512], in_=ps[b1][:, :], func=AF.Silu)),
                  reads=[("ps", b1)], writes=[("yT", ychunk, c4)])
        return ev

    def proj_tm(layer, name, nblk, tok_fn, evac, banks=(0, 1)):
        stt = {}

        def u(g4):
            if g4 == 0:
                w1, k1 = load_w(layer, _TIDX[name])
                stt["w1"] = (w1.rearrange("p (c n) -> p c n", n=128), k1)
            w1, k1 = stt["w1"]
            i = ppp["i"]; ppp["i"] += 1
            b1 = banks[i % len(banks)]
            for j in range(4):
                blk = g4 + j
                for c in range(8):
                    mm(ps[b1][:, j * 128:(j + 1) * 128], tok_fn(c, blk), w1[:, c, :], (c == 0 and j == 0), c == 7,
                       reads=[k1] + [("hT", q) for q in range(4)], writes=[("ps", b1)], inc=(c == 7 and j == 3))
            evac(g4, b1)
        return [(lambda g4=g4: u(g4)) for g4 in range(0, nblk, 4)]

    def mixer_A(layer):
        QA = [scr_view(0, [2, S], BF16), scr_view(8192, [2, S], BF16)]
        KA = [scr_view(16384, [2, S], BF16), scr_view(24576, [2, S], BF16)]
        VA = [scr_view(32768, [16, 256], BF16), scr_view(40960, [16, 256], BF16)]
        o0 = 49152
        km = scr_view(o0, [2, 8], BF16)
        kmf = scr_view(o0 + 64, [2, 8], F32)
        gt = scr_view(o0 + 256, [128], F32)
        cmp_ = scr_view(o0 + 1024, [128, 8], F32)
        rk = scr_view(o0 + 1024 + 4096, [128], F32)
        nb = scr_view(o0 + 1024 + 4096 + 512, [128], BF16)
        et = scr_view(57344, [2, 512], BF16)
        rd = scr_view(57344 + 2048, [512], F32)
        rn = scr_view(57344 + 4096, [512], F32)
        est = {"i": 0}

        def proj_units(p):
            bs_ = p % 2
            qa, ka, va = QA[bs_], KA[bs_], VA[bs_]
            units = []
            if p < 2:
                def init():
                    for e_ in range(2):
                        sc.op("pool", ("tensor_copy", dict(out=ka[64:72, e_, :], in_=cb[0:8, CB_ONEHOT:CB_ONEHOT + S])),
                              reads=["cb"], writes=[("ka", bs_, e_, c4) for c4 in range(4)])
                    for h in range(2):
                        sc.op("pool", ("tensor_copy", dict(out=va[:, :, 128 * h + 64:128 * h + 128],
                                                           in_=cb[:, CB_ONES:CB_ONES + 64].unsqueeze(1).to_broadcast([128, 16, 64]))),
                              reads=["cb"], writes=[("va1", bs_, h)])
                units.append(init)
            units += proj_fm(layer, f"AQ{p}", True, rope_evac(lambda c4, h: qa[0:64, h, c4 * 512:(c4 + 1) * 512],
                                                              lambda c4, h: [("qa", bs_, h, c4)], True))
            units += proj_fm(layer, f"AK{p}", True, rope_evac(lambda c4, h: ka[0:64, h, c4 * 512:(c4 + 1) * 512],
                                                              lambda c4, h: [("ka", bs_, h, c4)], True))
            units += proj_fm(layer, f"AG{p}", False, silu_evac(p))

            def vev(g4, b1):
                for h in range(2):
                    sc.op("act", ("copy", dict(out=va[:, g4:g4 + 4, 128 * h:128 * h + 64],
                                               in_=ps[b1][:, :].rearrange("p (j n) -> p j n", n=128)[:, :, 64 * h:64 * h + 64])),
                          reads=[("ps", b1)], writes=[("va", bs_, h, g4)])
            units += proj_tm(layer, f"AV{p}", 16, lambda c, blk: hT[:, c, blk * 128:(blk + 1) * 128], vev)
            return units

        def attn_units(p):
            bs_ = p % 2
            qa, ka, va = QA[bs_], KA[bs_], VA[bs_]
            units = []

            def gate(h):
                allk = [("ka", bs_, h, c4) for c4 in range(4)]
                allq = [("qa", bs_, h, c4) for c4 in range(4)]
                sc.op("dve", ("tensor_reduce", dict(out=kmf[0:64, h, :], in_=ka[0:64, h, :].rearrange("p (n b) -> p n b", b=256),
                                                    axis=AX.X, op=ALU.add)), reads=allk, writes=[("kmf", h)])
                sc.op("dve", ("tensor_copy", dict(out=km[0:64, h, :], in_=kmf[0:64, h, :])), reads=[("kmf", h)], writes=[("km", h)])
                gp = ps[4][:, 0:128]
                for qt in range(16):
                    mm(gp[:, qt * 8:(qt + 1) * 8], qa[0:64, h, qt * 128:(qt + 1) * 128], km[0:64, h, :], True, True,
                       reads=allq + [("km", h)], writes=[("ps", 4)], inc=(qt == 15))
                sc.op("dve", ("tensor_tensor", dict(out=gt, in0=gp, in1=cf[:, CF_AGM:CF_AGM + 128], op=ALU.add)),
                      reads=[("ps", 4), "cf"], writes=["gt"])
                g3 = gt.rearrange("p (q n) -> p q n", n=8)
                sc.op("dve", ("tensor_tensor", dict(out=cmp_.rearrange("p (q n) m -> p q n m", n=8),
                                                    in0=g3.unsqueeze(2).to_broadcast([128, 16, 8, 8]),
                                                    in1=g3.unsqueeze(3).to_broadcast([128, 16, 8, 8]), op=ALU.is_gt)),
                      reads=["gt"], writes=["cmp"])
                sc.op("dve", ("tensor_reduce", dict(out=rk, in_=cmp_, axis=AX.X, op=ALU.add)), reads=["cmp"], writes=["rk"])
                sc.op("dve", ("tensor_scalar", dict(out=rk, in0=rk, scalar1=2.5, scalar2=None, op0=ALU.is_lt)), reads=["rk"], writes=["rk"])
                sc.op("dve", ("tensor_tensor", dict(out=rk, in0=rk, in1=cf[:, CF_AVAL:CF_AVAL + 128], op=ALU.mult)), reads=["rk", "cf"], writes=["rk"])
                sc.op("dve", ("scalar_tensor_tensor", dict(out=rk, in0=rk, scalar=-NEG, in1=cf[:, CF_AOWN:CF_AOWN + 128],
                                                           op0=ALU.mult, op1=ALU.add)), reads=["rk", "cf"], writes=["rk"])
                sc.op("dve", ("tensor_copy", dict(out=nb, in_=rk)), reads=["rk"], writes=["nb"])
                tp = ps[5][:, 0:512].bitcast(BF16)
                for half in range(2):
                    for q8 in range(8):
                        qt = half * 8 + q8
                        tr(tp[0:8, q8 * 128:(q8 + 1) * 128], nb[:, qt * 8:(qt + 1) * 8], reads=["nb", "cb"], writes=[("ps", 5)], inc=(q8 == 7))
                    sc.op("act", ("copy", dict(out=qa[64:72, h, half * 1024:(half + 1) * 1024], in_=tp[0:8, :])),
                          reads=[("ps", 5)], writes=[("qa", bs_, h, 2 * half), ("qa", bs_, h, 2 * half + 1)])

            def tile_a(st_, h, c4, kt, nkt):
                q0 = max(kt * 128, c4 * 512)
                q1 = (c4 + 1) * 512
                n = q1 - q0
                ei = est["i"]; est["i"] += 1
                sb_ = 4 + (ei % 2)
                ej = ei % 2
                st_.update(q0=q0, n=n, ej=ej)
                mm(ps[sb_][:, 0:n], ka[0:72, h, kt * 128:(kt + 1) * 128], qa[0:72, h, q0:q1], True, True,
                   reads=[("ka", bs_, h, kt // 4), ("qa", bs_, h, c4)], writes=[("ps", sb_)])
                sc.op("act", ("activation", dict(out=et[:, ej, 0:n], in_=ps[sb_][:, 0:n], func=AF.Exp, scale=0.125)),
                      reads=[("ps", sb_)], writes=[("et", ej)])
                if q0 == kt * 128:
                    sc.op("dve", ("tensor_tensor", dict(out=et[:, ej, 0:128], in0=et[:, ej, 0:128],
                                                        in1=cb[:, CB_MOWN:CB_MOWN + 128], op=ALU.mult)),
                          reads=[("et", ej), "cb"], writes=[("et", ej)])

            def tile_b(st_, h, c4, kt, nkt):
                ob = 6 + (c4 % 2)
                q0, n, ej = st_["q0"], st_["n"], st_["ej"]
                mm(ps[ob][:, q0 - c4 * 512:512], va[:, kt, 128 * h:128 * h + 128], et[:, ej, 0:n], kt == 0, kt == nkt - 1,
                   reads=[("va", bs_, h, (kt // 4) * 4), ("va1", bs_, h), ("et", ej)], writes=[("ps", ob)], inc=True)

            def norm(h, c4):
                ob = 6 + (c4 % 2)
                sc.op("act", ("activation", dict(out=rd[64:128, :], in_=ps[ob][64:128, :], func=AF.Ln)), reads=[("ps", ob)], writes=["rd"])
                sc.op("act", ("activation", dict(out=rd[64:128, :], in_=rd[64:128, :], func=AF.Exp, scale=-1.0)), reads=["rd"], writes=["rd"])
                sc.op("dve", ("tensor_tensor", dict(out=rn[64 * h:64 * h + 64, :], in0=ps[ob][0:64, :], in1=rd[64:128, :], op=ALU.mult)),
                      reads=[("ps", ob), "rd"], writes=["rd0"])
                ydst = yT[64 * h:64 * h + 64, p, c4 * 512:(c4 + 1) * 512]
                sc.op("pool", ("tensor_tensor", dict(out=ydst, in0=ydst, in1=rn[64 * h:64 * h + 64, :], op=ALU.mult)),
                      reads=["rd0", ("yT", p, c4)], writes=[("yT", p, c4)])

            items = []
            for h in range(2):
                items.append(("g", h))
                for c4 in range(4):
                    nkt = 4 * c4 + 4
                    for kt in range(nkt):
                        items.append(("t", h, c4, kt, nkt))
                    items.append(("n", h, c4))
            skew(units, items, tile_a, tile_b, {"g": gate, "n": norm})
            return units

        run(proj_units(0))
        for p in range(3):
            run_merged(attn_units(p), proj_units(p + 1) if p < 2 else [])

    def mixer_B(layer):
        qb = scr_view(0, [3, S], BF16)
        kb = scr_view(12288, [S], BF16)
        ikb = scr_view(16384, [S], BF16)
        iqb = scr_view(20480, [2, S], BF16)
        vb = scr_view(28672, [16, 128], BF16)
        iw = scr_view(32768, [16, 4], F32)
        acc = scr_view(33024, [2, S], F32)
        tmp = scr_view(49408, [2, 512], F32)
        mk = scr_view(53504, [S], BF16)
        mt = scr_view(57600, [2, 16, 128], BF16)
        et = scr_view(65792, [2, 384], BF16)
        pt = scr_view(67328, [2, 384], BF16)
        rd = scr_view(68864, [384], F32)
        bs = scr_view(70400, [64], F32)
        rn = scr_view(70656, [384], F32)
        sc.op("pool", ("tensor_copy", dict(out=vb[:, :, 64:128], in_=cb[:, CB_ONES:CB_ONES + 64].unsqueeze(1).to_broadcast([128, 16, 64]))),
              reads=["cb"], writes=["vb1"])
        pu = []
        pu += proj_fm(layer, "IK", True, rope_evac(lambda c4, h: ikb[:, c4 * 512:(c4 + 1) * 512], lambda c4, h: [("ikb", c4)], False))
        for p in range(2):
            pu += proj_fm(layer, f"IQ{p}", True, rope_evac(lambda c4, h, p=p: iqb[:, p, c4 * 512:(c4 + 1) * 512],
                                                           lambda c4, h, p=p: [("iqb", c4)], False))

        def vev(g4, b1):
            v3 = ps[b1][:, :].rearrange("p (j n) -> p j n", n=128)
            sc.op("act", ("copy", dict(out=vb[:, g4:g4 + 4, 0:64], in_=v3[:, :, 0:64])), reads=[("ps", b1)], writes=[("vb", g4)])
            sc.op("act", ("copy", dict(out=iw[:, g4:g4 + 4, :], in_=v3[:, :, 64:68])), reads=[("ps", b1)], writes=["iw"])
        pu += proj_tm(layer, "BV", 16, lambda c, blk: hT[:, c, blk * 128:(blk + 1) * 128], vev)
        run(pu)
        PB = ((6,), (7,))
        pu2 = []
        for p in range(3):
            pu2 += proj_fm(layer, f"BQ{p}", True, rope_evac(lambda c4, h, p=p: qb[:, p, c4 * 512:(c4 + 1) * 512],
                                                            lambda c4, h, p=p: [("qb", c4)], False), banks=PB)
            pu2 += proj_fm(layer, f"BG{p}", False, silu_evac(3 + p), banks=PB)
        pu2 += proj_fm(layer, "BK", True, rope_evac(lambda c4, h: kb[:, c4 * 512:(c4 + 1) * 512], lambda c4, h: [("kb", c4)], False), banks=PB)
        cst = {"li": 0, "ei": 0}
        mkb = [mk, hn[:, :, :].rearrange("p a b -> p (a b)")]
        xtb = xt[:, :, :].rearrange("p a b -> p (a b)").bitcast(BF16).rearrange("p (i k n) -> p i k n", i=2, n=128)
        mtb = [mt[:, 0, :, :], mt[:, 1, :, :], xtb[:, 0, :, :], xtb[:, 1, :, :]]

        def stage1(qt):
            N = 128 * (qt + 1)
            a = qt % 2
            par = qt % 2
            m4 = qt % 4
            qs = slice(qt * 128, (qt + 1) * 128)
            mk_ = mkb[par]
            mt_ = mtb[m4]
            o = 32 * par
            thr = bs[:, o:o + 1]; hi = bs[:, o + 1:o + 2]; lo = bs[:, o + 2:o + 3]; cnt = bs[:, o + 3:o + 4]
            tt = bs[:, o + 4:o + 5]; nthr = bs[:, o + 5:o + 6]
            W = bs[:, o + 8:o + 8 + NBIS + 2]
            K = lambda nm: (nm, par)
            tpb = ps[2 + par][:, 0:512].bitcast(BF16)
            lb = par
            tj = par
            units = []

            def idx(j, h):
                k0 = j * 512
                n = min(512, N - k0)
                e_ = h % 2
                mm(ps[lb][:, 0:n], iqb[64 * e_:64 * e_ + 64, h // 2, qs], ikb[64 * e_:64 * e_ + 64, k0:k0 + n], True, True,
                   reads=[("iqb", qt // 4), ("ikb", j)], writes=[("ps", lb)])
                if h == 0:
                    sc.op("dve", ("tensor_scalar", dict(out=acc[:, a, k0:k0 + n], in0=ps[lb][:, 0:n], scalar1=0.0,
                                                        scalar2=iw[:, qt, 0:1], op0=ALU.max, op1=ALU.mult)),
                          reads=[("ps", lb), "iw"], writes=[("acc", a)])
                else:
                    sc.op("dve", ("tensor_scalar", dict(out=tmp[:, tj, 0:n], in0=ps[lb][:, 0:n], scalar1=0.0,
                                                        scalar2=iw[:, qt, h:h + 1], op0=ALU.max, op1=ALU.mult)),
                          reads=[("ps", lb), "iw"], writes=[("tmp", tj)])
                    sc.op("pool", ("tensor_tensor", dict(out=acc[:, a, k0:k0 + n], in0=acc[:, a, k0:k0 + n],
                                                         in1=tmp[:, tj, 0:n], op=ALU.add)),
                          reads=[("acc", a), ("tmp", tj)], writes=[("acc", a)])
            for j in range((N + 511) // 512):
                for h in range(4):
                    units.append(lambda j=j, h=h: idx(j, h))

            def bis_init():
                sc.op("pool", ("tensor_tensor", dict(out=acc[:, a, qs], in0=acc[:, a, qs], in1=cf[:, CF_NEGTRI:CF_NEGTRI + 128], op=ALU.add)),
                      reads=[("acc", a), "cf"], writes=[("acc", a)])
                if qt < 2:
                    sc.op("dve", ("memset", dict(ap=thr, constant=-1e29)), writes=[K("thr")])
                    return
                sc.op("dve", ("tensor_reduce", dict(out=hi, in_=acc[:, a, 0:N], axis=AX.X, op=ALU.max)), reads=[("acc", a)], writes=[K("bs_hi")])
                sc.op("dve", ("tensor_reduce", dict(out=lo, in_=acc[:, a, 0:N - 128], axis=AX.X, op=ALU.min)), reads=[("acc", a)], writes=[K("bs_lo")])
                sc.op("dve", ("tensor_tensor", dict(out=tt, in0=hi, in1=lo, op=ALU.subtract)), reads=[K("bs_hi"), K("bs_lo")], writes=[K("bs_tt")])
                sc.op("dve", ("tensor_scalar", dict(out=W, in0=cf[:, CF_POW2:CF_POW2 + NBIS + 2], scalar1=tt, scalar2=None, op0=ALU.mult)),
                      reads=[K("bs_tt"), "cf"], writes=[K("bs_W")])
                sc.op("dve", ("scalar_tensor_tensor", dict(out=nthr, in0=lo, scalar=-1.0, in1=W[:, 1:2], op0=ALU.mult, op1=ALU.subtract)),
                      reads=[K("bs_lo"), K("bs_W")], writes=[K("nthr")])
            units.append(bis_init)

            def bis(k):
                sc.op("act", ("activation", dict(out=mk_[:, 0:N], in_=acc[:, a, 0:N], func=AF.Sign, bias=nthr, scale=1.0, accum_out=cnt)),
                      reads=[("acc", a), K("nthr")], writes=[K("bs_cnt"), K("mk")])
                sc.op("dve", ("tensor_scalar", dict(out=tt, in0=cnt, scalar1=511.0 - N, scalar2=0.5, op0=ALU.is_lt, op1=ALU.subtract)),
                      reads=[K("bs_cnt")], writes=[K("bs_tt")])
                sc.op("dve", ("scalar_tensor_tensor", dict(out=nthr, in0=tt, scalar=W[:, k + 1:k + 2], in1=nthr, op0=ALU.mult, op1=ALU.add)),
                      reads=[K("bs_tt"), K("bs_W"), K("nthr")], writes=[K("nthr")])
            if qt >= 2:
                for k in range(NBIS):
                    units.append(lambda k=k: bis(k))

            def fin():
                if qt >= 2:
                    sc.op("dve", ("scalar_tensor_tensor", dict(out=thr, in0=nthr, scalar=-1.0, in1=W[:, NBIS + 1:NBIS + 2], op0=ALU.mult, op1=ALU.subtract)),
                          reads=[K("nthr"), K("bs_W")], writes=[K("thr")])
                sc.op("dve", ("tensor_scalar", dict(out=mk_[:, 0:N], in0=acc[:, a, 0:N], scalar1=thr, scalar2=None, op0=ALU.is_ge)),
                      reads=[("acc", a), K("thr")], writes=[K("mk")])
                for half in range((qt // 8) + 1):
                    nk = min(8, qt + 1 - 8 * half)
                    for i in range(nk):
                        kt = 8 * half + i
                        tr(tpb[:, i * 128:(i + 1) * 128], mk_[:, kt * 128:(kt + 1) * 128], reads=[K("mk"), "cb"], writes=[("ps", 2 + par)],
                           inc=(i == nk - 1))
                    sc.op("act", ("copy", dict(out=mt_[:, 8 * half:8 * half + nk, :],
                                               in_=tpb[:, 0:nk * 128].rearrange("p (k n) -> p k n", n=128))),
                          reads=[("ps", 2 + par)], writes=[("mt", m4)])
            units.append(fin)
            return units

        def stage2(qt):
            m = qt % 4
            mt_ = mtb[m]
            qs = slice(qt * 128, (qt + 1) * 128)
            units = []

            def tile_a(st_, kt, e_):
                ei = cst["ei"]; cst["ei"] += 1
                sb_ = 4 + (ei % 2)
                ej = ei % 2
                st_.update(ej=ej)
                mm(ps[sb_][:, 0:384], kb[64 * e_:64 * e_ + 64, kt * 128:(kt + 1) * 128], qb[64 * e_:64 * e_ + 64, :, qs], True, True,
                   reads=[("kb", kt // 4), ("qb", qt // 4)], writes=[("ps", sb_)])
                sc.op("act", ("activation", dict(out=et[:, ej, :], in_=ps[sb_][:, 0:384], func=AF.Exp, scale=0.125)),
                      reads=[("ps", sb_)], writes=[("et", ej)])
                sc.op("dve", ("tensor_tensor", dict(out=pt[:, ej, :].rearrange("p (h n) -> p h n", n=128),
                                                    in0=et[:, ej, :].rearrange("p (h n) -> p h n", n=128),
                                                    in1=mt_[:, kt, :].unsqueeze(1).to_broadcast([128, 3, 128]), op=ALU.mult)),
                      reads=[("et", ej), ("mt", m)], writes=[("pt", ej)])

            def tile_b(st_, kt, e_):
                ob = 6 + e_
                ej = st_["ej"]
                mm(ps[ob][:, 0:384], vb[:, kt, :], pt[:, ej, :], kt == 0, kt == qt,
                   reads=[("vb", (kt // 4) * 4), "vb1", ("pt", ej)], writes=[("ps", ob)], inc=True)
            items = [("t", kt, e_) for kt in range(qt + 1) for e_ in range(2)]

            def norm(e_):
                ob = 6 + e_
                sc.op("act", ("activation", dict(out=rd[64:128, :], in_=ps[ob][64:128, 0:384], func=AF.Ln)), reads=[("ps", ob)], writes=["rd"])
                sc.op("act", ("activation", dict(out=rd[64:128, :], in_=rd[64:128, :], func=AF.Exp, scale=-1.0)), reads=["rd"], writes=["rd"])
                sc.op("dve", ("tensor_tensor", dict(out=rn[64 * e_:64 * e_ + 64, :], in0=ps[ob][0:64, 0:384], in1=rd[64:128, :], op=ALU.mult)),
                      reads=[("ps", ob), "rd"], writes=["rd0"])
                ydst = yT[64 * e_:64 * e_ + 64, 3:6, qs]
                sc.op("pool", ("tensor_tensor", dict(out=ydst, in0=ydst, in1=rn[64 * e_:64 * e_ + 64, :].rearrange("p (h n) -> p h n", n=128), op=ALU.mult)),
                      reads=["rd0"] + [("yT", 3 + p, qt // 4) for p in range(3)], writes=[("yT", 3 + p, qt // 4) for p in range(3)])
            items += [("n", 0), ("n", 1)]
            skew(units, items, tile_a, tile_b, {"n": norm})
            return units

        run_merged(stage1(0), stage1(1), pu2)
        for P in range(8):
            nx = [stage1(2 * P + 2), stage1(2 * P + 3)] if P < 7 else []
            run_merged(stage2(2 * P) + stage2(2 * P + 1), *nx)

    def mixer_C(layer):
        qc = scr_view(0, [2, S], BF16)
        kc = scr_view(8192, [2, S], BF16)
        vc = scr_view(16384, [16, 512], BF16)
        ac = scr_view(32768, [4, S], F32)
        et = scr_view(65536, [2, 256], BF16)
        rd = scr_view(65536 + 1024, [512], F32)
        rn = scr_view(65536 + 3072, [512], F32)
        sc.op("pool", ("tensor_copy", dict(out=vc.rearrange("p b (h c) -> p b h c", c=128)[:, :, :, 64:128],
                                           in_=cb[:, CB_ONES:CB_ONES + 64].unsqueeze(1).unsqueeze(1).to_broadcast([128, 16, 4, 64]))),
              reads=["cb"], writes=["vc1"])
        gu = []
        for p in range(2):
            gu += proj_fm(layer, f"CG{p}", False, silu_evac(6 + p))
        run(gu)
        groups = [g for g in range(3) if g in getattr(_build, 'cgroups', (0, 1, 2))]
        dils = (1, 4, 16)
        cst = {"ei": 0}

        def toks_of(dil):
            def toks(r, m, cnt=128):
                st0 = r + dil * 128 * m
                return slice(st0, st0 + dil * (cnt - 1) + 1, dil)
            return toks

        def proj_units(g, p, pbanks):
            dil = dils[g]
            nblk = S // dil // 128
            toks = toks_of(dil)
            units = []
            units += proj_fm(layer, f"CQ{g}{p}", True, rope_evac(lambda c4, h: qc[:, p, c4 * 512:(c4 + 1) * 512], lambda c4, h: [("qc", p, c4)], False),
                             banks=pbanks)
            units += proj_fm(layer, f"CK{g}{p}", True, rope_evac(lambda c4, h: kc[:, p, c4 * 512:(c4 + 1) * 512], lambda c4, h: [("kc", p, c4)], False),
                             banks=pbanks)

            def vev(g4, b1):
                v4 = ps[b1][:, :].rearrange("p (j h c) -> p j h c", h=2, c=64)
                sc.op("act", ("copy", dict(out=vc.rearrange("p b (h c) -> p b h c", c=128)[:, g4:g4 + 4, 2 * p:2 * p + 2, 0:64], in_=v4)),
                      reads=[("ps", b1)], writes=[("vc", p, g4)])
            units += proj_tm(layer, f"CV{g}{p}", 16, lambda c, blk: hT[:, c, toks(blk // nblk, blk % nblk)], vev,
                             banks=(pbanks[0][0], pbanks[1][0]) if len(pbanks[0]) == 1 else (0, 1))
            return units

        def attn_units(g, j, first):
            dil = dils[g]
            nblk = S // dil // 128
            toks = toks_of(dil)
            p, e_ = j // 2, j % 2
            pr = slice(64 * e_, 64 * e_ + 64)
            allq = [("qc", p, c4) for c4 in range(4)]
            allk = [("kc", p, c4) for c4 in range(4)]
            started = [False] * 4
            units = []

            def blk_a(st_, r, m):
                nq = 256 if m + 1 < nblk else 128
                ei = cst["ei"]; cst["ei"] += 1
                sb_ = 4 + (ei % 2)
                ej = ei % 2
                st_.update(nq=nq, ej=ej)
                mm(ps[sb_][:, 0:nq], kc[pr, p, toks(r, m)], qc[pr, p, toks(r, m, nq)], True, True,
                   reads=allk + allq, writes=[("ps", sb_)])
                sc.op("act", ("activation", dict(out=et[:, ej, 0:nq], in_=ps[sb_][:, 0:nq], func=AF.Exp, scale=0.125)),
                      reads=[("ps", sb_)], writes=[("et", ej)])
                sc.op("dve", ("tensor_tensor", dict(out=et[:, ej, 0:nq], in0=et[:, ej, 0:nq], in1=cb[:, CB_MOWN:CB_MOWN + nq], op=ALU.mult)),
                      reads=[("et", ej), "cb"], writes=[("et", ej)])

            def blk_b(st_, r, m):
                blk = r * nblk + m
                nq, ej = st_["nq"], st_["ej"]
                pieces = []
                for part in range(nq // 128):
                    t0 = r + dil * 128 * (m + part)
                    if dil <= 4:
                        pieces.append((part * 128, 128, t0))
                    else:
                        for q4 in range(4):
                            pieces.append((part * 128 + 32 * q4, 32, t0 + dil * 32 * q4))
                for pi, (c0, cn, t0) in enumerate(pieces):
                    bnk = t0 // 512
                    o0 = t0 % 512
                    mm(ps[bnk][:, o0:o0 + dil * (cn - 1) + 1:dil], vc[:, blk, 128 * j:128 * j + 128], et[:, ej, c0:c0 + cn],
                       not started[bnk], False, reads=[("vc", p, (blk // 4) * 4), "vc1", ("et", ej)],
                       writes=[("ps", bnk)], inc=(pi == len(pieces) - 1))
                    started[bnk] = True
            items = [("t", r, m) for r in range(dil) for m in range(nblk)] + [("e",)]

            def evac():
                for c4 in range(4):
                    dst = ac[:, j, c4 * 512:(c4 + 1) * 512]
                    if first:
                        sc.op("act", ("copy", dict(out=dst, in_=ps[c4][:, :])), reads=[("ps", c4)], writes=[("ac", j, c4)])
                    else:
                        sc.op("dve", ("tensor_tensor", dict(out=dst, in0=ps[c4][:, :], in1=dst, op=ALU.add)),
                              reads=[("ps", c4), ("ac", j, c4)], writes=[("ac", j, c4)])
            skew(units, items, blk_a, blk_b, {"e": evac})
            return units

        PB = ((6,), (7,))
        seq = []
        for gi, g in enumerate(groups):
            seq.append((g, 0)); seq.append((g, 1))
        run(proj_units(seq[0][0], seq[0][1], ((0, 1), (2, 3))))
        for i, (g, p) in enumerate(seq):
            nxt = proj_units(seq[i + 1][0], seq[i + 1][1], PB) if i + 1 < len(seq) else []
            first = (g == groups[0])
            run_merged(attn_units(g, 2 * p, first) + attn_units(g, 2 * p + 1, first), nxt)
        for j in range(4):
            p, e_ = j // 2, j % 2
            for c4 in range(4):
                cs = slice(c4 * 512, (c4 + 1) * 512)
                sc.op("act", ("activation", dict(out=rd[64:128, :], in_=ac[64:128, j, cs], func=AF.Ln)), reads=[("ac", j, c4)], writes=["rd"])
                sc.op("act", ("activation", dict(out=rd[64:128, :], in_=rd[64:128, :], func=AF.Exp, scale=-1.0)), reads=["rd"], writes=["rd"])
                sc.op("act", ("copy", dict(out=rn[64:128, :], in_=ac[0:64, j, cs])), reads=[("ac", j, c4)], writes=[("rn", 1)])
                sc.op("dve", ("tensor_tensor", dict(out=rn[64 * e_:64 * e_ + 64, :], in0=rn[64:128, :], in1=rd[64:128, :], op=ALU.mult)),
                      reads=[("rn", 1), "rd"], writes=[("rn", e_)])
                ydst = yT[64 * e_:64 * e_ + 64, 6 + p, cs]
                sc.op("pool", ("tensor_tensor", dict(out=ydst, in0=ydst, in1=rn[64 * e_:64 * e_ + 64, :], op=ALU.mult)),
                      reads=[("rn", e_), ("yT", 6 + p, c4)], writes=[("yT", 6 + p, c4)])

    def phase_final(si, layer, src_d, dst_d, last):
        mg = scr_view(0, [8, S], BF16)
        wbr = scr_view(32768, [8, D], BF16)
        wo = scr_view(49152, [8, D], BF16)
        gs = scr_view(65536, [3, 512], BF16)
        tm = scr_view(65536 + 3072, [512], F32)
        for c in range(8):
            nm = (f"WA{c}" if c < 3 else f"WB{c - 3}" if c < 6 else f"WC{c - 6}")
            load_w(layer, _XIDX[nm], dest=wbr[:, c, :], dest_key=("wbr", c))
        for c in range(8):
            load_w(layer, _XIDX[f"WO{c}"], dest=wo[:, c, :], dest_key=("wo", c))
        if last:
            load_g(0, 2)
        kch = ((0, 3), (3, 6), (6, 8))
        for dt_ in range(8):
            gw = []
            for br in range(3):
                w_, k_ = load_w(layer, _TIDX[f"MG{8 * br + dt_}"])
                gw.append((w_.rearrange("p (c n) -> p c n", n=128), k_))
            for c4 in range(4):
                cs = slice(c4 * 512, (c4 + 1) * 512)
                for br in range(3):
                    for c in range(8):
                        mm(ps[br][:, :], gw[br][0][:, c, :], hT[:, c, cs], c == 0, c == 7, reads=[gw[br][1], ("hT", c4)], writes=[("ps", br)])
                    sc.op("act", ("activation", dict(out=gs[:, br, :], in_=ps[br][:, :], func=AF.Sigmoid)), reads=[("ps", br)], writes=[("gs", br)])
                for br in range(3):
                    a0, a1 = kch[br]
                    for c in range(a0, a1):
                        mm(ps[3 + br][:, :], wbr[:, c, dt_ * 128:(dt_ + 1) * 128], yT[:, c, cs], c == a0, c == a1 - 1,
                           reads=[("wbr", c), ("yT", c, c4)], writes=[("ps", 3 + br)])
                sc.op("dve", ("tensor_tensor", dict(out=tm, in0=ps[3][:, :], in1=gs[:, 0, :], op=ALU.mult)), reads=[("ps", 3), ("gs", 0)], writes=["tm"])
                sc.op("dve", ("tensor_tensor", dict(out=gs[:, 1, :], in0=ps[4][:, :], in1=gs[:, 1, :], op=ALU.mult)), reads=[("ps", 4), ("gs", 1)], writes=[("gs", 1)])
                sc.op("dve", ("tensor_tensor", dict(out=gs[:, 2, :], in0=ps[5][:, :], in1=gs[:, 2, :], op=ALU.mult)), reads=[("ps", 5), ("gs", 2)], writes=[("gs", 2)])
                sc.op("pool", ("tensor_tensor", dict(out=tm, in0=tm, in1=gs[:, 1, :], op=ALU.add)), reads=["tm", ("gs", 1)], writes=["tm"])
                sc.op("pool", ("tensor_tensor", dict(out=mg[:, dt_, cs], in0=tm, in1=gs[:, 2, :], op=ALU.add)),
                      reads=["tm", ("gs", 2)], writes=[("mg", c4)])
        for t in range(NT):
            s = t % 2
            ts = slice(t * 128, (t + 1) * 128)
            sc.dma(("x", s), ("dma_start", dict(out=xt[:, s, :], in_=src_d[si, ts, :])), writes=[("xt", s)])
            for hf in range(2):
                b = 6 + hf
                for c in range(8):
                    mm(ps[b][:, :], mg[:, c, ts], wo[:, c, hf * 512:(hf + 1) * 512], c == 0, c == 7,
                       reads=[("mg", t // 4), ("wo", c)], writes=[("ps", b)])
                sc.op("dve", ("tensor_tensor", dict(out=xt[:, s, hf * 512:(hf + 1) * 512], in0=ps[b][:, :],
                                                                        in1=xt[:, s, hf * 512:(hf + 1) * 512], op=ALU.add)),
                      reads=[("ps", b), ("xt", s)], writes=[("xt", s)])
            if last:
                ss = st[:, 8 + 2 * s:8 + 2 * s + 1]
                rs = st[:, 8 + 2 * s + 1:8 + 2 * s + 2]
                sc.op("act", ("activation", dict(out=hn[:, s, :], in_=xt[:, s, :], func=AF.Square, accum_out=ss)),
                      reads=[("xt", s)], writes=[("hn", s), ("st", s)])
                sc.op("dve", ("tensor_scalar", dict(out=rs, in0=ss, scalar1=1.0 / D, scalar2=1e-6, op0=ALU.mult, op1=ALU.add)),
                      reads=[("st", s)], writes=[("st", s)])
                sc.op("act", ("activation", dict(out=rs, in_=rs, func=AF.Sqrt)), reads=[("st", s)], writes=[("st", s)])
                sc.op("dve", ("reciprocal", dict(out=rs, in_=rs)), reads=[("st", s)], writes=[("st", s)])
                sc.op("dve", ("scalar_tensor_tensor", dict(out=xt[:, s, :], in0=xt[:, s, :], scalar=rs, in1=gbc[:, 0, :],
                                                                         op0=ALU.mult, op1=ALU.mult)),
                      reads=[("xt", s), ("st", s), ("gbc", 0)], writes=[("xt", s)])
            sc.dma(("o", s), ("dma_start", dict(out=dst_d[si, ts, :], in_=xt[:, s, :])), reads=[("xt", s)], writes=[("dram", si, t)])

    enabled = set(getattr(_build, "enabled", ("A", "B", "C")))

    def program():
        sc.dma("c0", ("dma_start", dict(out=cf[:], in_=cf_d[:, :])), writes=["cf"])
        sc.dma("c1", ("dma_start", dict(out=cb[:], in_=cb_d[:, :])), writes=["cb"])
        for si in range(nseq):
            for li_, layer in enumerate(layers_all):
                first = (li_ == 0)
                last = (li_ == len(layers_all) - 1)
                src = x_d if first else xs_d
                dst = out_d if last else xs_d
                sc.phase = "norm"
                phase_norm(si, layer, src)
                if "A" in enabled:
                    sc.barrier()
                    sc.phase = "A"
                    mixer_A(layer)
                if "B" in enabled:
                    sc.barrier()
                    sc.phase = "B"
                    mixer_B(layer)
                if "C" in enabled:
                    sc.barrier()
                    sc.phase = "C"
                    mixer_C(layer)
                sc.barrier()
                sc.phase = "final"
                for name, shape, dt in taps:
                    if name == f"yT{layer}" and si == 0:
                        sc.dma(("tap", name), ("dma_start", dict(out=tap_d[name][:, :, :], in_=yT[:])),
                               reads=[("yT", c, c4) for c in range(8) for c4 in range(4)])
                    if name == f"hT{layer}" and si == 0:
                        sc.dma(("tap", name), ("dma_start", dict(out=tap_d[name][:, :, :], in_=hT[:])),
                               reads=[("hT", c4) for c4 in range(4)])
                phase_final(si, layer, src, dst, last and final_norm)
        sc.final_wait("sp", [("o", 0), ("o", 1)] + [("tap", n) for n, _, _ in taps])


    program()
    sc = Sched()
    wstate.update(n=0, rec=False, issued=0)
    ppp["i"] = 0
    rtc["i"] = 0
    qsc["i"] = 0
    program()

    waited = {e: set() for e in ("pe", "act", "dve", "pool")}
    for e in Sched.ENG:
        for waits, fn, inc, ph in sc.q[e]:
            for de, dc in waits:
                if de in waited:
                    waited[de].add(dc)
    remap = {e: {c: i + 1 for i, c in enumerate(sorted(waited[e]))} for e in waited}
    for e in waited:
        c = 0
        newq = []
        for waits, fn, inc, ph in sc.q[e]:
            if inc is not None and inc[0] == "E":
                c += 1
                if c not in remap[e]:
                    inc = None
            newq.append((waits, fn, inc, ph))
        sc.q[e] = newq
    for e in Sched.ENG:
        sc.q[e] = [([(de, remap[de][dc]) if de in remap else (de, dc) for de, dc in waits], fn, inc, ph)
                   for waits, fn, inc, ph in sc.q[e]]

    sems = {}
    for e in Sched.ENG:
        sems[e] = es.enter_context(nc.semaphore("s_" + e))
    for de in sc.dcnt:
        sems[de] = es.enter_context(nc.semaphore("d%d" % len(sems)))
    block = es.enter_context(nc.Block())

    def replay(engname):
        def run(eng):
            for waits, fn, inc, ph in sc.q[engname]:
                for de, dc in waits:
                    eng.wait_ge(sems[de], dc)
                if fn is None:
                    continue
                ins = getattr(eng, fn[0])(**fn[1])
                if ANNOTATE:
                    ins.annotate(ph)
                if inc is not None:
                    if inc[0] == "E":
                        ins.then_inc(sems[inc[1]], 1)
                    else:
                        ins.then_inc(sems[inc[1]], 16)
        return run
    block.tensor(replay("pe"))
    block.scalar(replay("act"))
    block.vector(replay("dve"))
    block.gpsimd(replay("pool"))
    block.sync(replay("sp"))
    es.close()
    return nc


_CACHE = {}


def kernel(x, norm_g, w_in, w_br_a, w_br_b, w_br_c, w_out, final_norm_g):
    x = np.ascontiguousarray(np.asarray(x, np.float32))
    ncores = 8
    nseq = x.shape[0] // ncores
    wt = _pack_weights(np.asarray(w_in, np.float32), np.asarray(w_br_a, np.float32), np.asarray(w_br_b, np.float32),
                       np.asarray(w_br_c, np.float32), np.asarray(w_out, np.float32))
    gv = np.concatenate([np.asarray(norm_g, np.float32), np.asarray(final_norm_g, np.float32)[None, :]], axis=0)
    cf, cb = _consts()
    nc = _build(nseq)
    in_maps = [{"x": x[i * nseq:(i + 1) * nseq], "wt": wt, "gv": gv, "cf": cf, "cb": cb} for i in range(ncores)]
    res = run_bass_kernel_spmd(nc, in_maps, core_ids=list(range(ncores)))
    return np.concatenate([r["out"] for r in res.results], axis=0)
```

```python
import numpy as np
import ml_dtypes
import concourse.bass as bass
import concourse.mybir as mybir
from concourse.bass_utils import run_bass_kernel_spmd

F32 = mybir.dt.float32
BF16 = mybir.dt.bfloat16
AF = mybir.ActivationFunctionType
ALU = mybir.AluOpType
AX = mybir.AxisListType

S = 2048
D = 1024
NT = 16
NEG = -30000.0
NBIS = 12
ANNOTATE = False

_splits = (384, 384, 384, 384, 384, 64, 64, 384, 256, 64, 4, 768, 768, 768, 256, 3072)
_names = ("a_q", "a_k", "a_v", "a_g", "b_q", "b_k", "b_v", "b_g", "i_q", "i_k", "i_w",
          "c_q", "c_k", "c_v", "c_g", "m_g")
_off = {}
_o = 0
for _n, _s in zip(_names, _splits):
    _off[_n] = _o
    _o += _s


def _rot(cols):
    cols = np.asarray(cols).reshape(-1, 2, 32)
    return cols[:, ::-1, :].reshape(-1)


def _tile_cols():
    t = []
    rng = lambda n, a, b: np.arange(_off[n] + a, _off[n] + b)
    for p in range(3):
        c = rng("a_q", 128 * p, 128 * p + 128); t.append((f"AQ{p}", c)); t.append((f"AQR{p}", _rot(c)))
        c = rng("a_k", 128 * p, 128 * p + 128); t.append((f"AK{p}", c)); t.append((f"AKR{p}", _rot(c)))
        t.append((f"AG{p}", rng("a_g", 128 * p, 128 * p + 128)))
        t.append((f"AV{p}", rng("a_v", 128 * p, 128 * p + 128)))
    for p in range(3):
        c = rng("b_q", 128 * p, 128 * p + 128); t.append((f"BQ{p}", c)); t.append((f"BQR{p}", _rot(c)))
        t.append((f"BG{p}", rng("b_g", 128 * p, 128 * p + 128)))
    c = np.concatenate([rng("b_k", 0, 64), rng("b_k", 0, 64)]); t.append(("BK", c)); t.append(("BKR", _rot(c)))
    c = np.concatenate([rng("i_k", 0, 64), rng("i_k", 0, 64)]); t.append(("IK", c)); t.append(("IKR", _rot(c)))
    for p in range(2):
        c = rng("i_q", 128 * p, 128 * p + 128); t.append((f"IQ{p}", c)); t.append((f"IQR{p}", _rot(c)))
    c = np.concatenate([rng("b_v", 0, 64), rng("i_w", 0, 4), rng("i_w", 0, 4).repeat(15)]); t.append(("BV", c))
    for g in range(3):
        for p in range(2):
            c = rng("c_q", 256 * g + 128 * p, 256 * g + 128 * p + 128); t.append((f"CQ{g}{p}", c)); t.append((f"CQR{g}{p}", _rot(c)))
            c = rng("c_k", 256 * g + 128 * p, 256 * g + 128 * p + 128); t.append((f"CK{g}{p}", c)); t.append((f"CKR{g}{p}", _rot(c)))
            t.append((f"CV{g}{p}", rng("c_v", 256 * g + 128 * p, 256 * g + 128 * p + 128)))
    for p in range(2):
        t.append((f"CG{p}", rng("c_g", 128 * p, 128 * p + 128)))
    for j in range(24):
        t.append((f"MG{j}", rng("m_g", 128 * j, 128 * j + 128)))
    return t


_TILES = _tile_cols()
_TIDX = {n: i for i, (n, _) in enumerate(_TILES)}
NWT = len(_TILES)
_XIDX = {}
for _i, _n in enumerate([f"WA{c}" for c in range(3)] + [f"WB{c}" for c in range(3)] +
                        [f"WC{c}" for c in range(2)] + [f"WO{c}" for c in range(8)]):
    _XIDX[_n] = NWT + _i
NWALL = NWT + 16


def _pack_weights(w_in, w_br_a, w_br_b, w_br_c, w_out):
    L = w_in.shape[0]
    out = np.empty((L, NWALL, 128, 1024), np.float32)
    allc = np.concatenate([c for _, c in _TILES])
    for l in range(L):
        g = w_in[l][:, allc]
        g = g.reshape(8, 128, NWT, 128).transpose(2, 1, 0, 3)
        out[l, :NWT] = g.reshape(NWT, 128, 1024)
        out[l, NWT:NWT + 3] = w_br_a[l].reshape(3, 128, 1024)
        out[l, NWT + 3:NWT + 6] = w_br_b[l].reshape(3, 128, 1024)
        out[l, NWT + 6:NWT + 8] = w_br_c[l].reshape(2, 128, 1024)
        out[l, NWT + 8:NWT + 16] = w_out[l].reshape(8, 128, 1024)
    return out


CF_COS = 0
CF_SIN = CF_COS + S
CF_NEGTRI = CF_SIN + S
CF_AGM = CF_NEGTRI + 128
CF_AVAL = CF_AGM + 128
CF_AOWN = CF_AVAL + 128
CF_POW2 = CF_AOWN + 128
CF_N = CF_POW2 + 32
CB_ID = 0
CB_MOWN = CB_ID + 128
CB_MPREV = CB_MOWN + 128
CB_ONEHOT = CB_MPREV + 128
CB_ONES = CB_ONEHOT + S
CB_PERM = CB_ONES + 128
CB_N = CB_PERM + 128


def _consts():
    cf = np.zeros((128, CF_N), np.float32)
    inv = 1.0 / (10000.0 ** (np.arange(0, 64, 2, dtype=np.float32) / 64.0))
    ang = np.arange(S, dtype=np.float32)[None, :] * inv[:, None].astype(np.float32)
    cos = np.cos(ang).astype(np.float32)
    sin = np.sin(ang).astype(np.float32)
    for p in range(128):
        j = p % 32
        cf[p, CF_COS:CF_COS + S] = cos[j]
        cf[p, CF_SIN:CF_SIN + S] = sin[j] * (-1.0 if (p % 64) < 32 else 1.0)
    t = np.arange(128)[:, None]
    s_ = np.arange(128)[None, :]
    cf[:, CF_NEGTRI:CF_NEGTRI + 128] = np.where(s_ <= t, 0.0, -1e30)
    for qt in range(16):
        own = qt // 2
        for n in range(8):
            cf[:, CF_AGM + qt * 8 + n] = 0.0 if n < own else -1e30
            cf[:, CF_AVAL + qt * 8 + n] = 1.0 if n < own else 0.0
            cf[:, CF_AOWN + qt * 8 + n] = 0.0 if n == own else NEG
    for k in range(32):
        cf[:, CF_POW2 + k] = 2.0 ** (-k)
    cb = np.zeros((128, CB_N), np.float32)
    cb[:, CB_ID:CB_ID + 128] = np.eye(128)
    cb[:, CB_MOWN:CB_MOWN + 128] = (t <= s_)
    cb[:, CB_MPREV:CB_MPREV + 128] = (t >= s_)
    for n in range(8):
        cb[n, CB_ONEHOT + 256 * n:CB_ONEHOT + 256 * n + 256] = 1.0
    cb[:, CB_ONES:CB_ONES + 128] = 1.0
    for po in range(128):
        cb[(po // 64) * 64 + ((po % 64) + 32) % 64, CB_PERM + po] = 1.0
    return cf, cb.astype(ml_dtypes.bfloat16)


class Sched:
    ENG = ("pe", "act", "dve", "pool", "sp")

    def __init__(self):
        self.q = {e: [] for e in self.ENG}
        self.cnt = {e: 0 for e in self.ENG}
        self.seen = {e: {} for e in self.ENG}
        self.lastw = {}
        self.readers = {}
        self.dcnt = {}
        self.phase = "init"

    def _need(self, eng, reads, writes):
        need = {}

        def add(dep, raw):
            de, dc = dep
            if de == eng and (eng == "pe" or eng == "sp"):
                return
            if need.get(de, 0) < dc:
                need[de] = dc
        for r in reads:
            w = self.lastw.get(r)
            if w is not None:
                add(w, True)
        for w_ in writes:
            w = self.lastw.get(w_)
            if w is not None:
                add(w, False)
            for de, dc in self.readers.get(w_, {}).items():
                add((de, dc), False)
        waits = []
        for de, dc in need.items():
            if self.seen[eng].get(de, 0) < dc:
                self.seen[eng][de] = dc
                waits.append((de, dc))
        return waits

    def _record(self, tag, reads, writes):
        for r in reads:
            d = self.readers.setdefault(r, {})
            if d.get(tag[0], 0) < tag[1]:
                d[tag[0]] = tag[1]
        for w_ in writes:
            self.lastw[w_] = tag
            self.readers[w_] = {}

    def op(self, eng, fn, reads=(), writes=(), inc=True):
        waits = self._need(eng, reads, writes)
        tag = (eng, self.cnt[eng] + 1)
        if inc:
            self.cnt[eng] += 1
        self.q[eng].append((waits, fn, ("E", eng) if inc else None, self.phase))
        self._record(tag, reads, writes)

    def dma(self, key, fn, reads=(), writes=(), eng="sp"):
        waits = self._need(eng, reads, writes)
        de = ("dma", key)
        self.dcnt[de] = self.dcnt.get(de, 0) + 16
        self.q[eng].append((waits, fn, ("D", de), self.phase))
        self._record((de, self.dcnt[de]), reads, writes)

    def barrier(self):
        tgt = {e: self.cnt[e] for e in ("pe", "act", "dve", "pool")}
        tgt.update(self.dcnt)
        for e in self.ENG:
            waits = []
            for de, dc in tgt.items():
                if de == e or dc == 0:
                    continue
                if self.seen[e].get(de, 0) < dc:
                    self.seen[e][de] = dc
                    waits.append((de, dc))
            if waits:
                self.q[e].append((waits, None, None, None))

    def final_wait(self, eng, dma_keys):
        waits = [(("dma", k), self.dcnt[("dma", k)]) for k in dma_keys if ("dma", k) in self.dcnt]
        self.q[eng].append((waits, None, None, None))


def _build(nseq, layers_all=(0, 1), final_norm=True, taps=()):
    nc = bass.Bass("TRN2", target_bir_lowering=False)
    x_d = nc.dram_tensor("x", [nseq, S, D], F32, kind="ExternalInput").ap()
    wt_d = nc.dram_tensor("wt", [2, NWALL, 128, 1024], F32, kind="ExternalInput").ap()
    gv_d = nc.dram_tensor("gv", [3, D], F32, kind="ExternalInput").ap()
    cf_d = nc.dram_tensor("cf", [128, CF_N], F32, kind="ExternalInput").ap()
    cb_d = nc.dram_tensor("cb", [128, CB_N], BF16, kind="ExternalInput").ap()
    out_d = nc.dram_tensor("out", [nseq, S, D], F32, kind="ExternalOutput").ap()
    xs_d = nc.dram_tensor("xscr", [nseq, S, D], F32).ap()
    tap_d = {}
    for name, shape, dt in taps:
        tap_d[name] = nc.dram_tensor("tap_" + name, list(shape), dt, kind="ExternalOutput").ap()

    sc = Sched()
    sb = {}

    def alloc(name, shape, dt):
        t = nc.alloc_sbuf_tensor(name, list(shape), dt) if False else None
        return t

    from contextlib import ExitStack
    es = ExitStack()

    def SB(name, shape, dt):
        t = es.enter_context(nc.sbuf_tensor("sb_" + name, list(shape), dt))
        sb[name] = t
        return t

    def PS(name, shape, dt):
        return es.enter_context(nc.psum_tensor(name, list(shape), dt))

    cf = SB("cf", [128, CF_N], F32)
    cb = SB("cb", [128, CB_N], BF16)
    hT = SB("hT", [128, 8, S], BF16)
    yT = SB("yT", [128, 8, S], BF16)
    gbc = SB("gbc", [128, 2, D], F32)
    wbf = SB("wbf", [128, 8, 1024], BF16)
    xt = SB("xt", [128, 2, D], F32)
    hn = SB("hn", [128, 2, D], BF16)
    st = SB("st", [128, 64], F32)
    scr = SB("scr", [128, 36864], BF16)
    ps = [PS(f"ps{i}", [128, 512], F32) for i in range(8)]

    def scr_view(off_bytes, shape, dt):
        n = int(np.prod(shape))
        if dt == F32:
            assert off_bytes % 4 == 0
            v = scr[:, off_bytes // 2: off_bytes // 2 + 2 * n].bitcast(F32)
        else:
            v = scr[:, off_bytes // 2: off_bytes // 2 + n]
        if len(shape) == 2:
            v = v.rearrange("p (a b) -> p a b", b=shape[1])
        elif len(shape) == 3:
            v = v.rearrange("p (a b c) -> p a b c", b=shape[1], c=shape[2])
        return v

    ident = cb[:, CB_ID:CB_ID + 128]

    def mm(out, lhsT, rhs, start, stop, reads, writes, inc=None):
        if inc is None:
            inc = stop
        sc.op("pe", ("matmul", dict(out=out, lhsT=lhsT, rhs=rhs, start=start, stop=stop, skip_group_check=True)),
              reads=reads, writes=writes, inc=inc)

    def tr(out, in_, reads, writes, inc=True):
        sc.op("pe", ("transpose", dict(out=out, in_=in_, identity=ident[:in_.shape[0], :in_.shape[0]])), reads=reads, writes=writes, inc=inc)


    NSLOT = 8
    WDEPTH = 4
    wstate = {"n": 0, "ring": 0, "rec": True, "issued": 0, "xd": 0}
    wplan = []

    def _issue_w(ent):
        layer, idx, dest, dest_key, skey = ent
        sc.dma(skey, ("dma_start", dict(out=dest, in_=wt_d[layer, idx, :, :])), writes=[dest_key], eng="pool")

    def load_w(layer, idx, dest=None, dest_key=None):
        n = wstate["n"]; wstate["n"] += 1
        if wstate["rec"]:
            if dest is None:
                b = wstate["ring"] % NSLOT; wstate["ring"] += 1
                dest = wbf[:, b, :]
                dest_key = ("wbf", b)
                skey = ("w", b)
            else:
                skey = ("wx", wstate["xd"] % 16); wstate["xd"] += 1
            wplan.append((layer, idx, dest, dest_key, skey))
            return dest, dest_key
        while wstate["issued"] < len(wplan) and wstate["issued"] <= n + WDEPTH:
            ent = wplan[wstate["issued"]]
            if ent[4][0] == "wx" and wstate["issued"] > n:
                break
            _issue_w(ent)
            wstate["issued"] += 1
        ent = wplan[n]
        return ent[2], ent[3]

    def load_g(slot, row):
        src = gv_d[row:row + 1, :].partition_broadcast(128) if False else None
        from concourse.ap import AP
        src = AP(gv_d.tensor, row * D, [[0, 128], [1, D]])
        sc.dma(("g", slot), ("dma_start", dict(out=gbc[:, slot, :], in_=src)), writes=[("gbc", slot)])

    def phase_norm(si, layer, src_d):
        load_g(layer % 2, layer)
        pst = ps[7][:, 0:512].bitcast(BF16)
        def stats(t):
            s = t % 2
            sc.dma(("x", s), ("dma_start", dict(out=xt[:, s, :], in_=src_d[si, t * 128:(t + 1) * 128, :])),
                   writes=[("xt", s)])
            ss = st[:, 2 * s:2 * s + 1]
            rs = st[:, 2 * s + 1:2 * s + 2]
            sc.op("act", ("activation", dict(out=hn[:, s, :], in_=xt[:, s, :], func=AF.Square, accum_out=ss)),
                  reads=[("xt", s)], writes=[("hn", s), ("st", s)])
            sc.op("dve", ("tensor_scalar", dict(out=rs, in0=ss, scalar1=1.0 / D, scalar2=1e-6, op0=ALU.mult, op1=ALU.add)),
                  reads=[("st", s)], writes=[("st", s)])
            sc.op("act", ("activation", dict(out=rs, in_=rs, func=AF.Sqrt)), reads=[("st", s)], writes=[("st", s)])
            sc.op("dve", ("reciprocal", dict(out=rs, in_=rs)), reads=[("st", s)], writes=[("st", s)])
            sc.op("dve", ("scalar_tensor_tensor", dict(out=hn[:, s, :], in0=xt[:, s, :], scalar=rs, in1=gbc[:, layer % 2, :],
                                                                     op0=ALU.mult, op1=ALU.mult)),
                  reads=[("xt", s), ("st", s), ("gbc", layer % 2)], writes=[("hn", s)])

        def trans(t):
            s = t % 2
            for c in range(8):
                tr(pst[:, c * 128:(c + 1) * 128], hn[:, s, c * 128:(c + 1) * 128], reads=[("hn", s), "cb"], writes=[("ps", 7)], inc=(c == 7))
            sc.op("act", ("copy", dict(out=hT[:, :, t * 128:(t + 1) * 128], in_=pst.rearrange("p (c n) -> p c n", n=128))),
                  reads=[("ps", 7)], writes=[("hT", t // 4)])
        stats(0)
        for t in range(NT):
            if t + 1 < NT:
                stats(t + 1)
            trans(t)

    def run(units):
        for u in units:
            u()

    def run_merged(*lists):
        lists = [l for l in lists if l]
        idx = [0] * len(lists)
        while True:
            cand = [(idx[i] / len(l), i) for i, l in enumerate(lists) if idx[i] < len(l)]
            if not cand:
                break
            i = min(cand)[1]
            lists[i][idx[i]]()
            idx[i] += 1

    def skew(units, items, fa, fb, others):
        pend = None
        for it in items:
            if it[0] == "t":
                st_ = {}
                a_ = (lambda it=it, st_=st_: fa(st_, *it[1:]))
                if pend is None:
                    units.append(a_)
                else:
                    units.append(lambda a_=a_, b_=pend: (a_(), b_()))
                pend = (lambda it=it, st_=st_: fb(st_, *it[1:]))
            else:
                if pend is not None:
                    units.append(pend)
                    pend = None
                units.append(lambda it=it: others[it[0]](*it[1:]))
        if pend is not None:
            units.append(pend)

    ppp = {"i": 0}

    qsb = SB("qsb", [128, 2, 512], BF16)
    qsc = {"i": 0}

    def proj_fm(layer, name, rope, evac, banks=((0, 1), (2, 3))):
        stt = {"pend": None}

        def rot_evac(c4, j):
            i = ppp["i"]; ppp["i"] += 1
            b2 = banks[1][i % len(banks[1])]
            mm(ps[b2][:, :], cb[:, CB_PERM:CB_PERM + 128], qsb[:, j, :], True, True, reads=[("qsb", j), "cb"], writes=[("ps", b2)])
            evac(c4, j, b2)

        def u(c4):
            if c4 == 0:
                w1, k1 = load_w(layer, _TIDX[name])
                stt["w1"] = (w1.rearrange("p (c n) -> p c n", n=128), k1)
            w1, k1 = stt["w1"]
            i = ppp["i"]; ppp["i"] += 1
            b1 = banks[0][i % len(banks[0])]
            for c in range(8):
                mm(ps[b1][:, :], w1[:, c, :], hT[:, c, c4 * 512:(c4 + 1) * 512], c == 0, c == 7,
                   reads=[k1, ("hT", c4)], writes=[("ps", b1)])
            if rope:
                j = qsc["i"] % 2; qsc["i"] += 1
                sc.op("act", ("copy", dict(out=qsb[:, j, :], in_=ps[b1][:, :])), reads=[("ps", b1)], writes=[("qsb", j)])
                if stt["pend"] is not None:
                    rot_evac(*stt["pend"])
                stt["pend"] = (c4, j)
                if c4 == 3:
                    rot_evac(*stt["pend"])
                    stt["pend"] = None
            else:
                evac(c4, b1)
        return [(lambda c4=c4: u(c4)) for c4 in range(4)]

    rt = SB("rt", [128, 2, 2, 512], F32)
    rtc = {"i": 0}

    def rope_evac(dst_fn, dst_keys_fn, split_heads):
        def ev(c4, jq, b2):
            j = rtc["i"] % 2; rtc["i"] += 1
            t1 = rt[:, j, 0, :]
            t2 = rt[:, j, 1, :]
            cs = cf[:, CF_COS + c4 * 512:CF_COS + (c4 + 1) * 512]
            sn = cf[:, CF_SIN + c4 * 512:CF_SIN + (c4 + 1) * 512]
            sc.op("dve", ("tensor_tensor", dict(out=t1, in0=qsb[:, jq, :], in1=cs, op=ALU.mult)),
                  reads=[("qsb", jq), "cf"], writes=[("rt", j, 0)])
            sc.op("dve", ("tensor_tensor", dict(out=t2, in0=ps[b2][:, :], in1=sn, op=ALU.mult)),
                  reads=[("ps", b2), "cf"], writes=[("rt", j, 1)])
            if split_heads:
                for h in range(2):
                    d = dst_fn(c4, h)
                    sc.op("pool", ("tensor_tensor", dict(out=d, in0=t1[64 * h:64 * h + 64, :], in1=t2[64 * h:64 * h + 64, :], op=ALU.add)),
                          reads=[("rt", j, 0), ("rt", j, 1)], writes=dst_keys_fn(c4, h))
            else:
                d = dst_fn(c4, None)
                sc.op("pool", ("tensor_tensor", dict(out=d, in0=t1, in1=t2, op=ALU.add)),
                      reads=[("rt", j, 0), ("rt", j, 1)], writes=dst_keys_fn(c4, None))
        return ev

    def silu_evac(ychunk):
        def ev(c4, b1):
            sc.op("act", ("activation", dict(out=yT[:, ychunk, c4 * 512:(c4 + 1) * 512], in_=ps[b1][:, :], func=AF.Silu)),
                  reads=[("ps", b1)], writes=[("yT", ychunk, c4)])
        return ev

    def proj_tm(layer, name, nblk, tok_fn, evac, banks=(0, 1)):
        stt = {}

        def u(g4):
            if g4 == 0:
                w1, k1 = load_w(layer, _TIDX[name])
                stt["w1"] = (w1.rearrange("p (c n) -> p c n", n=128), k1)
            w1, k1 = stt["w1"]
            i = ppp["i"]; ppp["i"] += 1
            b1 = banks[i % len(banks)]
            for j in range(4):
                blk = g4 + j
                for c in range(8):
                    mm(ps[b1][:, j * 128:(j + 1) * 128], tok_fn(c, blk), w1[:, c, :], (c == 0 and j == 0), c == 7,
                       reads=[k1] + [("hT", q) for q in range(4)], writes=[("ps", b1)], inc=(c == 7 and j == 3))
            evac(g4, b1)
        return [(lambda g4=g4: u(g4)) for g4 in range(0, nblk, 4)]

    def mixer_A(layer):
        QA = [scr_view(0, [2, S], BF16), scr_view(8192, [2, S], BF16)]
        KA = [scr_view(16384, [2, S], BF16), scr_view(24576, [2, S], BF16)]
        VA = [scr_view(32768, [16, 256], BF16), scr_view(40960, [16, 256], BF16)]
        o0 = 49152
        km = scr_view(o0, [2, 8], BF16)
        kmf = scr_view(o0 + 64, [2, 8], F32)
        gt = scr_view(o0 + 256, [128], F32)
        cmp_ = scr_view(o0 + 1024, [128, 8], F32)
        rk = scr_view(o0 + 1024 + 4096, [128], F32)
        nb = scr_view(o0 + 1024 + 4096 + 512, [128], BF16)
        et = scr_view(57344, [2, 512], BF16)
        rd = scr_view(57344 + 2048, [512], F32)
        rn = scr_view(57344 + 4096, [512], F32)
        est = {"i": 0}

        def proj_units(p):
            bs_ = p % 2
            qa, ka, va = QA[bs_], KA[bs_], VA[bs_]
            units = []
            if p < 2:
                def init():
                    for e_ in range(2):
                        sc.op("pool", ("tensor_copy", dict(out=ka[64:72, e_, :], in_=cb[0:8, CB_ONEHOT:CB_ONEHOT + S])),
                              reads=["cb"], writes=[("ka", bs_, e_, c4) for c4 in range(4)])
                    for h in range(2):
                        sc.op("pool", ("tensor_copy", dict(out=va[:, :, 128 * h + 64:128 * h + 128],
                                                           in_=cb[:, CB_ONES:CB_ONES + 64].unsqueeze(1).to_broadcast([128, 16, 64]))),
                              reads=["cb"], writes=[("va1", bs_, h)])
                units.append(init)
            units += proj_fm(layer, f"AQ{p}", True, rope_evac(lambda c4, h: qa[0:64, h, c4 * 512:(c4 + 1) * 512],
                                                              lambda c4, h: [("qa", bs_, h, c4)], True))
            units += proj_fm(layer, f"AK{p}", True, rope_evac(lambda c4, h: ka[0:64, h, c4 * 512:(c4 + 1) * 512],
                                                              lambda c4, h: [("ka", bs_, h, c4)], True))
            units += proj_fm(layer, f"AG{p}", False, silu_evac(p))

            def vev(g4, b1):
                for h in range(2):
                    sc.op("act", ("copy", dict(out=va[:, g4:g4 + 4, 128 * h:128 * h + 64],
                                               in_=ps[b1][:, :].rearrange("p (j n) -> p j n", n=128)[:, :, 64 * h:64 * h + 64])),
                          reads=[("ps", b1)], writes=[("va", bs_, h, g4)])
            units += proj_tm(layer, f"AV{p}", 16, lambda c, blk: hT[:, c, blk * 128:(blk + 1) * 128], vev)
            return units

        def attn_units(p):
            bs_ = p % 2
            qa, ka, va = QA[bs_], KA[bs_], VA[bs_]
            units = []

            def gate(h):
                allk = [("ka", bs_, h, c4) for c4 in range(4)]
                allq = [("qa", bs_, h, c4) for c4 in range(4)]
                sc.op("dve", ("tensor_reduce", dict(out=kmf[0:64, h, :], in_=ka[0:64, h, :].rearrange("p (n b) -> p n b", b=256),
                                                    axis=AX.X, op=ALU.add)), reads=allk, writes=[("kmf", h)])
                sc.op("dve", ("tensor_copy", dict(out=km[0:64, h, :], in_=kmf[0:64, h, :])), reads=[("kmf", h)], writes=[("km", h)])
                gp = ps[4][:, 0:128]
                for qt in range(16):
                    mm(gp[:, qt * 8:(qt + 1) * 8], qa[0:64, h, qt * 128:(qt + 1) * 128], km[0:64, h, :], True, True,
                       reads=allq + [("km", h)], writes=[("ps", 4)], inc=(qt == 15))
                sc.op("dve", ("tensor_tensor", dict(out=gt, in0=gp, in1=cf[:, CF_AGM:CF_AGM + 128], op=ALU.add)),
                      reads=[("ps", 4), "cf"], writes=["gt"])
                g3 = gt.rearrange("p (q n) -> p q n", n=8)
                sc.op("dve", ("tensor_tensor", dict(out=cmp_.rearrange("p (q n) m -> p q n m", n=8),
                                                    in0=g3.unsqueeze(2).to_broadcast([128, 16, 8, 8]),
                                                    in1=g3.unsqueeze(3).to_broadcast([128, 16, 8, 8]), op=ALU.is_gt)),
                      reads=["gt"], writes=["cmp"])
                sc.op("dve", ("tensor_reduce", dict(out=rk, in_=cmp_, axis=AX.X, op=ALU.add)), reads=["cmp"], writes=["rk"])
                sc.op("dve", ("tensor_scalar", dict(out=rk, in0=rk, scalar1=2.5, scalar2=None, op0=ALU.is_lt)), reads=["rk"], writes=["rk"])
                sc.op("dve", ("tensor_tensor", dict(out=rk, in0=rk, in1=cf[:, CF_AVAL:CF_AVAL + 128], op=ALU.mult)), reads=["rk", "cf"], writes=["rk"])
                sc.op("dve", ("scalar_tensor_tensor", dict(out=rk, in0=rk, scalar=-NEG, in1=cf[:, CF_AOWN:CF_AOWN + 128],
                                                           op0=ALU.mult, op1=ALU.add)), reads=["rk", "cf"], writes=["rk"])
                sc.op("dve", ("tensor_copy", dict(out=nb, in_=rk)), reads=["rk"], writes=["nb"])
                tp = ps[5][:, 0:512].bitcast(BF16)
                for half in range(2):
                    for q8 in range(8):
                        qt = half * 8 + q8
                        tr(tp[0:8, q8 * 128:(q8 + 1) * 128], nb[:, qt * 8:(qt + 1) * 8], reads=["nb", "cb"], writes=[("ps", 5)], inc=(q8 == 7))
                    sc.op("act", ("copy", dict(out=qa[64:72, h, half * 1024:(half + 1) * 1024], in_=tp[0:8, :])),
                          reads=[("ps", 5)], writes=[("qa", bs_, h, 2 * half), ("qa", bs_, h, 2 * half + 1)])

            def tile_a(st_, h, c4, kt, nkt):
                q0 = max(kt * 128, c4 * 512)
                q1 = (c4 + 1) * 512
                n = q1 - q0
                ei = est["i"]; est["i"] += 1
                sb_ = 4 + (ei % 2)
                ej = ei % 2
                st_.update(q0=q0, n=n, ej=ej)
                mm(ps[sb_][:, 0:n], ka[0:72, h, kt * 128:(kt + 1) * 128], qa[0:72, h, q0:q1], True, True,
                   reads=[("ka", bs_, h, kt // 4), ("qa", bs_, h, c4)], writes=[("ps", sb_)])
                sc.op("act", ("activation", dict(out=et[:, ej, 0:n], in_=ps[sb_][:, 0:n], func=AF.Exp, scale=0.125)),
                      reads=[("ps", sb_)], writes=[("et", ej)])
                if q0 == kt * 128:
                    sc.op("dve", ("tensor_tensor", dict(out=et[:, ej, 0:128], in0=et[:, ej, 0:128],
                                                        in1=cb[:, CB_MOWN:CB_MOWN + 128], op=ALU.mult)),
                          reads=[("et", ej), "cb"], writes=[("et", ej)])

            def tile_b(st_, h, c4, kt, nkt):
                ob = 6 + (c4 % 2)
                q0, n, ej = st_["q0"], st_["n"], st_["ej"]
                mm(ps[ob][:, q0 - c4 * 512:512], va[:, kt, 128 * h:128 * h + 128], et[:, ej, 0:n], kt == 0, kt == nkt - 1,
                   reads=[("va", bs_, h, (kt // 4) * 4), ("va1", bs_, h), ("et", ej)], writes=[("ps", ob)], inc=True)

            def norm(h, c4):
                ob = 6 + (c4 % 2)
                sc.op("act", ("activation", dict(out=rd[64:128, :], in_=ps[ob][64:128, :], func=AF.Ln)), reads=[("ps", ob)], writes=["rd"])
                sc.op("act", ("activation", dict(out=rd[64:128, :], in_=rd[64:128, :], func=AF.Exp, scale=-1.0)), reads=["rd"], writes=["rd"])
                sc.op("dve", ("tensor_tensor", dict(out=rn[64 * h:64 * h + 64, :], in0=ps[ob][0:64, :], in1=rd[64:128, :], op=ALU.mult)),
                      reads=[("ps", ob), "rd"], writes=["rd0"])
                ydst = yT[64 * h:64 * h + 64, p, c4 * 512:(c4 + 1) * 512]
                sc.op("pool", ("tensor_tensor", dict(out=ydst, in0=ydst, in1=rn[64 * h:64 * h + 64, :], op=ALU.mult)),
                      reads=["rd0", ("yT", p, c4)], writes=[("yT", p, c4)])

            items = []
            for h in range(2):
                items.append(("g", h))
                for c4 in range(4):
                    nkt = 4 * c4 + 4
                    for kt in range(nkt):
                        items.append(("t", h, c4, kt, nkt))
                    items.append(("n", h, c4))
            skew(units, items, tile_a, tile_b, {"g": gate, "n": norm})
            return units

        run(proj_units(0))
        for p in range(3):
            run_merged(attn_units(p), proj_units(p + 1) if p < 2 else [])

    def mixer_B(layer):
        qb = scr_view(0, [3, S], BF16)
        kb = scr_view(12288, [S], BF16)
        ikb = scr_view(16384, [S], BF16)
        iqb = scr_view(20480, [2, S], BF16)
        vb = scr_view(28672, [16, 128], BF16)
        iw = scr_view(32768, [16, 4], F32)
        acc = scr_view(33024, [2, S], F32)
        tmp = scr_view(49408, [2, 512], F32)
        mk = scr_view(53504, [S], BF16)
        mt = scr_view(57600, [2, 16, 128], BF16)
        et = scr_view(65792, [2, 384], BF16)
        pt = scr_view(67328, [2, 384], BF16)
        rd = scr_view(68864, [384], F32)
        bs = scr_view(70400, [64], F32)
        rn = scr_view(70656, [384], F32)
        sc.op("pool", ("tensor_copy", dict(out=vb[:, :, 64:128], in_=cb[:, CB_ONES:CB_ONES + 64].unsqueeze(1).to_broadcast([128, 16, 64]))),
              reads=["cb"], writes=["vb1"])
        pu = []
        pu += proj_fm(layer, "IK", True, rope_evac(lambda c4, h: ikb[:, c4 * 512:(c4 + 1) * 512], lambda c4, h: [("ikb", c4)], False))
        for p in range(2):
            pu += proj_fm(layer, f"IQ{p}", True, rope_evac(lambda c4, h, p=p: iqb[:, p, c4 * 512:(c4 + 1) * 512],
                                                           lambda c4, h, p=p: [("iqb", c4)], False))

        def vev(g4, b1):
            v3 = ps[b1][:, :].rearrange("p (j n) -> p j n", n=128)
            sc.op("act", ("copy", dict(out=vb[:, g4:g4 + 4, 0:64], in_=v3[:, :, 0:64])), reads=[("ps", b1)], writes=[("vb", g4)])
            sc.op("act", ("copy", dict(out=iw[:, g4:g4 + 4, :], in_=v3[:, :, 64:68])), reads=[("ps", b1)], writes=["iw"])
        pu += proj_tm(layer, "BV", 16, lambda c, blk: hT[:, c, blk * 128:(blk + 1) * 128], vev)
        run(pu)
        PB = ((6,), (7,))
        pu2 = []
        for p in range(3):
            pu2 += proj_fm(layer, f"BQ{p}", True, rope_evac(lambda c4, h, p=p: qb[:, p, c4 * 512:(c4 + 1) * 512],
                                                            lambda c4, h, p=p: [("qb", c4)], False), banks=PB)
            pu2 += proj_fm(layer, f"BG{p}", False, silu_evac(3 + p), banks=PB)
        pu2 += proj_fm(layer, "BK", True, rope_evac(lambda c4, h: kb[:, c4 * 512:(c4 + 1) * 512], lambda c4, h: [("kb", c4)], False), banks=PB)
        cst = {"li": 0, "ei": 0}
        mkb = [mk, hn[:, :, :].rearrange("p a b -> p (a b)")]
        xtb = xt[:, :, :].rearrange("p a b -> p (a b)").bitcast(BF16).rearrange("p (i k n) -> p i k n", i=2, n=128)
        mtb = [mt[:, 0, :, :], mt[:, 1, :, :], xtb[:, 0, :, :], xtb[:, 1, :, :]]

        def stage1(qt):
            N = 128 * (qt + 1)
            a = qt % 2
            par = qt % 2
            m4 = qt % 4
            qs = slice(qt * 128, (qt + 1) * 128)
            mk_ = mkb[par]
            mt_ = mtb[m4]
            o = 32 * par
            thr = bs[:, o:o + 1]; hi = bs[:, o + 1:o + 2]; lo = bs[:, o + 2:o + 3]; cnt = bs[:, o + 3:o + 4]
            tt = bs[:, o + 4:o + 5]; nthr = bs[:, o + 5:o + 6]
            W = bs[:, o + 8:o + 8 + NBIS + 2]
            K = lambda nm: (nm, par)
            tpb = ps[2 + par][:, 0:512].bitcast(BF16)
            lb = par
            tj = par
            units = []

            def idx(j, h):
                k0 = j * 512
                n = min(512, N - k0)
                e_ = h % 2
                mm(ps[lb][:, 0:n], iqb[64 * e_:64 * e_ + 64, h // 2, qs], ikb[64 * e_:64 * e_ + 64, k0:k0 + n], True, True,
                   reads=[("iqb", qt // 4), ("ikb", j)], writes=[("ps", lb)])
                if h == 0:
                    sc.op("dve", ("tensor_scalar", dict(out=acc[:, a, k0:k0 + n], in0=ps[lb][:, 0:n], scalar1=0.0,
                                                        scalar2=iw[:, qt, 0:1], op0=ALU.max, op1=ALU.mult)),
                          reads=[("ps", lb), "iw"], writes=[("acc", a)])
                else:
                    sc.op("dve", ("tensor_scalar", dict(out=tmp[:, tj, 0:n], in0=ps[lb][:, 0:n], scalar1=0.0,
                                                        scalar2=iw[:, qt, h:h + 1], op0=ALU.max, op1=ALU.mult)),
                          reads=[("ps", lb), "iw"], writes=[("tmp", tj)])
                    sc.op("pool", ("tensor_tensor", dict(out=acc[:, a, k0:k0 + n], in0=acc[:, a, k0:k0 + n],
                                                         in1=tmp[:, tj, 0:n], op=ALU.add)),
                          reads=[("acc", a), ("tmp", tj)], writes=[("acc", a)])
            for j in range((N + 511) // 512):
                for h in range(4):
                    units.append(lambda j=j, h=h: idx(j, h))

            def bis_init():
                sc.op("pool", ("tensor_tensor", dict(out=acc[:, a, qs], in0=acc[:, a, qs], in1=cf[:, CF_NEGTRI:CF_NEGTRI + 128], op=ALU.add)),
                      reads=[("acc", a), "cf"], writes=[("acc", a)])
                if qt < 2:
                    sc.op("dve", ("memset", dict(ap=thr, constant=-1e29)), writes=[K("thr")])
                    return
                sc.op("dve", ("tensor_reduce", dict(out=hi, in_=acc[:, a, 0:N], axis=AX.X, op=ALU.max)), reads=[("acc", a)], writes=[K("bs_hi")])
                sc.op("dve", ("tensor_reduce", dict(out=lo, in_=acc[:, a, 0:N - 128], axis=AX.X, op=ALU.min)), reads=[("acc", a)], writes=[K("bs_lo")])
                sc.op("dve", ("tensor_tensor", dict(out=tt, in0=hi, in1=lo, op=ALU.subtract)), reads=[K("bs_hi"), K("bs_lo")], writes=[K("bs_tt")])
                sc.op("dve", ("tensor_scalar", dict(out=W, in0=cf[:, CF_POW2:CF_POW2 + NBIS + 2], scalar1=tt, scalar2=None, op0=ALU.mult)),
                      reads=[K("bs_tt"), "cf"], writes=[K("bs_W")])
                sc.op("dve", ("scalar_tensor_tensor", dict(out=nthr, in0=lo, scalar=-1.0, in1=W[:, 1:2], op0=ALU.mult, op1=ALU.subtract)),
                      reads=[K("bs_lo"), K("bs_W")], writes=[K("nthr")])
            units.append(bis_init)

            def bis(k):
                sc.op("act", ("activation", dict(out=mk_[:, 0:N], in_=acc[:, a, 0:N], func=AF.Sign, bias=nthr, scale=1.0, accum_out=cnt)),
                      reads=[("acc", a), K("nthr")], writes=[K("bs_cnt"), K("mk")])
                sc.op("dve", ("tensor_scalar", dict(out=tt, in0=cnt, scalar1=511.0 - N, scalar2=0.5, op0=ALU.is_lt, op1=ALU.subtract)),
                      reads=[K("bs_cnt")], writes=[K("bs_tt")])
                sc.op("dve", ("scalar_tensor_tensor", dict(out=nthr, in0=tt, scalar=W[:, k + 1:k + 2], in1=nthr, op0=ALU.mult, op1=ALU.add)),
                      reads=[K("bs_tt"), K("bs_W"), K("nthr")], writes=[K("nthr")])
            if qt >= 2:
                for k in range(NBIS):
                    units.append(lambda k=k: bis(k))

            def fin():
                if qt >= 2:
                    sc.op("dve", ("scalar_tensor_tensor", dict(out=thr, in0=nthr, scalar=-1.0, in1=W[:, NBIS + 1:NBIS + 2], op0=ALU.mult, op1=ALU.subtract)),
                          reads=[K("nthr"), K("bs_W")], writes=[K("thr")])
                sc.op("dve", ("tensor_scalar", dict(out=mk_[:, 0:N], in0=acc[:, a, 0:N], scalar1=thr, scalar2=None, op0=ALU.is_ge)),
                      reads=[("acc", a), K("thr")], writes=[K("mk")])
                for half in range((qt // 8) + 1):
                    nk = min(8, qt + 1 - 8 * half)
                    for i in range(nk):
                        kt = 8 * half + i
                        tr(tpb[:, i * 128:(i + 1) * 128], mk_[:, kt * 128:(kt + 1) * 128], reads=[K("mk"), "cb"], writes=[("ps", 2 + par)],
                           inc=(i == nk - 1))
                    sc.op("act", ("copy", dict(out=mt_[:, 8 * half:8 * half + nk, :],
                                               in_=tpb[:, 0:nk * 128].rearrange("p (k n) -> p k n", n=128))),
                          reads=[("ps", 2 + par)], writes=[("mt", m4)])
            units.append(fin)
            return units

        def stage2(qt):
            m = qt % 4
            mt_ = mtb[m]
            qs = slice(qt * 128, (qt + 1) * 128)
            units = []

            def tile_a(st_, kt, e_):
                ei = cst["ei"]; cst["ei"] += 1
                sb_ = 4 + (ei % 2)
                ej = ei % 2
                st_.update(ej=ej)
                mm(ps[sb_][:, 0:384], kb[64 * e_:64 * e_ + 64, kt * 128:(kt + 1) * 128], qb[64 * e_:64 * e_ + 64, :, qs], True, True,
                   reads=[("kb", kt // 4), ("qb", qt // 4)], writes=[("ps", sb_)])
                sc.op("act", ("activation", dict(out=et[:, ej, :], in_=ps[sb_][:, 0:384], func=AF.Exp, scale=0.125)),
                      reads=[("ps", sb_)], writes=[("et", ej)])
                sc.op("dve", ("tensor_tensor", dict(out=pt[:, ej, :].rearrange("p (h n) -> p h n", n=128),
                                                    in0=et[:, ej, :].rearrange("p (h n) -> p h n", n=128),
                                                    in1=mt_[:, kt, :].unsqueeze(1).to_broadcast([128, 3, 128]), op=ALU.mult)),
                      reads=[("et", ej), ("mt", m)], writes=[("pt", ej)])

            def tile_b(st_, kt, e_):
                ob = 6 + e_
                ej = st_["ej"]
                mm(ps[ob][:, 0:384], vb[:, kt, :], pt[:, ej, :], kt == 0, kt == qt,
                   reads=[("vb", (kt // 4) * 4), "vb1", ("pt", ej)], writes=[("ps", ob)], inc=True)
            items = [("t", kt, e_) for kt in range(qt + 1) for e_ in range(2)]

            def norm(e_):
                ob = 6 + e_
                sc.op("act", ("activation", dict(out=rd[64:128, :], in_=ps[ob][64:128, 0:384], func=AF.Ln)), reads=[("ps", ob)], writes=["rd"])
                sc.op("act", ("activation", dict(out=rd[64:128, :], in_=rd[64:128, :], func=AF.Exp, scale=-1.0)), reads=["rd"], writes=["rd"])
                sc.op("dve", ("tensor_tensor", dict(out=rn[64 * e_:64 * e_ + 64, :], in0=ps[ob][0:64, 0:384], in1=rd[64:128, :], op=ALU.mult)),
                      reads=[("ps", ob), "rd"], writes=["rd0"])
                ydst = yT[64 * e_:64 * e_ + 64, 3:6, qs]
                sc.op("pool", ("tensor_tensor", dict(out=ydst, in0=ydst, in1=rn[64 * e_:64 * e_ + 64, :].rearrange("p (h n) -> p h n", n=128), op=ALU.mult)),
                      reads=["rd0"] + [("yT", 3 + p, qt // 4) for p in range(3)], writes=[("yT", 3 + p, qt // 4) for p in range(3)])
            items += [("n", 0), ("n", 1)]
            skew(units, items, tile_a, tile_b, {"n": norm})
            return units

        run_merged(stage1(0), stage1(1), pu2)
        for P in range(8):
            nx = [stage1(2 * P + 2), stage1(2 * P + 3)] if P < 7 else []
            run_merged(stage2(2 * P) + stage2(2 * P + 1), *nx)

    def mixer_C(layer):
        qc = scr_view(0, [2, S], BF16)
        kc = scr_view(8192, [2, S], BF16)
        vc = scr_view(16384, [16, 512], BF16)
        ac = scr_view(32768, [4, S], F32)
        et = scr_view(65536, [2, 256], BF16)
        rd = scr_view(65536 + 1024, [512], F32)
        rn = scr_view(65536 + 3072, [512], F32)
        sc.op("pool", ("tensor_copy", dict(out=vc.rearrange("p b (h c) -> p b h c", c=128)[:, :, :, 64:128],
                                           in_=cb[:, CB_ONES:CB_ONES + 64].unsqueeze(1).unsqueeze(1).to_broadcast([128, 16, 4, 64]))),
              reads=["cb"], writes=["vc1"])
        gu = []
        for p in range(2):
            gu += proj_fm(layer, f"CG{p}", False, silu_evac(6 + p))
        run(gu)
        groups = [g for g in range(3) if g in getattr(_build, 'cgroups', (0, 1, 2))]
        dils = (1, 4, 16)
        cst = {"ei": 0}

        def toks_of(dil):
            def toks(r, m, cnt=128):
                st0 = r + dil * 128 * m
                return slice(st0, st0 + dil * (cnt - 1) + 1, dil)
            return toks

        def proj_units(g, p, pbanks):
            dil = dils[g]
            nblk = S // dil // 128
            toks = toks_of(dil)
            units = []
            units += proj_fm(layer, f"CQ{g}{p}", True, rope_evac(lambda c4, h: qc[:, p, c4 * 512:(c4 + 1) * 512], lambda c4, h: [("qc", p, c4)], False),
                             banks=pbanks)
            units += proj_fm(layer, f"CK{g}{p}", True, rope_evac(lambda c4, h: kc[:, p, c4 * 512:(c4 + 1) * 512], lambda c4, h: [("kc", p, c4)], False),
                             banks=pbanks)

            def vev(g4, b1):
                v4 = ps[b1][:, :].rearrange("p (j h c) -> p j h c", h=2, c=64)
                sc.op("act", ("copy", dict(out=vc.rearrange("p b (h c) -> p b h c", c=128)[:, g4:g4 + 4, 2 * p:2 * p + 2, 0:64], in_=v4)),
                      reads=[("ps", b1)], writes=[("vc", p, g4)])
            units += proj_tm(layer, f"CV{g}{p}", 16, lambda c, blk: hT[:, c, toks(blk // nblk, blk % nblk)], vev,
                             banks=(pbanks[0][0], pbanks[1][0]) if len(pbanks[0]) == 1 else (0, 1))
            return units

        def attn_units(g, j, first):
            dil = dils[g]
            nblk = S // dil // 128
            toks = toks_of(dil)
            p, e_ = j // 2, j % 2
            pr = slice(64 * e_, 64 * e_ + 64)
            allq = [("qc", p, c4) for c4 in range(4)]
            allk = [("kc", p, c4) for c4 in range(4)]
            started = [False] * 4
            units = []

            def blk_a(st_, r, m):
                nq = 256 if m + 1 < nblk else 128
                ei = cst["ei"]; cst["ei"] += 1
                sb_ = 4 + (ei % 2)
                ej = ei % 2
                st_.update(nq=nq, ej=ej)
                mm(ps[sb_][:, 0:nq], kc[pr, p, toks(r, m)], qc[pr, p, toks(r, m, nq)], True, True,
                   reads=allk + allq, writes=[("ps", sb_)])
                sc.op("act", ("activation", dict(out=et[:, ej, 0:nq], in_=ps[sb_][:, 0:nq], func=AF.Exp, scale=0.125)),
                      reads=[("ps", sb_)], writes=[("et", ej)])
                sc.op("dve", ("tensor_tensor", dict(out=et[:, ej, 0:nq], in0=et[:, ej, 0:nq], in1=cb[:, CB_MOWN:CB_MOWN + nq], op=ALU.mult)),
                      reads=[("et", ej), "cb"], writes=[("et", ej)])

            def blk_b(st_, r, m):
                blk = r * nblk + m
                nq, ej = st_["nq"], st_["ej"]
                pieces = []
                for part in range(nq // 128):
                    t0 = r + dil * 128 * (m + part)
                    if dil <= 4:
                        pieces.append((part * 128, 128, t0))
                    else:
                        for q4 in range(4):
                            pieces.append((part * 128 + 32 * q4, 32, t0 + dil * 32 * q4))
                for pi, (c0, cn, t0) in enumerate(pieces):
                    bnk = t0 // 512
                    o0 = t0 % 512
                    mm(ps[bnk][:, o0:o0 + dil * (cn - 1) + 1:dil], vc[:, blk, 128 * j:128 * j + 128], et[:, ej, c0:c0 + cn],
                       not started[bnk], False, reads=[("vc", p, (blk // 4) * 4), "vc1", ("et", ej)],
                       writes=[("ps", bnk)], inc=(pi == len(pieces) - 1))
                    started[bnk] = True
            items = [("t", r, m) for r in range(dil) for m in range(nblk)] + [("e",)]

            def evac():
                for c4 in range(4):
                    dst = ac[:, j, c4 * 512:(c4 + 1) * 512]
                    if first:
                        sc.op("act", ("copy", dict(out=dst, in_=ps[c4][:, :])), reads=[("ps", c4)], writes=[("ac", j, c4)])
                    else:
                        sc.op("dve", ("tensor_tensor", dict(out=dst, in0=ps[c4][:, :], in1=dst, op=ALU.add)),
                              reads=[("ps", c4), ("ac", j, c4)], writes=[("ac", j, c4)])
            skew(units, items, blk_a, blk_b, {"e": evac})
            return units

        PB = ((6,), (7,))
        seq = []
        for gi, g in enumerate(groups):
            seq.append((g, 0)); seq.append((g, 1))
        run(proj_units(seq[0][0], seq[0][1], ((0, 1), (2, 3))))
        for i, (g, p) in enumerate(seq):
            nxt = proj_units(seq[i + 1][0], seq[i + 1][1], PB) if i + 1 < len(seq) else []
            first = (g == groups[0])
            run_merged(attn_units(g, 2 * p, first) + attn_units(g, 2 * p + 1, first), nxt)
        for j in range(4):
            p, e_ = j // 2, j % 2
            for c4 in range(4):
                cs = slice(c4 * 512, (c4 + 1) * 512)
                sc.op("act", ("activation", dict(out=rd[64:128, :], in_=ac[64:128, j, cs], func=AF.Ln)), reads=[("ac", j, c4)], writes=["rd"])
                sc.op("act", ("activation", dict(out=rd[64:128, :], in_=rd[64:128, :], func=AF.Exp, scale=-1.0)), reads=["rd"], writes=["rd"])
                sc.op("act", ("copy", dict(out=rn[64:128, :], in_=ac[0:64, j, cs])), reads=[("ac", j, c4)], writes=[("rn", 1)])
                sc.op("dve", ("tensor_tensor", dict(out=rn[64 * e_:64 * e_ + 64, :], in0=rn[64:128, :], in1=rd[64:128, :], op=ALU.mult)),
                      reads=[("rn", 1), "rd"], writes=[("rn", e_)])
                ydst = yT[64 * e_:64 * e_ + 64, 6 + p, cs]
                sc.op("pool", ("tensor_tensor", dict(out=ydst, in0=ydst, in1=rn[64 * e_:64 * e_ + 64, :], op=ALU.mult)),
                      reads=[("rn", e_), ("yT", 6 + p, c4)], writes=[("yT", 6 + p, c4)])

    def phase_final(si, layer, src_d, dst_d, last):
        mg = scr_view(0, [8, S], BF16)
        wbr = scr_view(32768, [8, D], BF16)
        wo = scr_view(49152, [8, D], BF16)
        gs = scr_view(65536, [3, 512], BF16)
        tm = scr_view(65536 + 3072, [512], F32)
        if last:
            load_g(0, 2)
        kch = ((0, 3), (3, 6), (6, 8))
        for dt_ in range(8):
            gw = []
            for br in range(3):
                w_, k_ = load_w(layer, _TIDX[f"MG{8 * br + dt_}"])
                gw.append((w_.rearrange("p (c n) -> p c n", n=128), k_))
            if dt_ == 0:
                for c in range(8):
                    nm = (f"WA{c}" if c < 3 else f"WB{c - 3}" if c < 6 else f"WC{c - 6}")
                    load_w(layer, _XIDX[nm], dest=wbr[:, c, :], dest_key=("wbr", c))
            if dt_ == 1:
                for c in range(8):
                    load_w(layer, _XIDX[f"WO{c}"], dest=wo[:, c, :], dest_key=("wo", c))
            for c4 in range(4):
                cs = slice(c4 * 512, (c4 + 1) * 512)
                for br in range(3):
                    for c in range(8):
                        mm(ps[br][:, :], gw[br][0][:, c, :], hT[:, c, cs], c == 0, c == 7, reads=[gw[br][1], ("hT", c4)], writes=[("ps", br)])
                    sc.op("act", ("activation", dict(out=gs[:, br, :], in_=ps[br][:, :], func=AF.Sigmoid)), reads=[("ps", br)], writes=[("gs", br)])
                for br in range(3):
                    a0, a1 = kch[br]
                    for c in range(a0, a1):
                        mm(ps[3 + br][:, :], wbr[:, c, dt_ * 128:(dt_ + 1) * 128], yT[:, c, cs], c == a0, c == a1 - 1,
                           reads=[("wbr", c), ("yT", c, c4)], writes=[("ps", 3 + br)])
                sc.op("dve", ("tensor_tensor", dict(out=tm, in0=ps[3][:, :], in1=gs[:, 0, :], op=ALU.mult)), reads=[("ps", 3), ("gs", 0)], writes=["tm"])
                sc.op("dve", ("tensor_tensor", dict(out=gs[:, 1, :], in0=ps[4][:, :], in1=gs[:, 1, :], op=ALU.mult)), reads=[("ps", 4), ("gs", 1)], writes=[("gs", 1)])
                sc.op("dve", ("tensor_tensor", dict(out=gs[:, 2, :], in0=ps[5][:, :], in1=gs[:, 2, :], op=ALU.mult)), reads=[("ps", 5), ("gs", 2)], writes=[("gs", 2)])
                sc.op("pool", ("tensor_tensor", dict(out=tm, in0=tm, in1=gs[:, 1, :], op=ALU.add)), reads=["tm", ("gs", 1)], writes=["tm"])
                sc.op("pool", ("tensor_tensor", dict(out=mg[:, dt_, cs], in0=tm, in1=gs[:, 2, :], op=ALU.add)),
                      reads=["tm", ("gs", 2)], writes=[("mg", c4)])
        for t in range(NT):
            s = t % 2
            ts = slice(t * 128, (t + 1) * 128)
            sc.dma(("x", s), ("dma_start", dict(out=xt[:, s, :], in_=src_d[si, ts, :])), writes=[("xt", s)])
            for hf in range(2):
                b = 6 + hf
                for c in range(8):
                    mm(ps[b][:, :], mg[:, c, ts], wo[:, c, hf * 512:(hf + 1) * 512], c == 0, c == 7,
                       reads=[("mg", t // 4), ("wo", c)], writes=[("ps", b)])
                sc.op("dve", ("tensor_tensor", dict(out=xt[:, s, hf * 512:(hf + 1) * 512], in0=ps[b][:, :],
                                                                        in1=xt[:, s, hf * 512:(hf + 1) * 512], op=ALU.add)),
                      reads=[("ps", b), ("xt", s)], writes=[("xt", s)])
            if last:
                ss = st[:, 8 + 2 * s:8 + 2 * s + 1]
                rs = st[:, 8 + 2 * s + 1:8 + 2 * s + 2]
                sc.op("act", ("activation", dict(out=hn[:, s, :], in_=xt[:, s, :], func=AF.Square, accum_out=ss)),
                      reads=[("xt", s)], writes=[("hn", s), ("st", s)])
                sc.op("dve", ("tensor_scalar", dict(out=rs, in0=ss, scalar1=1.0 / D, scalar2=1e-6, op0=ALU.mult, op1=ALU.add)),
                      reads=[("st", s)], writes=[("st", s)])
                sc.op("act", ("activation", dict(out=rs, in_=rs, func=AF.Sqrt)), reads=[("st", s)], writes=[("st", s)])
                sc.op("dve", ("reciprocal", dict(out=rs, in_=rs)), reads=[("st", s)], writes=[("st", s)])
                sc.op("dve", ("scalar_tensor_tensor", dict(out=xt[:, s, :], in0=xt[:, s, :], scalar=rs, in1=gbc[:, 0, :],
                                                                         op0=ALU.mult, op1=ALU.mult)),
                      reads=[("xt", s), ("st", s), ("gbc", 0)], writes=[("xt", s)])
            sc.dma(("o", s), ("dma_start", dict(out=dst_d[si, ts, :], in_=xt[:, s, :])), reads=[("xt", s)], writes=[("dram", si, t)])

    enabled = set(getattr(_build, "enabled", ("A", "B", "C")))

    def program():
        sc.dma("c0", ("dma_start", dict(out=cf[:], in_=cf_d[:, :])), writes=["cf"])
        sc.dma("c1", ("dma_start", dict(out=cb[:], in_=cb_d[:, :])), writes=["cb"])
        for si in range(nseq):
            for li_, layer in enumerate(layers_all):
                first = (li_ == 0)
                last = (li_ == len(layers_all) - 1)
                src = x_d if first else xs_d
                dst = out_d if last else xs_d
                sc.phase = "norm"
                phase_norm(si, layer, src)
                if "A" in enabled:
                    sc.barrier()
                    sc.phase = "A"
                    mixer_A(layer)
                if "B" in enabled:
                    sc.barrier()
                    sc.phase = "B"
                    mixer_B(layer)
                if "C" in enabled:
                    sc.barrier()
                    sc.phase = "C"
                    mixer_C(layer)
                sc.barrier()
                sc.phase = "final"
                for name, shape, dt in taps:
                    if name == f"yT{layer}" and si == 0:
                        sc.dma(("tap", name), ("dma_start", dict(out=tap_d[name][:, :, :], in_=yT[:])),
                               reads=[("yT", c, c4) for c in range(8) for c4 in range(4)])
                    if name == f"hT{layer}" and si == 0:
                        sc.dma(("tap", name), ("dma_start", dict(out=tap_d[name][:, :, :], in_=hT[:])),
                               reads=[("hT", c4) for c4 in range(4)])
                phase_final(si, layer, src, dst, last and final_norm)
                sc.barrier()
        sc.final_wait("sp", [("o", 0), ("o", 1)] + [("tap", n) for n, _, _ in taps])


    program()
    sc = Sched()
    wstate.update(n=0, rec=False, issued=0)
    ppp["i"] = 0
    rtc["i"] = 0
    qsc["i"] = 0
    program()

    waited = {e: set() for e in ("pe", "act", "dve", "pool")}
    for e in Sched.ENG:
        for waits, fn, inc, ph in sc.q[e]:
            for de, dc in waits:
                if de in waited:
                    waited[de].add(dc)
    remap = {e: {c: i + 1 for i, c in enumerate(sorted(waited[e]))} for e in waited}
    for e in waited:
        c = 0
        newq = []
        for waits, fn, inc, ph in sc.q[e]:
            if inc is not None and inc[0] == "E":
                c += 1
                if c not in remap[e]:
                    inc = None
            newq.append((waits, fn, inc, ph))
        sc.q[e] = newq
    for e in Sched.ENG:
        sc.q[e] = [([(de, remap[de][dc]) if de in remap else (de, dc) for de, dc in waits], fn, inc, ph)
                   for waits, fn, inc, ph in sc.q[e]]

    sems = {}
    for e in Sched.ENG:
        sems[e] = es.enter_context(nc.semaphore("s_" + e))
    for de in sc.dcnt:
        sems[de] = es.enter_context(nc.semaphore("d%d" % len(sems)))
    block = es.enter_context(nc.Block())

    def replay(engname):
        def run(eng):
            for waits, fn, inc, ph in sc.q[engname]:
                for de, dc in waits:
                    eng.wait_ge(sems[de], dc)
                if fn is None:
                    continue
                ins = getattr(eng, fn[0])(**fn[1])
                if ANNOTATE:
                    ins.annotate(ph)
                if inc is not None:
                    if inc[0] == "E":
                        ins.then_inc(sems[inc[1]], 1)
                    else:
                        ins.then_inc(sems[inc[1]], 16)
        return run
    block.tensor(replay("pe"))
    block.scalar(replay("act"))
    block.vector(replay("dve"))
    block.gpsimd(replay("pool"))
    block.sync(replay("sp"))
    es.close()
    return nc


_CACHE = {}


def kernel(x, norm_g, w_in, w_br_a, w_br_b, w_br_c, w_out, final_norm_g):
    x = np.ascontiguousarray(np.asarray(x, np.float32))
    ncores = 8
    nseq = x.shape[0] // ncores
    wt = _pack_weights(np.asarray(w_in, np.float32), np.asarray(w_br_a, np.float32), np.asarray(w_br_b, np.float32),
                       np.asarray(w_br_c, np.float32), np.asarray(w_out, np.float32))
    gv = np.concatenate([np.asarray(norm_g, np.float32), np.asarray(final_norm_g, np.float32)[None, :]], axis=0)
    cf, cb = _consts()
    nc = _build(nseq)
    in_maps = [{"x": x[i * nseq:(i + 1) * nseq], "wt": wt, "gv": gv, "cf": cf, "cb": cb} for i in range(ncores)]
    res = run_bass_kernel_spmd(nc, in_maps, core_ids=list(range(ncores)))
    return np.concatenate([r["out"] for r in res.results], axis=0)
```
